# Optimizing a Trainium2 kernel written in Bass

```python
import jax, jax.numpy as jnp
from jax import lax
import numpy as np

D_MODEL = 1024
BATCH = 4
SEQ = 4096
DEPTH = 2

GRID_W = 64
CTX_LEN = 256
HEAD_DIM = 64
AXIS_DIM = HEAD_DIM // 2
ROPE_THETA = 10000.0
EPS = 1e-6
Q_BLOCK = 128
N_MOD = 6

GLA_HEADS = 4
GLA_DK = 64
GLA_DV = 128
GLA_GATE_RANK = 16
GLA_GATE_NORM = 16.0
GLA_CHUNK = 64
ATT_HEADS = 8
ATT_KV_HEADS = 2
SWA_HEADS = 16
SWA_KV_HEADS = 2
SWA_WINDOW = 128
MOE_GROUPS = 4
MOE_EXPERTS_PER_GROUP = 4
MOE_TOPK = 2
D_EXPERT = 256

EVEN_WIDTHS = (GLA_HEADS * GLA_DK, GLA_HEADS * GLA_DK, GLA_HEADS * GLA_DV, GLA_HEADS * GLA_DV,
               2 * GLA_GATE_RANK, ATT_HEADS * HEAD_DIM, ATT_KV_HEADS * HEAD_DIM, ATT_KV_HEADS * HEAD_DIM)
EVEN_IN = sum(EVEN_WIDTHS)
EVEN_MIX = GLA_HEADS * GLA_DV + ATT_HEADS * HEAD_DIM
ODD_Q = SWA_HEADS * HEAD_DIM
ODD_KV = SWA_KV_HEADS * HEAD_DIM
ODD_IN = ODD_Q + 2 * ODD_KV
ODD_MIX = ODD_Q
N_EVEN = (DEPTH + 1) // 2
N_ODD = DEPTH // 2

kernel_name = 'hybrid_gla_gqa_swa_hmoe_diffusion_block'


def _split(z, widths):
    return jnp.split(z, np.cumsum(widths)[:-1].tolist(), axis=-1)


def rms_norm(x, gain):
    xf = x.astype(jnp.float32)
    y = xf * lax.rsqrt(jnp.mean(xf * xf, axis=-1, keepdims=True) + EPS)
    return (y * gain.astype(jnp.float32)).astype(x.dtype)


def modulate(x, gain, shift, scale):
    return rms_norm(x, gain) * (1 + scale) + shift


def axial_rope_tables(n):
    rows = n // GRID_W
    row = jnp.repeat(jnp.arange(rows, dtype=jnp.int32), GRID_W)
    col = jnp.tile(jnp.arange(GRID_W, dtype=jnp.int32), rows)
    inv_freq = ROPE_THETA ** (-jnp.arange(0, AXIS_DIM, 2, dtype=jnp.float32) / AXIS_DIM)
    ang = jnp.stack([row[:, None] * inv_freq, col[:, None] * inv_freq], axis=1)
    return jnp.cos(ang), jnp.sin(ang)


def apply_axial_rope(x, cos, sin):
    B, n, H, _ = x.shape
    xs = x.reshape(B, n, H, 2, 2, AXIS_DIM // 2)
    x1, x2 = xs[..., 0, :], xs[..., 1, :]
    c = cos[None, :, None].astype(x.dtype)
    s = sin[None, :, None].astype(x.dtype)
    out = jnp.stack([x1 * c - x2 * s, x2 * c + x1 * s], axis=-2)
    return out.reshape(B, n, H, HEAD_DIM)


def heads(z, n_heads, gain):
    B, L, _ = z.shape
    return rms_norm(z.reshape(B, L, n_heads, HEAD_DIM), gain)


def group_q(q, n_kv):
    B, L, H, Dh = q.shape
    return q.reshape(B, L, n_kv, H // n_kv, Dh)


def softmax_attend(q, k, v, mask=None, sink=None):
    B, Q, KV, G, Dh = q.shape
    s = jnp.einsum('bqhgd,bkhd->bhgqk', q, k).astype(jnp.float32) * (Dh ** -0.5)
    if mask is not None:
        s = jnp.where(mask, s, -jnp.inf)
    if sink is not None:
        sink_col = jnp.broadcast_to(sink.reshape(1, KV, G, 1, 1).astype(jnp.float32), s.shape[:-1] + (1,))
        p = jax.nn.softmax(jnp.concatenate([s, sink_col], axis=-1), axis=-1)[..., :-1]
    else:
        p = jax.nn.softmax(s, axis=-1)
    o = jnp.einsum('bhgqk,bkhd->bqhgd', p.astype(v.dtype), v)
    return o.reshape(B, Q, KV * G * Dh)


def blocked_dense_attention(q, k, v):
    B, S = q.shape[:2]
    nb = S // Q_BLOCK
    qb = jnp.moveaxis(q.reshape(B, nb, Q_BLOCK, *q.shape[2:]), 1, 0)
    ob = lax.map(lambda qi: softmax_attend(qi, k, v), qb)
    return jnp.moveaxis(ob, 0, 1).reshape(B, S, -1)


def banded_window_attention(q, k, v, k_ctx, v_ctx, sink):
    B, S = q.shape[:2]
    nb = S // Q_BLOCK
    span = Q_BLOCK + 2 * SWA_WINDOW
    pad = ((0, 0), (SWA_WINDOW, SWA_WINDOW), (0, 0), (0, 0))
    kp = jnp.pad(k, pad)
    vp = jnp.pad(v, pad)
    qb = jnp.moveaxis(q.reshape(B, nb, Q_BLOCK, *q.shape[2:]), 1, 0)
    ctx_ok = jnp.ones((Q_BLOCK, k_ctx.shape[1]), dtype=bool)

    def block(args):
        qi, bi = args
        start = bi * Q_BLOCK
        kb = lax.dynamic_slice_in_dim(kp, start, span, axis=1)
        vb = lax.dynamic_slice_in_dim(vp, start, span, axis=1)
        qpos = start + jnp.arange(Q_BLOCK, dtype=jnp.int32)
        kpos = start - SWA_WINDOW + jnp.arange(span, dtype=jnp.int32)
        band = (kpos[None, :] >= 0) & (kpos[None, :] < S) & (jnp.abs(kpos[None, :] - qpos[:, None]) <= SWA_WINDOW)
        mask = jnp.concatenate([band, ctx_ok], axis=1)
        return softmax_attend(qi, jnp.concatenate([kb, k_ctx], axis=1), jnp.concatenate([vb, v_ctx], axis=1), mask, sink)

    ob = lax.map(block, (qb, jnp.arange(nb, dtype=jnp.int32)))
    return jnp.moveaxis(ob, 0, 1).reshape(B, S, -1)


def gla_chunk_scan(q, k, v, g, s0):
    B, H, L, DK = q.shape
    DV = v.shape[-1]
    n = L // GLA_CHUNK

    def chunks(t):
        return jnp.moveaxis(t.reshape(B, H, n, GLA_CHUNK, t.shape[-1]), 2, 0)

    lower_tri = jnp.tril(jnp.ones((GLA_CHUNK, GLA_CHUNK), dtype=bool))[:, :, None]

    def step(s, blk):
        qc, kc, vc, gc = blk
        b = jnp.cumsum(gc, axis=2)
        o = jnp.einsum('bhtk,bhkv->bhtv', qc * jnp.exp(b), s)
        diff = b[:, :, :, None, :] - b[:, :, None, :, :]
        decay = jnp.exp(jnp.where(lower_tri, diff, -jnp.inf))
        att = jnp.einsum('bhtk,bhsk,bhtsk->bhts', qc, kc, decay)
        o = o + jnp.einsum('bhts,bhsv->bhtv', att, vc)
        b_end = b[:, :, -1:, :]
        s = jnp.exp(b_end[:, :, 0, :, None]) * s + jnp.einsum('bhsk,bhsv->bhkv', kc * jnp.exp(b_end - b), vc)
        return s, o

    s_fin, o = lax.scan(step, s0, (chunks(q), chunks(k), chunks(v), chunks(g)))
    return jnp.moveaxis(o, 0, 2).reshape(B, H, L, DV), s_fin


def gla_inputs(q, k, v, lr, gate_w, gate_b):
    B, L, _ = q.shape

    def hd(t, d):
        return t.reshape(B, L, GLA_HEADS, d).transpose(0, 2, 1, 3).astype(jnp.float32)

    z = jnp.einsum('blzr,zrk->zblk', lr.reshape(B, L, 2, GLA_GATE_RANK), gate_w) + gate_b[:, None, None, :]
    g = jax.nn.log_sigmoid(z.astype(jnp.float32)) / GLA_GATE_NORM
    g = g.reshape(2, B, L, GLA_HEADS, GLA_DK).transpose(0, 1, 3, 2, 4)
    return hd(q, GLA_DK) * (GLA_DK ** -0.5), hd(k, GLA_DK), hd(v, GLA_DV), g


def gla_bidirectional(lat, ctx):
    ql, kl, vl, gl = lat
    qc, kc, vc, gc = ctx
    B = ql.shape[0]
    s0 = jnp.zeros((B, GLA_HEADS, GLA_DK, GLA_DV), jnp.float32)

    def rev(t):
        return jnp.flip(t, axis=2)

    oc_f, sc_f = gla_chunk_scan(qc, kc, vc, gc[0], s0)
    oc_b, sc_b = gla_chunk_scan(rev(qc), rev(kc), rev(vc), rev(gc[1]), s0)
    ol_f, _ = gla_chunk_scan(ql, kl, vl, gl[0], sc_f)
    ol_b, _ = gla_chunk_scan(rev(ql), rev(kl), rev(vl), rev(gl[1]), sc_b)
    return ol_f + rev(ol_b), oc_f + rev(oc_b)


def gla_output(o, r, gain):
    B, H, L, _ = o.shape
    o = rms_norm(o.transpose(0, 2, 1, 3), gain).astype(r.dtype)
    return (o * jax.nn.silu(r.reshape(B, L, H, GLA_DV))).reshape(B, L, H * GLA_DV)


def even_mixer(h_lat, h_ctx, w_in, w_out, gate_w, gate_b, gla_norm, q_norm, k_norm, cos, sin):
    B, S, _ = h_lat.shape
    Lc = h_ctx.shape[1]
    z_l = _split(h_lat @ w_in, EVEN_WIDTHS)
    z_c = _split(h_ctx @ w_in, EVEN_WIDTHS)
    o_gla_l, o_gla_c = gla_bidirectional(gla_inputs(z_l[0], z_l[1], z_l[2], z_l[4], gate_w, gate_b),
                                         gla_inputs(z_c[0], z_c[1], z_c[2], z_c[4], gate_w, gate_b))
    a_l = gla_output(o_gla_l, z_l[3], gla_norm)
    a_c = gla_output(o_gla_c, z_c[3], gla_norm)
    q_l = group_q(apply_axial_rope(heads(z_l[5], ATT_HEADS, q_norm), cos, sin), ATT_KV_HEADS)
    k_l = apply_axial_rope(heads(z_l[6], ATT_KV_HEADS, k_norm), cos, sin)
    v_l = z_l[7].reshape(B, S, ATT_KV_HEADS, HEAD_DIM)
    q_c = group_q(heads(z_c[5], ATT_HEADS, q_norm), ATT_KV_HEADS)
    k_c = heads(z_c[6], ATT_KV_HEADS, k_norm)
    v_c = z_c[7].reshape(B, Lc, ATT_KV_HEADS, HEAD_DIM)
    b_l = blocked_dense_attention(q_l, jnp.concatenate([k_l, k_c], axis=1), jnp.concatenate([v_l, v_c], axis=1))
    b_c = softmax_attend(q_c, k_c, v_c)
    return jnp.concatenate([a_l, b_l], axis=-1) @ w_out, jnp.concatenate([a_c, b_c], axis=-1) @ w_out


def odd_mixer(h_lat, h_ctx, w_in, w_out, sink, q_norm, k_norm, cos, sin, need_ctx):
    B, S, _ = h_lat.shape
    Lc = h_ctx.shape[1]
    q_l, k_l, v_l = _split(h_lat @ w_in, (ODD_Q, ODD_KV, ODD_KV))
    k_c, v_c = _split(h_ctx @ w_in[:, ODD_Q:], (ODD_KV, ODD_KV))
    q = group_q(apply_axial_rope(heads(q_l, SWA_HEADS, q_norm), cos, sin), SWA_KV_HEADS)
    k = apply_axial_rope(heads(k_l, SWA_KV_HEADS, k_norm), cos, sin)
    v = v_l.reshape(B, S, SWA_KV_HEADS, HEAD_DIM)
    kc = heads(k_c, SWA_KV_HEADS, k_norm)
    vc = v_c.reshape(B, Lc, SWA_KV_HEADS, HEAD_DIM)
    m_l = banded_window_attention(q, k, v, kc, vc, sink) @ w_out
    if not need_ctx:
        return m_l, None
    qc = group_q(heads(h_ctx @ w_in[:, :ODD_Q], SWA_HEADS, q_norm), SWA_KV_HEADS)
    return m_l, softmax_attend(qc, kc, vc, None, sink) @ w_out


def hier_moe(h, wg, bg, we, be, w_gate, w_up, w_down):
    N = h.shape[0]
    g_logits = (h @ wg + bg).astype(jnp.float32)
    g_prob = jax.nn.softmax(g_logits, axis=-1)
    g_idx = jnp.argmax(g_logits, axis=-1)
    g_weight = jnp.take_along_axis(g_prob, g_idx[:, None], axis=-1)
    e_logits = (h @ we + be).astype(jnp.float32).reshape(N, MOE_GROUPS, MOE_EXPERTS_PER_GROUP)
    e_sel = jnp.take_along_axis(e_logits, g_idx[:, None, None], axis=1)[:, 0]
    top_p, top_i = lax.top_k(jax.nn.softmax(e_sel, axis=-1), MOE_TOPK)
    top_p = top_p / jnp.sum(top_p, axis=-1, keepdims=True)
    e_weight = jnp.sum(jax.nn.one_hot(top_i, MOE_EXPERTS_PER_GROUP, dtype=jnp.float32) * top_p[..., None], axis=1)
    combine = (jax.nn.one_hot(g_idx, MOE_GROUPS, dtype=jnp.float32)[:, :, None]
               * (g_weight * e_weight)[:, None, :]).astype(h.dtype)
    out = jnp.zeros_like(h)
    for g in range(MOE_GROUPS):
        a = jnp.einsum('nd,edf->nef', h, w_gate[g])
        u = jnp.einsum('nd,edf->nef', h, w_up[g])
        hid = jax.nn.silu(a) * u * combine[:, g, :, None]
        out = out + jnp.einsum('nef,efd->nd', hid, w_down[g])
    return out


def setup_inputs(seed: int = 0) -> dict:
    key = jax.random.key(seed)
    ks = iter(jax.random.split(key, 32))
    D = D_MODEL
    G, E = MOE_GROUPS, MOE_EXPERTS_PER_GROUP

    def nrm(shape, scale):
        return scale * jax.random.normal(next(ks), shape, jnp.float32)

    def gain(shape):
        return 1.0 + 0.02 * jax.random.normal(next(ks), shape, jnp.float32)

    return {
        'x': nrm((BATCH, SEQ, D), 1.0),
        'c': nrm((BATCH, D), 1.0),
        'ctx': nrm((BATCH, CTX_LEN, D), 1.0),
        'c_ctx': nrm((D,), 1.0),
        'mod_w': nrm((DEPTH, D, N_MOD * D), 0.5 * D ** -0.5),
        'mod_b': nrm((DEPTH, N_MOD * D), 0.02),
        'norm_mix': gain((DEPTH, D)),
        'norm_ffn': gain((DEPTH, D)),
        'ev_w_in': nrm((N_EVEN, D, EVEN_IN), D ** -0.5),
        'ev_w_out': nrm((N_EVEN, EVEN_MIX, D), EVEN_MIX ** -0.5),
        'gla_gate_w': nrm((N_EVEN, 2, GLA_GATE_RANK, GLA_HEADS * GLA_DK), GLA_GATE_RANK ** -0.5),
        'gla_gate_b': nrm((N_EVEN, 2, GLA_HEADS * GLA_DK), 0.1),
        'gla_out_norm': gain((N_EVEN, GLA_DV)),
        'att_q_norm': gain((N_EVEN, HEAD_DIM)),
        'att_k_norm': gain((N_EVEN, HEAD_DIM)),
        'od_w_in': nrm((N_ODD, D, ODD_IN), D ** -0.5),
        'od_w_out': nrm((N_ODD, ODD_MIX, D), ODD_MIX ** -0.5),
        'swa_sink': nrm((N_ODD, SWA_HEADS), 1.0),
        'swa_q_norm': gain((N_ODD, HEAD_DIM)),
        'swa_k_norm': gain((N_ODD, HEAD_DIM)),
        'router_group_w': nrm((DEPTH, D, G), D ** -0.5),
        'router_group_b': nrm((DEPTH, G), 0.01),
        'router_expert_w': nrm((DEPTH, D, G * E), D ** -0.5),
        'router_expert_b': nrm((DEPTH, G * E), 0.01),
        'exp_w_gate': nrm((DEPTH, G, E, D, D_EXPERT), D ** -0.5),
        'exp_w_up': nrm((DEPTH, G, E, D, D_EXPERT), D ** -0.5),
        'exp_w_down': nrm((DEPTH, G, E, D_EXPERT, D), D_EXPERT ** -0.5),
    }


def reference(x, c, ctx, c_ctx, mod_w, mod_b, norm_mix, norm_ffn, ev_w_in, ev_w_out, gla_gate_w, gla_gate_b,
              gla_out_norm, att_q_norm, att_k_norm, od_w_in, od_w_out, swa_sink, swa_q_norm, swa_k_norm,
              router_group_w, router_group_b, router_expert_w, router_expert_b, exp_w_gate, exp_w_up, exp_w_down):
    B, S, D = x.shape
    cos, sin = axial_rope_tables(S)
    sc = jax.nn.silu(c)
    scc = jax.nn.silu(c_ctx)[None]
    for i in range(DEPTH):
        last = i == DEPTH - 1
        j = i // 2
        mod_l = jnp.split((sc @ mod_w[i] + mod_b[i])[:, None, :], N_MOD, axis=-1)
        mod_c = jnp.split((scc @ mod_w[i] + mod_b[i])[:, None, :], N_MOD, axis=-1)
        h_l = modulate(x, norm_mix[i], mod_l[0], mod_l[1])
        h_c = modulate(ctx, norm_mix[i], mod_c[0], mod_c[1])
        if i % 2 == 0:
            m_l, m_c = even_mixer(h_l, h_c, ev_w_in[j], ev_w_out[j], gla_gate_w[j], gla_gate_b[j],
                                  gla_out_norm[j], att_q_norm[j], att_k_norm[j], cos, sin)
        else:
            m_l, m_c = odd_mixer(h_l, h_c, od_w_in[j], od_w_out[j], swa_sink[j], swa_q_norm[j],
                                 swa_k_norm[j], cos, sin, not last)
        x = x + mod_l[2] * m_l
        moe_p = (router_group_w[i], router_group_b[i], router_expert_w[i], router_expert_b[i],
                 exp_w_gate[i], exp_w_up[i], exp_w_down[i])
        if last:
            f_l = hier_moe(modulate(x, norm_ffn[i], mod_l[3], mod_l[4]).reshape(B * S, D), *moe_p)
            x = x + mod_l[5] * f_l.reshape(B, S, D)
        else:
            ctx = ctx + mod_c[2] * m_c
            tok = jnp.concatenate([modulate(x, norm_ffn[i], mod_l[3], mod_l[4]).reshape(B * S, D),
                                   modulate(ctx, norm_ffn[i], mod_c[3], mod_c[4]).reshape(-1, D)], axis=0)
            f = hier_moe(tok, *moe_p)
            x = x + mod_l[5] * f[:B * S].reshape(B, S, D)
            ctx = ctx + mod_c[5] * f[B * S:].reshape(ctx.shape)
    return x
```

```python
import numpy as np
import ml_dtypes
from contextlib import ExitStack
import concourse.bass as bass
import concourse.mybir as mybir
from concourse.bass_utils import run_bass_kernel_spmd

F32 = mybir.dt.float32
BF16 = mybir.dt.bfloat16
ALU = mybir.AluOpType
AF = mybir.ActivationFunctionType
AX = mybir.AxisListType

import os
COMPUTE = ("pe", "act", "dve", "pool")
SCHED_W = int(os.environ.get("SCHED_W", "128"))
NDMASEM = 12

D = 1024
SEQ = 4096
NOWN = 17
NTOK0 = 19
EVEN_IN = 2336
NEG_BIG = 1.0e30


class Buf:
    __slots__ = ("name", "w", "r")

    def __init__(self, name=""):
        self.name = name
        self.w = None
        self.r = {}


class Prog:
    def __init__(self, nc):
        self.nc = nc
        self.ops = []
        self.cost = []
        self.reorder = True
        self.base = set()
        self.eng = {"pe": nc.tensor, "act": nc.scalar, "dve": nc.vector,
                    "pool": nc.gpsimd, "sp": nc.sync}

    def op(self, eng, fn, reads=(), writes=(), dma=False, cost=0.4):
        idx = len(self.ops)
        deps = set(self.base)
        for b in reads:
            if b.w is not None:
                deps.add(b.w)
        for b in writes:
            if b.w is not None:
                deps.add(b.w)
            for v in b.r.values():
                if isinstance(v, list):
                    deps.update(v)
                else:
                    deps.add(v)
        key = (eng, dma)
        for b in reads:
            if dma:
                b.r.setdefault(key, []).append(idx)
            else:
                b.r[key] = idx
        for b in writes:
            b.w = idx
            b.r = {}
        deps.discard(idx)
        self.ops.append((eng, dma, fn, deps))
        self.cost.append(cost)
        return idx

    def barrier(self):
        last = {}
        dm = {}
        for i, (eng, dma, fn, deps) in enumerate(self.ops):
            if dma:
                dm.setdefault(eng, []).append(i)
            else:
                last[eng] = i
        base = set(last.values())
        for q, lst in dm.items():
            base.update(lst[-NDMASEM:])
        self.base = base

    @staticmethod
    def _fs(ap):
        n = 1
        for s in ap.shape[1:]:
            n *= s
        return n

    def dma(self, out, in_, reads=(), writes=(), eng="sp"):
        e = self.eng[eng]
        nb = self._fs(out) * out.shape[0] * (2 if out.dtype == BF16 else 4)
        return self.op(eng, lambda: e.dma_start(out=out, in_=in_), reads, writes, dma=True,
                       cost=2.2 + nb / 150e3)

    def mm(self, out, lhsT, rhs, start=True, stop=True, reads=(), writes=()):
        nc = self.nc
        c = 0.07 + self._fs(rhs) * 0.0006
        if rhs.dtype == F32:
            c *= 4
        return self.op("pe", lambda: nc.tensor.matmul(out, lhsT, rhs, start=start, stop=stop),
                       reads, writes, cost=c)

    def tr(self, out, in_, ident, reads=(), writes=()):
        nc = self.nc
        return self.op("pe", lambda: nc.tensor.transpose(out, in_, ident), reads, writes, cost=0.12)

    def act(self, out, in_, func, reads=(), writes=(), **kw):
        nc = self.nc
        return self.op("act", lambda: nc.scalar.activation(out=out, in_=in_, func=func, **kw),
                       reads, writes, cost=0.25 + self._fs(in_) * 0.0009)

    def v(self, eng, name, *args, reads=(), writes=(), **kw):
        f = getattr(self.eng[eng], name)
        c = 0.15 + self._fs(args[0]) * 0.0011
        if eng == "pool":
            c = 0.3 + self._fs(args[0]) * 0.0025
        return self.op(eng, lambda: f(*args, **kw), reads, writes, cost=c)

    def schedule(self, W=SCHED_W):
        ops, cost = self.ops, self.cost
        n = len(ops)
        fin = [None] * n
        start = [0.0] * n
        rem = {}
        for i, (eng, dma, fn, deps) in enumerate(ops):
            rem.setdefault(eng, []).append(i)
        etime = {e: 0.0 for e in rem}
        nsched = 0
        while nsched < n:
            best = None
            for e, lst in rem.items():
                if not lst:
                    continue
                te = etime[e]
                for i in lst[:W]:
                    deps = ops[i][3]
                    r = te
                    ok = True
                    for d in deps:
                        f = fin[d]
                        if f is None:
                            ok = False
                            break
                        if f > r:
                            r = f
                    if not ok:
                        continue
                    key = (r, i)
                    if best is None or key < best[0]:
                        best = (key, e, i)
                    if r <= te:
                        break
            (r, i), e, _ = best
            eng, dma, fn, deps = ops[i]
            start[i] = r
            if dma:
                etime[e] = r + 0.5
                fin[i] = r + cost[i]
            else:
                etime[e] = r + cost[i]
                fin[i] = r + cost[i] + 0.1
            rem[e].remove(i)
            nsched += 1
        order = sorted(range(n), key=lambda j: (start[j], j))
        self.sim_time = max(f for f in fin)
        self.sim_start, self.sim_fin = start, fin
        return order

    def emit(self, sems):
        ops = self.ops
        n = len(ops)
        need = [False] * n
        for (eng, dma, fn, deps) in ops:
            for d in deps:
                deng, ddma, _, _ = ops[d]
                if (not ddma) and deng == "pe" and eng == "pe" and not dma:
                    continue
                need[d] = True
        sig = [None] * n
        cnt = {e: 0 for e in COMPUTE}
        dcnt = {}
        waited = {}
        order = self.schedule() if self.reorder else range(n)
        for i in order:
            eng, dma, fn, deps = ops[i]
            e = self.eng[eng]
            w = {}
            for d in deps:
                deng, ddma, _, _ = ops[d]
                if (not ddma) and deng == "pe" and eng == "pe" and not dma:
                    continue
                s, val = sig[d]
                if w.get(id(s), (None, -1))[1] < val:
                    w[id(s)] = (s, val)
            if dma:
                j = dcnt.get(eng, 0)
                pool = sems[("dma", eng)]
                s = pool[j % len(pool)]
                val = 16 * (j // len(pool) + 1)
                if val > 16 and w.get(id(s), (None, -1))[1] < val - 16:
                    w[id(s)] = (s, val - 16)
                dcnt[eng] = j + 1
                sig[i] = (s, val)
            for sid, (s, val) in w.items():
                k = (eng, sid)
                if waited.get(k, -1) >= val:
                    continue
                waited[k] = val
                e.wait_ge(s, val)
            ins = fn()
            if dma:
                ins.then_inc(sig[i][0], 16)
            elif need[i]:
                cnt[eng] += 1
                sig[i] = (sems[eng], cnt[eng])
                ins.then_inc(sems[eng], 1)
        for (k, pool) in sems.items():
            if isinstance(k, tuple):
                eng = k[1]
                j = dcnt.get(eng, 0)
                e = self.eng[eng]
                for q, s in enumerate(pool):
                    c = (j - q + len(pool) - 1) // len(pool) if j > q else 0
                    if c > 0:
                        e.wait_ge(s, 16 * c)
        return dict(n_ops=n, sig=cnt, dmas=dcnt)


class K:
    pass


def build(stages=("all",), dbg=()):
    nc = bass.Bass("TRN2", target_bir_lowering=False)
    P = Prog(nc)
    k = K()
    k.nc, k.P = nc, P
    k.dbgset = set(dbg)
    es = ExitStack()
    k.es = es
    k.dbg = {}

    def din(name, shape, dt=F32):
        return nc.dram_tensor(name, list(shape), dt, kind="ExternalInput").ap()

    def dscr(name, shape, dt=F32):
        if name in dbg:
            return nc.dram_tensor(name, list(shape), dt, kind="ExternalOutput").ap()
        return nc.dram_tensor(name, list(shape), dt).ap()

    def dout(name, shape, dt=F32):
        return nc.dram_tensor(name, list(shape), dt, kind="ExternalOutput").ap()

    def sb(name, shape, dt=F32):
        return es.enter_context(nc.sbuf_tensor("sb_" + name, list(shape), dt))

    def ps(name, shape, dt=F32):
        return es.enter_context(nc.psum_tensor("ps_" + name, list(shape), dt))

    k.din, k.dscr, k.dout, k.sb, k.ps = din, dscr, dout, sb, ps
    ARW = 41700
    k.arena = None
    k.aoff = 0

    def ar(name, shape, dt=F32):
        if k.arena is None:
            k.arena = sb("arena", [128, ARW])
        n = 1
        for s in shape[1:]:
            n *= s
        w = n if dt == F32 else (n + 1) // 2
        off = k.aoff
        k.aoff += w + (w % 2)
        assert k.aoff <= ARW, (name, k.aoff)
        a = k.arena[0:shape[0], off:off + w]
        if dt != F32:
            a = a.bitcast(dt)
        if len(shape) > 2:
            names = " ".join("d%d" % i for i in range(1, len(shape)))
            kw = {"d%d" % i: shape[i] for i in range(1, len(shape))}
            a = a.rearrange("p (%s) -> p %s" % (names, names), **kw)
        return a

    def areset(mark=0):
        P.barrier()
        k.peak = getattr(k, "peak", [])
        k.peak.append(k.aoff * 4 // 1024)
        k.marks = getattr(k, "marks", [])
        k.marks.append(len(P.ops))
        k.aoff = mark

    k.ar, k.areset = ar, areset

    with es:
        sems = {e: es.enter_context(nc.semaphore("s_" + e)) for e in COMPUTE}
        for q in ("sp", "pool"):
            sems[("dma", q)] = [es.enter_context(nc.semaphore(f"d_{q}{i}")) for i in range(NDMASEM)]
        body(k, stages, dbg)
        st = P.emit(sems)
        k.stats = st
    return nc, k


def body(k, stages, dbg):
    nc, P = k.nc, k.P
    din, dscr, dout, sb, ps, ar = k.din, k.dscr, k.dout, k.sb, k.ps, k.ar

    x_d = din("x", [SEQ, D])
    ctx_d = din("ctx", [256, D])
    crow_d = din("crow", [2, D])
    modw_d = din("mod_w", [2, D, 6 * D])
    modb_d = din("mod_b", [2, 6 * D])
    nmix_d = din("norm_mix", [2, D])
    nffn_d = din("norm_ffn", [2, D])
    evin_d = din("ev_w_in", [D, EVEN_IN])
    evout_d = din("ev_w_out", [D, D])
    gwext_d = din("gw_ext", [33, 512])
    glan_d = din("gla_out_norm", [1, 128])
    aqn_d = din("att_q_norm", [1, 64])
    akn_d = din("att_k_norm", [1, 64])
    odin_d = din("od_w_in", [D, 1280])
    odout_d = din("od_w_out", [D, D])
    sink_d = din("swa_sink", [1, 16])
    sqn_d = din("swa_q_norm", [1, 64])
    skn_d = din("swa_k_norm", [1, 64])
    rw_d = din("router_w", [2, D, 20])
    rb_d = din("router_b", [2, 1, 20])
    wg_d = din("exp_w_gate", [2, 16, D, 256])
    wu_d = din("exp_w_up", [2, 16, D, 256])
    wd_d = din("exp_w_down", [2, 16, 256, D])
    ropeC_d = din("ropeC", [SEQ, 64])
    ropeS_d = din("ropeS", [SEQ, 64])
    identF_d = din("identF", [128, 128])
    identB_d = din("identB", [128, 128], BF16)
    tri_d = din("tri", [128, 4, 128])
    maskAB_d = din("maskAB", [128, 2, 256], BF16)
    sel2_d = din("sel2", [2, 2, 128])
    wmask_d = din("wmask", [128, 2, 128], BF16)
    out_d = dout("out", [2048, D])

    X1_d = dscr("X1", [NTOK0 * 128, D])
    X2_d = dscr("X2", [NTOK0 * 128, D])
    X3_d = dscr("X3", [2048, D])
    OA_d = dscr("OA", [NTOK0, 128, 512])
    QT_d = dscr("QT", [NTOK0, 64, 8, 128], BF16)
    MIXT_d = dscr("MIXT", [NTOK0, 128, 8, 128], BF16)
    PB_lt = dscr("PB_lt", [NTOK0, 64, 512], BF16)
    PB_ke = dscr("PB_ke", [NTOK0, 64, 512], BF16)
    PB_kd = dscr("PB_kd", [NTOK0, 128, 256], BF16)
    PB_v = dscr("PB_v", [NTOK0, 128, 512], BF16)
    PB_ee = dscr("PB_ee", [NTOK0, 64, 8])
    PB_rg = dscr("PB_rg", [NTOK0, 128, 512])
    QT1_d = dscr("QT1", [16, 64, 16, 128], BF16)

    identF = sb("identF", [128, 128]); identB = sb("identB", [128, 128], BF16)
    b_const = Buf("const")
    for t, d in ((identF, identF_d), (identB, identB_d)):
        P.dma(t[:], d, writes=[b_const], eng="pool")
    k.identF, k.identB, k.b_const = identF, identB, b_const

    psall = ps("psall", [128, 4096])
    k.psall = psall
    pA, pB, pC, pD, pE, pF, pG, pH = [psall[:, i * 512:(i + 1) * 512] for i in range(8)]
    bA, bB, bC, bD, bE, bF_, bG, bH = [Buf("ps%d" % i) for i in range(8)]
    k.psum = [(pA, bA), (pB, bB), (pC, bC), (pD, bD), (pE, bE), (pF, bF_), (pG, bG), (pH, bH)]

    crow = ar("crow", [2, D]); b_crow = Buf()
    scT = sb("scT", [128, 8, 2]); b_scT = Buf()
    P.dma(crow[:], crow_d, writes=[b_crow])
    P.act(crow[:], crow[:], AF.Silu, reads=[b_crow], writes=[b_crow])
    for c in range(8):
        P.tr(pA[:, 2 * c:2 * c + 2], crow[:, c * 128:(c + 1) * 128], identF[0:2, 0:2],
             reads=[b_crow, b_const], writes=[bA])
    P.v("dve", "tensor_copy", scT[:].rearrange("p c t -> p (c t)"), pA[:, 0:16], reads=[bA], writes=[b_scT])
    gcol = sb("gcol", [128, 2, 2, 2, 8, 2])
    b_gcol = [Buf(), Buf()]
    gateB_all = sb("gateB", [128, 6, D])
    b_gateB = [Buf(), Buf()]
    k.gslot = {(0, 0, 0): 0, (0, 0, 1): 1, (0, 1, 0): 2, (0, 1, 1): 3, (1, 0, 0): 4, (1, 1, 0): 5}
    k.gcol, k.b_gcol = gcol, b_gcol
    k.gateB_all, k.b_gateB = gateB_all, b_gateB

    def s0_steps(l, tg, bankT, bank1, bank2):
        st = {}

        def alloc():
            st["modv"] = ar("modv" + tg, [2, 6 * D]); st["b_modv"] = Buf()
            st["modb"] = [ar("modb%d" % i + tg, [2, 512]) for i in range(2)]; st["b_modb"] = [Buf(), Buf()]
            st["wst"] = [ar("modwst%d" % i + tg, [128, 8, 512]) for i in range(2)]; st["b_wst"] = [Buf(), Buf()]
            st["nrm"] = ar("nrm" + tg, [2, D]); st["b_nrm"] = Buf()
            st["grow"] = st["nrm"]; st["b_grow"] = st["b_nrm"]
            st["sel2"] = ar("sel2" + tg, [2, 2, 128]); st["b_sel2"] = Buf()
            P.dma(st["sel2"][:], sel2_d, writes=[st["b_sel2"]], eng="pool")

        def blk(j):
            if j == 0:
                alloc()
            modv, b_modv = st["modv"], st["b_modv"]
            w = st["wst"][j % 2]; bw = st["b_wst"][j % 2]
            mb = st["modb"][j % 2]; bmb = st["b_modb"][j % 2]
            pz, bz = (bank1, bank2)[j % 2]
            for r in range(2):
                P.dma(mb[r:r + 1, :], modb_d[l:l + 1, j * 512:(j + 1) * 512], writes=[bmb], eng="pool")
            P.dma(w[:], modw_d[l].rearrange("(c p) n -> p c n", p=128)[:, :, j * 512:(j + 1) * 512],
                  writes=[bw], eng=("sp" if j % 2 == 0 else "pool"))
            for c in range(8):
                P.mm(pz[0:2, :], scT[:, c, :], w[:, c, :], c == 0, c == 7, reads=[b_scT, bw], writes=[bz])
            P.v("dve", "tensor_tensor", modv[:, j * 512:(j + 1) * 512], pz[0:2, :], mb[:],
                ALU.add, reads=[bz, bmb], writes=[b_modv])

        def fin():
            modv, b_modv = st["modv"], st["b_modv"]
            nrm, b_nrm, grow, b_grow = st["nrm"], st["b_nrm"], st["grow"], st["b_grow"]
            pT_, bT_ = bankT
            for sub in range(2):
                nd = (nmix_d, nffn_d)[sub]
                for r in range(2):
                    P.dma(nrm[r:r + 1, :], nd[l:l + 1, :], writes=[b_nrm], eng="pool")
                base = sub * 3 * D
                P.v("dve", "scalar_tensor_tensor", grow[:], modv[:, base + D:base + 2 * D], 1.0, nrm[:],
                    ALU.add, ALU.mult, reads=[b_modv, b_nrm], writes=[b_grow])
                for gi, src_ in enumerate((grow[:], modv[:, base:base + D])):
                    for c in range(8):
                        P.tr(pT_[:, 2 * c:2 * c + 2], src_[:, c * 128:(c + 1) * 128], identF[0:2, 0:2],
                             reads=[b_grow, b_modv, b_const], writes=[bT_])
                    P.v("dve", "tensor_copy", gcol[:, l, sub, gi].rearrange("p c t -> p (c t)"), pT_[:, 0:16],
                        reads=[bT_], writes=[b_gcol[l]])
            for sub in range(2):
                for w in range(2):
                    if l == 1 and w == 1:
                        continue
                    for hh in range(2):
                        pz, bz = (bank1, bank2)[hh]
                        col = (sub * 3 + 2) * D + hh * 512
                        P.mm(pz[:], st["sel2"][:, w, :], modv[:, col:col + 512], reads=[st["b_sel2"], b_modv], writes=[bz])
                        P.v("dve", "tensor_copy", gateB_all[:, k.gslot[(l, sub, w)], hh * 512:(hh + 1) * 512], pz[:],
                            reads=[bz], writes=[b_gateB[l]])

        return [(lambda j=j: blk(j)) for j in range(12)] + [fin]

    k.s0_steps = s0_steps
    for stp in s0_steps(0, "L0", k.psum[0], k.psum[1], k.psum[2]):
        stp()

    xt = [sb("xt%d" % i, [128, D]) for i in range(2)]; b_xt = [Buf(), Buf()]
    stat = [sb("stat%d" % i, [128, 8]) for i in range(2)]; b_stat = [Buf(), Buf()]
    xn = [sb("xn%d" % i, [128, D], BF16) for i in range(2)]; b_xn = [Buf(), Buf()]
    b_xnf = Buf()
    hT = [sb("hT%d" % i, [128, 8, 128], BF16) for i in range(2)]; b_hT = [Buf(), Buf()]
    b_hTf = Buf()
    k.cnt = 0

    def modulate(src_ap, l, sub, w, fp32=False, src_buf=None, xt_out=None):
        i = k.cnt % 2
        k.cnt += 1
        X, bX = xt[i], b_xt[i]
        P.dma(X[:], src_ap, reads=([src_buf] if src_buf else []), writes=[bX])
        st, bst = stat[i], b_stat[i]
        P.v("dve", "scalar_tensor_tensor", xn[i][:], X[:], 1.0, X[:], ALU.mult, ALU.mult,
            accum_out=st[:, 0:1], reads=[bX], writes=[b_xn[i], bst])
        P.act(st[:, 1:2], st[:, 0:1], AF.Ln, scale=1.0 / D, bias=1e-6, reads=[bst], writes=[bst])
        P.act(st[:, 2:3], st[:, 1:2], AF.Exp, scale=-0.5, reads=[bst], writes=[bst])
        if not fp32:
            N_, bN = xn[i], b_xn[i]
            H, bH_ = hT[i], b_hT[i]
            pz, bz = k.psum[0]
            pzv = pz[:].bitcast(BF16).rearrange("p (c t) -> p c t", c=8)
            P.v("dve", "tensor_scalar", N_[:], X[:], st[:, 2:3], None, ALU.mult, reads=[bX, bst], writes=[bN])
            for c in range(8):
                P.tr(pzv[:, c, :], N_[:, c * 128:(c + 1) * 128], identB[:], reads=[bN, b_const], writes=[bz])
            for c in range(8):
                P.act(H[:, c, :], pzv[:, c, :], AF.Identity, scale=gcol[:, l, sub, 0, c, w:w + 1],
                      bias=gcol[:, l, sub, 1, c, w:w + 1], reads=[bz, b_gcol[l]], writes=[bH_])
            return H, bH_, X, bX
        else:
            xnf, hTf = k.xnf, k.hTf
            P.v("dve", "tensor_scalar", xnf[:], X[:], st[:, 2:3], None, ALU.mult, reads=[bX, bst], writes=[b_xnf])
            for hh in range(2):
                pz, bz = k.psum[hh]
                for c in range(4):
                    cc = hh * 4 + c
                    P.tr(pz[:, c * 128:(c + 1) * 128], xnf[:, cc * 128:(cc + 1) * 128], identF[:],
                         reads=[b_xnf, b_const], writes=[bz])
                for c in range(4):
                    cc = hh * 4 + c
                    P.act(hTf[:, cc, :], pz[:, c * 128:(c + 1) * 128], AF.Identity,
                          scale=gcol[:, l, sub, 0, cc, w:w + 1], bias=gcol[:, l, sub, 1, cc, w:w + 1],
                          reads=[bz, b_gcol[l]], writes=[b_hTf])
            return hTf, b_hTf, X, bX

    k.modulate = modulate

    def cast(eng, dst, src, reads, writes):
        if eng == "act":
            P.act(dst, src, AF.Copy, reads=reads, writes=writes)
        else:
            P.v(eng, "tensor_copy", dst, src, reads=reads, writes=writes)

    k.cast = cast

    def load_weight_bf16(dst, b_dst, src_ap_fn, nchunk, ncol, stage, b_stage, cast_engs=("pool", "dve", "act")):
        for c in range(nchunk):
            s, bs = stage[c % 2], b_stage[c % 2]
            P.dma(s[:, 0:ncol], src_ap_fn(c), writes=[bs], eng=("sp" if c % 2 == 0 else "pool"))
            cast(cast_engs[c % len(cast_engs)], dst[:, c, :], s[:, 0:ncol], [bs], [b_dst])

    k.b_wstage = [Buf(), Buf()]
    k.load_weight_bf16 = load_weight_bf16

    def bcast_row(name, d_ap, n):
        t = sb(name, [128, n])
        P.dma(t[:], d_ap.partition_broadcast(128).rearrange("p o n -> p (o n)"), writes=[b_const], eng="pool")
        return t

    aqn = bcast_row("aqn", aqn_d, 64); akn = bcast_row("akn", akn_d, 64)
    sqn = bcast_row("sqn", sqn_d, 64); skn = bcast_row("skn", skn_d, 64)
    glan = bcast_row("glan", glan_d, 128)
    sinkb = bcast_row("sinkb", sink_d, 16)

    bnd = sb("bnd", [128, 8]); b_bnd = Buf()

    def make_bound(col, gq, gk):
        P.v("dve", "tensor_reduce", bnd[:, col:col + 1], gq[:], AX.X, ALU.max, apply_absolute_value=True,
            reads=[b_const], writes=[b_bnd])
        P.v("dve", "tensor_reduce", bnd[:, col + 1:col + 2], gk[:], AX.X, ALU.max, apply_absolute_value=True,
            reads=[b_const], writes=[b_bnd])
        P.v("dve", "scalar_tensor_tensor", bnd[:, col + 2:col + 3], bnd[:, col:col + 1], -8.0, bnd[:, col + 1:col + 2],
            ALU.mult, ALU.mult, reads=[b_bnd], writes=[b_bnd])
    make_bound(0, aqn, akn)
    make_bound(4, sqn, skn)
    k.bnd, k.b_bnd = bnd, b_bnd

    def head_norm_rope(dst, b_dst, src, b_src, nh, gain, rope, tmp, b_tmp, scr, b_scr):
        s3 = src.rearrange("p (h d) -> p h d", h=nh)
        t3 = tmp[:, 0:nh * 64].rearrange("p (h d) -> p h d", h=nh)
        P.v("dve", "tensor_tensor", t3, s3, s3, ALU.mult, reads=[b_src], writes=[b_tmp])
        P.v("dve", "tensor_reduce", scr[:, 0:nh], t3, AX.X, ALU.add, reads=[b_tmp], writes=[b_scr])
        P.act(scr[:, 16:16 + nh], scr[:, 0:nh], AF.Ln, scale=1.0 / 64, bias=1e-6, reads=[b_scr], writes=[b_scr])
        P.act(scr[:, 32:32 + nh], scr[:, 16:16 + nh], AF.Exp, scale=-0.5, reads=[b_scr], writes=[b_scr])
        rs = scr[:, 32:32 + nh]
        P.v("dve", "tensor_tensor", t3, s3, rs.unsqueeze(2).to_broadcast([128, nh, 64]), ALU.mult,
            reads=[b_src, b_scr], writes=[b_tmp])
        g3 = gain[:, 0:64].unsqueeze(1).to_broadcast([128, nh, 64])
        if rope is None:
            d3 = dst.rearrange("p (h d) -> p h d", h=nh)
            P.v("dve", "tensor_tensor", d3, t3, g3, ALU.mult, reads=[b_tmp, b_const], writes=[b_dst])
            return
        C, S, b_rope, tmp2, b_tmp2 = rope
        P.v("dve", "tensor_tensor", t3, t3, g3, ALU.mult, reads=[b_tmp, b_const], writes=[b_tmp])
        u3 = tmp2[:, 0:nh * 64].rearrange("p (h d) -> p h d", h=nh)
        P.v("pool", "tensor_tensor", u3, t3, C[:, 0:64].unsqueeze(1).to_broadcast([128, nh, 64]), ALU.mult,
            reads=[b_tmp, b_rope], writes=[b_tmp2])
        t5 = tmp[:, 0:nh * 64].rearrange("p (h a f e) -> p h a f e", h=nh, a=2, f=2)
        S5 = S[:, 0:64].rearrange("p (a f e) -> p a f e", a=2, f=2)
        d5 = dst.rearrange("p (h a f e) -> p h a f e", h=nh, a=2, f=2)
        u5 = tmp2[:, 0:nh * 64].rearrange("p (h a f e) -> p h a f e", h=nh, a=2, f=2)
        for f in range(2):
            sw = t5[:, :, :, 1 - f, :]
            sv = S5[:, :, f, :].unsqueeze(1).to_broadcast([128, nh, 2, 16])
            w_ = scr
            P.v("dve", "tensor_tensor", sw_tmp(k, nh)[:, :, :, f, :], sw, sv, ALU.mult,
                reads=[b_tmp, b_rope], writes=[k.b_swt])
        st5 = sw_tmp(k, nh)
        P.v("dve", "tensor_tensor", d5, st5, u5, ALU.add, reads=[k.b_swt, b_tmp2], writes=[b_dst])

    k.b_swt = Buf()

    def sw_tmp(k_, nh):
        return k_.swt[:, 0:nh * 64].rearrange("p (h a f e) -> p h a f e", h=nh, a=2, f=2)

    k.head_norm_rope = head_norm_rope

    from_l0 = dict(locals())
    k.env = from_l0
    k.areset(0)
    if "stopS0" in dbg:
        return
    layer0(k, stages, dbg)


def layer0(k, stages, dbg):
    g = k.env
    nc, P = k.nc, k.P
    sb, ps, dout = k.ar, k.ps, k.dout
    identF, identB, b_const = g["identF"], g["identB"], g["b_const"]
    modulate = k.modulate
    psum = k.psum
    x_d, ctx_d = g["x_d"], g["ctx_d"]
    (OA_d, QT_d, MIXT_d, PB_lt, PB_ke, PB_kd, PB_v, PB_ee, PB_rg, X1_d) = (
        g["OA_d"], g["QT_d"], g["MIXT_d"], g["PB_lt"], g["PB_ke"], g["PB_kd"], g["PB_v"], g["PB_ee"], g["PB_rg"], g["X1_d"])
    aqn, akn, glan = g["aqn"], g["akn"], g["glan"]
    bnd, b_bnd = k.bnd, k.b_bnd
    gateB_all, b_gateB = k.gateB_all, k.b_gateB
    b_scr = {n: Buf(n) for n in ("OA", "QT", "MIXT", "PBlt", "PBke", "PBkd", "PBv", "PBee", "PBrg", "X1")}
    k.b_scr = b_scr

    KT = sb("KT", [128, 2, 34 * 128], BF16); b_KT = Buf()
    P.v("pool", "memset", KT[64:128], 0.0, writes=[b_KT])
    Vt = sb("Vt", [128, 34, 2, 66], BF16); b_Vt = Buf()
    P.v("pool", "memset", Vt[:], 1.0, writes=[b_Vt])
    mark = k.aoff
    k.swt = sb("swt", [128, 1024])
    w_in = sb("w_in", [128, 8, EVEN_IN], BF16); b_win = Buf()
    tri = sb("tri", [128, 4, 128]); maskAB = sb("maskAB", [128, 2, 256], BF16)
    P.dma(tri[:], g["tri_d"], writes=[b_const], eng="pool")
    P.dma(maskAB[:], g["maskAB_d"], writes=[b_const], eng="pool")
    gwext = sb("gwext", [33, 512]); P.dma(gwext[:], g["gwext_d"], writes=[b_const], eng="pool")
    mark_w = k.aoff
    k.wstage = [sb("wstage%d" % i, [128, 1168]) for i in range(2)]
    evin_d = g["evin_d"]
    for hf in range(2):
        lo_ = hf * 1168
        k.load_weight_bf16(w_in[:, :, lo_:lo_ + 1168], b_win,
                           lambda c, lo_=lo_: evin_d[c * 128:(c + 1) * 128, lo_:lo_ + 1168], 8, 1168, k.wstage, k.b_wstage)
    P.barrier()
    k.aoff = mark_w

    z_sb = sb("z_sb", [128, EVEN_IN]); b_z = Buf()
    ropeC = sb("ropeC", [128, 64]); ropeS = sb("ropeS", [128, 64]); b_rope = Buf()
    tmpA = sb("tmpA", [128, 1024]); b_tmpA = Buf()
    tmpB = sb("tmpB", [128, 1024]); b_tmpB = Buf()
    scr = sb("scr", [128, 64]); b_scr_ = Buf()
    qb = sb("qb", [128, 512], BF16); b_qb = Buf()
    kb = sb("kb", [128, 128], BF16); b_kb = Buf()
    qT_sb = sb("qT_sb", [64, 8, 128], BF16); b_qTsb = Buf()
    lrT = sb("lrT", [33, 128]); b_lrT = Buf()
    P.v("pool", "memset", lrT[:], 1.0, writes=[b_lrT])
    e_sb = sb("e_sb", [128, 512]); b_e = Buf()
    sp_ = sb("sp_", [128, 512]); b_sp = Buf()
    gqT = sb("gqT", [64, 4, 128]); gkT = sb("gkT", [64, 4, 128]); b_gq = Buf(); b_gk = Buf()
    EbT = [sb("EbT%d" % i, [64, 4, 128]) for i in range(2)]; b_EbT = [Buf(), Buf()]
    EnbT = [sb("EnbT%d" % i, [64, 4, 128]) for i in range(2)]; b_EnbT = [Buf(), Buf()]
    kdec = sb("kdec", [128, 256]); b_kdec = Buf()
    LT = [sb("LT%d" % i, [128, 2, 4, 64], BF16) for i in range(2)]
    b_LTq = [Buf(), Buf()]; b_LTa = [Buf(), Buf()]
    keT = [sb("keT%d" % i, [64, 4, 128], BF16) for i in range(2)]; b_keT = [Buf(), Buf()]
    kd = [sb("kd%d" % i, [128, 256], BF16) for i in range(2)]; b_kd = [Buf(), Buf()]
    v_bf = sb("v_bf", [128, 512], BF16); b_vbf = Buf()
    vhi = sb("vhi", [128, 2, 512], BF16); b_vhi = Buf()
    eend = [sb("eend%d" % i, [64, 4, 2]) for i in range(2)]; b_eend = [Buf(), Buf()]
    RH = [[sb("RH%d%d" % (i, j), [128, 4, 128], BF16) for j in range(2)] for i in range(2)]
    b_RHs = [[Buf(), Buf()], [Buf(), Buf()]]; b_RHv = [[Buf(), Buf()], [Buf(), Buf()]]
    S32 = [sb("S32_%d" % i, [64, 4, 128]) for i in range(2)]; b_S32 = [Buf(), Buf()]
    par = [0, 0]
    for d_ in range(2):
        P.v("pool", "memset", S32[d_][:], 0.0, writes=[b_S32[d_]])
        for j in range(2):
            P.v("pool", "memset", RH[d_][j][:], 0.0, writes=[b_RHs[d_][j], b_RHv[d_][j]])
    oa_sb = sb("oa_sb", [128, 512]); b_oa = Buf()
    rg_sb = sb("rg_sb", [128, 512]); b_rg = Buf()
    mixT_sb = sb("mixT_sb", [128, 8, 128], BF16); b_mixT = Buf()

    def mkset2():
        z2 = sb("z_sb2", [128, EVEN_IN])
        qb2 = sb("qb2", [128, 512], BF16); kb2 = sb("kb2", [128, 128], BF16); qT2 = sb("qT_sb2", [64, 8, 128], BF16)
        lrT2 = sb("lrT2", [33, 128]); bl2 = Buf()
        P.v("pool", "memset", lrT2[:], 1.0, writes=[bl2])
        gq2 = sb("gqT2", [64, 4, 128]); gk2 = sb("gkT2", [64, 4, 128])
        kdec2 = sb("kdec2", [128, 256])
        LT2 = [sb("LT2%d" % i, [128, 2, 4, 64], BF16) for i in range(2)]
        keT2 = [sb("keT2%d" % i, [64, 4, 128], BF16) for i in range(2)]
        kd2 = [sb("kd2%d" % i, [128, 256], BF16) for i in range(2)]
        v2 = sb("v_bf2", [128, 512], BF16); vh2 = sb("vhi2", [128, 2, 512], BF16)
        ee2 = [sb("eend2%d" % i, [64, 4, 2]) for i in range(2)]
        oa2 = sb("oa_sb2", [128, 512]); rg2 = sb("rg_sb2", [128, 512])
        return (z2, Buf(), qb2, Buf(), kb2, Buf(), qT2, Buf(), lrT2, bl2, gq2, gk2, Buf(), Buf(), kdec2, Buf(),
                LT2, [Buf(), Buf()], [Buf(), Buf()], keT2, [Buf(), Buf()], kd2, [Buf(), Buf()], v2, Buf(), vh2, Buf(),
                ee2, [Buf(), Buf()], oa2, Buf(), rg2, Buf())

    SETS = [(z_sb, b_z, qb, b_qb, kb, b_kb, qT_sb, b_qTsb, lrT, b_lrT, gqT, gkT, b_gq, b_gk, kdec, b_kdec,
             LT, b_LTq, b_LTa, keT, b_keT, kd, b_kd, v_bf, b_vbf, vhi, b_vhi, eend, b_eend, oa_sb, b_oa, rg_sb, b_rg),
            mkset2()]

    pMisc, bMisc = psum[3]
    pZG, bZG = psum[4]
    pBT, bBT = psum[5]
    pAT, bAT = psum[7][0], Buf("pAT")
    pS, bS = psum[7]
    pPO, bPO = psum[6]

    def scan_tile(d_, lt, b_ltq, b_lta, ke, b_ke, kdt, b_kdt, vb, b_vb, vh, b_vh, ee, b_ee, with_out):
        order = (0, 1) if d_ == 0 else (1, 0)
        pAT4 = pAT[:, 0:256].rearrange("p (h t) -> p h t", h=4)
        pS4 = pS[0:64, :].rearrange("p (h v) -> p h v", h=4)
        po, bpo = pPO, bPO
        for ch in order:
            p_ = par[d_]
            rh, brs, brv = RH[d_][p_], b_RHs[d_][p_], b_RHv[d_][p_]
            if with_out:
                for h in range(4):
                    P.mm(pAT4[64:128, h, :], ke[0:64, h, ch * 64:(ch + 1) * 64], lt[0:64, ch, h, :],
                         reads=[b_ke, b_ltq], writes=[bAT])
                P.v("dve", "tensor_tensor", lt[64:128, ch, :, :], pAT4[64:128, :, :],
                    maskAB[64:128, d_, :].rearrange("p (h t) -> p h t", h=4), ALU.mult,
                    reads=[bAT, b_const], writes=[b_lta])
                P.v("pool", "tensor_copy", rh[64:128, :, :], vh[64:128, ch, :].rearrange("p (h v) -> p h v", h=4),
                    reads=[b_vh], writes=[brv])
                for h in range(4):
                    P.mm(po[ch * 64:(ch + 1) * 64, h * 128:(h + 1) * 128], lt[:, ch, h, :], rh[:, h, :],
                         reads=[b_ltq, b_lta, brs, brv], writes=[bpo])
            for h in range(4):
                P.mm(pS4[:, h, :], kdt[ch * 64:(ch + 1) * 64, h * 64:(h + 1) * 64],
                     vb[ch * 64:(ch + 1) * 64, h * 128:(h + 1) * 128], reads=[b_kdt, b_vb], writes=[bS])
            for h in range(4):
                P.v("dve", "scalar_tensor_tensor", S32[d_][:, h, :], S32[d_][:, h, :], ee[:, h, ch:ch + 1],
                    pS4[:, h, :], ALU.mult, ALU.add, reads=[b_S32[d_], b_ee, bS], writes=[b_S32[d_]])
            nrh = RH[d_][1 - p_]
            P.act(nrh[0:64, :, :], S32[d_][:], AF.Copy, reads=[b_S32[d_]], writes=[b_RHs[d_][1 - p_]])
            par[d_] = 1 - p_

    def gla_prep(dirs, need_q):
        P.tr(pBT[0:32, 0:128], z_sb[:, 1536:1568], identF[:], reads=[b_z, b_const], writes=[bBT])
        P.act(lrT[0:32, :], pBT[0:32, 0:128], AF.Copy, reads=[bBT], writes=[b_lrT])
        P.mm(pZG[:], lrT[:], gwext[:], reads=[b_lrT, b_const], writes=[bZG])
        P.act(e_sb[:], pZG[:], AF.Exp, scale=-1.0, reads=[bZG], writes=[b_e])
        P.act(sp_[:], e_sb[:], AF.Ln, bias=1.0, reads=[b_e], writes=[b_sp])
        pBT4 = pBT[0:64, :].rearrange("p (h t) -> p h t", h=4)
        if need_q:
            for h in range(4):
                P.tr(pBT4[:, h, :], z_sb[:, h * 64:(h + 1) * 64], identF[:], reads=[b_z, b_const], writes=[bBT])
            P.act(gqT[:], pBT4, AF.Copy, reads=[bBT], writes=[b_gq])
        for h in range(4):
            P.tr(pBT4[:, h, :], z_sb[:, 256 + h * 64:256 + (h + 1) * 64], identF[:], reads=[b_z, b_const], writes=[bBT])
        P.act(gkT[:], pBT4, AF.Copy, reads=[bBT], writes=[b_gk])
        P.v("pool", "tensor_copy", v_bf[:], z_sb[:, 512:1024], reads=[b_z], writes=[b_vbf])
        P.dma(vhi[64:128, 0, :], v_bf[0:64, :], reads=[b_vbf], writes=[b_vhi], eng="pool")
        P.v("pool", "tensor_copy", vhi[64:128, 1, :], v_bf[64:128, :], reads=[b_vbf], writes=[b_vhi])
        for d_ in dirs:
            for h in range(4):
                P.mm(pBT4[:, h, :], sp_[:, d_ * 256 + h * 64:d_ * 256 + (h + 1) * 64], tri[:, d_, :],
                     reads=[b_sp, b_const], writes=[bBT])
            P.act(EbT[d_][:], pBT4, AF.Exp, scale=-1.0 / 16, reads=[bBT], writes=[b_EbT[d_]])
            P.act(EnbT[d_][:], pBT4, AF.Exp, scale=1.0 / 16, reads=[bBT], writes=[b_EnbT[d_]])
            P.mm(pZG[:, 0:256], tri[:, 2 + d_, :], sp_[:, d_ * 256:(d_ + 1) * 256], reads=[b_sp, b_const], writes=[bZG])
            P.act(kdec[:], pZG[:, 0:256], AF.Exp, scale=-1.0 / 16, reads=[bZG], writes=[b_kdec])
            if need_q:
                for ch in range(2):
                    P.v("dve", "scalar_tensor_tensor", LT[d_][0:64, ch, :, :],
                        gqT[:, :, ch * 64:(ch + 1) * 64], 0.125,
                        EbT[d_][:, :, ch * 64:(ch + 1) * 64], ALU.mult, ALU.mult,
                        reads=[b_gq, b_EbT[d_]], writes=[b_LTq[d_]])
            P.v("dve", "tensor_tensor", keT[d_][:], gkT[:], EnbT[d_][:], ALU.mult,
                reads=[b_gk, b_EnbT[d_]], writes=[b_keT[d_]])
            P.v("dve", "tensor_tensor", kd[d_][:], z_sb[:, 256:512], kdec[:], ALU.mult,
                reads=[b_z, b_kdec], writes=[b_kd[d_]])
            cols = (63, 127) if d_ == 0 else (0, 64)
            for ch in range(2):
                P.v("dve", "tensor_copy", eend[d_][:, :, ch:ch + 1], EbT[d_][:, :, cols[ch]:cols[ch] + 1],
                    reads=[b_EbT[d_]], writes=[b_eend[d_]])

    def inproj(H, bH, blocks):
        for bi, (lo, hi) in enumerate(blocks):
            pz, bz = psum[1 + bi % 2]
            for c in range(8):
                P.mm(pz[:, 0:hi - lo], H[:, c, :], w_in[:, c, lo:hi], c == 0, c == 7, reads=[bH, b_win], writes=[bz])
            if bi % 2 == 0:
                P.act(z_sb[:, lo:hi], pz[:, 0:hi - lo], AF.Copy, reads=[bz], writes=[b_z])
            else:
                P.v("dve", "tensor_copy", z_sb[:, lo:hi], pz[:, 0:hi - lo], reads=[bz], writes=[b_z])

    FULL = ((0, 512), (512, 1024), (1024, 1536), (1536, 2048), (2048, 2336))
    RBLK = ((256, 768), (768, 1024), (1536, 1568), (2080, 2336))

    def modA(kind, ti):
        if kind == "ctx":
            return modulate(ctx_d[ti * 128:(ti + 1) * 128, :], 0, 0, 1)
        return modulate(x_d[ti * 128:(ti + 1) * 128, :], 0, 0, 0)

    def tileA(kind, ti, pre):
        if kind == "ctx":
            src, w, slot, kidx = ctx_d[ti * 128:(ti + 1) * 128, :], 1, NOWN + ti, ti
        else:
            src, w, slot, kidx = x_d[ti * 128:(ti + 1) * 128, :], 0, ti, 2 + ti
        H, bH, X, bX = pre
        inproj(H, bH, RBLK if kind == "R" else FULL)
        rope = None
        if kind != "ctx":
            P.dma(ropeC[:], g["ropeC_d"][ti * 128:(ti + 1) * 128, :], writes=[b_rope], eng="pool")
            P.dma(ropeS[:], g["ropeS_d"][ti * 128:(ti + 1) * 128, :], writes=[b_rope], eng="pool")
            rope = (ropeC, ropeS, b_rope, tmpB, b_tmpB)
        pM8 = pMisc[0:64, :].bitcast(BF16).rearrange("p (h t) -> p h t", h=8)
        if kind != "R":
            k.head_norm_rope(qb[:], b_qb, z_sb[:, 1568:2080], b_z, 8, aqn, rope, tmpA, b_tmpA, scr, b_scr_)
            for h in range(8):
                P.tr(pM8[:, h, :], qb[:, h * 64:(h + 1) * 64], identB[:], reads=[b_qb, b_const], writes=[bMisc])
            P.act(qT_sb[:], pM8, AF.Copy, reads=[bMisc], writes=[b_qTsb])
            P.dma(QT_d[slot], qT_sb[:], reads=[b_qTsb], writes=[b_scr["QT"]])
        k.head_norm_rope(kb[:], b_kb, z_sb[:, 2080:2208], b_z, 2, akn, rope, tmpA, b_tmpA, scr, b_scr_)
        for h in range(2):
            P.tr(pM8[:, h, :], kb[:, h * 64:(h + 1) * 64], identB[:], reads=[b_kb, b_const], writes=[bMisc])
        P.act(KT[0:64, :, kidx * 128:(kidx + 1) * 128], pM8[:, 0:2, :], AF.Copy, reads=[bMisc], writes=[b_KT])
        P.v("pool", "tensor_copy", Vt[:, kidx, :, 0:64], z_sb[:, 2208:2336].rearrange("p (h d) -> p h d", h=2),
            reads=[b_z], writes=[b_Vt])
        if kind == "R":
            gla_prep((1,), False)
            scan_tile(1, LT[1], b_LTq[1], b_LTa[1], keT[1], b_keT[1], kd[1], b_kd[1], v_bf, b_vbf, vhi, b_vhi,
                      eend[1], b_eend[1], False)
            return
        gla_prep((0, 1), True)
        P.act(rg_sb[:], z_sb[:, 1024:1536], AF.Silu, reads=[b_z], writes=[b_rg])
        P.v("pool", "tensor_tensor", rg_sb[:].rearrange("p (h v) -> p h v", h=4), rg_sb[:].rearrange("p (h v) -> p h v", h=4),
            glan[:, 0:128].unsqueeze(1).to_broadcast([128, 4, 128]), ALU.mult, reads=[b_rg, b_const], writes=[b_rg])
        P.dma(PB_rg[slot], rg_sb[:], reads=[b_rg], writes=[b_scr["PBrg"]])
        P.dma(PB_lt[slot], LT[1][0:64].rearrange("p c h t -> p (c h t)"), reads=[b_LTq[1]], writes=[b_scr["PBlt"]])
        P.dma(PB_ke[slot], keT[1][:].rearrange("p h t -> p (h t)"), reads=[b_keT[1]], writes=[b_scr["PBke"]])
        P.dma(PB_kd[slot], kd[1][:], reads=[b_kd[1]], writes=[b_scr["PBkd"]])
        P.dma(PB_v[slot], v_bf[:], reads=[b_vbf], writes=[b_scr["PBv"]])
        P.dma(PB_ee[slot], eend[1][:].rearrange("p h c -> p (h c)"), reads=[b_eend[1]], writes=[b_scr["PBee"]])
        scan_tile(0, LT[0], b_LTq[0], b_LTa[0], keT[0], b_keT[0], kd[0], b_kd[0], v_bf, b_vbf, vhi, b_vhi,
                  eend[0], b_eend[0], True)
        P.act(oa_sb[:], pPO[:], AF.Copy, reads=[bPO], writes=[b_oa])
        P.dma(OA_d[slot], oa_sb[:], reads=[b_oa], writes=[b_scr["OA"]])

    ob_sb = tmpB[:, 0:512]; b_ob = b_tmpB
    ab_sb = tmpB[:, 512:768].bitcast(BF16); b_ab = Buf()

    def tileB(slot):
        P.dma(LT[1][0:64].rearrange("p c h t -> p (c h t)"), PB_lt[slot], reads=[b_scr["PBlt"]], writes=[b_LTq[1]])
        P.dma(keT[1][:].rearrange("p h t -> p (h t)"), PB_ke[slot], reads=[b_scr["PBke"]], writes=[b_keT[1]])
        P.dma(kd[1][:], PB_kd[slot], reads=[b_scr["PBkd"]], writes=[b_kd[1]])
        P.dma(v_bf[:], PB_v[slot], reads=[b_scr["PBv"]], writes=[b_vbf])
        P.dma(vhi[64:128, 0, :], PB_v[slot][0:64, :], reads=[b_scr["PBv"]], writes=[b_vhi], eng="pool")
        P.dma(vhi[64:128, 1, :], PB_v[slot][64:128, :], reads=[b_scr["PBv"]], writes=[b_vhi], eng="pool")
        P.dma(eend[1][:].rearrange("p h c -> p (h c)"), PB_ee[slot], reads=[b_scr["PBee"]], writes=[b_eend[1]])
        P.dma(oa_sb[:], OA_d[slot], reads=[b_scr["OA"]], writes=[b_oa], eng="pool")
        P.dma(rg_sb[:], PB_rg[slot], reads=[b_scr["PBrg"]], writes=[b_rg], eng="pool")
        scan_tile(1, LT[1], b_LTq[1], b_LTa[1], keT[1], b_keT[1], kd[1], b_kd[1], v_bf, b_vbf, vhi, b_vhi,
                  eend[1], b_eend[1], True)
        P.v("dve", "tensor_tensor", ob_sb[:], pPO[:], oa_sb[:], ALU.add, reads=[bPO, b_oa], writes=[b_ob])
        o3 = ob_sb[:].rearrange("p (h v) -> p h v", h=4)
        t3 = tmpA[:, 0:512].rearrange("p (h v) -> p h v", h=4)
        P.v("dve", "tensor_tensor", t3, o3, o3, ALU.mult, reads=[b_ob], writes=[b_tmpA])
        P.v("dve", "tensor_reduce", scr[:, 0:4], t3, AX.X, ALU.add, reads=[b_tmpA], writes=[b_scr_])
        P.act(scr[:, 16:20], scr[:, 0:4], AF.Ln, scale=1.0 / 128, bias=1e-6, reads=[b_scr_], writes=[b_scr_])
        P.act(scr[:, 32:36], scr[:, 16:20], AF.Exp, scale=-0.5, reads=[b_scr_], writes=[b_scr_])
        P.v("dve", "tensor_tensor", t3, o3, scr[:, 32:36].unsqueeze(2).to_broadcast([128, 4, 128]), ALU.mult,
            reads=[b_ob, b_scr_], writes=[b_tmpA])
        P.v("dve", "tensor_tensor", ab_sb[:], tmpA[:, 0:512], rg_sb[:], ALU.mult, reads=[b_tmpA, b_rg], writes=[b_ab])
        pM4 = pMisc[:].bitcast(BF16)[:, 0:512].rearrange("p (c t) -> p c t", c=4)
        for c in range(4):
            P.tr(pM4[:, c, :], ab_sb[:, c * 128:(c + 1) * 128], identB[:], reads=[b_ab, b_const], writes=[bMisc])
        P.act(mixT_sb[:, 0:4, :], pM4, AF.Copy, reads=[bMisc], writes=[b_mixT])
        P.dma(MIXT_d[slot][:, 0:4, :], mixT_sb[:, 0:4, :], reads=[b_mixT], writes=[b_scr["MIXT"]])

    seqA = [("ctx", 0), ("ctx", 1)] + [("R", ti) for ti in range(31, NOWN - 1, -1)] + [("own", ti) for ti in range(NOWN)]
    pre = modA(*seqA[0])
    nset = 0
    for si, (kind, ti) in enumerate(seqA):
        nxt = modA(*seqA[si + 1]) if si + 1 < len(seqA) else None
        (z_sb, b_z, qb, b_qb, kb, b_kb, qT_sb, b_qTsb, lrT, b_lrT, gqT, gkT, b_gq, b_gk, kdec, b_kdec,
         LT, b_LTq, b_LTa, keT, b_keT, kd, b_kd, v_bf, b_vbf, vhi, b_vhi, eend, b_eend, oa_sb, b_oa, rg_sb, b_rg) = SETS[nset % 2]
        nset += 1
        tileA(kind, ti, pre)
        pre = nxt
        if (kind, ti) == ("ctx", 1):
            for sl in (NOWN + 1, NOWN + 0):
                (z_sb, b_z, qb, b_qb, kb, b_kb, qT_sb, b_qTsb, lrT, b_lrT, gqT, gkT, b_gq, b_gk, kdec, b_kdec,
                 LT, b_LTq, b_LTa, keT, b_keT, kd, b_kd, v_bf, b_vbf, vhi, b_vhi, eend, b_eend, oa_sb, b_oa, rg_sb, b_rg) = SETS[nset % 2]
                nset += 1
                tileB(sl)
    for ti in range(NOWN - 1, -1, -1):
        (z_sb, b_z, qb, b_qb, kb, b_kb, qT_sb, b_qTsb, lrT, b_lrT, gqT, gkT, b_gq, b_gk, kdec, b_kdec,
         LT, b_LTq, b_LTa, keT, b_keT, kd, b_kd, v_bf, b_vbf, vhi, b_vhi, eend, b_eend, oa_sb, b_oa, rg_sb, b_rg) = SETS[nset % 2]
        nset += 1
        tileB(ti)

    if "stopAB" in dbg:
        return
    k.areset(mark)
    k.wstage = [sb("wstage%d" % i, [128, D]) for i in range(2)]
    w_out = sb("w_out", [128, 4, D], BF16); b_wout = Buf()
    w_outB = sb("w_outB", [64, 8, D], BF16); b_woutB = Buf()
    evout_d = g["evout_d"]
    k.load_weight_bf16(w_out, b_wout, lambda c: evout_d[c * 128:(c + 1) * 128, :], 4, D, k.wstage, k.b_wstage)
    for h in range(8):
        s_, bs_ = k.wstage[h % 2], k.b_wstage[h % 2]
        P.dma(s_[0:64, :], evout_d[512 + h * 64:512 + (h + 1) * 64, :], writes=[bs_], eng=("sp" if h % 2 == 0 else "pool"))
        k.cast(("pool", "dve", "act")[h % 3], w_outB[:, h, :], s_[0:64, :], [bs_], [b_woutB])
    ones_f = sb("ones_f", [128, 64]); P.v("pool", "memset", ones_f[:], 1.0, writes=[b_const])
    qT_in = [sb("qT_in%d" % i, [128, 8, 128], BF16) for i in range(2)]; b_qTin = [Buf(), Buf()]
    for i_ in range(2):
        P.v("pool", "memset", qT_in[i_][64:128], 0.0, writes=[b_qTin[i_]])
    rsr = sb("rsr", [128, 512]); b_rsr = Buf()
    bc_sb = sb("bc_sb", [64, 512]); b_bc = Buf()
    OT = sb("OT", [64, 8, 128], BF16); b_OT = Buf()
    mT_in = sb("mT_in", [128, 4, 128], BF16); b_mTin = Buf()
    x1_sb = sb("x1_sb", [128, D]); b_x1 = Buf()
    pO = [psum[6], psum[7]]
    SG = [(k.psall[:, 2 * g_ * 512:(2 * g_ + 2) * 512], [psum[2 * g_][1], psum[2 * g_ + 1][1]]) for g_ in range(3)]
    pBC, bBC = psum[0]
    NPT = 4
    PT = [sb("PTp%d" % i, [128, 1024], BF16) for i in range(NPT)]; b_PT = [Buf() for _ in range(NPT)]
    cstate = dict(it=0)

    mT_ins = [mT_in, sb("mT_in2", [128, 4, 128], BF16)]; b_mTins = [b_mTin, Buf()]
    x_ins = [sb("xC%d" % i, [128, D]) for i in range(2)]; b_xins = [Buf(), Buf()]
    ones_b = sb("ones_b", [128, 64], BF16); P.v("pool", "memset", ones_b[:], 1.0, writes=[b_const])
    rsb = sb("rsb", [128, 2, 512], BF16); b_rsb = Buf()
    bcs = [sb("bcs%d" % i, [64, 512]) for i in range(2)]; b_bcs = [Buf(), Buf()]
    cpre = dict(n=0)

    def loadC(kind, ti):
        if kind == "ctx":
            slot, src = NOWN + ti, ctx_d[ti * 128:(ti + 1) * 128, :]
        else:
            slot, src = ti, x_d[ti * 128:(ti + 1) * 128, :]
        i = cpre["n"] % 2
        cpre["n"] += 1
        P.dma(qT_in[i][0:64], QT_d[slot], reads=[b_scr["QT"]], writes=[b_qTin[i]])
        P.dma(mT_ins[i][:], MIXT_d[slot][:, 0:4, :], reads=[b_scr["MIXT"]], writes=[b_mTins[i]], eng="pool")
        P.dma(x_ins[i][:], src, writes=[b_xins[i]])
        return (qT_in[i], b_qTin[i], mT_ins[i], b_mTins[i], x_ins[i], b_xins[i])

    def tileC(kind, ti, pre):
        if kind == "ctx":
            slot, w, src, keys = NOWN + ti, 1, ctx_d[ti * 128:(ti + 1) * 128, :], [0, 1]
        else:
            slot, w, src, keys = ti, 0, x_d[ti * 128:(ti + 1) * 128, :], list(range(34))
        Q, bQ, mT_in, b_mTin, X, bX = pre
        units = [(kvh, keys[a_:a_ + 2]) for kvh in range(2) for a_ in range(0, len(keys), 2)]
        pend = []

        def pv(u, pt, bpt):
            kvh, kts = u
            po, bpo = pO[kvh]
            for j, kt in enumerate(kts):
                P.mm(po[0:65, :], Vt[:, kt, kvh, 0:65], pt[:, j * 512:(j + 1) * 512], kt == keys[0], kt == keys[-1],
                     reads=[bpt, b_Vt], writes=[bpo])

        for u in units:
            kvh, kts = u
            it = cstate["it"]
            cstate["it"] += 1
            psc, bscs = SG[it % 3]
            pt, bpt = PT[it % NPT], b_PT[it % NPT]
            n = len(kts)
            for j, kt in enumerate(kts):
                P.mm(psc[:, j * 512:(j + 1) * 512], KT[:, kvh, kt * 128:(kt + 1) * 128],
                     Q[:, kvh * 4:(kvh + 1) * 4, :].rearrange("p h t -> p (h t)"),
                     reads=[b_KT, bQ], writes=[bscs[j]])
            P.act(pt[:, 0:n * 512], psc[:, 0:n * 512], AF.Exp, scale=0.125, bias=bnd[:, 2:3],
                  reads=bscs[0:n] + [b_bnd], writes=[bpt])
            pend.append((u, pt, bpt))
            if len(pend) > 2:
                pv(*pend.pop(0))
        while pend:
            pv(*pend.pop(0))
        for kvh in range(2):
            po, bpo = pO[kvh]
            P.act(rsr[64:65, :], po[64:65, :], AF.Ln, reads=[bpo], writes=[b_rsr])
            P.act(rsb[64:65, kvh, :], rsr[64:65, :], AF.Exp, scale=-1.0, reads=[b_rsr], writes=[b_rsb])
        for kvh in range(2):
            pb_, bb_ = psum[2 + kvh]
            P.mm(pb_[0:64, :], ones_b[64:65, 0:64], rsb[64:65, kvh, :], reads=[b_const, b_rsb], writes=[bb_])
        for kvh in range(2):
            pb_, bb_ = psum[2 + kvh]
            P.v("dve", "tensor_copy", bcs[kvh][:], pb_[0:64, :], reads=[bb_], writes=[b_bcs[kvh]])
        for kvh in range(2):
            po, bpo = pO[kvh]
            P.v("dve", "tensor_tensor", OT[:, kvh * 4:(kvh + 1) * 4, :].rearrange("p h t -> p (h t)"), po[0:64, :], bcs[kvh][:],
                ALU.mult, reads=[bpo, b_bcs[kvh]], writes=[b_OT])
        for hh in range(2):
            pz, bz = psum[hh]
            for c in range(4):
                P.mm(pz[:], mT_in[:, c, :], w_out[:, c, hh * 512:(hh + 1) * 512], c == 0, False,
                     reads=[b_mTin, b_wout], writes=[bz])
            for h in range(8):
                P.mm(pz[:], OT[:, h, :], w_outB[:, h, hh * 512:(hh + 1) * 512], False, h == 7,
                     reads=[b_OT, b_woutB], writes=[bz])
            P.v("dve", "tensor_tensor", x1_sb[:, hh * 512:(hh + 1) * 512], pz[:],
                gateB_all[:, k.gslot[(0, 0, w)], hh * 512:(hh + 1) * 512],
                ALU.mult, reads=[bz, b_gateB[0]], writes=[b_x1])
        P.v("pool", "tensor_tensor", x1_sb[:], x1_sb[:], X[:], ALU.add, reads=[b_x1, bX], writes=[b_x1])
        P.dma(X1_d[slot * 128:(slot + 1) * 128, :], x1_sb[:], reads=[b_x1], writes=[b_scr["X1"]])

    tiles_c = [("ctx", 0), ("ctx", 1)] + [("own", t) for t in range(NOWN)]
    if "fewC" in dbg:
        tiles_c = [("ctx", 0), ("own", 0), ("own", 16)]
    s1 = k.s0_steps(1, "L1", psum[0], psum[0], psum[1])
    preC = loadC(*tiles_c[0])
    for si, (kind, ti) in enumerate(tiles_c):
        nxtC = loadC(*tiles_c[si + 1]) if si + 1 < len(tiles_c) else None
        if s1:
            s1.pop(0)()
        tileC(kind, ti, preC)
        preC = nxtC
    while s1:
        s1.pop(0)()
    if "stopL0mix" in dbg:
        return
    X2_d, X3_d, out_d = g["X2_d"], g["X3_d"], g["out_d"]
    b_scr["X2"] = Buf("X2"); b_scr["X3"] = Buf("X3")
    tiles = []
    for slot in range(NTOK0):
        w = 1 if slot >= NOWN else 0
        tiles.append((X1_d[slot * 128:(slot + 1) * 128, :], b_scr["X1"], X2_d[slot * 128:(slot + 1) * 128, :], b_scr["X2"], w))
    if "fewM" in dbg:
        tiles = [tiles[0], tiles[16], tiles[17]]
    moe(k, 0, tiles, "a")
    if "stopL0" in dbg or "stopD1" in dbg or "stopD2" in dbg:
        return
    layer1(k, dbg)
    if "stopL1" in dbg:
        return
    tiles = [(X3_d[t * 128:(t + 1) * 128, :], b_scr["X3"], out_d[t * 128:(t + 1) * 128, :], None, 0) for t in range(16)]
    moe(k, 1, tiles, "b")


def moe(k, l, tiles, tag):
    g = k.env
    nc, P = k.nc, k.P
    ar, psum = k.ar, k.psum
    identF, b_const = g["identF"], g["b_const"]
    NT = len(tiles)
    NTOK = NT * 128
    k.areset(0)
    hT_all = ar("hT_all" + tag, [128, 8, NTOK], BF16); b_hTall = Buf()
    yacc = ar("yacc" + tag, [128, NT, D]); b_yacc = [Buf() for _ in range(NT)]
    comb_all = ar("comb" + tag, [128, NT, 16]); b_combt = [Buf() for _ in range(NT)]
    mark = k.aoff
    k.xnf = ar("xnf" + tag, [128, D]); k.hTf = ar("hTf" + tag, [128, 8, 128])
    rw = ar("rw" + tag, [128, 8, 20]); rb = ar("rb" + tag, [128, 20]); b_rw = Buf()
    P.dma(rw[:], g["rw_d"][l].rearrange("(c p) n -> p c n", p=128), writes=[b_rw], eng="pool")
    P.dma(rb[:], g["rb_d"][l].partition_broadcast(128).rearrange("p o n -> p (o n)"), writes=[b_rw], eng="pool")
    lg = ar("lg" + tag, [128, 20]); b_lg = Buf()
    rt = ar("rt" + tag, [128, 64]); b_rt = Buf()
    pR, bR = psum[2]
    b_hTt = [Buf() for _ in range(NT)]

    def d1(ti):
        (src, sbuf_, dst, dbuf_, w) = tiles[ti]
        H, bH, X, bX = k.modulate(src, l, 1, w, fp32=True, src_buf=sbuf_)
        P.v("pool", "tensor_copy", hT_all[:, :, ti * 128:(ti + 1) * 128], H[:], reads=[bH], writes=[b_hTt[ti]])
        for c_ in range(8):
            P.mm(pR[:, 0:20], H[:, c_, :], rw[:, c_, :], c_ == 0, c_ == 7, reads=[bH, b_rw], writes=[bR])
        P.v("dve", "tensor_tensor", lg[:], pR[:, 0:20], rb[:], ALU.add, reads=[bR, b_rw], writes=[b_lg])
        gl, el = lg[:, 0:4], lg[:, 4:20]
        R_ = lambda a, b: rt[:, a:b]
        gmax, ngmax, sume, gw = R_(0, 1), R_(1, 2), R_(2, 3), R_(3, 4)
        ohg, eg, esel, oh1, msk, oh2, ew, sg = R_(4, 8), R_(8, 12), R_(12, 16), R_(16, 20), R_(20, 24), R_(24, 28), R_(28, 32), R_(32, 36)
        m1, m2, dd, e2, w1, w2 = R_(36, 37), R_(37, 38), R_(38, 39), R_(39, 40), R_(40, 41), R_(41, 42)
        rd, wr = [b_lg, b_rt], [b_rt]
        V = lambda name, *a, **kw: P.v("dve", name, *a, reads=rd, writes=wr, **kw)
        V("tensor_reduce", gmax, gl, AX.X, ALU.max)
        V("tensor_scalar", ohg, gl, gmax, None, ALU.is_equal)
        V("tensor_scalar", ngmax, gmax, -1.0, None, ALU.mult)
        P.act(eg, gl, AF.Exp, bias=ngmax, accum_out=sume, reads=rd, writes=wr)
        V("reciprocal", gw, sume)
        V("tensor_scalar", esel, el[:, 0:4], ohg[:, 0:1], None, ALU.mult)
        for gi in range(1, 4):
            V("scalar_tensor_tensor", esel, el[:, gi * 4:(gi + 1) * 4], ohg[:, gi:gi + 1], esel, ALU.mult, ALU.add)
        V("tensor_reduce", m1, esel, AX.X, ALU.max)
        V("tensor_scalar", oh1, esel, m1, None, ALU.is_equal)
        V("scalar_tensor_tensor", msk, oh1, -NEG_BIG, esel, ALU.mult, ALU.add)
        V("tensor_reduce", m2, msk, AX.X, ALU.max)
        V("tensor_scalar", oh2, msk, m2, None, ALU.is_equal)
        V("tensor_tensor", dd, m2, m1, ALU.subtract)
        P.act(e2, dd, AF.Exp, reads=rd, writes=wr)
        V("tensor_scalar", w1, e2, 1.0, None, ALU.add)
        V("reciprocal", w1, w1)
        V("tensor_tensor", w2, e2, w1, ALU.mult)
        V("tensor_scalar", ew, oh1, w1, None, ALU.mult)
        V("scalar_tensor_tensor", ew, oh2, w2, ew, ALU.mult, ALU.add)
        V("tensor_scalar", sg, ohg, gw, None, ALU.mult)
        for gi in range(4):
            P.v("dve", "tensor_scalar", comb_all[:, ti, gi * 4:(gi + 1) * 4], ew, sg[:, gi:gi + 1], None, ALU.mult,
                reads=[b_rt], writes=[b_combt[ti]])
    wst = [ar("mwst%d" % i + tag, [128, 512]) for i in range(2)]; b_wst = [Buf(), Buf()]
    Wg = [ar("Wg%d" % i + tag, [128, 8, 256], BF16) for i in range(2)]
    Wu = [ar("Wu%d" % i + tag, [128, 8, 256], BF16) for i in range(2)]
    Wd = [ar("Wd%d" % i + tag, [128, 2, D], BF16) for i in range(2)]
    b_W = [[Buf(), Buf(), Buf()] for _ in range(2)]
    sa = [ar("sa%d" % i + tag, [128, 512]) for i in range(2)]; b_sa = [Buf(), Buf()]
    hid = [ar("hid%d" % i + tag, [128, 2, 512], BF16) for i in range(2)]; b_hid = [Buf(), Buf()]
    pCW, bCW = psum[0]
    pAs = [psum[1], psum[2]]
    pUs = [psum[3], psum[4]]
    pYs = [psum[5], psum[6], psum[0]]
    wcnt = 0
    blocks = [(s, min(512, NTOK - s)) for s in range(0, NTOK, 512)]
    it = 0
    yi = 0
    def load_w(e):
        nonlocal wcnt
        pe_ = e % 2
        srcs = (g["wg_d"][l, e].rearrange("(c p) f -> p c f", p=128), g["wu_d"][l, e].rearrange("(c p) f -> p c f", p=128),
                g["wd_d"][l, e].rearrange("(c p) n -> p c n", p=128))
        dsts = (Wg[pe_], Wu[pe_], Wd[pe_])
        for wi in range(3):
            for q4 in range(4):
                s_, bs_ = wst[wcnt % 2], b_wst[wcnt % 2]
                if wi < 2:
                    sv = s_[:].rearrange("p (c f) -> p c f", c=2)
                    sview = srcs[wi][:, 2 * q4:2 * q4 + 2, :]
                    dview = dsts[wi][:, 2 * q4:2 * q4 + 2, :]
                else:
                    sv = s_[:]
                    sview = srcs[wi][:, q4 // 2, (q4 % 2) * 512:(q4 % 2 + 1) * 512]
                    dview = dsts[wi][:, q4 // 2, (q4 % 2) * 512:(q4 % 2 + 1) * 512]
                P.dma(sv, sview, writes=[bs_], eng=("sp" if wcnt % 2 == 0 else "pool"))
                P.v("pool", "tensor_copy", dview, sv, reads=[bs_], writes=[b_W[pe_][wi]])
                wcnt += 1

    def gu(e, t0, n, i):
        pe_ = e % 2
        for fc in range(2):
            pa, ba = pAs[fc]
            pu, bu = pUs[fc]
            for c_ in range(8):
                P.mm(pa[:, 0:n], Wg[pe_][:, c_, fc * 128:(fc + 1) * 128], hT_all[:, c_, t0:t0 + n], c_ == 0, c_ == 7,
                     reads=[b_W[pe_][0]] + b_hTt[t0 // 128:(t0 + n) // 128], writes=[ba])
            for c_ in range(8):
                P.mm(pu[:, 0:n], Wu[pe_][:, c_, fc * 128:(fc + 1) * 128], hT_all[:, c_, t0:t0 + n], c_ == 0, c_ == 7,
                     reads=[b_W[pe_][1]] + b_hTt[t0 // 128:(t0 + n) // 128], writes=[bu])
            P.act(sa[fc][:, 0:n], pa[:, 0:n], AF.Silu, reads=[ba], writes=[b_sa[fc]])
            P.v("dve", "tensor_tensor", hid[i][:, fc, 0:n], sa[fc][:, 0:n], pu[:, 0:n], ALU.mult,
                reads=[b_sa[fc], bu], writes=[b_hid[i]])

    def dn(e, t0, n, i):
        nonlocal yi
        pe_ = e % 2
        ntile = n // 128
        for j in range(ntile):
            tile_i = t0 // 128 + j
            for dh in range(2):
                py, by = pYs[yi % 3]
                yi += 1
                for fc in range(2):
                    P.mm(py[:], hid[i][:, fc, j * 128:(j + 1) * 128], Wd[pe_][:, fc, dh * 512:(dh + 1) * 512],
                         fc == 0, fc == 1, reads=[b_hid[i], b_W[pe_][2]], writes=[by])
                ya = yacc[:, tile_i, dh * 512:(dh + 1) * 512]
                cs = comb_all[:, tile_i, e:e + 1]
                if e == 0:
                    P.v("dve", "tensor_scalar", ya, py[:], cs, None, ALU.mult, reads=[by, b_combt[tile_i]], writes=[b_yacc[tile_i]])
                else:
                    P.v("dve", "scalar_tensor_tensor", ya, py[:], cs, ya, ALU.mult, ALU.add,
                        reads=[by, b_combt[tile_i], b_yacc[tile_i]], writes=[b_yacc[tile_i]])

    items = [(e, t0, n) for e in range(16) for (t0, n) in blocks]
    pend = None
    last_e = -1
    for idx, (e, t0, n) in enumerate(items):
        if e != last_e:
            if e == 0:
                load_w(0)
            if e + 1 < 16:
                pass
            last_e = e
        if e == 0:
            for tq in range(t0 // 128, (t0 + n) // 128):
                d1(tq)
        gu(e, t0, n, idx % 2)
        if pend is not None:
            dn(*pend)
        pend = (e, t0, n, idx % 2)
        if t0 == blocks[0][0] and e + 1 < 16:
            load_w(e + 1)
    dn(*pend)
    xo = [k.xnf, k.hTf[:].rearrange("p c t -> p (c t)")]; b_xo = [k.env["b_xnf"], k.env["b_hTf"]]
    for ti, (src, sbuf_, dst, dbuf_, w) in enumerate(tiles):
        i = ti % 2
        X, bX = g["xt"][i], g["b_xt"][i]
        P.dma(X[:], src, reads=([sbuf_] if sbuf_ else []), writes=[bX], eng="pool")
        gs = k.gslot[(l, 1, w)]
        P.v("dve", "tensor_tensor", xo[i][:], yacc[:, ti, :], k.gateB_all[:, gs, :], ALU.mult,
            reads=[b_yacc[ti], k.b_gateB[l]], writes=[b_xo[i]])
        P.v("pool", "tensor_tensor", xo[i][:], xo[i][:], X[:], ALU.add, reads=[b_xo[i], bX], writes=[b_xo[i]])
        P.dma(dst, xo[i][:], reads=[b_xo[i]], writes=([dbuf_] if dbuf_ else []))


def layer1(k, dbg):
    g = k.env
    nc, P = k.nc, k.P
    ar, psum = k.ar, k.psum
    identF, identB, b_const = g["identF"], g["identB"], g["b_const"]
    X2_d, X3_d, QT1_d = g["X2_d"], g["X3_d"], g["QT1_d"]
    bnd, b_bnd = k.bnd, k.b_bnd
    sqn, skn, sinkb = g["sqn"], g["skn"], g["sinkb"]
    b_X2, b_X3 = k.b_scr["X2"], k.b_scr["X3"]
    b_QT1 = Buf()
    k.areset(0)
    NK = 19
    KT = ar("KT1", [128, 2, NK * 128], BF16); b_KT = Buf()
    P.v("pool", "memset", KT[64:128], 0.0, writes=[b_KT])
    Vt = ar("Vt1", [128, NK, 2, 66], BF16); b_Vt = Buf()
    P.v("pool", "memset", Vt[:], 1.0, writes=[b_Vt])
    wmask = ar("wmask", [128, 2, 128], BF16)
    P.dma(wmask[:], g["wmask_d"], writes=[b_const], eng="pool")
    k.wstage = [ar("wstage1%d" % i, [128, 1280]) for i in range(2)]
    k.swt = ar("swt1", [128, 1024])
    w_in = ar("w_in1", [128, 8, 1280], BF16); b_win = Buf()
    odin_d, odout_d = g["odin_d"], g["odout_d"]
    k.load_weight_bf16(w_in, b_win, lambda c: odin_d[c * 128:(c + 1) * 128, :], 8, 1280, k.wstage, k.b_wstage)
    w_outB = ar("w_out1B", [64, 16, D], BF16); b_woutB = Buf()
    for h in range(16):
        s_, bs_ = k.wstage[h % 2], k.b_wstage[h % 2]
        P.dma(s_[0:64, 0:D], odout_d[h * 64:(h + 1) * 64, :], writes=[bs_], eng=("sp" if h % 2 == 0 else "pool"))
        k.cast(("pool", "dve", "act")[h % 3], w_outB[:, h, :], s_[0:64, 0:D], [bs_], [b_woutB])
    ones_f = ar("ones_f1", [128, 64]); P.v("pool", "memset", ones_f[:], 1.0, writes=[b_const])
    z_sb = ar("z_sb1", [128, 1280]); b_z = Buf()
    ropeC = ar("ropeC1", [128, 64]); ropeS = ar("ropeS1", [128, 64]); b_rope = Buf()
    tmpA = ar("tmpA1", [128, 1024]); b_tmpA = Buf()
    tmpB = ar("tmpB1", [128, 1024]); b_tmpB = Buf()
    scr = ar("scr1", [128, 64]); b_scr_ = Buf()
    qb = ar("qb1", [128, 1024], BF16); b_qb = Buf()
    kb = ar("kb1", [128, 128], BF16); b_kb = Buf()
    qT_sb = ar("qT_sb1", [64, 16, 128], BF16); b_qTsb = Buf()
    pMisc, bMisc = psum[3]
    pMisc2, bMisc2 = psum[4]

    def modE(kind, ti):
        if kind == "ctx":
            return k.modulate(X2_d[(NOWN + ti) * 128:(NOWN + ti + 1) * 128, :], 1, 0, 1, src_buf=b_X2)
        return k.modulate(X2_d[ti * 128:(ti + 1) * 128, :], 1, 0, 0, src_buf=b_X2)

    def tileE(kind, ti, pre):
        if kind == "ctx":
            src, w, kidx = X2_d[(NOWN + ti) * 128:(NOWN + ti + 1) * 128, :], 1, ti
        else:
            src, w, kidx = X2_d[ti * 128:(ti + 1) * 128, :], 0, 2 + ti
        need_q = (kind != "ctx" and ti < 16)
        H, bH, X, bX = pre
        blocks = ((0, 512), (512, 1024), (1024, 1280)) if need_q else ((1024, 1280),)
        for bi, (lo, hi) in enumerate(blocks):
            pz, bz = psum[1 + bi % 2]
            for c in range(8):
                P.mm(pz[:, 0:hi - lo], H[:, c, :], w_in[:, c, lo:hi], c == 0, c == 7, reads=[bH, b_win], writes=[bz])
            P.act(z_sb[:, lo:hi], pz[:, 0:hi - lo], AF.Copy, reads=[bz], writes=[b_z])
        rope = None
        if kind != "ctx":
            P.dma(ropeC[:], g["ropeC_d"][ti * 128:(ti + 1) * 128, :], writes=[b_rope], eng="pool")
            P.dma(ropeS[:], g["ropeS_d"][ti * 128:(ti + 1) * 128, :], writes=[b_rope], eng="pool")
            rope = (ropeC, ropeS, b_rope, tmpB, b_tmpB)
        pM8 = pMisc[0:64, :].bitcast(BF16).rearrange("p (h t) -> p h t", h=8)
        pM8b = pMisc2[0:64, :].bitcast(BF16).rearrange("p (h t) -> p h t", h=8)
        if need_q:
            k.head_norm_rope(qb[:], b_qb, z_sb[:, 0:1024], b_z, 16, sqn, rope, tmpA, b_tmpA, scr, b_scr_)
            for h in range(16):
                pm, bm = (pM8, bMisc) if h < 8 else (pM8b, bMisc2)
                P.tr(pm[:, h % 8, :], qb[:, h * 64:(h + 1) * 64], identB[:], reads=[b_qb, b_const], writes=[bm])
            P.act(qT_sb[:, 0:8, :], pM8, AF.Copy, reads=[bMisc], writes=[b_qTsb])
            P.act(qT_sb[:, 8:16, :], pM8b, AF.Copy, reads=[bMisc2], writes=[b_qTsb])
            P.dma(QT1_d[ti], qT_sb[:], reads=[b_qTsb], writes=[b_QT1])
        k.head_norm_rope(kb[:], b_kb, z_sb[:, 1024:1152], b_z, 2, skn, rope, tmpA, b_tmpA, scr, b_scr_)
        for h in range(2):
            P.tr(pM8[:, h, :], kb[:, h * 64:(h + 1) * 64], identB[:], reads=[b_kb, b_const], writes=[bMisc])
        P.act(KT[0:64, :, kidx * 128:(kidx + 1) * 128], pM8[:, 0:2, :], AF.Copy, reads=[bMisc], writes=[b_KT])
        P.v("pool", "tensor_copy", Vt[:, kidx, :, 0:64], z_sb[:, 1152:1280].rearrange("p (h d) -> p h d", h=2),
            reads=[b_z], writes=[b_Vt])

    seqE = [("ctx", 0), ("ctx", 1)] + [("own", ti) for ti in range(17)]
    SETE = [(z_sb, b_z, qb, b_qb, kb, b_kb, qT_sb, b_qTsb),
            (ar("z_sb1b", [128, 1280]), Buf(), ar("qb1b", [128, 1024], BF16), Buf(), ar("kb1b", [128, 128], BF16), Buf(),
             ar("qT_sb1b", [64, 16, 128], BF16), Buf())]
    pre = modE(*seqE[0])
    for si, (kind, ti) in enumerate(seqE):
        nxt = modE(*seqE[si + 1]) if si + 1 < len(seqE) else None
        (z_sb, b_z, qb, b_qb, kb, b_kb, qT_sb, b_qTsb) = SETE[si % 2]
        tileE(kind, ti, pre)
        pre = nxt

    if "stopE1" in dbg:
        return
    qT_in = [ar("qT_in1%d" % i, [128, 16, 128], BF16) for i in range(2)]; b_qTin = [Buf(), Buf()]
    for i_ in range(2):
        P.v("pool", "memset", qT_in[i_][64:128], 0.0, writes=[b_qTin[i_]])
    NPT = 3
    PT = [ar("PT1%d" % i, [128, 1024], BF16) for i in range(NPT)]; b_PT = [Buf() for _ in range(NPT)]
    esink = ar("esink1", [128, 16]); b_esink = Buf()
    P.act(esink[:], sinkb[:], AF.Exp, bias=bnd[:, 6:7], reads=[b_const, b_bnd], writes=[b_esink])
    rsr = ar("rsr1", [128, 512]); b_rsr = Buf()
    bc_sb = ar("bc_sb1", [64, 512]); b_bc = Buf()
    OT = ar("OT1", [64, 16, 128], BF16); b_OT = Buf()
    x3_sb = ar("x3_sb", [128, D]); b_x3 = Buf()
    pO = [psum[4], psum[5], psum[6], psum[7]]
    SG = [(k.psall[:, 2 * g_ * 512:(2 * g_ + 2) * 512], [psum[2 * g_][1], psum[2 * g_ + 1][1]]) for g_ in range(2)]
    pBC, bBC = psum[0]
    ones_b = ar("ones_b1", [128, 64], BF16); P.v("pool", "memset", ones_b[:], 1.0, writes=[b_const])
    rsb = ar("rsb1", [128, 4, 512], BF16); b_rsb = Buf()
    bcs = [ar("bcs1%d" % i_, [64, 512]) for i_ in range(4)]; b_bcs = [Buf() for _ in range(4)]
    x_ins = [ar("xE%d" % i_, [128, D]) for i_ in range(2)]; b_xins = [Buf(), Buf()]
    vsink = ar("vsink", [128, 66], BF16); b_vs = Buf()
    P.v("pool", "memset", vsink[:], 0.0, writes=[b_vs])
    P.v("pool", "memset", vsink[:, 64:65], 1.0, writes=[b_vs])
    esrow = ar("esrow", [128, 16, 128], BF16); b_esrow = Buf()
    P.v("dve", "tensor_copy", esrow[64:65], esink[64:65, :].unsqueeze(2).to_broadcast([1, 16, 128]),
        reads=[b_esink], writes=[b_esrow])

    def loadQ(qi):
        i_ = qi % 2
        P.dma(qT_in[i_][0:64], QT1_d[qi], reads=[b_QT1], writes=[b_qTin[i_]])
        P.dma(x_ins[i_][:], X2_d[qi * 128:(qi + 1) * 128, :], reads=[b_X2], writes=[b_xins[i_]])

    it = 0
    loadQ(0)
    for qi in range(16):
        i = qi % 2
        Q, bQ = qT_in[i], b_qTin[i]
        if qi + 1 < 16:
            loadQ(qi + 1)
        keys = [(0, None), (1, None)]
        if qi > 0:
            keys.append((2 + qi - 1, 0))
        keys.append((2 + qi, None))
        keys.append((2 + qi + 1, 1))
        nk = len(keys)
        units = [(gq, list(range(a_, min(a_ + 2, nk)))) for gq in range(4) for a_ in range(0, nk, 2)]
        pend = None

        def pv(gq, kks, pt, bpt):
            po, bpo = pO[gq]
            for j, kk in enumerate(kks):
                kt, mk = keys[kk]
                P.mm(po[0:65, :], Vt[:, kt, gq // 2, 0:65], pt[:, j * 512:(j + 1) * 512], kk == 0, False,
                     reads=[bpt, b_Vt], writes=[bpo])
            if kks[-1] == nk - 1:
                P.mm(po[0:65, :], vsink[64:65, 0:65], esrow[64:65, gq * 4:(gq + 1) * 4, :].rearrange("p h t -> p (h t)"),
                     False, True, reads=[b_vs, b_esrow], writes=[bpo])

        for (gq, kks) in units:
            kvh = gq // 2
            psc, bscs = SG[it % 2]
            pt, bpt = PT[it % NPT], b_PT[it % NPT]
            it += 1
            n = len(kks)
            for j, kk in enumerate(kks):
                kt, mk = keys[kk]
                P.mm(psc[:, j * 512:(j + 1) * 512], KT[:, kvh, kt * 128:(kt + 1) * 128],
                     Q[:, gq * 4:(gq + 1) * 4, :].rearrange("p h t -> p (h t)"), reads=[b_KT, bQ], writes=[bscs[j]])
            P.act(pt[:, 0:n * 512], psc[:, 0:n * 512], AF.Exp, scale=0.125, bias=bnd[:, 6:7],
                  reads=bscs[0:n] + [b_bnd], writes=[bpt])
            for j, kk in enumerate(kks):
                kt, mk = keys[kk]
                if mk is not None:
                    p3 = pt[:, j * 512:(j + 1) * 512].rearrange("p (h t) -> p h t", h=4)
                    P.v("dve", "tensor_tensor", p3, p3, wmask[:, mk, :].unsqueeze(1).to_broadcast([128, 4, 128]), ALU.mult,
                        reads=[bpt, b_const], writes=[bpt])
            if pend is not None:
                pv(*pend)
            pend = (gq, kks, pt, bpt)
        pv(*pend)
        for gq in range(4):
            po, bpo = pO[gq]
            P.act(rsr[64:65, :], po[64:65, :], AF.Ln, reads=[bpo], writes=[b_rsr])
            P.act(rsb[64:65, gq, :], rsr[64:65, :], AF.Exp, scale=-1.0, reads=[b_rsr], writes=[b_rsb])
        for gq in range(4):
            pb_, bb_ = psum[gq]
            P.mm(pb_[0:64, :], ones_b[64:65, 0:64], rsb[64:65, gq, :], reads=[b_const, b_rsb], writes=[bb_])
        for gq in range(4):
            pb_, bb_ = psum[gq]
            P.v("dve", "tensor_copy", bcs[gq][:], pb_[0:64, :], reads=[bb_], writes=[b_bcs[gq]])
        for gq in range(4):
            po, bpo = pO[gq]
            P.v("dve", "tensor_tensor", OT[:, gq * 4:(gq + 1) * 4, :].rearrange("p h t -> p (h t)"), po[0:64, :], bcs[gq][:],
                ALU.mult, reads=[bpo, b_bcs[gq]], writes=[b_OT])
        X, bX = x_ins[i], b_xins[i]
        for hh in range(2):
            pz, bz = psum[1 + hh]
            for h in range(16):
                P.mm(pz[:], OT[:, h, :], w_outB[:, h, hh * 512:(hh + 1) * 512], h == 0, h == 15,
                     reads=[b_OT, b_woutB], writes=[bz])
            P.v("dve", "tensor_tensor", x3_sb[:, hh * 512:(hh + 1) * 512], pz[:],
                k.gateB_all[:, k.gslot[(1, 0, 0)], hh * 512:(hh + 1) * 512], ALU.mult,
                reads=[bz, k.b_gateB[1]], writes=[b_x3])
        P.v("pool", "tensor_tensor", x3_sb[:], x3_sb[:], X[:], ALU.add, reads=[b_x3, bX], writes=[b_x3])
        P.dma(X3_d[qi * 128:(qi + 1) * 128, :], x3_sb[:], reads=[b_x3], writes=[b_X3])


def rope_tables():
    t = np.arange(SEQ)
    row = (t // 64).astype(np.float32)
    col = (t % 64).astype(np.float32)
    inv = (10000.0 ** (-np.arange(0, 32, 2, dtype=np.float32) / 32)).astype(np.float32)
    ang = np.stack([row[:, None] * inv, col[:, None] * inv], axis=1)
    c = np.cos(ang).astype(np.float32)
    s = np.sin(ang).astype(np.float32)
    C = np.zeros((SEQ, 2, 2, 16), np.float32)
    S = np.zeros((SEQ, 2, 2, 16), np.float32)
    C[:, :, 0] = c
    C[:, :, 1] = c
    S[:, :, 0] = -s
    S[:, :, 1] = s
    return C.reshape(SEQ, 64), S.reshape(SEQ, 64)


def host_consts():
    p = np.arange(128)
    same = (p[:, None] // 64) == (p[None, :] // 64)
    s, t = p[:, None], p[None, :]
    tri = np.stack([same & (s <= t), same & (s >= t), same & (s > t), same & (s < t)], axis=1).astype(np.float32)
    sm = (p % 64)[:, None]
    tt = np.arange(64)[None, :]
    mA = np.tile((sm <= tt), (1, 4))
    mB = np.tile((sm >= tt), (1, 4))
    maskAB = np.stack([mA, mB], axis=1).astype(ml_dtypes.bfloat16)
    sel2 = np.zeros((2, 2, 128), np.float32)
    sel2[0, 0] = 1
    sel2[1, 1] = 1
    wmask = np.stack([(s >= t), (s <= t)], axis=1).astype(ml_dtypes.bfloat16)
    return dict(identF=np.eye(128, dtype=np.float32), identB=np.eye(128).astype(ml_dtypes.bfloat16),
                tri=tri, maskAB=maskAB, sel2=sel2, wmask=wmask)


def make_in_maps(inp):
    f = lambda a: np.ascontiguousarray(np.asarray(a, dtype=np.float32))
    C, S = rope_tables()
    consts = host_consts()
    maps = []
    rw = np.concatenate([f(inp["router_group_w"]), f(inp["router_expert_w"])], axis=-1)
    rb = np.concatenate([f(inp["router_group_b"]), f(inp["router_expert_b"])], axis=-1)[:, None, :]
    shared = dict(
        mod_w=f(inp["mod_w"]), mod_b=f(inp["mod_b"]), norm_mix=f(inp["norm_mix"]), norm_ffn=f(inp["norm_ffn"]),
        ev_w_in=f(inp["ev_w_in"])[0], ev_w_out=f(inp["ev_w_out"])[0],
        gla_out_norm=f(inp["gla_out_norm"]), att_q_norm=f(inp["att_q_norm"]), att_k_norm=f(inp["att_k_norm"]),
        od_w_in=f(inp["od_w_in"])[0], od_w_out=f(inp["od_w_out"])[0], swa_sink=f(inp["swa_sink"]),
        swa_q_norm=f(inp["swa_q_norm"]), swa_k_norm=f(inp["swa_k_norm"]),
        router_w=np.ascontiguousarray(rw), router_b=np.ascontiguousarray(rb),
        exp_w_gate=f(inp["exp_w_gate"]).reshape(2, 16, D, 256), exp_w_up=f(inp["exp_w_up"]).reshape(2, 16, D, 256),
        exp_w_down=f(inp["exp_w_down"]).reshape(2, 16, 256, D), **consts)
    gw = f(inp["gla_gate_w"])[0]
    gb = f(inp["gla_gate_b"])[0]
    for core in range(8):
        b, half = core // 2, core % 2
        x = f(inp["x"])[b]
        cx = f(inp["ctx"])[b]
        order = (0, 1)
        if half == 1:
            x = x[::-1]
            cx = cx[::-1]
            order = (1, 0)
        gwe = np.zeros((33, 512), np.float32)
        for i, dr in enumerate(order):
            gwe[16 * dr:16 * dr + 16, 256 * i:256 * i + 256] = gw[dr]
            gwe[32, 256 * i:256 * i + 256] = gb[dr]
        m = dict(shared)
        m.update(x=np.ascontiguousarray(x), ctx=np.ascontiguousarray(cx),
                 crow=np.ascontiguousarray(np.stack([f(inp["c"])[b], f(inp["c_ctx"])])),
                 gw_ext=gwe,
                 ropeC=np.ascontiguousarray(C[::-1] if half else C),
                 ropeS=np.ascontiguousarray(S[::-1] if half else S))
        maps.append(m)
    return maps


_CACHE = {}


def kernel(**inputs):
    if "nc" not in _CACHE:
        _CACHE["nc"] = build()[0]
    nc = _CACHE["nc"]
    maps = make_in_maps(inputs)
    res = run_bass_kernel_spmd(nc, maps, core_ids=list(range(8)))
    out = np.zeros((4, SEQ, D), np.float32)
    for core in range(8):
        b, half = core // 2, core % 2
        o = np.asarray(res.results[core]["out"], dtype=np.float32)
        if half == 0:
            out[b, 0:2048] = o
        else:
            out[b, 2048:] = o[::-1]
    return out
```

```python
import numpy as np
import ml_dtypes
from contextlib import ExitStack
import concourse.bass as bass
import concourse.mybir as mybir
from concourse.bass_utils import run_bass_kernel_spmd

F32 = mybir.dt.float32
BF16 = mybir.dt.bfloat16
ALU = mybir.AluOpType
AF = mybir.ActivationFunctionType
AX = mybir.AxisListType

import os
COMPUTE = ("pe", "act", "dve", "pool")
SCHED_W = int(os.environ.get("SCHED_W", "128"))
PE_A = float(os.environ.get("PE_A", "0.05"))
PE_B = float(os.environ.get("PE_B", "0.00035"))
PE_F = float(os.environ.get("PE_F", "2.5"))
DMA_L = float(os.environ.get("DMA_L", "2.2"))
ACT_S = float(os.environ.get("ACT_S", "0.8"))
DVE_S = float(os.environ.get("DVE_S", "1.0"))
POOL_S = float(os.environ.get("POOL_S", "1.0"))
SEM_L = float(os.environ.get("SEM_L", "0.1"))
NDMASEM = 12

D = 1024
SEQ = 4096
NOWN = 17
NTOK0 = 19
EVEN_IN = 2336
NEG_BIG = 1.0e30


class Buf:
    __slots__ = ("name", "w", "r")

    def __init__(self, name=""):
        self.name = name
        self.w = None
        self.r = {}


class Prog:
    def __init__(self, nc):
        self.nc = nc
        self.ops = []
        self.cost = []
        self.reorder = True
        self.base = set()
        self.eng = {"pe": nc.tensor, "act": nc.scalar, "dve": nc.vector,
                    "pool": nc.gpsimd, "sp": nc.sync}

    def op(self, eng, fn, reads=(), writes=(), dma=False, cost=0.4):
        idx = len(self.ops)
        deps = set(self.base)
        for b in reads:
            if b.w is not None:
                deps.add(b.w)
        for b in writes:
            if b.w is not None:
                deps.add(b.w)
            for v in b.r.values():
                if isinstance(v, list):
                    deps.update(v)
                else:
                    deps.add(v)
        key = (eng, dma)
        for b in reads:
            if dma:
                b.r.setdefault(key, []).append(idx)
            else:
                b.r[key] = idx
        for b in writes:
            b.w = idx
            b.r = {}
        deps.discard(idx)
        self.ops.append((eng, dma, fn, deps))
        self.cost.append(cost)
        return idx

    def barrier(self):
        last = {}
        dm = {}
        for i, (eng, dma, fn, deps) in enumerate(self.ops):
            if dma:
                dm.setdefault(eng, []).append(i)
            else:
                last[eng] = i
        base = set(last.values())
        for q, lst in dm.items():
            base.update(lst[-NDMASEM:])
        self.base = base

    @staticmethod
    def _fs(ap):
        n = 1
        for s in ap.shape[1:]:
            n *= s
        return n

    def dma(self, out, in_, reads=(), writes=(), eng="sp"):
        e = self.eng[eng]
        nb = self._fs(out) * out.shape[0] * (2 if out.dtype == BF16 else 4)
        return self.op(eng, lambda: e.dma_start(out=out, in_=in_), reads, writes, dma=True,
                       cost=DMA_L + nb / 150e3)

    def mm(self, out, lhsT, rhs, start=True, stop=True, reads=(), writes=()):
        nc = self.nc
        c = PE_A + self._fs(rhs) * PE_B
        if rhs.dtype == F32:
            c *= PE_F
        return self.op("pe", lambda: nc.tensor.matmul(out, lhsT, rhs, start=start, stop=stop),
                       reads, writes, cost=c)

    def tr(self, out, in_, ident, reads=(), writes=()):
        nc = self.nc
        return self.op("pe", lambda: nc.tensor.transpose(out, in_, ident), reads, writes, cost=0.12)

    def act(self, out, in_, func, reads=(), writes=(), **kw):
        nc = self.nc
        return self.op("act", lambda: nc.scalar.activation(out=out, in_=in_, func=func, **kw),
                       reads, writes, cost=ACT_S * (0.25 + self._fs(in_) * 0.0009))

    def v(self, eng, name, *args, reads=(), writes=(), **kw):
        f = getattr(self.eng[eng], name)
        c = DVE_S * (0.15 + self._fs(args[0]) * 0.0011)
        if eng == "pool":
            c = POOL_S * (0.3 + self._fs(args[0]) * 0.0025)
        return self.op(eng, lambda: f(*args, **kw), reads, writes, cost=c)

    def schedule(self, W=SCHED_W):
        ops, cost = self.ops, self.cost
        n = len(ops)
        fin = [None] * n
        start = [0.0] * n
        rem = {}
        for i, (eng, dma, fn, deps) in enumerate(ops):
            rem.setdefault(eng, []).append(i)
        etime = {e: 0.0 for e in rem}
        nsched = 0
        while nsched < n:
            best = None
            for e, lst in rem.items():
                if not lst:
                    continue
                te = etime[e]
                for i in lst[:W]:
                    deps = ops[i][3]
                    r = te
                    ok = True
                    for d in deps:
                        f = fin[d]
                        if f is None:
                            ok = False
                            break
                        if f > r:
                            r = f
                    if not ok:
                        continue
                    key = (r, i)
                    if best is None or key < best[0]:
                        best = (key, e, i)
                    if r <= te:
                        break
            (r, i), e, _ = best
            eng, dma, fn, deps = ops[i]
            start[i] = r
            if dma:
                etime[e] = r + 0.5
                fin[i] = r + cost[i]
            else:
                etime[e] = r + cost[i]
                fin[i] = r + cost[i] + SEM_L
            rem[e].remove(i)
            nsched += 1
        order = sorted(range(n), key=lambda j: (start[j], j))
        self.sim_time = max(f for f in fin)
        self.sim_start, self.sim_fin = start, fin
        return order

    def emit(self, sems):
        ops = self.ops
        n = len(ops)
        need = [False] * n
        for (eng, dma, fn, deps) in ops:
            for d in deps:
                deng, ddma, _, _ = ops[d]
                if (not ddma) and deng == "pe" and eng == "pe" and not dma:
                    continue
                need[d] = True
        sig = [None] * n
        cnt = {e: 0 for e in COMPUTE}
        dcnt = {}
        waited = {}
        order = self.schedule() if self.reorder else range(n)
        for i in order:
            eng, dma, fn, deps = ops[i]
            e = self.eng[eng]
            w = {}
            for d in deps:
                deng, ddma, _, _ = ops[d]
                if (not ddma) and deng == "pe" and eng == "pe" and not dma:
                    continue
                s, val = sig[d]
                if w.get(id(s), (None, -1))[1] < val:
                    w[id(s)] = (s, val)
            if dma:
                j = dcnt.get(eng, 0)
                pool = sems[("dma", eng)]
                s = pool[j % len(pool)]
                val = 16 * (j // len(pool) + 1)
                if val > 16 and w.get(id(s), (None, -1))[1] < val - 16:
                    w[id(s)] = (s, val - 16)
                dcnt[eng] = j + 1
                sig[i] = (s, val)
            for sid, (s, val) in w.items():
                k = (eng, sid)
                if waited.get(k, -1) >= val:
                    continue
                waited[k] = val
                e.wait_ge(s, val)
            ins = fn()
            if dma:
                ins.then_inc(sig[i][0], 16)
            elif need[i]:
                cnt[eng] += 1
                sig[i] = (sems[eng], cnt[eng])
                ins.then_inc(sems[eng], 1)
        for (k, pool) in sems.items():
            if isinstance(k, tuple):
                eng = k[1]
                j = dcnt.get(eng, 0)
                e = self.eng[eng]
                for q, s in enumerate(pool):
                    c = (j - q + len(pool) - 1) // len(pool) if j > q else 0
                    if c > 0:
                        e.wait_ge(s, 16 * c)
        return dict(n_ops=n, sig=cnt, dmas=dcnt)


class K:
    pass


def build(stages=("all",), dbg=()):
    nc = bass.Bass("TRN2", target_bir_lowering=False)
    P = Prog(nc)
    k = K()
    k.nc, k.P = nc, P
    k.dbgset = set(dbg)
    es = ExitStack()
    k.es = es
    k.dbg = {}

    def din(name, shape, dt=F32):
        return nc.dram_tensor(name, list(shape), dt, kind="ExternalInput").ap()

    def dscr(name, shape, dt=F32):
        if name in dbg:
            return nc.dram_tensor(name, list(shape), dt, kind="ExternalOutput").ap()
        return nc.dram_tensor(name, list(shape), dt).ap()

    def dout(name, shape, dt=F32):
        return nc.dram_tensor(name, list(shape), dt, kind="ExternalOutput").ap()

    def sb(name, shape, dt=F32):
        return es.enter_context(nc.sbuf_tensor("sb_" + name, list(shape), dt))

    def ps(name, shape, dt=F32):
        return es.enter_context(nc.psum_tensor("ps_" + name, list(shape), dt))

    k.din, k.dscr, k.dout, k.sb, k.ps = din, dscr, dout, sb, ps
    ARW = 41700
    k.arena = None
    k.aoff = 0

    def ar(name, shape, dt=F32):
        if k.arena is None:
            k.arena = sb("arena", [128, ARW])
        n = 1
        for s in shape[1:]:
            n *= s
        w = n if dt == F32 else (n + 1) // 2
        off = k.aoff
        k.aoff += w + (w % 2)
        assert k.aoff <= ARW, (name, k.aoff)
        a = k.arena[0:shape[0], off:off + w]
        if dt != F32:
            a = a.bitcast(dt)
        if len(shape) > 2:
            names = " ".join("d%d" % i for i in range(1, len(shape)))
            kw = {"d%d" % i: shape[i] for i in range(1, len(shape))}
            a = a.rearrange("p (%s) -> p %s" % (names, names), **kw)
        return a

    def areset(mark=0):
        P.barrier()
        k.peak = getattr(k, "peak", [])
        k.peak.append(k.aoff * 4 // 1024)
        k.marks = getattr(k, "marks", [])
        k.marks.append(len(P.ops))
        k.aoff = mark

    k.ar, k.areset = ar, areset

    with es:
        sems = {e: es.enter_context(nc.semaphore("s_" + e)) for e in COMPUTE}
        for q in ("sp", "pool"):
            sems[("dma", q)] = [es.enter_context(nc.semaphore(f"d_{q}{i}")) for i in range(NDMASEM)]
        body(k, stages, dbg)
        st = P.emit(sems)
        k.stats = st
    return nc, k


def body(k, stages, dbg):
    nc, P = k.nc, k.P
    din, dscr, dout, sb, ps, ar = k.din, k.dscr, k.dout, k.sb, k.ps, k.ar

    x_d = din("x", [SEQ, D])
    ctx_d = din("ctx", [256, D])
    crow_d = din("crow", [2, D])
    modw_d = din("mod_w", [2, D, 6 * D])
    modb_d = din("mod_b", [2, 6 * D])
    nmix_d = din("norm_mix", [2, D])
    nffn_d = din("norm_ffn", [2, D])
    evin_d = din("ev_w_in", [D, EVEN_IN])
    evout_d = din("ev_w_out", [D, D])
    gwext_d = din("gw_ext", [33, 512])
    glan_d = din("gla_out_norm", [1, 128])
    aqn_d = din("att_q_norm", [1, 64])
    akn_d = din("att_k_norm", [1, 64])
    odin_d = din("od_w_in", [D, 1280])
    odout_d = din("od_w_out", [D, D])
    sink_d = din("swa_sink", [1, 16])
    sqn_d = din("swa_q_norm", [1, 64])
    skn_d = din("swa_k_norm", [1, 64])
    rw_d = din("router_w", [2, D, 20])
    rb_d = din("router_b", [2, 1, 20])
    wg_d = din("exp_w_gate", [2, 16, D, 256])
    wu_d = din("exp_w_up", [2, 16, D, 256])
    wd_d = din("exp_w_down", [2, 16, 256, D])
    ropeC_d = din("ropeC", [SEQ, 64])
    ropeS_d = din("ropeS", [SEQ, 64])
    identF_d = din("identF", [128, 128])
    identB_d = din("identB", [128, 128], BF16)
    tri_d = din("tri", [128, 4, 128])
    maskAB_d = din("maskAB", [128, 2, 256], BF16)
    sel2_d = din("sel2", [2, 2, 128])
    wmask_d = din("wmask", [128, 2, 128], BF16)
    out_d = dout("out", [2048, D])

    X1_d = dscr("X1", [NTOK0 * 128, D])
    X2_d = dscr("X2", [NTOK0 * 128, D])
    X3_d = dscr("X3", [2048, D])
    OA_d = dscr("OA", [NTOK0, 128, 512])
    QT_d = dscr("QT", [NTOK0, 64, 8, 128], BF16)
    MIXT_d = dscr("MIXT", [NTOK0, 128, 8, 128], BF16)
    PB_lt = dscr("PB_lt", [NTOK0, 64, 512], BF16)
    PB_ke = dscr("PB_ke", [NTOK0, 64, 512], BF16)
    PB_kd = dscr("PB_kd", [NTOK0, 128, 256], BF16)
    PB_v = dscr("PB_v", [NTOK0, 128, 512], BF16)
    PB_ee = dscr("PB_ee", [NTOK0, 64, 8])
    PB_rg = dscr("PB_rg", [NTOK0, 128, 512])
    QT1_d = dscr("QT1", [16, 64, 16, 128], BF16)

    identF = sb("identF", [128, 128]); identB = sb("identB", [128, 128], BF16)
    b_const = Buf("const")
    for t, d in ((identF, identF_d), (identB, identB_d)):
        P.dma(t[:], d, writes=[b_const], eng="pool")
    k.identF, k.identB, k.b_const = identF, identB, b_const

    psall = ps("psall", [128, 4096])
    k.psall = psall
    pA, pB, pC, pD, pE, pF, pG, pH = [psall[:, i * 512:(i + 1) * 512] for i in range(8)]
    bA, bB, bC, bD, bE, bF_, bG, bH = [Buf("ps%d" % i) for i in range(8)]
    k.psum = [(pA, bA), (pB, bB), (pC, bC), (pD, bD), (pE, bE), (pF, bF_), (pG, bG), (pH, bH)]

    crow = ar("crow", [2, D]); b_crow = Buf()
    scT = sb("scT", [128, 8, 2]); b_scT = Buf()
    P.dma(crow[:], crow_d, writes=[b_crow])
    P.act(crow[:], crow[:], AF.Silu, reads=[b_crow], writes=[b_crow])
    for c in range(8):
        P.tr(pA[:, 2 * c:2 * c + 2], crow[:, c * 128:(c + 1) * 128], identF[0:2, 0:2],
             reads=[b_crow, b_const], writes=[bA])
    P.v("dve", "tensor_copy", scT[:].rearrange("p c t -> p (c t)"), pA[:, 0:16], reads=[bA], writes=[b_scT])
    gcol = sb("gcol", [128, 2, 2, 2, 8, 2])
    b_gcol = [Buf(), Buf()]
    gateB_all = sb("gateB", [128, 6, D])
    b_gateB = [Buf(), Buf()]
    k.gslot = {(0, 0, 0): 0, (0, 0, 1): 1, (0, 1, 0): 2, (0, 1, 1): 3, (1, 0, 0): 4, (1, 1, 0): 5}
    k.gcol, k.b_gcol = gcol, b_gcol
    k.gateB_all, k.b_gateB = gateB_all, b_gateB

    def s0_steps(l, tg, bankT, bank1, bank2):
        st = {}

        def alloc():
            st["modv"] = ar("modv" + tg, [2, 6 * D]); st["b_modv"] = Buf()
            st["modb"] = [ar("modb%d" % i + tg, [2, 512]) for i in range(2)]; st["b_modb"] = [Buf(), Buf()]
            st["wst"] = [ar("modwst%d" % i + tg, [128, 8, 512]) for i in range(2)]; st["b_wst"] = [Buf(), Buf()]
            st["nrm"] = ar("nrm" + tg, [2, D]); st["b_nrm"] = Buf()
            st["grow"] = st["nrm"]; st["b_grow"] = st["b_nrm"]
            st["sel2"] = ar("sel2" + tg, [2, 2, 128]); st["b_sel2"] = Buf()
            P.dma(st["sel2"][:], sel2_d, writes=[st["b_sel2"]], eng="pool")

        def blk(j):
            if j == 0:
                alloc()
            modv, b_modv = st["modv"], st["b_modv"]
            w = st["wst"][j % 2]; bw = st["b_wst"][j % 2]
            mb = st["modb"][j % 2]; bmb = st["b_modb"][j % 2]
            pz, bz = (bank1, bank2)[j % 2]
            for r in range(2):
                P.dma(mb[r:r + 1, :], modb_d[l:l + 1, j * 512:(j + 1) * 512], writes=[bmb], eng="pool")
            P.dma(w[:], modw_d[l].rearrange("(c p) n -> p c n", p=128)[:, :, j * 512:(j + 1) * 512],
                  writes=[bw], eng=("sp" if j % 2 == 0 else "pool"))
            for c in range(8):
                P.mm(pz[0:2, :], scT[:, c, :], w[:, c, :], c == 0, c == 7, reads=[b_scT, bw], writes=[bz])
            P.v("dve", "tensor_tensor", modv[:, j * 512:(j + 1) * 512], pz[0:2, :], mb[:],
                ALU.add, reads=[bz, bmb], writes=[b_modv])

        def fin():
            modv, b_modv = st["modv"], st["b_modv"]
            nrm, b_nrm, grow, b_grow = st["nrm"], st["b_nrm"], st["grow"], st["b_grow"]
            pT_, bT_ = bankT
            for sub in range(2):
                nd = (nmix_d, nffn_d)[sub]
                for r in range(2):
                    P.dma(nrm[r:r + 1, :], nd[l:l + 1, :], writes=[b_nrm], eng="pool")
                base = sub * 3 * D
                P.v("dve", "scalar_tensor_tensor", grow[:], modv[:, base + D:base + 2 * D], 1.0, nrm[:],
                    ALU.add, ALU.mult, reads=[b_modv, b_nrm], writes=[b_grow])
                for gi, src_ in enumerate((grow[:], modv[:, base:base + D])):
                    for c in range(8):
                        P.tr(pT_[:, 2 * c:2 * c + 2], src_[:, c * 128:(c + 1) * 128], identF[0:2, 0:2],
                             reads=[b_grow, b_modv, b_const], writes=[bT_])
                    P.v("dve", "tensor_copy", gcol[:, l, sub, gi].rearrange("p c t -> p (c t)"), pT_[:, 0:16],
                        reads=[bT_], writes=[b_gcol[l]])
            for sub in range(2):
                for w in range(2):
                    if l == 1 and w == 1:
                        continue
                    for hh in range(2):
                        pz, bz = (bank1, bank2)[hh]
                        col = (sub * 3 + 2) * D + hh * 512
                        P.mm(pz[:], st["sel2"][:, w, :], modv[:, col:col + 512], reads=[st["b_sel2"], b_modv], writes=[bz])
                        P.v("dve", "tensor_copy", gateB_all[:, k.gslot[(l, sub, w)], hh * 512:(hh + 1) * 512], pz[:],
                            reads=[bz], writes=[b_gateB[l]])

        return [(lambda j=j: blk(j)) for j in range(12)] + [fin]

    k.s0_steps = s0_steps
    for stp in s0_steps(0, "L0", k.psum[0], k.psum[1], k.psum[2]):
        stp()

    xt = [sb("xt%d" % i, [128, D]) for i in range(2)]; b_xt = [Buf(), Buf()]
    stat = [sb("stat%d" % i, [128, 8]) for i in range(2)]; b_stat = [Buf(), Buf()]
    xn = [sb("xn%d" % i, [128, D], BF16) for i in range(2)]; b_xn = [Buf(), Buf()]
    b_xnf = Buf()
    hT = [sb("hT%d" % i, [128, 8, 128], BF16) for i in range(2)]; b_hT = [Buf(), Buf()]
    b_hTf = Buf()
    k.cnt = 0

    def modulate(src_ap, l, sub, w, fp32=False, src_buf=None, xt_out=None):
        i = k.cnt % 2
        k.cnt += 1
        X, bX = xt[i], b_xt[i]
        P.dma(X[:], src_ap, reads=([src_buf] if src_buf else []), writes=[bX])
        st, bst = stat[i], b_stat[i]
        P.v("dve", "scalar_tensor_tensor", xn[i][:], X[:], 1.0, X[:], ALU.mult, ALU.mult,
            accum_out=st[:, 0:1], reads=[bX], writes=[b_xn[i], bst])
        P.act(st[:, 1:2], st[:, 0:1], AF.Ln, scale=1.0 / D, bias=1e-6, reads=[bst], writes=[bst])
        P.act(st[:, 2:3], st[:, 1:2], AF.Exp, scale=-0.5, reads=[bst], writes=[bst])
        if not fp32:
            N_, bN = xn[i], b_xn[i]
            H, bH_ = hT[i], b_hT[i]
            pz, bz = k.psum[0]
            pzv = pz[:].bitcast(BF16).rearrange("p (c t) -> p c t", c=8)
            P.v("dve", "tensor_scalar", N_[:], X[:], st[:, 2:3], None, ALU.mult, reads=[bX, bst], writes=[bN])
            for c in range(8):
                P.tr(pzv[:, c, :], N_[:, c * 128:(c + 1) * 128], identB[:], reads=[bN, b_const], writes=[bz])
            for c in range(8):
                P.act(H[:, c, :], pzv[:, c, :], AF.Identity, scale=gcol[:, l, sub, 0, c, w:w + 1],
                      bias=gcol[:, l, sub, 1, c, w:w + 1], reads=[bz, b_gcol[l]], writes=[bH_])
            return H, bH_, X, bX
        else:
            xnf, hTf = k.xnf, k.hTf
            P.v("dve", "tensor_scalar", xnf[:], X[:], st[:, 2:3], None, ALU.mult, reads=[bX, bst], writes=[b_xnf])
            for hh in range(2):
                pz, bz = k.psum[hh]
                for c in range(4):
                    cc = hh * 4 + c
                    P.tr(pz[:, c * 128:(c + 1) * 128], xnf[:, cc * 128:(cc + 1) * 128], identF[:],
                         reads=[b_xnf, b_const], writes=[bz])
                for c in range(4):
                    cc = hh * 4 + c
                    P.act(hTf[:, cc, :], pz[:, c * 128:(c + 1) * 128], AF.Identity,
                          scale=gcol[:, l, sub, 0, cc, w:w + 1], bias=gcol[:, l, sub, 1, cc, w:w + 1],
                          reads=[bz, b_gcol[l]], writes=[b_hTf])
            return hTf, b_hTf, X, bX

    k.modulate = modulate

    def cast(eng, dst, src, reads, writes):
        if eng == "act":
            P.act(dst, src, AF.Copy, reads=reads, writes=writes)
        else:
            P.v(eng, "tensor_copy", dst, src, reads=reads, writes=writes)

    k.cast = cast

    def load_weight_bf16(dst, b_dst, src_ap_fn, nchunk, ncol, stage, b_stage, cast_engs=("pool", "dve", "act")):
        for c in range(nchunk):
            s, bs = stage[c % 2], b_stage[c % 2]
            P.dma(s[:, 0:ncol], src_ap_fn(c), writes=[bs], eng=("sp" if c % 2 == 0 else "pool"))
            cast(cast_engs[c % len(cast_engs)], dst[:, c, :], s[:, 0:ncol], [bs], [b_dst])

    k.b_wstage = [Buf(), Buf()]
    k.load_weight_bf16 = load_weight_bf16

    def bcast_row(name, d_ap, n):
        t = sb(name, [128, n])
        P.dma(t[:], d_ap.partition_broadcast(128).rearrange("p o n -> p (o n)"), writes=[b_const], eng="pool")
        return t

    aqn = bcast_row("aqn", aqn_d, 64); akn = bcast_row("akn", akn_d, 64)
    sqn = bcast_row("sqn", sqn_d, 64); skn = bcast_row("skn", skn_d, 64)
    glan = bcast_row("glan", glan_d, 128)
    sinkb = bcast_row("sinkb", sink_d, 16)

    bnd = sb("bnd", [128, 8]); b_bnd = Buf()

    def make_bound(col, gq, gk):
        P.v("dve", "tensor_reduce", bnd[:, col:col + 1], gq[:], AX.X, ALU.max, apply_absolute_value=True,
            reads=[b_const], writes=[b_bnd])
        P.v("dve", "tensor_reduce", bnd[:, col + 1:col + 2], gk[:], AX.X, ALU.max, apply_absolute_value=True,
            reads=[b_const], writes=[b_bnd])
        P.v("dve", "scalar_tensor_tensor", bnd[:, col + 2:col + 3], bnd[:, col:col + 1], -8.0, bnd[:, col + 1:col + 2],
            ALU.mult, ALU.mult, reads=[b_bnd], writes=[b_bnd])
    make_bound(0, aqn, akn)
    make_bound(4, sqn, skn)
    k.bnd, k.b_bnd = bnd, b_bnd

    def head_norm_rope(dst, b_dst, src, b_src, nh, gain, rope, tmp, b_tmp, scr, b_scr):
        s3 = src.rearrange("p (h d) -> p h d", h=nh)
        t3 = tmp[:, 0:nh * 64].rearrange("p (h d) -> p h d", h=nh)
        P.v("dve", "tensor_tensor", t3, s3, s3, ALU.mult, reads=[b_src], writes=[b_tmp])
        P.v("dve", "tensor_reduce", scr[:, 0:nh], t3, AX.X, ALU.add, reads=[b_tmp], writes=[b_scr])
        P.act(scr[:, 16:16 + nh], scr[:, 0:nh], AF.Ln, scale=1.0 / 64, bias=1e-6, reads=[b_scr], writes=[b_scr])
        P.act(scr[:, 32:32 + nh], scr[:, 16:16 + nh], AF.Exp, scale=-0.5, reads=[b_scr], writes=[b_scr])
        rs = scr[:, 32:32 + nh]
        P.v("dve", "tensor_tensor", t3, s3, rs.unsqueeze(2).to_broadcast([128, nh, 64]), ALU.mult,
            reads=[b_src, b_scr], writes=[b_tmp])
        g3 = gain[:, 0:64].unsqueeze(1).to_broadcast([128, nh, 64])
        if rope is None:
            d3 = dst.rearrange("p (h d) -> p h d", h=nh)
            P.v("dve", "tensor_tensor", d3, t3, g3, ALU.mult, reads=[b_tmp, b_const], writes=[b_dst])
            return
        C, S, b_rope, tmp2, b_tmp2 = rope
        P.v("dve", "tensor_tensor", t3, t3, g3, ALU.mult, reads=[b_tmp, b_const], writes=[b_tmp])
        u3 = tmp2[:, 0:nh * 64].rearrange("p (h d) -> p h d", h=nh)
        P.v("pool", "tensor_tensor", u3, t3, C[:, 0:64].unsqueeze(1).to_broadcast([128, nh, 64]), ALU.mult,
            reads=[b_tmp, b_rope], writes=[b_tmp2])
        t5 = tmp[:, 0:nh * 64].rearrange("p (h a f e) -> p h a f e", h=nh, a=2, f=2)
        S5 = S[:, 0:64].rearrange("p (a f e) -> p a f e", a=2, f=2)
        d5 = dst.rearrange("p (h a f e) -> p h a f e", h=nh, a=2, f=2)
        u5 = tmp2[:, 0:nh * 64].rearrange("p (h a f e) -> p h a f e", h=nh, a=2, f=2)
        for f in range(2):
            sw = t5[:, :, :, 1 - f, :]
            sv = S5[:, :, f, :].unsqueeze(1).to_broadcast([128, nh, 2, 16])
            w_ = scr
            P.v("dve", "tensor_tensor", sw_tmp(k, nh)[:, :, :, f, :], sw, sv, ALU.mult,
                reads=[b_tmp, b_rope], writes=[k.b_swt])
        st5 = sw_tmp(k, nh)
        P.v("dve", "tensor_tensor", d5, st5, u5, ALU.add, reads=[k.b_swt, b_tmp2], writes=[b_dst])

    k.b_swt = Buf()

    def sw_tmp(k_, nh):
        return k_.swt[:, 0:nh * 64].rearrange("p (h a f e) -> p h a f e", h=nh, a=2, f=2)

    k.head_norm_rope = head_norm_rope

    from_l0 = dict(locals())
    k.env = from_l0
    k.areset(0)
    if "stopS0" in dbg:
        return
    layer0(k, stages, dbg)


def layer0(k, stages, dbg):
    g = k.env
    nc, P = k.nc, k.P
    sb, ps, dout = k.ar, k.ps, k.dout
    identF, identB, b_const = g["identF"], g["identB"], g["b_const"]
    modulate = k.modulate
    psum = k.psum
    x_d, ctx_d = g["x_d"], g["ctx_d"]
    (OA_d, QT_d, MIXT_d, PB_lt, PB_ke, PB_kd, PB_v, PB_ee, PB_rg, X1_d) = (
        g["OA_d"], g["QT_d"], g["MIXT_d"], g["PB_lt"], g["PB_ke"], g["PB_kd"], g["PB_v"], g["PB_ee"], g["PB_rg"], g["X1_d"])
    aqn, akn, glan = g["aqn"], g["akn"], g["glan"]
    bnd, b_bnd = k.bnd, k.b_bnd
    gateB_all, b_gateB = k.gateB_all, k.b_gateB
    b_scr = {n: Buf(n) for n in ("OA", "QT", "MIXT", "PBlt", "PBke", "PBkd", "PBv", "PBee", "PBrg", "X1")}
    k.b_scr = b_scr

    KT = sb("KT", [128, 2, 34 * 128], BF16); b_KT = Buf()
    P.v("pool", "memset", KT[64:128], 0.0, writes=[b_KT])
    Vt = sb("Vt", [128, 34, 2, 66], BF16); b_Vt = Buf()
    P.v("pool", "memset", Vt[:], 1.0, writes=[b_Vt])
    mark = k.aoff
    k.swt = sb("swt", [128, 1024])
    w_in = sb("w_in", [128, 8, EVEN_IN], BF16); b_win = Buf()
    tri = sb("tri", [128, 4, 128]); maskAB = sb("maskAB", [128, 2, 256], BF16)
    P.dma(tri[:], g["tri_d"], writes=[b_const], eng="pool")
    P.dma(maskAB[:], g["maskAB_d"], writes=[b_const], eng="pool")
    gwext = sb("gwext", [33, 512]); P.dma(gwext[:], g["gwext_d"], writes=[b_const], eng="pool")
    mark_w = k.aoff
    k.wstage = [sb("wstage%d" % i, [128, 1168]) for i in range(2)]
    evin_d = g["evin_d"]
    for hf in range(2):
        lo_ = hf * 1168
        k.load_weight_bf16(w_in[:, :, lo_:lo_ + 1168], b_win,
                           lambda c, lo_=lo_: evin_d[c * 128:(c + 1) * 128, lo_:lo_ + 1168], 8, 1168, k.wstage, k.b_wstage)
    P.barrier()
    k.aoff = mark_w

    z_sb = sb("z_sb", [128, EVEN_IN]); b_z = Buf()
    ropeC = sb("ropeC", [128, 64]); ropeS = sb("ropeS", [128, 64]); b_rope = Buf()
    tmpA = sb("tmpA", [128, 1024]); b_tmpA = Buf()
    tmpB = sb("tmpB", [128, 1024]); b_tmpB = Buf()
    scr = sb("scr", [128, 64]); b_scr_ = Buf()
    qb = sb("qb", [128, 512], BF16); b_qb = Buf()
    kb = sb("kb", [128, 128], BF16); b_kb = Buf()
    qT_sb = sb("qT_sb", [64, 8, 128], BF16); b_qTsb = Buf()
    lrT = sb("lrT", [33, 128]); b_lrT = Buf()
    P.v("pool", "memset", lrT[:], 1.0, writes=[b_lrT])
    e_sb = sb("e_sb", [128, 512]); b_e = Buf()
    sp_ = sb("sp_", [128, 512]); b_sp = Buf()
    gqT = sb("gqT", [64, 4, 128]); gkT = sb("gkT", [64, 4, 128]); b_gq = Buf(); b_gk = Buf()
    EbT = [sb("EbT%d" % i, [64, 4, 128]) for i in range(2)]; b_EbT = [Buf(), Buf()]
    EnbT = [sb("EnbT%d" % i, [64, 4, 128]) for i in range(2)]; b_EnbT = [Buf(), Buf()]
    kdec = sb("kdec", [128, 256]); b_kdec = Buf()
    LT = [sb("LT%d" % i, [128, 2, 4, 64], BF16) for i in range(2)]
    b_LTq = [Buf(), Buf()]; b_LTa = [Buf(), Buf()]
    keT = [sb("keT%d" % i, [64, 4, 128], BF16) for i in range(2)]; b_keT = [Buf(), Buf()]
    kd = [sb("kd%d" % i, [128, 256], BF16) for i in range(2)]; b_kd = [Buf(), Buf()]
    v_bf = sb("v_bf", [128, 512], BF16); b_vbf = Buf()
    vhi = sb("vhi", [128, 2, 512], BF16); b_vhi = Buf()
    eend = [sb("eend%d" % i, [64, 4, 2]) for i in range(2)]; b_eend = [Buf(), Buf()]
    RH = [[sb("RH%d%d" % (i, j), [128, 4, 128], BF16) for j in range(2)] for i in range(2)]
    b_RHs = [[Buf(), Buf()], [Buf(), Buf()]]; b_RHv = [[Buf(), Buf()], [Buf(), Buf()]]
    S32 = [sb("S32_%d" % i, [64, 4, 128]) for i in range(2)]; b_S32 = [Buf(), Buf()]
    par = [0, 0]
    for d_ in range(2):
        P.v("pool", "memset", S32[d_][:], 0.0, writes=[b_S32[d_]])
        for j in range(2):
            P.v("pool", "memset", RH[d_][j][:], 0.0, writes=[b_RHs[d_][j], b_RHv[d_][j]])
    oa_sb = sb("oa_sb", [128, 512]); b_oa = Buf()
    rg_sb = sb("rg_sb", [128, 512]); b_rg = Buf()
    mixT_sb = sb("mixT_sb", [128, 8, 128], BF16); b_mixT = Buf()

    def mkset2():
        z2 = sb("z_sb2", [128, EVEN_IN])
        qb2 = sb("qb2", [128, 512], BF16); kb2 = sb("kb2", [128, 128], BF16); qT2 = sb("qT_sb2", [64, 8, 128], BF16)
        lrT2 = sb("lrT2", [33, 128]); bl2 = Buf()
        P.v("pool", "memset", lrT2[:], 1.0, writes=[bl2])
        gq2 = sb("gqT2", [64, 4, 128]); gk2 = sb("gkT2", [64, 4, 128])
        kdec2 = sb("kdec2", [128, 256])
        LT2 = [sb("LT2%d" % i, [128, 2, 4, 64], BF16) for i in range(2)]
        keT2 = [sb("keT2%d" % i, [64, 4, 128], BF16) for i in range(2)]
        kd2 = [sb("kd2%d" % i, [128, 256], BF16) for i in range(2)]
        v2 = sb("v_bf2", [128, 512], BF16); vh2 = sb("vhi2", [128, 2, 512], BF16)
        ee2 = [sb("eend2%d" % i, [64, 4, 2]) for i in range(2)]
        oa2 = sb("oa_sb2", [128, 512]); rg2 = sb("rg_sb2", [128, 512])
        return (z2, Buf(), qb2, Buf(), kb2, Buf(), qT2, Buf(), lrT2, bl2, gq2, gk2, Buf(), Buf(), kdec2, Buf(),
                LT2, [Buf(), Buf()], [Buf(), Buf()], keT2, [Buf(), Buf()], kd2, [Buf(), Buf()], v2, Buf(), vh2, Buf(),
                ee2, [Buf(), Buf()], oa2, Buf(), rg2, Buf())

    SETS = [(z_sb, b_z, qb, b_qb, kb, b_kb, qT_sb, b_qTsb, lrT, b_lrT, gqT, gkT, b_gq, b_gk, kdec, b_kdec,
             LT, b_LTq, b_LTa, keT, b_keT, kd, b_kd, v_bf, b_vbf, vhi, b_vhi, eend, b_eend, oa_sb, b_oa, rg_sb, b_rg),
            mkset2()]

    pMisc, bMisc = psum[3]
    pZG, bZG = psum[4]
    pBT, bBT = psum[5]
    pAT, bAT = psum[7][0], Buf("pAT")
    pS, bS = psum[7]
    pPO, bPO = psum[6]

    def scan_tile(d_, lt, b_ltq, b_lta, ke, b_ke, kdt, b_kdt, vb, b_vb, vh, b_vh, ee, b_ee, with_out):
        order = (0, 1) if d_ == 0 else (1, 0)
        pAT4 = pAT[:, 0:256].rearrange("p (h t) -> p h t", h=4)
        pS4 = pS[0:64, :].rearrange("p (h v) -> p h v", h=4)
        po, bpo = pPO, bPO
        for ch in order:
            p_ = par[d_]
            rh, brs, brv = RH[d_][p_], b_RHs[d_][p_], b_RHv[d_][p_]
            if with_out:
                for h in range(4):
                    P.mm(pAT4[64:128, h, :], ke[0:64, h, ch * 64:(ch + 1) * 64], lt[0:64, ch, h, :],
                         reads=[b_ke, b_ltq], writes=[bAT])
                P.v("dve", "tensor_tensor", lt[64:128, ch, :, :], pAT4[64:128, :, :],
                    maskAB[64:128, d_, :].rearrange("p (h t) -> p h t", h=4), ALU.mult,
                    reads=[bAT, b_const], writes=[b_lta])
                P.v("pool", "tensor_copy", rh[64:128, :, :], vh[64:128, ch, :].rearrange("p (h v) -> p h v", h=4),
                    reads=[b_vh], writes=[brv])
                for h in range(4):
                    P.mm(po[ch * 64:(ch + 1) * 64, h * 128:(h + 1) * 128], lt[:, ch, h, :], rh[:, h, :],
                         reads=[b_ltq, b_lta, brs, brv], writes=[bpo])
            for h in range(4):
                P.mm(pS4[:, h, :], kdt[ch * 64:(ch + 1) * 64, h * 64:(h + 1) * 64],
                     vb[ch * 64:(ch + 1) * 64, h * 128:(h + 1) * 128], reads=[b_kdt, b_vb], writes=[bS])
            for h in range(4):
                P.v("dve", "scalar_tensor_tensor", S32[d_][:, h, :], S32[d_][:, h, :], ee[:, h, ch:ch + 1],
                    pS4[:, h, :], ALU.mult, ALU.add, reads=[b_S32[d_], b_ee, bS], writes=[b_S32[d_]])
            nrh = RH[d_][1 - p_]
            P.act(nrh[0:64, :, :], S32[d_][:], AF.Copy, reads=[b_S32[d_]], writes=[b_RHs[d_][1 - p_]])
            par[d_] = 1 - p_

    def gla_prep(dirs, need_q):
        P.tr(pBT[0:32, 0:128], z_sb[:, 1536:1568], identF[:], reads=[b_z, b_const], writes=[bBT])
        P.act(lrT[0:32, :], pBT[0:32, 0:128], AF.Copy, reads=[bBT], writes=[b_lrT])
        P.mm(pZG[:], lrT[:], gwext[:], reads=[b_lrT, b_const], writes=[bZG])
        P.act(e_sb[:], pZG[:], AF.Exp, scale=-1.0, reads=[bZG], writes=[b_e])
        P.act(sp_[:], e_sb[:], AF.Ln, bias=1.0, reads=[b_e], writes=[b_sp])
        pBT4 = pBT[0:64, :].rearrange("p (h t) -> p h t", h=4)
        if need_q:
            for h in range(4):
                P.tr(pBT4[:, h, :], z_sb[:, h * 64:(h + 1) * 64], identF[:], reads=[b_z, b_const], writes=[bBT])
            P.act(gqT[:], pBT4, AF.Copy, reads=[bBT], writes=[b_gq])
        for h in range(4):
            P.tr(pBT4[:, h, :], z_sb[:, 256 + h * 64:256 + (h + 1) * 64], identF[:], reads=[b_z, b_const], writes=[bBT])
        P.act(gkT[:], pBT4, AF.Copy, reads=[bBT], writes=[b_gk])
        P.v("pool", "tensor_copy", v_bf[:], z_sb[:, 512:1024], reads=[b_z], writes=[b_vbf])
        P.dma(vhi[64:128, 0, :], v_bf[0:64, :], reads=[b_vbf], writes=[b_vhi], eng="pool")
        P.v("pool", "tensor_copy", vhi[64:128, 1, :], v_bf[64:128, :], reads=[b_vbf], writes=[b_vhi])
        for d_ in dirs:
            for h in range(4):
                P.mm(pBT4[:, h, :], sp_[:, d_ * 256 + h * 64:d_ * 256 + (h + 1) * 64], tri[:, d_, :],
                     reads=[b_sp, b_const], writes=[bBT])
            P.act(EbT[d_][:], pBT4, AF.Exp, scale=-1.0 / 16, reads=[bBT], writes=[b_EbT[d_]])
            P.act(EnbT[d_][:], pBT4, AF.Exp, scale=1.0 / 16, reads=[bBT], writes=[b_EnbT[d_]])
            P.mm(pZG[:, 0:256], tri[:, 2 + d_, :], sp_[:, d_ * 256:(d_ + 1) * 256], reads=[b_sp, b_const], writes=[bZG])
            P.act(kdec[:], pZG[:, 0:256], AF.Exp, scale=-1.0 / 16, reads=[bZG], writes=[b_kdec])
            if need_q:
                for ch in range(2):
                    P.v("dve", "scalar_tensor_tensor", LT[d_][0:64, ch, :, :],
                        gqT[:, :, ch * 64:(ch + 1) * 64], 0.125,
                        EbT[d_][:, :, ch * 64:(ch + 1) * 64], ALU.mult, ALU.mult,
                        reads=[b_gq, b_EbT[d_]], writes=[b_LTq[d_]])
            P.v("dve", "tensor_tensor", keT[d_][:], gkT[:], EnbT[d_][:], ALU.mult,
                reads=[b_gk, b_EnbT[d_]], writes=[b_keT[d_]])
            P.v("dve", "tensor_tensor", kd[d_][:], z_sb[:, 256:512], kdec[:], ALU.mult,
                reads=[b_z, b_kdec], writes=[b_kd[d_]])
            cols = (63, 127) if d_ == 0 else (0, 64)
            for ch in range(2):
                P.v("dve", "tensor_copy", eend[d_][:, :, ch:ch + 1], EbT[d_][:, :, cols[ch]:cols[ch] + 1],
                    reads=[b_EbT[d_]], writes=[b_eend[d_]])

    def inproj(H, bH, blocks):
        for bi, (lo, hi) in enumerate(blocks):
            pz, bz = psum[1 + bi % 2]
            for c in range(8):
                P.mm(pz[:, 0:hi - lo], H[:, c, :], w_in[:, c, lo:hi], c == 0, c == 7, reads=[bH, b_win], writes=[bz])
            if bi % 2 == 0:
                P.act(z_sb[:, lo:hi], pz[:, 0:hi - lo], AF.Copy, reads=[bz], writes=[b_z])
            else:
                P.v("dve", "tensor_copy", z_sb[:, lo:hi], pz[:, 0:hi - lo], reads=[bz], writes=[b_z])

    FULL = ((0, 512), (512, 1024), (1024, 1536), (1536, 2048), (2048, 2336))
    RBLK = ((256, 768), (768, 1024), (1536, 1568), (2080, 2336))

    def modA(kind, ti):
        if kind == "ctx":
            return modulate(ctx_d[ti * 128:(ti + 1) * 128, :], 0, 0, 1)
        return modulate(x_d[ti * 128:(ti + 1) * 128, :], 0, 0, 0)

    def tileA(kind, ti, pre):
        if kind == "ctx":
            src, w, slot, kidx = ctx_d[ti * 128:(ti + 1) * 128, :], 1, NOWN + ti, ti
        else:
            src, w, slot, kidx = x_d[ti * 128:(ti + 1) * 128, :], 0, ti, 2 + ti
        H, bH, X, bX = pre
        inproj(H, bH, RBLK if kind == "R" else FULL)
        rope = None
        if kind != "ctx":
            P.dma(ropeC[:], g["ropeC_d"][ti * 128:(ti + 1) * 128, :], writes=[b_rope], eng="pool")
            P.dma(ropeS[:], g["ropeS_d"][ti * 128:(ti + 1) * 128, :], writes=[b_rope], eng="pool")
            rope = (ropeC, ropeS, b_rope, tmpB, b_tmpB)
        pM8 = pMisc[0:64, :].bitcast(BF16).rearrange("p (h t) -> p h t", h=8)
        if kind != "R":
            k.head_norm_rope(qb[:], b_qb, z_sb[:, 1568:2080], b_z, 8, aqn, rope, tmpA, b_tmpA, scr, b_scr_)
            for h in range(8):
                P.tr(pM8[:, h, :], qb[:, h * 64:(h + 1) * 64], identB[:], reads=[b_qb, b_const], writes=[bMisc])
            P.act(qT_sb[:], pM8, AF.Copy, reads=[bMisc], writes=[b_qTsb])
            P.dma(QT_d[slot], qT_sb[:], reads=[b_qTsb], writes=[b_scr["QT"]])
        k.head_norm_rope(kb[:], b_kb, z_sb[:, 2080:2208], b_z, 2, akn, rope, tmpA, b_tmpA, scr, b_scr_)
        for h in range(2):
            P.tr(pM8[:, h, :], kb[:, h * 64:(h + 1) * 64], identB[:], reads=[b_kb, b_const], writes=[bMisc])
        P.act(KT[0:64, :, kidx * 128:(kidx + 1) * 128], pM8[:, 0:2, :], AF.Copy, reads=[bMisc], writes=[b_KT])
        P.v("pool", "tensor_copy", Vt[:, kidx, :, 0:64], z_sb[:, 2208:2336].rearrange("p (h d) -> p h d", h=2),
            reads=[b_z], writes=[b_Vt])
        if kind == "R":
            gla_prep((1,), False)
            scan_tile(1, LT[1], b_LTq[1], b_LTa[1], keT[1], b_keT[1], kd[1], b_kd[1], v_bf, b_vbf, vhi, b_vhi,
                      eend[1], b_eend[1], False)
            return
        gla_prep((0, 1), True)
        P.act(rg_sb[:], z_sb[:, 1024:1536], AF.Silu, reads=[b_z], writes=[b_rg])
        P.v("pool", "tensor_tensor", rg_sb[:].rearrange("p (h v) -> p h v", h=4), rg_sb[:].rearrange("p (h v) -> p h v", h=4),
            glan[:, 0:128].unsqueeze(1).to_broadcast([128, 4, 128]), ALU.mult, reads=[b_rg, b_const], writes=[b_rg])
        P.dma(PB_rg[slot], rg_sb[:], reads=[b_rg], writes=[b_scr["PBrg"]])
        P.dma(PB_lt[slot], LT[1][0:64].rearrange("p c h t -> p (c h t)"), reads=[b_LTq[1]], writes=[b_scr["PBlt"]])
        P.dma(PB_ke[slot], keT[1][:].rearrange("p h t -> p (h t)"), reads=[b_keT[1]], writes=[b_scr["PBke"]])
        P.dma(PB_kd[slot], kd[1][:], reads=[b_kd[1]], writes=[b_scr["PBkd"]])
        P.dma(PB_v[slot], v_bf[:], reads=[b_vbf], writes=[b_scr["PBv"]])
        P.dma(PB_ee[slot], eend[1][:].rearrange("p h c -> p (h c)"), reads=[b_eend[1]], writes=[b_scr["PBee"]])
        scan_tile(0, LT[0], b_LTq[0], b_LTa[0], keT[0], b_keT[0], kd[0], b_kd[0], v_bf, b_vbf, vhi, b_vhi,
                  eend[0], b_eend[0], True)
        P.act(oa_sb[:], pPO[:], AF.Copy, reads=[bPO], writes=[b_oa])
        P.dma(OA_d[slot], oa_sb[:], reads=[b_oa], writes=[b_scr["OA"]])

    ob_sb = tmpB[:, 0:512]; b_ob = b_tmpB
    ab_sb = tmpB[:, 512:768].bitcast(BF16); b_ab = Buf()

    def tileB(slot):
        P.dma(LT[1][0:64].rearrange("p c h t -> p (c h t)"), PB_lt[slot], reads=[b_scr["PBlt"]], writes=[b_LTq[1]])
        P.dma(keT[1][:].rearrange("p h t -> p (h t)"), PB_ke[slot], reads=[b_scr["PBke"]], writes=[b_keT[1]])
        P.dma(kd[1][:], PB_kd[slot], reads=[b_scr["PBkd"]], writes=[b_kd[1]])
        P.dma(v_bf[:], PB_v[slot], reads=[b_scr["PBv"]], writes=[b_vbf])
        P.dma(vhi[64:128, 0, :], PB_v[slot][0:64, :], reads=[b_scr["PBv"]], writes=[b_vhi], eng="pool")
        P.dma(vhi[64:128, 1, :], PB_v[slot][64:128, :], reads=[b_scr["PBv"]], writes=[b_vhi], eng="pool")
        P.dma(eend[1][:].rearrange("p h c -> p (h c)"), PB_ee[slot], reads=[b_scr["PBee"]], writes=[b_eend[1]])
        P.dma(oa_sb[:], OA_d[slot], reads=[b_scr["OA"]], writes=[b_oa], eng="pool")
        P.dma(rg_sb[:], PB_rg[slot], reads=[b_scr["PBrg"]], writes=[b_rg], eng="pool")
        scan_tile(1, LT[1], b_LTq[1], b_LTa[1], keT[1], b_keT[1], kd[1], b_kd[1], v_bf, b_vbf, vhi, b_vhi,
                  eend[1], b_eend[1], True)
        P.v("dve", "tensor_tensor", ob_sb[:], pPO[:], oa_sb[:], ALU.add, reads=[bPO, b_oa], writes=[b_ob])
        o3 = ob_sb[:].rearrange("p (h v) -> p h v", h=4)
        t3 = tmpA[:, 0:512].rearrange("p (h v) -> p h v", h=4)
        P.v("dve", "tensor_tensor", t3, o3, o3, ALU.mult, reads=[b_ob], writes=[b_tmpA])
        P.v("dve", "tensor_reduce", scr[:, 0:4], t3, AX.X, ALU.add, reads=[b_tmpA], writes=[b_scr_])
        P.act(scr[:, 16:20], scr[:, 0:4], AF.Ln, scale=1.0 / 128, bias=1e-6, reads=[b_scr_], writes=[b_scr_])
        P.act(scr[:, 32:36], scr[:, 16:20], AF.Exp, scale=-0.5, reads=[b_scr_], writes=[b_scr_])
        P.v("dve", "tensor_tensor", t3, o3, scr[:, 32:36].unsqueeze(2).to_broadcast([128, 4, 128]), ALU.mult,
            reads=[b_ob, b_scr_], writes=[b_tmpA])
        P.v("dve", "tensor_tensor", ab_sb[:], tmpA[:, 0:512], rg_sb[:], ALU.mult, reads=[b_tmpA, b_rg], writes=[b_ab])
        pM4 = pMisc[:].bitcast(BF16)[:, 0:512].rearrange("p (c t) -> p c t", c=4)
        for c in range(4):
            P.tr(pM4[:, c, :], ab_sb[:, c * 128:(c + 1) * 128], identB[:], reads=[b_ab, b_const], writes=[bMisc])
        P.act(mixT_sb[:, 0:4, :], pM4, AF.Copy, reads=[bMisc], writes=[b_mixT])
        P.dma(MIXT_d[slot][:, 0:4, :], mixT_sb[:, 0:4, :], reads=[b_mixT], writes=[b_scr["MIXT"]])

    seqA = [("ctx", 0), ("ctx", 1)] + [("R", ti) for ti in range(31, NOWN - 1, -1)] + [("own", ti) for ti in range(NOWN)]
    pre = modA(*seqA[0])
    nset = 0
    for si, (kind, ti) in enumerate(seqA):
        nxt = modA(*seqA[si + 1]) if si + 1 < len(seqA) else None
        (z_sb, b_z, qb, b_qb, kb, b_kb, qT_sb, b_qTsb, lrT, b_lrT, gqT, gkT, b_gq, b_gk, kdec, b_kdec,
         LT, b_LTq, b_LTa, keT, b_keT, kd, b_kd, v_bf, b_vbf, vhi, b_vhi, eend, b_eend, oa_sb, b_oa, rg_sb, b_rg) = SETS[nset % 2]
        nset += 1
        tileA(kind, ti, pre)
        pre = nxt
        if (kind, ti) == ("ctx", 1):
            for sl in (NOWN + 1, NOWN + 0):
                (z_sb, b_z, qb, b_qb, kb, b_kb, qT_sb, b_qTsb, lrT, b_lrT, gqT, gkT, b_gq, b_gk, kdec, b_kdec,
                 LT, b_LTq, b_LTa, keT, b_keT, kd, b_kd, v_bf, b_vbf, vhi, b_vhi, eend, b_eend, oa_sb, b_oa, rg_sb, b_rg) = SETS[nset % 2]
                nset += 1
                tileB(sl)
    for ti in range(NOWN - 1, -1, -1):
        (z_sb, b_z, qb, b_qb, kb, b_kb, qT_sb, b_qTsb, lrT, b_lrT, gqT, gkT, b_gq, b_gk, kdec, b_kdec,
         LT, b_LTq, b_LTa, keT, b_keT, kd, b_kd, v_bf, b_vbf, vhi, b_vhi, eend, b_eend, oa_sb, b_oa, rg_sb, b_rg) = SETS[nset % 2]
        nset += 1
        tileB(ti)

    if "stopAB" in dbg:
        return
    k.areset(mark)
    k.wstage = [sb("wstage%d" % i, [128, D]) for i in range(2)]
    w_out = sb("w_out", [128, 4, D], BF16); b_wout = Buf()
    w_outB = sb("w_outB", [64, 8, D], BF16); b_woutB = Buf()
    evout_d = g["evout_d"]
    k.load_weight_bf16(w_out, b_wout, lambda c: evout_d[c * 128:(c + 1) * 128, :], 4, D, k.wstage, k.b_wstage)
    for h in range(8):
        s_, bs_ = k.wstage[h % 2], k.b_wstage[h % 2]
        P.dma(s_[0:64, :], evout_d[512 + h * 64:512 + (h + 1) * 64, :], writes=[bs_], eng=("sp" if h % 2 == 0 else "pool"))
        k.cast(("pool", "dve", "act")[h % 3], w_outB[:, h, :], s_[0:64, :], [bs_], [b_woutB])
    ones_f = sb("ones_f", [128, 64]); P.v("pool", "memset", ones_f[:], 1.0, writes=[b_const])
    qT_in = [sb("qT_in%d" % i, [128, 8, 128], BF16) for i in range(2)]; b_qTin = [Buf(), Buf()]
    for i_ in range(2):
        P.v("pool", "memset", qT_in[i_][64:128], 0.0, writes=[b_qTin[i_]])
    rsr = sb("rsr", [128, 512]); b_rsr = Buf()
    bc_sb = sb("bc_sb", [64, 512]); b_bc = Buf()
    OT = sb("OT", [64, 8, 128], BF16); b_OT = Buf()
    mT_in = sb("mT_in", [128, 4, 128], BF16); b_mTin = Buf()
    x1_sb = sb("x1_sb", [128, D]); b_x1 = Buf()
    pO = [psum[6], psum[7]]
    SG = [(k.psall[:, 2 * g_ * 512:(2 * g_ + 2) * 512], [psum[2 * g_][1], psum[2 * g_ + 1][1]]) for g_ in range(3)]
    pBC, bBC = psum[0]
    NPT = 4
    PT = [sb("PTp%d" % i, [128, 1024], BF16) for i in range(NPT)]; b_PT = [Buf() for _ in range(NPT)]
    cstate = dict(it=0)

    mT_ins = [mT_in, sb("mT_in2", [128, 4, 128], BF16)]; b_mTins = [b_mTin, Buf()]
    x_ins = [sb("xC%d" % i, [128, D]) for i in range(2)]; b_xins = [Buf(), Buf()]
    ones_b = sb("ones_b", [128, 64], BF16); P.v("pool", "memset", ones_b[:], 1.0, writes=[b_const])
    rsb = sb("rsb", [128, 2, 512], BF16); b_rsb = Buf()
    bcs = [sb("bcs%d" % i, [64, 512]) for i in range(2)]; b_bcs = [Buf(), Buf()]
    cpre = dict(n=0)

    def loadC(kind, ti):
        if kind == "ctx":
            slot, src = NOWN + ti, ctx_d[ti * 128:(ti + 1) * 128, :]
        else:
            slot, src = ti, x_d[ti * 128:(ti + 1) * 128, :]
        i = cpre["n"] % 2
        cpre["n"] += 1
        P.dma(qT_in[i][0:64], QT_d[slot], reads=[b_scr["QT"]], writes=[b_qTin[i]])
        P.dma(mT_ins[i][:], MIXT_d[slot][:, 0:4, :], reads=[b_scr["MIXT"]], writes=[b_mTins[i]], eng="pool")
        P.dma(x_ins[i][:], src, writes=[b_xins[i]])
        return (qT_in[i], b_qTin[i], mT_ins[i], b_mTins[i], x_ins[i], b_xins[i])

    def tileC(kind, ti, pre):
        if kind == "ctx":
            slot, w, src, keys = NOWN + ti, 1, ctx_d[ti * 128:(ti + 1) * 128, :], [0, 1]
        else:
            slot, w, src, keys = ti, 0, x_d[ti * 128:(ti + 1) * 128, :], list(range(34))
        Q, bQ, mT_in, b_mTin, X, bX = pre
        units = [(kvh, keys[a_:a_ + 2]) for kvh in range(2) for a_ in range(0, len(keys), 2)]
        pend = []

        def pv(u, pt, bpt):
            kvh, kts = u
            po, bpo = pO[kvh]
            for j, kt in enumerate(kts):
                P.mm(po[0:65, :], Vt[:, kt, kvh, 0:65], pt[:, j * 512:(j + 1) * 512], kt == keys[0], kt == keys[-1],
                     reads=[bpt, b_Vt], writes=[bpo])

        for u in units:
            kvh, kts = u
            it = cstate["it"]
            cstate["it"] += 1
            psc, bscs = SG[it % 3]
            pt, bpt = PT[it % NPT], b_PT[it % NPT]
            n = len(kts)
            for j, kt in enumerate(kts):
                P.mm(psc[:, j * 512:(j + 1) * 512], KT[:, kvh, kt * 128:(kt + 1) * 128],
                     Q[:, kvh * 4:(kvh + 1) * 4, :].rearrange("p h t -> p (h t)"),
                     reads=[b_KT, bQ], writes=[bscs[j]])
            P.act(pt[:, 0:n * 512], psc[:, 0:n * 512], AF.Exp, scale=0.125, bias=bnd[:, 2:3],
                  reads=bscs[0:n] + [b_bnd], writes=[bpt])
            pend.append((u, pt, bpt))
            if len(pend) > 2:
                pv(*pend.pop(0))
        while pend:
            pv(*pend.pop(0))
        for kvh in range(2):
            po, bpo = pO[kvh]
            P.act(rsr[64:65, :], po[64:65, :], AF.Ln, reads=[bpo], writes=[b_rsr])
            P.act(rsb[64:65, kvh, :], rsr[64:65, :], AF.Exp, scale=-1.0, reads=[b_rsr], writes=[b_rsb])
        for kvh in range(2):
            pb_, bb_ = psum[2 + kvh]
            P.mm(pb_[0:64, :], ones_b[64:65, 0:64], rsb[64:65, kvh, :], reads=[b_const, b_rsb], writes=[bb_])
        for kvh in range(2):
            pb_, bb_ = psum[2 + kvh]
            P.v("dve", "tensor_copy", bcs[kvh][:], pb_[0:64, :], reads=[bb_], writes=[b_bcs[kvh]])
        for kvh in range(2):
            po, bpo = pO[kvh]
            P.v("dve", "tensor_tensor", OT[:, kvh * 4:(kvh + 1) * 4, :].rearrange("p h t -> p (h t)"), po[0:64, :], bcs[kvh][:],
                ALU.mult, reads=[bpo, b_bcs[kvh]], writes=[b_OT])
        for hh in range(2):
            pz, bz = psum[hh]
            for c in range(4):
                P.mm(pz[:], mT_in[:, c, :], w_out[:, c, hh * 512:(hh + 1) * 512], c == 0, False,
                     reads=[b_mTin, b_wout], writes=[bz])
            for h in range(8):
                P.mm(pz[:], OT[:, h, :], w_outB[:, h, hh * 512:(hh + 1) * 512], False, h == 7,
                     reads=[b_OT, b_woutB], writes=[bz])
            P.v("dve", "tensor_tensor", x1_sb[:, hh * 512:(hh + 1) * 512], pz[:],
                gateB_all[:, k.gslot[(0, 0, w)], hh * 512:(hh + 1) * 512],
                ALU.mult, reads=[bz, b_gateB[0]], writes=[b_x1])
        P.v("pool", "tensor_tensor", x1_sb[:], x1_sb[:], X[:], ALU.add, reads=[b_x1, bX], writes=[b_x1])
        P.dma(X1_d[slot * 128:(slot + 1) * 128, :], x1_sb[:], reads=[b_x1], writes=[b_scr["X1"]])

    tiles_c = [("ctx", 0), ("ctx", 1)] + [("own", t) for t in range(NOWN)]
    if "fewC" in dbg:
        tiles_c = [("ctx", 0), ("own", 0), ("own", 16)]
    s1 = k.s0_steps(1, "L1", psum[0], psum[0], psum[1])
    preC = loadC(*tiles_c[0])
    for si, (kind, ti) in enumerate(tiles_c):
        nxtC = loadC(*tiles_c[si + 1]) if si + 1 < len(tiles_c) else None
        if s1:
            s1.pop(0)()
        tileC(kind, ti, preC)
        preC = nxtC
    while s1:
        s1.pop(0)()
    if "stopL0mix" in dbg:
        return
    X2_d, X3_d, out_d = g["X2_d"], g["X3_d"], g["out_d"]
    b_scr["X2"] = Buf("X2"); b_scr["X3"] = Buf("X3")
    tiles = []
    for slot in range(NTOK0):
        w = 1 if slot >= NOWN else 0
        tiles.append((X1_d[slot * 128:(slot + 1) * 128, :], b_scr["X1"], X2_d[slot * 128:(slot + 1) * 128, :], b_scr["X2"], w))
    if "fewM" in dbg:
        tiles = [tiles[0], tiles[16], tiles[17]]
    moe(k, 0, tiles, "a")
    if "stopL0" in dbg or "stopD1" in dbg or "stopD2" in dbg:
        return
    layer1(k, dbg)
    if "stopL1" in dbg:
        return
    tiles = [(X3_d[t * 128:(t + 1) * 128, :], b_scr["X3"], out_d[t * 128:(t + 1) * 128, :], None, 0) for t in range(16)]
    moe(k, 1, tiles, "b")


def moe(k, l, tiles, tag):
    g = k.env
    nc, P = k.nc, k.P
    ar, psum = k.ar, k.psum
    identF, b_const = g["identF"], g["b_const"]
    NT = len(tiles)
    NTOK = NT * 128
    k.areset(0)
    hT_all = ar("hT_all" + tag, [128, 8, NTOK], BF16); b_hTall = Buf()
    yacc = ar("yacc" + tag, [128, NT, D]); b_yacc = [Buf() for _ in range(NT)]
    comb_all = ar("comb" + tag, [128, NT, 16]); b_combt = [Buf() for _ in range(NT)]
    mark = k.aoff
    k.xnf = ar("xnf" + tag, [128, D]); k.hTf = ar("hTf" + tag, [128, 8, 128])
    rw = ar("rw" + tag, [128, 8, 20]); rb = ar("rb" + tag, [128, 20]); b_rw = Buf()
    P.dma(rw[:], g["rw_d"][l].rearrange("(c p) n -> p c n", p=128), writes=[b_rw], eng="pool")
    P.dma(rb[:], g["rb_d"][l].partition_broadcast(128).rearrange("p o n -> p (o n)"), writes=[b_rw], eng="pool")
    lg = ar("lg" + tag, [128, 20]); b_lg = Buf()
    rt = ar("rt" + tag, [128, 64]); b_rt = Buf()
    pR, bR = psum[2]
    b_hTt = [Buf() for _ in range(NT)]

    def d1(ti):
        (src, sbuf_, dst, dbuf_, w) = tiles[ti]
        H, bH, X, bX = k.modulate(src, l, 1, w, fp32=True, src_buf=sbuf_)
        P.v("pool", "tensor_copy", hT_all[:, :, ti * 128:(ti + 1) * 128], H[:], reads=[bH], writes=[b_hTt[ti]])
        for c_ in range(8):
            P.mm(pR[:, 0:20], H[:, c_, :], rw[:, c_, :], c_ == 0, c_ == 7, reads=[bH, b_rw], writes=[bR])
        P.v("dve", "tensor_tensor", lg[:], pR[:, 0:20], rb[:], ALU.add, reads=[bR, b_rw], writes=[b_lg])
        gl, el = lg[:, 0:4], lg[:, 4:20]
        R_ = lambda a, b: rt[:, a:b]
        gmax, ngmax, sume, gw = R_(0, 1), R_(1, 2), R_(2, 3), R_(3, 4)
        ohg, eg, esel, oh1, msk, oh2, ew, sg = R_(4, 8), R_(8, 12), R_(12, 16), R_(16, 20), R_(20, 24), R_(24, 28), R_(28, 32), R_(32, 36)
        m1, m2, dd, e2, w1, w2 = R_(36, 37), R_(37, 38), R_(38, 39), R_(39, 40), R_(40, 41), R_(41, 42)
        rd, wr = [b_lg, b_rt], [b_rt]
        V = lambda name, *a, **kw: P.v("dve", name, *a, reads=rd, writes=wr, **kw)
        V("tensor_reduce", gmax, gl, AX.X, ALU.max)
        V("tensor_scalar", ohg, gl, gmax, None, ALU.is_equal)
        V("tensor_scalar", ngmax, gmax, -1.0, None, ALU.mult)
        P.act(eg, gl, AF.Exp, bias=ngmax, accum_out=sume, reads=rd, writes=wr)
        V("reciprocal", gw, sume)
        V("tensor_scalar", esel, el[:, 0:4], ohg[:, 0:1], None, ALU.mult)
        for gi in range(1, 4):
            V("scalar_tensor_tensor", esel, el[:, gi * 4:(gi + 1) * 4], ohg[:, gi:gi + 1], esel, ALU.mult, ALU.add)
        V("tensor_reduce", m1, esel, AX.X, ALU.max)
        V("tensor_scalar", oh1, esel, m1, None, ALU.is_equal)
        V("scalar_tensor_tensor", msk, oh1, -NEG_BIG, esel, ALU.mult, ALU.add)
        V("tensor_reduce", m2, msk, AX.X, ALU.max)
        V("tensor_scalar", oh2, msk, m2, None, ALU.is_equal)
        V("tensor_tensor", dd, m2, m1, ALU.subtract)
        P.act(e2, dd, AF.Exp, reads=rd, writes=wr)
        V("tensor_scalar", w1, e2, 1.0, None, ALU.add)
        V("reciprocal", w1, w1)
        V("tensor_tensor", w2, e2, w1, ALU.mult)
        V("tensor_scalar", ew, oh1, w1, None, ALU.mult)
        V("scalar_tensor_tensor", ew, oh2, w2, ew, ALU.mult, ALU.add)
        V("tensor_scalar", sg, ohg, gw, None, ALU.mult)
        for gi in range(4):
            P.v("dve", "tensor_scalar", comb_all[:, ti, gi * 4:(gi + 1) * 4], ew, sg[:, gi:gi + 1], None, ALU.mult,
                reads=[b_rt], writes=[b_combt[ti]])
    wst = [ar("mwst%d" % i + tag, [128, 512]) for i in range(2)]; b_wst = [Buf(), Buf()]
    Wg = [ar("Wg%d" % i + tag, [128, 8, 256], BF16) for i in range(2)]
    Wu = [ar("Wu%d" % i + tag, [128, 8, 256], BF16) for i in range(2)]
    Wd = [ar("Wd%d" % i + tag, [128, 2, D], BF16) for i in range(2)]
    b_W = [[Buf(), Buf(), Buf()] for _ in range(2)]
    sa = [ar("sa%d" % i + tag, [128, 512]) for i in range(2)]; b_sa = [Buf(), Buf()]
    hid = [ar("hid%d" % i + tag, [128, 2, 512], BF16) for i in range(2)]; b_hid = [Buf(), Buf()]
    pCW, bCW = psum[0]
    pAs = [psum[1], psum[2]]
    pUs = [psum[3], psum[4]]
    pYs = [psum[5], psum[6], psum[0]]
    wcnt = 0
    blocks = [(s, min(512, NTOK - s)) for s in range(0, NTOK, 512)]
    it = 0
    yi = 0
    def load_w(e):
        nonlocal wcnt
        pe_ = e % 2
        srcs = (g["wg_d"][l, e].rearrange("(c p) f -> p c f", p=128), g["wu_d"][l, e].rearrange("(c p) f -> p c f", p=128),
                g["wd_d"][l, e].rearrange("(c p) n -> p c n", p=128))
        dsts = (Wg[pe_], Wu[pe_], Wd[pe_])
        for wi in range(3):
            for q4 in range(4):
                s_, bs_ = wst[wcnt % 2], b_wst[wcnt % 2]
                if wi < 2:
                    sv = s_[:].rearrange("p (c f) -> p c f", c=2)
                    sview = srcs[wi][:, 2 * q4:2 * q4 + 2, :]
                    dview = dsts[wi][:, 2 * q4:2 * q4 + 2, :]
                else:
                    sv = s_[:]
                    sview = srcs[wi][:, q4 // 2, (q4 % 2) * 512:(q4 % 2 + 1) * 512]
                    dview = dsts[wi][:, q4 // 2, (q4 % 2) * 512:(q4 % 2 + 1) * 512]
                P.dma(sv, sview, writes=[bs_], eng=("sp" if wcnt % 2 == 0 else "pool"))
                P.v("pool", "tensor_copy", dview, sv, reads=[bs_], writes=[b_W[pe_][wi]])
                wcnt += 1

    def gu(e, t0, n, i):
        pe_ = e % 2
        for fc in range(2):
            pa, ba = pAs[fc]
            pu, bu = pUs[fc]
            for c_ in range(8):
                P.mm(pa[:, 0:n], Wg[pe_][:, c_, fc * 128:(fc + 1) * 128], hT_all[:, c_, t0:t0 + n], c_ == 0, c_ == 7,
                     reads=[b_W[pe_][0]] + b_hTt[t0 // 128:(t0 + n) // 128], writes=[ba])
            for c_ in range(8):
                P.mm(pu[:, 0:n], Wu[pe_][:, c_, fc * 128:(fc + 1) * 128], hT_all[:, c_, t0:t0 + n], c_ == 0, c_ == 7,
                     reads=[b_W[pe_][1]] + b_hTt[t0 // 128:(t0 + n) // 128], writes=[bu])
            P.act(sa[fc][:, 0:n], pa[:, 0:n], AF.Silu, reads=[ba], writes=[b_sa[fc]])
            P.v("dve", "tensor_tensor", hid[i][:, fc, 0:n], sa[fc][:, 0:n], pu[:, 0:n], ALU.mult,
                reads=[b_sa[fc], bu], writes=[b_hid[i]])

    def dn(e, t0, n, i):
        nonlocal yi
        pe_ = e % 2
        ntile = n // 128
        for j in range(ntile):
            tile_i = t0 // 128 + j
            for dh in range(2):
                py, by = pYs[yi % 3]
                yi += 1
                for fc in range(2):
                    P.mm(py[:], hid[i][:, fc, j * 128:(j + 1) * 128], Wd[pe_][:, fc, dh * 512:(dh + 1) * 512],
                         fc == 0, fc == 1, reads=[b_hid[i], b_W[pe_][2]], writes=[by])
                ya = yacc[:, tile_i, dh * 512:(dh + 1) * 512]
                cs = comb_all[:, tile_i, e:e + 1]
                if e == 0:
                    P.v("dve", "tensor_scalar", ya, py[:], cs, None, ALU.mult, reads=[by, b_combt[tile_i]], writes=[b_yacc[tile_i]])
                else:
                    P.v("dve", "scalar_tensor_tensor", ya, py[:], cs, ya, ALU.mult, ALU.add,
                        reads=[by, b_combt[tile_i], b_yacc[tile_i]], writes=[b_yacc[tile_i]])

    items = [(e, t0, n) for e in range(16) for (t0, n) in blocks]
    pend = None
    last_e = -1
    for idx, (e, t0, n) in enumerate(items):
        if e != last_e:
            if e == 0:
                load_w(0)
            if e + 1 < 16:
                pass
            last_e = e
        if e == 0:
            for tq in range(t0 // 128, (t0 + n) // 128):
                d1(tq)
        gu(e, t0, n, idx % 2)
        if pend is not None:
            dn(*pend)
        pend = (e, t0, n, idx % 2)
        if t0 == blocks[0][0] and e + 1 < 16:
            load_w(e + 1)
    dn(*pend)
    xo = [k.xnf, k.hTf[:].rearrange("p c t -> p (c t)")]; b_xo = [k.env["b_xnf"], k.env["b_hTf"]]
    for ti, (src, sbuf_, dst, dbuf_, w) in enumerate(tiles):
        i = ti % 2
        X, bX = g["xt"][i], g["b_xt"][i]
        P.dma(X[:], src, reads=([sbuf_] if sbuf_ else []), writes=[bX], eng="pool")
        gs = k.gslot[(l, 1, w)]
        P.v("dve", "tensor_tensor", xo[i][:], yacc[:, ti, :], k.gateB_all[:, gs, :], ALU.mult,
            reads=[b_yacc[ti], k.b_gateB[l]], writes=[b_xo[i]])
        P.v("pool", "tensor_tensor", xo[i][:], xo[i][:], X[:], ALU.add, reads=[b_xo[i], bX], writes=[b_xo[i]])
        P.dma(dst, xo[i][:], reads=[b_xo[i]], writes=([dbuf_] if dbuf_ else []))


def layer1(k, dbg):
    g = k.env
    nc, P = k.nc, k.P
    ar, psum = k.ar, k.psum
    identF, identB, b_const = g["identF"], g["identB"], g["b_const"]
    X2_d, X3_d, QT1_d = g["X2_d"], g["X3_d"], g["QT1_d"]
    bnd, b_bnd = k.bnd, k.b_bnd
    sqn, skn, sinkb = g["sqn"], g["skn"], g["sinkb"]
    b_X2, b_X3 = k.b_scr["X2"], k.b_scr["X3"]
    b_QT1 = Buf()
    k.areset(0)
    NK = 19
    KT = ar("KT1", [128, 2, NK * 128], BF16); b_KT = Buf()
    P.v("pool", "memset", KT[64:128], 0.0, writes=[b_KT])
    Vt = ar("Vt1", [128, NK, 2, 66], BF16); b_Vt = Buf()
    P.v("pool", "memset", Vt[:], 1.0, writes=[b_Vt])
    wmask = ar("wmask", [128, 2, 128], BF16)
    P.dma(wmask[:], g["wmask_d"], writes=[b_const], eng="pool")
    k.wstage = [ar("wstage1%d" % i, [128, 1280]) for i in range(2)]
    k.swt = ar("swt1", [128, 1024])
    w_in = ar("w_in1", [128, 8, 1280], BF16); b_win = Buf()
    odin_d, odout_d = g["odin_d"], g["odout_d"]
    k.load_weight_bf16(w_in, b_win, lambda c: odin_d[c * 128:(c + 1) * 128, :], 8, 1280, k.wstage, k.b_wstage)
    w_outB = ar("w_out1B", [64, 16, D], BF16); b_woutB = Buf()
    for h in range(16):
        s_, bs_ = k.wstage[h % 2], k.b_wstage[h % 2]
        P.dma(s_[0:64, 0:D], odout_d[h * 64:(h + 1) * 64, :], writes=[bs_], eng=("sp" if h % 2 == 0 else "pool"))
        k.cast(("pool", "dve", "act")[h % 3], w_outB[:, h, :], s_[0:64, 0:D], [bs_], [b_woutB])
    ones_f = ar("ones_f1", [128, 64]); P.v("pool", "memset", ones_f[:], 1.0, writes=[b_const])
    z_sb = ar("z_sb1", [128, 1280]); b_z = Buf()
    ropeC = ar("ropeC1", [128, 64]); ropeS = ar("ropeS1", [128, 64]); b_rope = Buf()
    tmpA = ar("tmpA1", [128, 1024]); b_tmpA = Buf()
    tmpB = ar("tmpB1", [128, 1024]); b_tmpB = Buf()
    scr = ar("scr1", [128, 64]); b_scr_ = Buf()
    qb = ar("qb1", [128, 1024], BF16); b_qb = Buf()
    kb = ar("kb1", [128, 128], BF16); b_kb = Buf()
    qT_sb = ar("qT_sb1", [64, 16, 128], BF16); b_qTsb = Buf()
    pMisc, bMisc = psum[3]
    pMisc2, bMisc2 = psum[4]

    def modE(kind, ti):
        if kind == "ctx":
            return k.modulate(X2_d[(NOWN + ti) * 128:(NOWN + ti + 1) * 128, :], 1, 0, 1, src_buf=b_X2)
        return k.modulate(X2_d[ti * 128:(ti + 1) * 128, :], 1, 0, 0, src_buf=b_X2)

    def tileE(kind, ti, pre):
        if kind == "ctx":
            src, w, kidx = X2_d[(NOWN + ti) * 128:(NOWN + ti + 1) * 128, :], 1, ti
        else:
            src, w, kidx = X2_d[ti * 128:(ti + 1) * 128, :], 0, 2 + ti
        need_q = (kind != "ctx" and ti < 16)
        H, bH, X, bX = pre
        blocks = ((0, 512), (512, 1024), (1024, 1280)) if need_q else ((1024, 1280),)
        for bi, (lo, hi) in enumerate(blocks):
            pz, bz = psum[1 + bi % 2]
            for c in range(8):
                P.mm(pz[:, 0:hi - lo], H[:, c, :], w_in[:, c, lo:hi], c == 0, c == 7, reads=[bH, b_win], writes=[bz])
            P.act(z_sb[:, lo:hi], pz[:, 0:hi - lo], AF.Copy, reads=[bz], writes=[b_z])
        rope = None
        if kind != "ctx":
            P.dma(ropeC[:], g["ropeC_d"][ti * 128:(ti + 1) * 128, :], writes=[b_rope], eng="pool")
            P.dma(ropeS[:], g["ropeS_d"][ti * 128:(ti + 1) * 128, :], writes=[b_rope], eng="pool")
            rope = (ropeC, ropeS, b_rope, tmpB, b_tmpB)
        pM8 = pMisc[0:64, :].bitcast(BF16).rearrange("p (h t) -> p h t", h=8)
        pM8b = pMisc2[0:64, :].bitcast(BF16).rearrange("p (h t) -> p h t", h=8)
        if need_q:
            k.head_norm_rope(qb[:], b_qb, z_sb[:, 0:1024], b_z, 16, sqn, rope, tmpA, b_tmpA, scr, b_scr_)
            for h in range(16):
                pm, bm = (pM8, bMisc) if h < 8 else (pM8b, bMisc2)
                P.tr(pm[:, h % 8, :], qb[:, h * 64:(h + 1) * 64], identB[:], reads=[b_qb, b_const], writes=[bm])
            P.act(qT_sb[:, 0:8, :], pM8, AF.Copy, reads=[bMisc], writes=[b_qTsb])
            P.act(qT_sb[:, 8:16, :], pM8b, AF.Copy, reads=[bMisc2], writes=[b_qTsb])
            P.dma(QT1_d[ti], qT_sb[:], reads=[b_qTsb], writes=[b_QT1])
        k.head_norm_rope(kb[:], b_kb, z_sb[:, 1024:1152], b_z, 2, skn, rope, tmpA, b_tmpA, scr, b_scr_)
        for h in range(2):
            P.tr(pM8[:, h, :], kb[:, h * 64:(h + 1) * 64], identB[:], reads=[b_kb, b_const], writes=[bMisc])
        P.act(KT[0:64, :, kidx * 128:(kidx + 1) * 128], pM8[:, 0:2, :], AF.Copy, reads=[bMisc], writes=[b_KT])
        P.v("pool", "tensor_copy", Vt[:, kidx, :, 0:64], z_sb[:, 1152:1280].rearrange("p (h d) -> p h d", h=2),
            reads=[b_z], writes=[b_Vt])

    seqE = [("ctx", 0), ("ctx", 1)] + [("own", ti) for ti in range(17)]
    SETE = [(z_sb, b_z, qb, b_qb, kb, b_kb, qT_sb, b_qTsb),
            (ar("z_sb1b", [128, 1280]), Buf(), ar("qb1b", [128, 1024], BF16), Buf(), ar("kb1b", [128, 128], BF16), Buf(),
             ar("qT_sb1b", [64, 16, 128], BF16), Buf())]
    pre = modE(*seqE[0])
    for si, (kind, ti) in enumerate(seqE):
        nxt = modE(*seqE[si + 1]) if si + 1 < len(seqE) else None
        (z_sb, b_z, qb, b_qb, kb, b_kb, qT_sb, b_qTsb) = SETE[si % 2]
        tileE(kind, ti, pre)
        pre = nxt

    if "stopE1" in dbg:
        return
    qT_in = [ar("qT_in1%d" % i, [128, 16, 128], BF16) for i in range(2)]; b_qTin = [Buf(), Buf()]
    for i_ in range(2):
        P.v("pool", "memset", qT_in[i_][64:128], 0.0, writes=[b_qTin[i_]])
    NPT = 3
    PT = [ar("PT1%d" % i, [128, 1024], BF16) for i in range(NPT)]; b_PT = [Buf() for _ in range(NPT)]
    esink = ar("esink1", [128, 16]); b_esink = Buf()
    P.act(esink[:], sinkb[:], AF.Exp, bias=bnd[:, 6:7], reads=[b_const, b_bnd], writes=[b_esink])
    rsr = ar("rsr1", [128, 512]); b_rsr = Buf()
    bc_sb = ar("bc_sb1", [64, 512]); b_bc = Buf()
    OT = ar("OT1", [64, 16, 128], BF16); b_OT = Buf()
    x3_sb = ar("x3_sb", [128, D]); b_x3 = Buf()
    pO = [psum[4], psum[5], psum[6], psum[7]]
    SG = [(k.psall[:, 2 * g_ * 512:(2 * g_ + 2) * 512], [psum[2 * g_][1], psum[2 * g_ + 1][1]]) for g_ in range(2)]
    pBC, bBC = psum[0]
    ones_b = ar("ones_b1", [128, 64], BF16); P.v("pool", "memset", ones_b[:], 1.0, writes=[b_const])
    rsb = ar("rsb1", [128, 4, 512], BF16); b_rsb = Buf()
    bcs = [ar("bcs1%d" % i_, [64, 512]) for i_ in range(4)]; b_bcs = [Buf() for _ in range(4)]
    x_ins = [ar("xE%d" % i_, [128, D]) for i_ in range(2)]; b_xins = [Buf(), Buf()]
    vsink = ar("vsink", [128, 66], BF16); b_vs = Buf()
    P.v("pool", "memset", vsink[:], 0.0, writes=[b_vs])
    P.v("pool", "memset", vsink[:, 64:65], 1.0, writes=[b_vs])
    esrow = ar("esrow", [128, 16, 128], BF16); b_esrow = Buf()
    P.v("dve", "tensor_copy", esrow[64:65], esink[64:65, :].unsqueeze(2).to_broadcast([1, 16, 128]),
        reads=[b_esink], writes=[b_esrow])

    def loadQ(qi):
        i_ = qi % 2
        P.dma(qT_in[i_][0:64], QT1_d[qi], reads=[b_QT1], writes=[b_qTin[i_]])
        P.dma(x_ins[i_][:], X2_d[qi * 128:(qi + 1) * 128, :], reads=[b_X2], writes=[b_xins[i_]])

    it = 0
    loadQ(0)
    for qi in range(16):
        i = qi % 2
        Q, bQ = qT_in[i], b_qTin[i]
        if qi + 1 < 16:
            loadQ(qi + 1)
        keys = [(0, None), (1, None)]
        if qi > 0:
            keys.append((2 + qi - 1, 0))
        keys.append((2 + qi, None))
        keys.append((2 + qi + 1, 1))
        nk = len(keys)
        units = [(gq, list(range(a_, min(a_ + 2, nk)))) for gq in range(4) for a_ in range(0, nk, 2)]
        pend = None

        def pv(gq, kks, pt, bpt):
            po, bpo = pO[gq]
            for j, kk in enumerate(kks):
                kt, mk = keys[kk]
                P.mm(po[0:65, :], Vt[:, kt, gq // 2, 0:65], pt[:, j * 512:(j + 1) * 512], kk == 0, False,
                     reads=[bpt, b_Vt], writes=[bpo])
            if kks[-1] == nk - 1:
                P.mm(po[0:65, :], vsink[64:65, 0:65], esrow[64:65, gq * 4:(gq + 1) * 4, :].rearrange("p h t -> p (h t)"),
                     False, True, reads=[b_vs, b_esrow], writes=[bpo])

        for (gq, kks) in units:
            kvh = gq // 2
            psc, bscs = SG[it % 2]
            pt, bpt = PT[it % NPT], b_PT[it % NPT]
            it += 1
            n = len(kks)
            for j, kk in enumerate(kks):
                kt, mk = keys[kk]
                P.mm(psc[:, j * 512:(j + 1) * 512], KT[:, kvh, kt * 128:(kt + 1) * 128],
                     Q[:, gq * 4:(gq + 1) * 4, :].rearrange("p h t -> p (h t)"), reads=[b_KT, bQ], writes=[bscs[j]])
            P.act(pt[:, 0:n * 512], psc[:, 0:n * 512], AF.Exp, scale=0.125, bias=bnd[:, 6:7],
                  reads=bscs[0:n] + [b_bnd], writes=[bpt])
            for j, kk in enumerate(kks):
                kt, mk = keys[kk]
                if mk is not None:
                    p3 = pt[:, j * 512:(j + 1) * 512].rearrange("p (h t) -> p h t", h=4)
                    P.v("dve", "tensor_tensor", p3, p3, wmask[:, mk, :].unsqueeze(1).to_broadcast([128, 4, 128]), ALU.mult,
                        reads=[bpt, b_const], writes=[bpt])
            if pend is not None:
                pv(*pend)
            pend = (gq, kks, pt, bpt)
        pv(*pend)
        for gq in range(4):
            po, bpo = pO[gq]
            P.act(rsr[64:65, :], po[64:65, :], AF.Ln, reads=[bpo], writes=[b_rsr])
            P.act(rsb[64:65, gq, :], rsr[64:65, :], AF.Exp, scale=-1.0, reads=[b_rsr], writes=[b_rsb])
        for gq in range(4):
            pb_, bb_ = psum[gq]
            P.mm(pb_[0:64, :], ones_b[64:65, 0:64], rsb[64:65, gq, :], reads=[b_const, b_rsb], writes=[bb_])
        for gq in range(4):
            pb_, bb_ = psum[gq]
            P.v("dve", "tensor_copy", bcs[gq][:], pb_[0:64, :], reads=[bb_], writes=[b_bcs[gq]])
        for gq in range(4):
            po, bpo = pO[gq]
            P.v("dve", "tensor_tensor", OT[:, gq * 4:(gq + 1) * 4, :].rearrange("p h t -> p (h t)"), po[0:64, :], bcs[gq][:],
                ALU.mult, reads=[bpo, b_bcs[gq]], writes=[b_OT])
        X, bX = x_ins[i], b_xins[i]
        for hh in range(2):
            pz, bz = psum[1 + hh]
            for h in range(16):
                P.mm(pz[:], OT[:, h, :], w_outB[:, h, hh * 512:(hh + 1) * 512], h == 0, h == 15,
                     reads=[b_OT, b_woutB], writes=[bz])
            P.v("dve", "tensor_tensor", x3_sb[:, hh * 512:(hh + 1) * 512], pz[:],
                k.gateB_all[:, k.gslot[(1, 0, 0)], hh * 512:(hh + 1) * 512], ALU.mult,
                reads=[bz, k.b_gateB[1]], writes=[b_x3])
        P.v("pool", "tensor_tensor", x3_sb[:], x3_sb[:], X[:], ALU.add, reads=[b_x3, bX], writes=[b_x3])
        P.dma(X3_d[qi * 128:(qi + 1) * 128, :], x3_sb[:], reads=[b_x3], writes=[b_X3])


def rope_tables():
    t = np.arange(SEQ)
    row = (t // 64).astype(np.float32)
    col = (t % 64).astype(np.float32)
    inv = (10000.0 ** (-np.arange(0, 32, 2, dtype=np.float32) / 32)).astype(np.float32)
    ang = np.stack([row[:, None] * inv, col[:, None] * inv], axis=1)
    c = np.cos(ang).astype(np.float32)
    s = np.sin(ang).astype(np.float32)
    C = np.zeros((SEQ, 2, 2, 16), np.float32)
    S = np.zeros((SEQ, 2, 2, 16), np.float32)
    C[:, :, 0] = c
    C[:, :, 1] = c
    S[:, :, 0] = -s
    S[:, :, 1] = s
    return C.reshape(SEQ, 64), S.reshape(SEQ, 64)


def host_consts():
    p = np.arange(128)
    same = (p[:, None] // 64) == (p[None, :] // 64)
    s, t = p[:, None], p[None, :]
    tri = np.stack([same & (s <= t), same & (s >= t), same & (s > t), same & (s < t)], axis=1).astype(np.float32)
    sm = (p % 64)[:, None]
    tt = np.arange(64)[None, :]
    mA = np.tile((sm <= tt), (1, 4))
    mB = np.tile((sm >= tt), (1, 4))
    maskAB = np.stack([mA, mB], axis=1).astype(ml_dtypes.bfloat16)
    sel2 = np.zeros((2, 2, 128), np.float32)
    sel2[0, 0] = 1
    sel2[1, 1] = 1
    wmask = np.stack([(s >= t), (s <= t)], axis=1).astype(ml_dtypes.bfloat16)
    return dict(identF=np.eye(128, dtype=np.float32), identB=np.eye(128).astype(ml_dtypes.bfloat16),
                tri=tri, maskAB=maskAB, sel2=sel2, wmask=wmask)


def make_in_maps(inp):
    f = lambda a: np.ascontiguousarray(np.asarray(a, dtype=np.float32))
    C, S = rope_tables()
    consts = host_consts()
    maps = []
    rw = np.concatenate([f(inp["router_group_w"]), f(inp["router_expert_w"])], axis=-1)
    rb = np.concatenate([f(inp["router_group_b"]), f(inp["router_expert_b"])], axis=-1)[:, None, :]
    shared = dict(
        mod_w=f(inp["mod_w"]), mod_b=f(inp["mod_b"]), norm_mix=f(inp["norm_mix"]), norm_ffn=f(inp["norm_ffn"]),
        ev_w_in=f(inp["ev_w_in"])[0], ev_w_out=f(inp["ev_w_out"])[0],
        gla_out_norm=f(inp["gla_out_norm"]), att_q_norm=f(inp["att_q_norm"]), att_k_norm=f(inp["att_k_norm"]),
        od_w_in=f(inp["od_w_in"])[0], od_w_out=f(inp["od_w_out"])[0], swa_sink=f(inp["swa_sink"]),
        swa_q_norm=f(inp["swa_q_norm"]), swa_k_norm=f(inp["swa_k_norm"]),
        router_w=np.ascontiguousarray(rw), router_b=np.ascontiguousarray(rb),
        exp_w_gate=f(inp["exp_w_gate"]).reshape(2, 16, D, 256), exp_w_up=f(inp["exp_w_up"]).reshape(2, 16, D, 256),
        exp_w_down=f(inp["exp_w_down"]).reshape(2, 16, 256, D), **consts)
    gw = f(inp["gla_gate_w"])[0]
    gb = f(inp["gla_gate_b"])[0]
    for core in range(8):
        b, half = core // 2, core % 2
        x = f(inp["x"])[b]
        cx = f(inp["ctx"])[b]
        order = (0, 1)
        if half == 1:
            x = x[::-1]
            cx = cx[::-1]
            order = (1, 0)
        gwe = np.zeros((33, 512), np.float32)
        for i, dr in enumerate(order):
            gwe[16 * dr:16 * dr + 16, 256 * i:256 * i + 256] = gw[dr]
            gwe[32, 256 * i:256 * i + 256] = gb[dr]
        m = dict(shared)
        m.update(x=np.ascontiguousarray(x), ctx=np.ascontiguousarray(cx),
                 crow=np.ascontiguousarray(np.stack([f(inp["c"])[b], f(inp["c_ctx"])])),
                 gw_ext=gwe,
                 ropeC=np.ascontiguousarray(C[::-1] if half else C),
                 ropeS=np.ascontiguousarray(S[::-1] if half else S))
        maps.append(m)
    return maps


_CACHE = {}


def kernel(**inputs):
    if "nc" not in _CACHE:
        _CACHE["nc"] = build()[0]
    nc = _CACHE["nc"]
    maps = make_in_maps(inputs)
    res = run_bass_kernel_spmd(nc, maps, core_ids=list(range(8)))
    out = np.zeros((4, SEQ, D), np.float32)
    for core in range(8):
        b, half = core // 2, core % 2
        o = np.asarray(res.results[core]["out"], dtype=np.float32)
        if half == 0:
            out[b, 0:2048] = o
        else:
            out[b, 2048:] = o[::-1]
    return out
```

```python
import numpy as np
import ml_dtypes
from contextlib import ExitStack
import concourse.bass as bass
import concourse.mybir as mybir
from concourse.bass_utils import run_bass_kernel_spmd

F32 = mybir.dt.float32
BF16 = mybir.dt.bfloat16
ALU = mybir.AluOpType
AF = mybir.ActivationFunctionType
AX = mybir.AxisListType

import os
COMPUTE = ("pe", "act", "dve", "pool")
SCHED_W = int(os.environ.get("SCHED_W", "128"))
PE_A = float(os.environ.get("PE_A", "0.05"))
PE_B = float(os.environ.get("PE_B", "0.00035"))
PE_F = float(os.environ.get("PE_F", "2.5"))
DMA_L = float(os.environ.get("DMA_L", "2.2"))
ACT_S = float(os.environ.get("ACT_S", "0.8"))
DVE_S = float(os.environ.get("DVE_S", "1.0"))
POOL_S = float(os.environ.get("POOL_S", "1.0"))
SEM_L = float(os.environ.get("SEM_L", "0.1"))
NDMASEM = 12

D = 1024
SEQ = 4096
NOWN = 17
NTOK0 = 19
EVEN_IN = 2336
NEG_BIG = 1.0e30


class Buf:
    __slots__ = ("name", "w", "r")

    def __init__(self, name=""):
        self.name = name
        self.w = None
        self.r = {}


class Prog:
    def __init__(self, nc):
        self.nc = nc
        self.ops = []
        self.cost = []
        self.reorder = True
        self.base = set()
        self.eng = {"pe": nc.tensor, "act": nc.scalar, "dve": nc.vector,
                    "pool": nc.gpsimd, "sp": nc.sync}

    def op(self, eng, fn, reads=(), writes=(), dma=False, cost=0.4):
        idx = len(self.ops)
        deps = set(self.base)
        for b in reads:
            if b.w is not None:
                deps.add(b.w)
        for b in writes:
            if b.w is not None:
                deps.add(b.w)
            for v in b.r.values():
                if isinstance(v, list):
                    deps.update(v)
                else:
                    deps.add(v)
        key = (eng, dma)
        for b in reads:
            b.r.setdefault(key, []).append(idx)
        for b in writes:
            b.w = idx
            b.r = {}
        deps.discard(idx)
        self.ops.append((eng, dma, fn, deps))
        self.cost.append(cost)
        return idx

    def barrier(self):
        last = {}
        dm = {}
        for i, (eng, dma, fn, deps) in enumerate(self.ops):
            if dma:
                dm.setdefault(eng, []).append(i)
            else:
                last[eng] = i
        base = set(last.values())
        for q, lst in dm.items():
            base.update(lst[-NDMASEM:])
        self.base = base

    @staticmethod
    def _fs(ap):
        n = 1
        for s in ap.shape[1:]:
            n *= s
        return n

    def dma(self, out, in_, reads=(), writes=(), eng="sp"):
        e = self.eng[eng]
        nb = self._fs(out) * out.shape[0] * (2 if out.dtype == BF16 else 4)
        return self.op(eng, lambda: e.dma_start(out=out, in_=in_), reads, writes, dma=True,
                       cost=DMA_L + nb / 150e3)

    def mm(self, out, lhsT, rhs, start=True, stop=True, reads=(), writes=()):
        nc = self.nc
        c = PE_A + self._fs(rhs) * PE_B
        if rhs.dtype == F32:
            c *= PE_F
        return self.op("pe", lambda: nc.tensor.matmul(out, lhsT, rhs, start=start, stop=stop),
                       reads, writes, cost=c)

    def tr(self, out, in_, ident, reads=(), writes=()):
        nc = self.nc
        return self.op("pe", lambda: nc.tensor.transpose(out, in_, ident), reads, writes, cost=0.12)

    def act(self, out, in_, func, reads=(), writes=(), **kw):
        nc = self.nc
        return self.op("act", lambda: nc.scalar.activation(out=out, in_=in_, func=func, **kw),
                       reads, writes, cost=ACT_S * (0.25 + self._fs(in_) * 0.0009))

    def v(self, eng, name, *args, reads=(), writes=(), **kw):
        f = getattr(self.eng[eng], name)
        c = DVE_S * (0.15 + self._fs(args[0]) * 0.0011)
        if eng == "pool":
            c = POOL_S * (0.3 + self._fs(args[0]) * 0.0025)
        return self.op(eng, lambda: f(*args, **kw), reads, writes, cost=c)

    def schedule(self, W=SCHED_W):
        ops, cost = self.ops, self.cost
        n = len(ops)
        fin = [None] * n
        start = [0.0] * n
        rem = {}
        for i, (eng, dma, fn, deps) in enumerate(ops):
            rem.setdefault(eng, []).append(i)
        etime = {e: 0.0 for e in rem}
        nsched = 0
        while nsched < n:
            best = None
            for e, lst in rem.items():
                if not lst:
                    continue
                te = etime[e]
                for i in lst[:W]:
                    deps = ops[i][3]
                    r = te
                    ok = True
                    for d in deps:
                        f = fin[d]
                        if f is None:
                            ok = False
                            break
                        if f > r:
                            r = f
                    if not ok:
                        continue
                    key = (r, i)
                    if best is None or key < best[0]:
                        best = (key, e, i)
                    if r <= te:
                        break
            (r, i), e, _ = best
            eng, dma, fn, deps = ops[i]
            start[i] = r
            if dma:
                etime[e] = r + 0.5
                fin[i] = r + cost[i]
            else:
                etime[e] = r + cost[i]
                fin[i] = r + cost[i] + SEM_L
            rem[e].remove(i)
            nsched += 1
        order = sorted(range(n), key=lambda j: (start[j], j))
        self.sim_time = max(f for f in fin)
        self.sim_start, self.sim_fin = start, fin
        return order

    def emit(self, sems):
        ops = self.ops
        n = len(ops)
        need = [False] * n
        for (eng, dma, fn, deps) in ops:
            for d in deps:
                deng, ddma, _, _ = ops[d]
                if (not ddma) and deng == "pe" and eng == "pe" and not dma:
                    continue
                need[d] = True
        sig = [None] * n
        cnt = {e: 0 for e in COMPUTE}
        dcnt = {}
        waited = {}
        order = self.schedule() if self.reorder else range(n)
        for i in order:
            eng, dma, fn, deps = ops[i]
            e = self.eng[eng]
            w = {}
            for d in deps:
                deng, ddma, _, _ = ops[d]
                if (not ddma) and deng == "pe" and eng == "pe" and not dma:
                    continue
                s, val = sig[d]
                if w.get(id(s), (None, -1))[1] < val:
                    w[id(s)] = (s, val)
            if dma:
                j = dcnt.get(eng, 0)
                pool = sems[("dma", eng)]
                s = pool[j % len(pool)]
                val = 16 * (j // len(pool) + 1)
                if val > 16 and w.get(id(s), (None, -1))[1] < val - 16:
                    w[id(s)] = (s, val - 16)
                dcnt[eng] = j + 1
                sig[i] = (s, val)
            for sid, (s, val) in w.items():
                k = (eng, sid)
                if waited.get(k, -1) >= val:
                    continue
                waited[k] = val
                e.wait_ge(s, val)
            ins = fn()
            if dma:
                ins.then_inc(sig[i][0], 16)
            elif need[i]:
                cnt[eng] += 1
                sig[i] = (sems[eng], cnt[eng])
                ins.then_inc(sems[eng], 1)
        for (k, pool) in sems.items():
            if isinstance(k, tuple):
                eng = k[1]
                j = dcnt.get(eng, 0)
                e = self.eng[eng]
                for q, s in enumerate(pool):
                    c = (j - q + len(pool) - 1) // len(pool) if j > q else 0
                    if c > 0:
                        e.wait_ge(s, 16 * c)
        return dict(n_ops=n, sig=cnt, dmas=dcnt)


class K:
    pass


def build(stages=("all",), dbg=()):
    nc = bass.Bass("TRN2", target_bir_lowering=False)
    P = Prog(nc)
    k = K()
    k.nc, k.P = nc, P
    k.dbgset = set(dbg)
    es = ExitStack()
    k.es = es
    k.dbg = {}

    def din(name, shape, dt=F32):
        return nc.dram_tensor(name, list(shape), dt, kind="ExternalInput").ap()

    def dscr(name, shape, dt=F32):
        if name in dbg:
            return nc.dram_tensor(name, list(shape), dt, kind="ExternalOutput").ap()
        return nc.dram_tensor(name, list(shape), dt).ap()

    def dout(name, shape, dt=F32):
        return nc.dram_tensor(name, list(shape), dt, kind="ExternalOutput").ap()

    def sb(name, shape, dt=F32):
        return es.enter_context(nc.sbuf_tensor("sb_" + name, list(shape), dt))

    def ps(name, shape, dt=F32):
        return es.enter_context(nc.psum_tensor("ps_" + name, list(shape), dt))

    k.din, k.dscr, k.dout, k.sb, k.ps = din, dscr, dout, sb, ps
    ARW = 41700
    k.arena = None
    k.aoff = 0

    def ar(name, shape, dt=F32):
        if k.arena is None:
            k.arena = sb("arena", [128, ARW])
        n = 1
        for s in shape[1:]:
            n *= s
        w = n if dt == F32 else (n + 1) // 2
        off = k.aoff
        k.aoff += w + (w % 2)
        assert k.aoff <= ARW, (name, k.aoff)
        a = k.arena[0:shape[0], off:off + w]
        if dt != F32:
            a = a.bitcast(dt)
        if len(shape) > 2:
            names = " ".join("d%d" % i for i in range(1, len(shape)))
            kw = {"d%d" % i: shape[i] for i in range(1, len(shape))}
            a = a.rearrange("p (%s) -> p %s" % (names, names), **kw)
        return a

    def areset(mark=0):
        P.barrier()
        k.peak = getattr(k, "peak", [])
        k.peak.append(k.aoff * 4 // 1024)
        k.marks = getattr(k, "marks", [])
        k.marks.append(len(P.ops))
        k.aoff = mark

    k.ar, k.areset = ar, areset

    with es:
        sems = {e: es.enter_context(nc.semaphore("s_" + e)) for e in COMPUTE}
        for q in ("sp", "pool"):
            sems[("dma", q)] = [es.enter_context(nc.semaphore(f"d_{q}{i}")) for i in range(NDMASEM)]
        body(k, stages, dbg)
        st = P.emit(sems)
        k.stats = st
    return nc, k


def body(k, stages, dbg):
    nc, P = k.nc, k.P
    din, dscr, dout, sb, ps, ar = k.din, k.dscr, k.dout, k.sb, k.ps, k.ar

    x_d = din("x", [SEQ, D])
    ctx_d = din("ctx", [256, D])
    crow_d = din("crow", [2, D])
    modw_d = din("mod_w", [2, D, 6 * D])
    modb_d = din("mod_b", [2, 6 * D])
    nmix_d = din("norm_mix", [2, D])
    nffn_d = din("norm_ffn", [2, D])
    evin_d = din("ev_w_in", [D, EVEN_IN])
    evout_d = din("ev_w_out", [D, D])
    gwext_d = din("gw_ext", [33, 512])
    glan_d = din("gla_out_norm", [1, 128])
    aqn_d = din("att_q_norm", [1, 64])
    akn_d = din("att_k_norm", [1, 64])
    odin_d = din("od_w_in", [D, 1280])
    odout_d = din("od_w_out", [D, D])
    sink_d = din("swa_sink", [1, 16])
    sqn_d = din("swa_q_norm", [1, 64])
    skn_d = din("swa_k_norm", [1, 64])
    rw_d = din("router_w", [2, D, 20])
    rb_d = din("router_b", [2, 1, 20])
    wg_d = din("exp_w_gate", [2, 16, D, 256])
    wu_d = din("exp_w_up", [2, 16, D, 256])
    wd_d = din("exp_w_down", [2, 16, 256, D])
    ropeC_d = din("ropeC", [SEQ, 64])
    ropeS_d = din("ropeS", [SEQ, 64])
    identF_d = din("identF", [128, 128])
    identB_d = din("identB", [128, 128], BF16)
    tri_d = din("tri", [128, 4, 128])
    maskAB_d = din("maskAB", [128, 2, 256], BF16)
    sel2_d = din("sel2", [2, 2, 128])
    wmask_d = din("wmask", [128, 2, 128], BF16)
    out_d = dout("out", [2048, D])

    X1_d = dscr("X1", [NTOK0 * 128, D])
    X2_d = dscr("X2", [NTOK0 * 128, D])
    X3_d = dscr("X3", [2048, D])
    OA_d = dscr("OA", [NTOK0, 128, 512])
    QT_d = dscr("QT", [NTOK0, 64, 8, 128], BF16)
    MIXT_d = dscr("MIXT", [NTOK0, 128, 8, 128], BF16)
    PB_lt = dscr("PB_lt", [NTOK0, 64, 512], BF16)
    PB_ke = dscr("PB_ke", [NTOK0, 64, 512], BF16)
    PB_kd = dscr("PB_kd", [NTOK0, 128, 256], BF16)
    PB_v = dscr("PB_v", [NTOK0, 128, 512], BF16)
    PB_ee = dscr("PB_ee", [NTOK0, 64, 8])
    PB_rg = dscr("PB_rg", [NTOK0, 128, 512])
    QT1_d = dscr("QT1", [16, 64, 16, 128], BF16)

    identF = sb("identF", [128, 128]); identB = sb("identB", [128, 128], BF16)
    b_const = Buf("const")
    for t, d in ((identF, identF_d), (identB, identB_d)):
        P.dma(t[:], d, writes=[b_const], eng="pool")
    k.identF, k.identB, k.b_const = identF, identB, b_const

    psall = ps("psall", [128, 4096])
    k.psall = psall
    pA, pB, pC, pD, pE, pF, pG, pH = [psall[:, i * 512:(i + 1) * 512] for i in range(8)]
    bA, bB, bC, bD, bE, bF_, bG, bH = [Buf("ps%d" % i) for i in range(8)]
    k.psum = [(pA, bA), (pB, bB), (pC, bC), (pD, bD), (pE, bE), (pF, bF_), (pG, bG), (pH, bH)]

    crow = ar("crow", [2, D]); b_crow = Buf()
    scT = sb("scT", [128, 8, 2]); b_scT = Buf()
    P.dma(crow[:], crow_d, writes=[b_crow])
    P.act(crow[:], crow[:], AF.Silu, reads=[b_crow], writes=[b_crow])
    for c in range(8):
        P.tr(pA[:, 2 * c:2 * c + 2], crow[:, c * 128:(c + 1) * 128], identF[0:2, 0:2],
             reads=[b_crow, b_const], writes=[bA])
    P.v("dve", "tensor_copy", scT[:].rearrange("p c t -> p (c t)"), pA[:, 0:16], reads=[bA], writes=[b_scT])
    gcol = sb("gcol", [128, 2, 2, 2, 8, 2])
    b_gcol = [Buf(), Buf()]
    gateB_all = sb("gateB", [128, 6, D])
    b_gateB = [Buf(), Buf()]
    k.gslot = {(0, 0, 0): 0, (0, 0, 1): 1, (0, 1, 0): 2, (0, 1, 1): 3, (1, 0, 0): 4, (1, 1, 0): 5}
    k.gcol, k.b_gcol = gcol, b_gcol
    k.gateB_all, k.b_gateB = gateB_all, b_gateB

    def s0_steps(l, tg, bankT, bank1, bank2):
        st = {}

        def alloc():
            st["modv"] = ar("modv" + tg, [2, 6 * D]); st["b_modv"] = Buf()
            st["modb"] = [ar("modb%d" % i + tg, [2, 512]) for i in range(2)]; st["b_modb"] = [Buf(), Buf()]
            st["wst"] = [ar("modwst%d" % i + tg, [128, 8, 512]) for i in range(2)]; st["b_wst"] = [Buf(), Buf()]
            st["nrm"] = ar("nrm" + tg, [2, D]); st["b_nrm"] = Buf()
            st["grow"] = st["nrm"]; st["b_grow"] = st["b_nrm"]
            st["sel2"] = ar("sel2" + tg, [2, 2, 128]); st["b_sel2"] = Buf()
            P.dma(st["sel2"][:], sel2_d, writes=[st["b_sel2"]], eng="pool")

        def blk(j):
            if j == 0:
                alloc()
            modv, b_modv = st["modv"], st["b_modv"]
            w = st["wst"][j % 2]; bw = st["b_wst"][j % 2]
            mb = st["modb"][j % 2]; bmb = st["b_modb"][j % 2]
            pz, bz = (bank1, bank2)[j % 2]
            for r in range(2):
                P.dma(mb[r:r + 1, :], modb_d[l:l + 1, j * 512:(j + 1) * 512], writes=[bmb], eng="pool")
            P.dma(w[:], modw_d[l].rearrange("(c p) n -> p c n", p=128)[:, :, j * 512:(j + 1) * 512],
                  writes=[bw], eng=("sp" if j % 2 == 0 else "pool"))
            for c in range(8):
                P.mm(pz[0:2, :], scT[:, c, :], w[:, c, :], c == 0, c == 7, reads=[b_scT, bw], writes=[bz])
            P.v("dve", "tensor_tensor", modv[:, j * 512:(j + 1) * 512], pz[0:2, :], mb[:],
                ALU.add, reads=[bz, bmb], writes=[b_modv])

        def fin():
            modv, b_modv = st["modv"], st["b_modv"]
            nrm, b_nrm, grow, b_grow = st["nrm"], st["b_nrm"], st["grow"], st["b_grow"]
            pT_, bT_ = bankT
            for sub in range(2):
                nd = (nmix_d, nffn_d)[sub]
                for r in range(2):
                    P.dma(nrm[r:r + 1, :], nd[l:l + 1, :], writes=[b_nrm], eng="pool")
                base = sub * 3 * D
                P.v("dve", "scalar_tensor_tensor", grow[:], modv[:, base + D:base + 2 * D], 1.0, nrm[:],
                    ALU.add, ALU.mult, reads=[b_modv, b_nrm], writes=[b_grow])
                for gi, src_ in enumerate((grow[:], modv[:, base:base + D])):
                    for c in range(8):
                        P.tr(pT_[:, 2 * c:2 * c + 2], src_[:, c * 128:(c + 1) * 128], identF[0:2, 0:2],
                             reads=[b_grow, b_modv, b_const], writes=[bT_])
                    P.v("dve", "tensor_copy", gcol[:, l, sub, gi].rearrange("p c t -> p (c t)"), pT_[:, 0:16],
                        reads=[bT_], writes=[b_gcol[l]])
            for sub in range(2):
                for w in range(2):
                    if l == 1 and w == 1:
                        continue
                    for hh in range(2):
                        pz, bz = (bank1, bank2)[hh]
                        col = (sub * 3 + 2) * D + hh * 512
                        P.mm(pz[:], st["sel2"][:, w, :], modv[:, col:col + 512], reads=[st["b_sel2"], b_modv], writes=[bz])
                        P.v("dve", "tensor_copy", gateB_all[:, k.gslot[(l, sub, w)], hh * 512:(hh + 1) * 512], pz[:],
                            reads=[bz], writes=[b_gateB[l]])

        return [(lambda j=j: blk(j)) for j in range(12)] + [fin]

    k.s0_steps = s0_steps
    for stp in s0_steps(0, "L0", k.psum[0], k.psum[1], k.psum[2]):
        stp()

    xt = [sb("xt%d" % i, [128, D]) for i in range(2)]; b_xt = [Buf(), Buf()]
    stat = [sb("stat%d" % i, [128, 8]) for i in range(2)]; b_stat = [Buf(), Buf()]
    xn = [sb("xn%d" % i, [128, D], BF16) for i in range(2)]; b_xn = [Buf(), Buf()]
    b_xnf = Buf()
    hT = [sb("hT%d" % i, [128, 8, 128], BF16) for i in range(2)]; b_hT = [Buf(), Buf()]
    b_hTf = Buf()
    k.cnt = 0

    def modulate(src_ap, l, sub, w, fp32=False, src_buf=None, xt_out=None):
        i = k.cnt % 2
        k.cnt += 1
        X, bX = xt[i], b_xt[i]
        P.dma(X[:], src_ap, reads=([src_buf] if src_buf else []), writes=[bX])
        st, bst = stat[i], b_stat[i]
        P.v("dve", "scalar_tensor_tensor", xn[i][:], X[:], 1.0, X[:], ALU.mult, ALU.mult,
            accum_out=st[:, 0:1], reads=[bX], writes=[b_xn[i], bst])
        P.act(st[:, 1:2], st[:, 0:1], AF.Ln, scale=1.0 / D, bias=1e-6, reads=[bst], writes=[bst])
        P.act(st[:, 2:3], st[:, 1:2], AF.Exp, scale=-0.5, reads=[bst], writes=[bst])
        if not fp32:
            N_, bN = xn[i], b_xn[i]
            H, bH_ = hT[i], b_hT[i]
            pz, bz = k.psum[0]
            pzv = pz[:].bitcast(BF16).rearrange("p (c t) -> p c t", c=8)
            P.v("dve", "tensor_scalar", N_[:], X[:], st[:, 2:3], None, ALU.mult, reads=[bX, bst], writes=[bN])
            for c in range(8):
                P.tr(pzv[:, c, :], N_[:, c * 128:(c + 1) * 128], identB[:], reads=[bN, b_const], writes=[bz])
            for c in range(8):
                P.act(H[:, c, :], pzv[:, c, :], AF.Identity, scale=gcol[:, l, sub, 0, c, w:w + 1],
                      bias=gcol[:, l, sub, 1, c, w:w + 1], reads=[bz, b_gcol[l]], writes=[bH_])
            return H, bH_, X, bX
        else:
            xnf, hTf = k.xnf, k.hTf
            P.v("dve", "tensor_scalar", xnf[:], X[:], st[:, 2:3], None, ALU.mult, reads=[bX, bst], writes=[b_xnf])
            for hh in range(2):
                pz, bz = k.psum[hh]
                for c in range(4):
                    cc = hh * 4 + c
                    P.tr(pz[:, c * 128:(c + 1) * 128], xnf[:, cc * 128:(cc + 1) * 128], identF[:],
                         reads=[b_xnf, b_const], writes=[bz])
                for c in range(4):
                    cc = hh * 4 + c
                    P.act(hTf[:, cc, :], pz[:, c * 128:(c + 1) * 128], AF.Identity,
                          scale=gcol[:, l, sub, 0, cc, w:w + 1], bias=gcol[:, l, sub, 1, cc, w:w + 1],
                          reads=[bz, b_gcol[l]], writes=[b_hTf])
            return hTf, b_hTf, X, bX

    k.modulate = modulate

    def cast(eng, dst, src, reads, writes):
        if eng == "act":
            P.act(dst, src, AF.Copy, reads=reads, writes=writes)
        else:
            P.v(eng, "tensor_copy", dst, src, reads=reads, writes=writes)

    k.cast = cast

    def load_weight_bf16(dst, b_dst, src_ap_fn, nchunk, ncol, stage, b_stage, cast_engs=("pool", "dve", "act")):
        for c in range(nchunk):
            s, bs = stage[c % 2], b_stage[c % 2]
            P.dma(s[:, 0:ncol], src_ap_fn(c), writes=[bs], eng=("sp" if c % 2 == 0 else "pool"))
            cast(cast_engs[c % len(cast_engs)], dst[:, c, :], s[:, 0:ncol], [bs], [b_dst])

    k.b_wstage = [Buf(), Buf()]
    k.load_weight_bf16 = load_weight_bf16

    def bcast_row(name, d_ap, n):
        t = sb(name, [128, n])
        P.dma(t[:], d_ap.partition_broadcast(128).rearrange("p o n -> p (o n)"), writes=[b_const], eng="pool")
        return t

    aqn = bcast_row("aqn", aqn_d, 64); akn = bcast_row("akn", akn_d, 64)
    sqn = bcast_row("sqn", sqn_d, 64); skn = bcast_row("skn", skn_d, 64)
    glan = bcast_row("glan", glan_d, 128)
    sinkb = bcast_row("sinkb", sink_d, 16)

    bnd = sb("bnd", [128, 8]); b_bnd = Buf()

    def make_bound(col, gq, gk):
        P.v("dve", "tensor_reduce", bnd[:, col:col + 1], gq[:], AX.X, ALU.max, apply_absolute_value=True,
            reads=[b_const], writes=[b_bnd])
        P.v("dve", "tensor_reduce", bnd[:, col + 1:col + 2], gk[:], AX.X, ALU.max, apply_absolute_value=True,
            reads=[b_const], writes=[b_bnd])
        P.v("dve", "scalar_tensor_tensor", bnd[:, col + 2:col + 3], bnd[:, col:col + 1], -8.0, bnd[:, col + 1:col + 2],
            ALU.mult, ALU.mult, reads=[b_bnd], writes=[b_bnd])
    make_bound(0, aqn, akn)
    make_bound(4, sqn, skn)
    k.bnd, k.b_bnd = bnd, b_bnd

    def head_norm_rope(dst, b_dst, src, b_src, nh, gain, rope, tmp, b_tmp, scr, b_scr):
        s3 = src.rearrange("p (h d) -> p h d", h=nh)
        t3 = tmp[:, 0:nh * 64].rearrange("p (h d) -> p h d", h=nh)
        P.v("dve", "tensor_tensor", t3, s3, s3, ALU.mult, reads=[b_src], writes=[b_tmp])
        P.v("dve", "tensor_reduce", scr[:, 0:nh], t3, AX.X, ALU.add, reads=[b_tmp], writes=[b_scr])
        P.act(scr[:, 16:16 + nh], scr[:, 0:nh], AF.Ln, scale=1.0 / 64, bias=1e-6, reads=[b_scr], writes=[b_scr])
        P.act(scr[:, 32:32 + nh], scr[:, 16:16 + nh], AF.Exp, scale=-0.5, reads=[b_scr], writes=[b_scr])
        rs = scr[:, 32:32 + nh]
        P.v("dve", "tensor_tensor", t3, s3, rs.unsqueeze(2).to_broadcast([128, nh, 64]), ALU.mult,
            reads=[b_src, b_scr], writes=[b_tmp])
        g3 = gain[:, 0:64].unsqueeze(1).to_broadcast([128, nh, 64])
        if rope is None:
            d3 = dst.rearrange("p (h d) -> p h d", h=nh)
            P.v("dve", "tensor_tensor", d3, t3, g3, ALU.mult, reads=[b_tmp, b_const], writes=[b_dst])
            return
        C, S, b_rope, tmp2, b_tmp2 = rope
        P.v("dve", "tensor_tensor", t3, t3, g3, ALU.mult, reads=[b_tmp, b_const], writes=[b_tmp])
        u3 = tmp2[:, 0:nh * 64].rearrange("p (h d) -> p h d", h=nh)
        P.v("pool", "tensor_tensor", u3, t3, C[:, 0:64].unsqueeze(1).to_broadcast([128, nh, 64]), ALU.mult,
            reads=[b_tmp, b_rope], writes=[b_tmp2])
        t5 = tmp[:, 0:nh * 64].rearrange("p (h a f e) -> p h a f e", h=nh, a=2, f=2)
        S5 = S[:, 0:64].rearrange("p (a f e) -> p a f e", a=2, f=2)
        d5 = dst.rearrange("p (h a f e) -> p h a f e", h=nh, a=2, f=2)
        u5 = tmp2[:, 0:nh * 64].rearrange("p (h a f e) -> p h a f e", h=nh, a=2, f=2)
        for f in range(2):
            sw = t5[:, :, :, 1 - f, :]
            sv = S5[:, :, f, :].unsqueeze(1).to_broadcast([128, nh, 2, 16])
            w_ = scr
            P.v("dve", "tensor_tensor", sw_tmp(k, nh)[:, :, :, f, :], sw, sv, ALU.mult,
                reads=[b_tmp, b_rope], writes=[k.b_swt])
        st5 = sw_tmp(k, nh)
        P.v("dve", "tensor_tensor", d5, st5, u5, ALU.add, reads=[k.b_swt, b_tmp2], writes=[b_dst])

    k.b_swt = Buf()

    def sw_tmp(k_, nh):
        return k_.swt[:, 0:nh * 64].rearrange("p (h a f e) -> p h a f e", h=nh, a=2, f=2)

    k.head_norm_rope = head_norm_rope

    from_l0 = dict(locals())
    k.env = from_l0
    k.areset(0)
    if "stopS0" in dbg:
        return
    layer0(k, stages, dbg)


def layer0(k, stages, dbg):
    g = k.env
    nc, P = k.nc, k.P
    sb, ps, dout = k.ar, k.ps, k.dout
    identF, identB, b_const = g["identF"], g["identB"], g["b_const"]
    modulate = k.modulate
    psum = k.psum
    x_d, ctx_d = g["x_d"], g["ctx_d"]
    (OA_d, QT_d, MIXT_d, PB_lt, PB_ke, PB_kd, PB_v, PB_ee, PB_rg, X1_d) = (
        g["OA_d"], g["QT_d"], g["MIXT_d"], g["PB_lt"], g["PB_ke"], g["PB_kd"], g["PB_v"], g["PB_ee"], g["PB_rg"], g["X1_d"])
    aqn, akn, glan = g["aqn"], g["akn"], g["glan"]
    bnd, b_bnd = k.bnd, k.b_bnd
    gateB_all, b_gateB = k.gateB_all, k.b_gateB
    b_scr = {n: Buf(n) for n in ("OA", "QT", "MIXT", "PBlt", "PBke", "PBkd", "PBv", "PBee", "PBrg", "X1")}
    k.b_scr = b_scr

    KT = sb("KT", [128, 2, 34 * 128], BF16); b_KT = Buf()
    P.v("pool", "memset", KT[64:128], 0.0, writes=[b_KT])
    Vt = sb("Vt", [128, 34, 2, 66], BF16); b_Vt = Buf()
    P.v("pool", "memset", Vt[:], 1.0, writes=[b_Vt])
    mark = k.aoff
    k.swt = sb("swt", [128, 1024])
    w_in = sb("w_in", [128, 8, EVEN_IN], BF16); b_win = Buf()
    tri = sb("tri", [128, 4, 128]); maskAB = sb("maskAB", [128, 2, 256], BF16)
    P.dma(tri[:], g["tri_d"], writes=[b_const], eng="pool")
    P.dma(maskAB[:], g["maskAB_d"], writes=[b_const], eng="pool")
    gwext = sb("gwext", [33, 512]); P.dma(gwext[:], g["gwext_d"], writes=[b_const], eng="pool")
    mark_w = k.aoff
    k.wstage = [sb("wstage%d" % i, [128, 1168]) for i in range(2)]
    evin_d = g["evin_d"]
    for hf in range(2):
        lo_ = hf * 1168
        k.load_weight_bf16(w_in[:, :, lo_:lo_ + 1168], b_win,
                           lambda c, lo_=lo_: evin_d[c * 128:(c + 1) * 128, lo_:lo_ + 1168], 8, 1168, k.wstage, k.b_wstage)
    P.barrier()
    k.aoff = mark_w

    z_sb = sb("z_sb", [128, EVEN_IN]); b_z = Buf()
    ropeC = sb("ropeC", [128, 64]); ropeS = sb("ropeS", [128, 64]); b_rope = Buf()
    tmpA = sb("tmpA", [128, 1024]); b_tmpA = Buf()
    tmpB = sb("tmpB", [128, 1024]); b_tmpB = Buf()
    scr = sb("scr", [128, 64]); b_scr_ = Buf()
    qb = sb("qb", [128, 512], BF16); b_qb = Buf()
    kb = sb("kb", [128, 128], BF16); b_kb = Buf()
    qT_sb = sb("qT_sb", [64, 8, 128], BF16); b_qTsb = Buf()
    lrT = sb("lrT", [33, 128]); b_lrT = Buf()
    P.v("pool", "memset", lrT[:], 1.0, writes=[b_lrT])
    e_sb = sb("e_sb", [128, 512]); b_e = Buf()
    sp_ = sb("sp_", [128, 512]); b_sp = Buf()
    gqT = sb("gqT", [64, 4, 128]); gkT = sb("gkT", [64, 4, 128]); b_gq = Buf(); b_gk = Buf()
    EbT = [sb("EbT%d" % i, [64, 4, 128]) for i in range(2)]; b_EbT = [Buf(), Buf()]
    EnbT = [sb("EnbT%d" % i, [64, 4, 128]) for i in range(2)]; b_EnbT = [Buf(), Buf()]
    kdec = sb("kdec", [128, 256]); b_kdec = Buf()
    LT = [sb("LT%d" % i, [128, 2, 4, 64], BF16) for i in range(2)]
    b_LTq = [Buf(), Buf()]; b_LTa = [Buf(), Buf()]
    keT = [sb("keT%d" % i, [64, 4, 128], BF16) for i in range(2)]; b_keT = [Buf(), Buf()]
    kd = [sb("kd%d" % i, [128, 256], BF16) for i in range(2)]; b_kd = [Buf(), Buf()]
    v_bf = sb("v_bf", [128, 512], BF16); b_vbf = Buf()
    vhi = sb("vhi", [128, 2, 512], BF16); b_vhi = Buf()
    eend = [sb("eend%d" % i, [64, 4, 2]) for i in range(2)]; b_eend = [Buf(), Buf()]
    RH = [[sb("RH%d%d" % (i, j), [128, 4, 128], BF16) for j in range(2)] for i in range(2)]
    b_RHs = [[Buf(), Buf()], [Buf(), Buf()]]; b_RHv = [[Buf(), Buf()], [Buf(), Buf()]]
    S32 = [sb("S32_%d" % i, [64, 4, 128]) for i in range(2)]; b_S32 = [Buf(), Buf()]
    par = [0, 0]
    for d_ in range(2):
        P.v("pool", "memset", S32[d_][:], 0.0, writes=[b_S32[d_]])
        for j in range(2):
            P.v("pool", "memset", RH[d_][j][:], 0.0, writes=[b_RHs[d_][j], b_RHv[d_][j]])
    oa_sb = sb("oa_sb", [128, 512]); b_oa = Buf()
    rg_sb = sb("rg_sb", [128, 512]); b_rg = Buf()
    mixT_sb = sb("mixT_sb", [128, 8, 128], BF16); b_mixT = Buf()

    def mkset2():
        z2 = sb("z_sb2", [128, EVEN_IN])
        qb2 = sb("qb2", [128, 512], BF16); kb2 = sb("kb2", [128, 128], BF16); qT2 = sb("qT_sb2", [64, 8, 128], BF16)
        lrT2 = sb("lrT2", [33, 128]); bl2 = Buf()
        P.v("pool", "memset", lrT2[:], 1.0, writes=[bl2])
        gq2 = sb("gqT2", [64, 4, 128]); gk2 = sb("gkT2", [64, 4, 128])
        kdec2 = sb("kdec2", [128, 256])
        LT2 = [sb("LT2%d" % i, [128, 2, 4, 64], BF16) for i in range(2)]
        keT2 = [sb("keT2%d" % i, [64, 4, 128], BF16) for i in range(2)]
        kd2 = [sb("kd2%d" % i, [128, 256], BF16) for i in range(2)]
        v2 = sb("v_bf2", [128, 512], BF16); vh2 = sb("vhi2", [128, 2, 512], BF16)
        ee2 = [sb("eend2%d" % i, [64, 4, 2]) for i in range(2)]
        oa2 = sb("oa_sb2", [128, 512]); rg2 = sb("rg_sb2", [128, 512])
        return (z2, Buf(), qb2, Buf(), kb2, Buf(), qT2, Buf(), lrT2, bl2, gq2, gk2, Buf(), Buf(), kdec2, Buf(),
                LT2, [Buf(), Buf()], [Buf(), Buf()], keT2, [Buf(), Buf()], kd2, [Buf(), Buf()], v2, Buf(), vh2, Buf(),
                ee2, [Buf(), Buf()], oa2, Buf(), rg2, Buf())

    SETS = [(z_sb, b_z, qb, b_qb, kb, b_kb, qT_sb, b_qTsb, lrT, b_lrT, gqT, gkT, b_gq, b_gk, kdec, b_kdec,
             LT, b_LTq, b_LTa, keT, b_keT, kd, b_kd, v_bf, b_vbf, vhi, b_vhi, eend, b_eend, oa_sb, b_oa, rg_sb, b_rg),
            mkset2()]

    pMisc, bMisc = psum[3]
    pZG, bZG = psum[4]
    pBT, bBT = psum[5]
    pAT, bAT = psum[7][0], Buf("pAT")
    pS, bS = psum[7]
    pPO, bPO = psum[6]

    def scan_tile(d_, lt, b_ltq, b_lta, ke, b_ke, kdt, b_kdt, vb, b_vb, vh, b_vh, ee, b_ee, with_out):
        order = (0, 1) if d_ == 0 else (1, 0)
        pAT4 = pAT[:, 0:256].rearrange("p (h t) -> p h t", h=4)
        pS4 = pS[0:64, :].rearrange("p (h v) -> p h v", h=4)
        po, bpo = pPO, bPO
        for ch in order:
            p_ = par[d_]
            rh, brs, brv = RH[d_][p_], b_RHs[d_][p_], b_RHv[d_][p_]
            if with_out:
                for h in range(4):
                    P.mm(pAT4[64:128, h, :], ke[0:64, h, ch * 64:(ch + 1) * 64], lt[0:64, ch, h, :],
                         reads=[b_ke, b_ltq], writes=[bAT])
                P.v("dve", "tensor_tensor", lt[64:128, ch, :, :], pAT4[64:128, :, :],
                    maskAB[64:128, d_, :].rearrange("p (h t) -> p h t", h=4), ALU.mult,
                    reads=[bAT, b_const], writes=[b_lta])
                P.v("pool", "tensor_copy", rh[64:128, :, :], vh[64:128, ch, :].rearrange("p (h v) -> p h v", h=4),
                    reads=[b_vh], writes=[brv])
                for h in range(4):
                    P.mm(po[ch * 64:(ch + 1) * 64, h * 128:(h + 1) * 128], lt[:, ch, h, :], rh[:, h, :],
                         reads=[b_ltq, b_lta, brs, brv], writes=[bpo])
            for h in range(4):
                P.mm(pS4[:, h, :], kdt[ch * 64:(ch + 1) * 64, h * 64:(h + 1) * 64],
                     vb[ch * 64:(ch + 1) * 64, h * 128:(h + 1) * 128], reads=[b_kdt, b_vb], writes=[bS])
            for h in range(4):
                P.v("dve", "scalar_tensor_tensor", S32[d_][:, h, :], S32[d_][:, h, :], ee[:, h, ch:ch + 1],
                    pS4[:, h, :], ALU.mult, ALU.add, reads=[b_S32[d_], b_ee, bS], writes=[b_S32[d_]])
            nrh = RH[d_][1 - p_]
            P.act(nrh[0:64, :, :], S32[d_][:], AF.Copy, reads=[b_S32[d_]], writes=[b_RHs[d_][1 - p_]])
            par[d_] = 1 - p_

    def gla_prep(dirs, need_q):
        P.tr(pBT[0:32, 0:128], z_sb[:, 1536:1568], identF[:], reads=[b_z, b_const], writes=[bBT])
        P.act(lrT[0:32, :], pBT[0:32, 0:128], AF.Copy, reads=[bBT], writes=[b_lrT])
        P.mm(pZG[:], lrT[:], gwext[:], reads=[b_lrT, b_const], writes=[bZG])
        P.act(e_sb[:], pZG[:], AF.Exp, scale=-1.0, reads=[bZG], writes=[b_e])
        P.act(sp_[:], e_sb[:], AF.Ln, bias=1.0, reads=[b_e], writes=[b_sp])
        pBT4 = pBT[0:64, :].rearrange("p (h t) -> p h t", h=4)
        if need_q:
            for h in range(4):
                P.tr(pBT4[:, h, :], z_sb[:, h * 64:(h + 1) * 64], identF[:], reads=[b_z, b_const], writes=[bBT])
            P.act(gqT[:], pBT4, AF.Copy, reads=[bBT], writes=[b_gq])
        for h in range(4):
            P.tr(pBT4[:, h, :], z_sb[:, 256 + h * 64:256 + (h + 1) * 64], identF[:], reads=[b_z, b_const], writes=[bBT])
        P.act(gkT[:], pBT4, AF.Copy, reads=[bBT], writes=[b_gk])
        P.v("pool", "tensor_copy", v_bf[:], z_sb[:, 512:1024], reads=[b_z], writes=[b_vbf])
        P.dma(vhi[64:128, 0, :], v_bf[0:64, :], reads=[b_vbf], writes=[b_vhi], eng="pool")
        P.v("pool", "tensor_copy", vhi[64:128, 1, :], v_bf[64:128, :], reads=[b_vbf], writes=[b_vhi])
        for d_ in dirs:
            for h in range(4):
                P.mm(pBT4[:, h, :], sp_[:, d_ * 256 + h * 64:d_ * 256 + (h + 1) * 64], tri[:, d_, :],
                     reads=[b_sp, b_const], writes=[bBT])
            P.act(EbT[d_][:], pBT4, AF.Exp, scale=-1.0 / 16, reads=[bBT], writes=[b_EbT[d_]])
            P.act(EnbT[d_][:], pBT4, AF.Exp, scale=1.0 / 16, reads=[bBT], writes=[b_EnbT[d_]])
            P.mm(pZG[:, 0:256], tri[:, 2 + d_, :], sp_[:, d_ * 256:(d_ + 1) * 256], reads=[b_sp, b_const], writes=[bZG])
            P.act(kdec[:], pZG[:, 0:256], AF.Exp, scale=-1.0 / 16, reads=[bZG], writes=[b_kdec])
            if need_q:
                for ch in range(2):
                    P.v("dve", "scalar_tensor_tensor", LT[d_][0:64, ch, :, :],
                        gqT[:, :, ch * 64:(ch + 1) * 64], 0.125,
                        EbT[d_][:, :, ch * 64:(ch + 1) * 64], ALU.mult, ALU.mult,
                        reads=[b_gq, b_EbT[d_]], writes=[b_LTq[d_]])
            P.v("dve", "tensor_tensor", keT[d_][:], gkT[:], EnbT[d_][:], ALU.mult,
                reads=[b_gk, b_EnbT[d_]], writes=[b_keT[d_]])
            P.v("dve", "tensor_tensor", kd[d_][:], z_sb[:, 256:512], kdec[:], ALU.mult,
                reads=[b_z, b_kdec], writes=[b_kd[d_]])
            cols = (63, 127) if d_ == 0 else (0, 64)
            for ch in range(2):
                P.v("dve", "tensor_copy", eend[d_][:, :, ch:ch + 1], EbT[d_][:, :, cols[ch]:cols[ch] + 1],
                    reads=[b_EbT[d_]], writes=[b_eend[d_]])

    def inproj(H, bH, blocks):
        for bi, (lo, hi) in enumerate(blocks):
            pz, bz = psum[1 + bi % 2]
            for c in range(8):
                P.mm(pz[:, 0:hi - lo], H[:, c, :], w_in[:, c, lo:hi], c == 0, c == 7, reads=[bH, b_win], writes=[bz])
            if bi % 2 == 0:
                P.act(z_sb[:, lo:hi], pz[:, 0:hi - lo], AF.Copy, reads=[bz], writes=[b_z])
            else:
                P.v("dve", "tensor_copy", z_sb[:, lo:hi], pz[:, 0:hi - lo], reads=[bz], writes=[b_z])

    FULL = ((0, 512), (512, 1024), (1024, 1536), (1536, 2048), (2048, 2336))
    RBLK = ((256, 768), (768, 1024), (1536, 1568), (2080, 2336))

    def modA(kind, ti):
        if kind == "ctx":
            return modulate(ctx_d[ti * 128:(ti + 1) * 128, :], 0, 0, 1)
        return modulate(x_d[ti * 128:(ti + 1) * 128, :], 0, 0, 0)

    def tileA(kind, ti, pre):
        if kind == "ctx":
            src, w, slot, kidx = ctx_d[ti * 128:(ti + 1) * 128, :], 1, NOWN + ti, ti
        else:
            src, w, slot, kidx = x_d[ti * 128:(ti + 1) * 128, :], 0, ti, 2 + ti
        H, bH, X, bX = pre
        inproj(H, bH, RBLK if kind == "R" else FULL)
        rope = None
        if kind != "ctx":
            P.dma(ropeC[:], g["ropeC_d"][ti * 128:(ti + 1) * 128, :], writes=[b_rope], eng="pool")
            P.dma(ropeS[:], g["ropeS_d"][ti * 128:(ti + 1) * 128, :], writes=[b_rope], eng="pool")
            rope = (ropeC, ropeS, b_rope, tmpB, b_tmpB)
        pM8 = pMisc[0:64, :].bitcast(BF16).rearrange("p (h t) -> p h t", h=8)
        if kind != "R":
            k.head_norm_rope(qb[:], b_qb, z_sb[:, 1568:2080], b_z, 8, aqn, rope, tmpA, b_tmpA, scr, b_scr_)
            for h in range(8):
                P.tr(pM8[:, h, :], qb[:, h * 64:(h + 1) * 64], identB[:], reads=[b_qb, b_const], writes=[bMisc])
            P.act(qT_sb[:], pM8, AF.Copy, reads=[bMisc], writes=[b_qTsb])
            P.dma(QT_d[slot], qT_sb[:], reads=[b_qTsb], writes=[b_scr["QT"]])
        k.head_norm_rope(kb[:], b_kb, z_sb[:, 2080:2208], b_z, 2, akn, rope, tmpA, b_tmpA, scr, b_scr_)
        for h in range(2):
            P.tr(pM8[:, h, :], kb[:, h * 64:(h + 1) * 64], identB[:], reads=[b_kb, b_const], writes=[bMisc])
        P.act(KT[0:64, :, kidx * 128:(kidx + 1) * 128], pM8[:, 0:2, :], AF.Copy, reads=[bMisc], writes=[b_KT])
        P.v("pool", "tensor_copy", Vt[:, kidx, :, 0:64], z_sb[:, 2208:2336].rearrange("p (h d) -> p h d", h=2),
            reads=[b_z], writes=[b_Vt])
        if kind == "R":
            gla_prep((1,), False)
            scan_tile(1, LT[1], b_LTq[1], b_LTa[1], keT[1], b_keT[1], kd[1], b_kd[1], v_bf, b_vbf, vhi, b_vhi,
                      eend[1], b_eend[1], False)
            return
        gla_prep((0, 1), True)
        P.act(rg_sb[:], z_sb[:, 1024:1536], AF.Silu, reads=[b_z], writes=[b_rg])
        P.v("pool", "tensor_tensor", rg_sb[:].rearrange("p (h v) -> p h v", h=4), rg_sb[:].rearrange("p (h v) -> p h v", h=4),
            glan[:, 0:128].unsqueeze(1).to_broadcast([128, 4, 128]), ALU.mult, reads=[b_rg, b_const], writes=[b_rg])
        P.dma(PB_rg[slot], rg_sb[:], reads=[b_rg], writes=[b_scr["PBrg"]])
        P.dma(PB_lt[slot], LT[1][0:64].rearrange("p c h t -> p (c h t)"), reads=[b_LTq[1]], writes=[b_scr["PBlt"]])
        P.dma(PB_ke[slot], keT[1][:].rearrange("p h t -> p (h t)"), reads=[b_keT[1]], writes=[b_scr["PBke"]])
        P.dma(PB_kd[slot], kd[1][:], reads=[b_kd[1]], writes=[b_scr["PBkd"]])
        P.dma(PB_v[slot], v_bf[:], reads=[b_vbf], writes=[b_scr["PBv"]])
        P.dma(PB_ee[slot], eend[1][:].rearrange("p h c -> p (h c)"), reads=[b_eend[1]], writes=[b_scr["PBee"]])
        scan_tile(0, LT[0], b_LTq[0], b_LTa[0], keT[0], b_keT[0], kd[0], b_kd[0], v_bf, b_vbf, vhi, b_vhi,
                  eend[0], b_eend[0], True)
        P.act(oa_sb[:], pPO[:], AF.Copy, reads=[bPO], writes=[b_oa])
        P.dma(OA_d[slot], oa_sb[:], reads=[b_oa], writes=[b_scr["OA"]])

    ob_sb = tmpB[:, 0:512]; b_ob = b_tmpB
    ab_sb = tmpB[:, 512:768].bitcast(BF16); b_ab = Buf()

    def tileB(slot):
        P.dma(LT[1][0:64].rearrange("p c h t -> p (c h t)"), PB_lt[slot], reads=[b_scr["PBlt"]], writes=[b_LTq[1]])
        P.dma(keT[1][:].rearrange("p h t -> p (h t)"), PB_ke[slot], reads=[b_scr["PBke"]], writes=[b_keT[1]])
        P.dma(kd[1][:], PB_kd[slot], reads=[b_scr["PBkd"]], writes=[b_kd[1]])
        P.dma(v_bf[:], PB_v[slot], reads=[b_scr["PBv"]], writes=[b_vbf])
        P.dma(vhi[64:128, 0, :], PB_v[slot][0:64, :], reads=[b_scr["PBv"]], writes=[b_vhi], eng="pool")
        P.dma(vhi[64:128, 1, :], PB_v[slot][64:128, :], reads=[b_scr["PBv"]], writes=[b_vhi], eng="pool")
        P.dma(eend[1][:].rearrange("p h c -> p (h c)"), PB_ee[slot], reads=[b_scr["PBee"]], writes=[b_eend[1]])
        P.dma(oa_sb[:], OA_d[slot], reads=[b_scr["OA"]], writes=[b_oa], eng="pool")
        P.dma(rg_sb[:], PB_rg[slot], reads=[b_scr["PBrg"]], writes=[b_rg], eng="pool")
        scan_tile(1, LT[1], b_LTq[1], b_LTa[1], keT[1], b_keT[1], kd[1], b_kd[1], v_bf, b_vbf, vhi, b_vhi,
                  eend[1], b_eend[1], True)
        P.v("dve", "tensor_tensor", ob_sb[:], pPO[:], oa_sb[:], ALU.add, reads=[bPO, b_oa], writes=[b_ob])
        o3 = ob_sb[:].rearrange("p (h v) -> p h v", h=4)
        t3 = tmpA[:, 0:512].rearrange("p (h v) -> p h v", h=4)
        P.v("dve", "tensor_tensor", t3, o3, o3, ALU.mult, reads=[b_ob], writes=[b_tmpA])
        P.v("dve", "tensor_reduce", scr[:, 0:4], t3, AX.X, ALU.add, reads=[b_tmpA], writes=[b_scr_])
        P.act(scr[:, 16:20], scr[:, 0:4], AF.Ln, scale=1.0 / 128, bias=1e-6, reads=[b_scr_], writes=[b_scr_])
        P.act(scr[:, 32:36], scr[:, 16:20], AF.Exp, scale=-0.5, reads=[b_scr_], writes=[b_scr_])
        P.v("dve", "tensor_tensor", t3, o3, scr[:, 32:36].unsqueeze(2).to_broadcast([128, 4, 128]), ALU.mult,
            reads=[b_ob, b_scr_], writes=[b_tmpA])
        P.v("dve", "tensor_tensor", ab_sb[:], tmpA[:, 0:512], rg_sb[:], ALU.mult, reads=[b_tmpA, b_rg], writes=[b_ab])
        pM4 = pMisc[:].bitcast(BF16)[:, 0:512].rearrange("p (c t) -> p c t", c=4)
        for c in range(4):
            P.tr(pM4[:, c, :], ab_sb[:, c * 128:(c + 1) * 128], identB[:], reads=[b_ab, b_const], writes=[bMisc])
        P.act(mixT_sb[:, 0:4, :], pM4, AF.Copy, reads=[bMisc], writes=[b_mixT])
        P.dma(MIXT_d[slot][:, 0:4, :], mixT_sb[:, 0:4, :], reads=[b_mixT], writes=[b_scr["MIXT"]])

    seqA = [("ctx", 0), ("ctx", 1)] + [("R", ti) for ti in range(31, NOWN - 1, -1)] + [("own", ti) for ti in range(NOWN)]
    pre = modA(*seqA[0])
    nset = 0
    for si, (kind, ti) in enumerate(seqA):
        nxt = modA(*seqA[si + 1]) if si + 1 < len(seqA) else None
        (z_sb, b_z, qb, b_qb, kb, b_kb, qT_sb, b_qTsb, lrT, b_lrT, gqT, gkT, b_gq, b_gk, kdec, b_kdec,
         LT, b_LTq, b_LTa, keT, b_keT, kd, b_kd, v_bf, b_vbf, vhi, b_vhi, eend, b_eend, oa_sb, b_oa, rg_sb, b_rg) = SETS[nset % 2]
        nset += 1
        tileA(kind, ti, pre)
        pre = nxt
        if (kind, ti) == ("ctx", 1):
            for sl in (NOWN + 1, NOWN + 0):
                (z_sb, b_z, qb, b_qb, kb, b_kb, qT_sb, b_qTsb, lrT, b_lrT, gqT, gkT, b_gq, b_gk, kdec, b_kdec,
                 LT, b_LTq, b_LTa, keT, b_keT, kd, b_kd, v_bf, b_vbf, vhi, b_vhi, eend, b_eend, oa_sb, b_oa, rg_sb, b_rg) = SETS[nset % 2]
                nset += 1
                tileB(sl)
    for ti in range(NOWN - 1, -1, -1):
        (z_sb, b_z, qb, b_qb, kb, b_kb, qT_sb, b_qTsb, lrT, b_lrT, gqT, gkT, b_gq, b_gk, kdec, b_kdec,
         LT, b_LTq, b_LTa, keT, b_keT, kd, b_kd, v_bf, b_vbf, vhi, b_vhi, eend, b_eend, oa_sb, b_oa, rg_sb, b_rg) = SETS[nset % 2]
        nset += 1
        tileB(ti)

    if "stopAB" in dbg:
        return
    k.areset(mark)
    k.wstage = [sb("wstage%d" % i, [128, D]) for i in range(2)]
    w_out = sb("w_out", [128, 4, D], BF16); b_wout = Buf()
    w_outB = sb("w_outB", [64, 8, D], BF16); b_woutB = Buf()
    evout_d = g["evout_d"]
    k.load_weight_bf16(w_out, b_wout, lambda c: evout_d[c * 128:(c + 1) * 128, :], 4, D, k.wstage, k.b_wstage)
    for h in range(8):
        s_, bs_ = k.wstage[h % 2], k.b_wstage[h % 2]
        P.dma(s_[0:64, :], evout_d[512 + h * 64:512 + (h + 1) * 64, :], writes=[bs_], eng=("sp" if h % 2 == 0 else "pool"))
        k.cast(("pool", "dve", "act")[h % 3], w_outB[:, h, :], s_[0:64, :], [bs_], [b_woutB])
    ones_f = sb("ones_f", [128, 64]); P.v("pool", "memset", ones_f[:], 1.0, writes=[b_const])
    qT_in = [sb("qT_in%d" % i, [128, 8, 128], BF16) for i in range(2)]; b_qTin = [Buf(), Buf()]
    for i_ in range(2):
        P.v("pool", "memset", qT_in[i_][64:128], 0.0, writes=[b_qTin[i_]])
    rsr = sb("rsr", [128, 512]); b_rsr = Buf()
    bc_sb = sb("bc_sb", [64, 512]); b_bc = Buf()
    OT = sb("OT", [64, 8, 128], BF16); b_OT = Buf()
    mT_in = sb("mT_in", [128, 4, 128], BF16); b_mTin = Buf()
    x1_sb = sb("x1_sb", [128, D]); b_x1 = Buf()
    pO = [psum[6], psum[7]]
    SG = [(k.psall[:, 2 * g_ * 512:(2 * g_ + 2) * 512], [psum[2 * g_][1], psum[2 * g_ + 1][1]]) for g_ in range(3)]
    pBC, bBC = psum[0]
    NPT = 4
    PT = [sb("PTp%d" % i, [128, 1024], BF16) for i in range(NPT)]; b_PT = [Buf() for _ in range(NPT)]
    cstate = dict(it=0)

    mT_ins = [mT_in, sb("mT_in2", [128, 4, 128], BF16)]; b_mTins = [b_mTin, Buf()]
    x_ins = [sb("xC%d" % i, [128, D]) for i in range(2)]; b_xins = [Buf(), Buf()]
    ones_b = sb("ones_b", [128, 64], BF16); P.v("pool", "memset", ones_b[:], 1.0, writes=[b_const])
    rsb = sb("rsb", [128, 2, 512], BF16); b_rsb = Buf()
    bcs = [sb("bcs%d" % i, [64, 512]) for i in range(2)]; b_bcs = [Buf(), Buf()]
    cpre = dict(n=0)

    def loadC(kind, ti):
        if kind == "ctx":
            slot, src = NOWN + ti, ctx_d[ti * 128:(ti + 1) * 128, :]
        else:
            slot, src = ti, x_d[ti * 128:(ti + 1) * 128, :]
        i = cpre["n"] % 2
        cpre["n"] += 1
        P.dma(qT_in[i][0:64], QT_d[slot], reads=[b_scr["QT"]], writes=[b_qTin[i]])
        P.dma(mT_ins[i][:], MIXT_d[slot][:, 0:4, :], reads=[b_scr["MIXT"]], writes=[b_mTins[i]], eng="pool")
        P.dma(x_ins[i][:], src, writes=[b_xins[i]])
        return (qT_in[i], b_qTin[i], mT_ins[i], b_mTins[i], x_ins[i], b_xins[i])

    def tileC(kind, ti, pre):
        if kind == "ctx":
            slot, w, src, keys = NOWN + ti, 1, ctx_d[ti * 128:(ti + 1) * 128, :], [0, 1]
        else:
            slot, w, src, keys = ti, 0, x_d[ti * 128:(ti + 1) * 128, :], list(range(34))
        Q, bQ, mT_in, b_mTin, X, bX = pre
        units = [(kvh, keys[a_:a_ + 2]) for kvh in range(2) for a_ in range(0, len(keys), 2)]
        pend = []

        def pv(u, pt, bpt):
            kvh, kts = u
            po, bpo = pO[kvh]
            for j, kt in enumerate(kts):
                P.mm(po[0:65, :], Vt[:, kt, kvh, 0:65], pt[:, j * 512:(j + 1) * 512], kt == keys[0], kt == keys[-1],
                     reads=[bpt, b_Vt], writes=[bpo])

        for u in units:
            kvh, kts = u
            it = cstate["it"]
            cstate["it"] += 1
            psc, bscs = SG[it % 3]
            pt, bpt = PT[it % NPT], b_PT[it % NPT]
            n = len(kts)
            for j, kt in enumerate(kts):
                P.mm(psc[:, j * 512:(j + 1) * 512], KT[:, kvh, kt * 128:(kt + 1) * 128],
                     Q[:, kvh * 4:(kvh + 1) * 4, :].rearrange("p h t -> p (h t)"),
                     reads=[b_KT, bQ], writes=[bscs[j]])
            P.act(pt[:, 0:n * 512], psc[:, 0:n * 512], AF.Exp, scale=0.125, bias=bnd[:, 2:3],
                  reads=bscs[0:n] + [b_bnd], writes=[bpt])
            pend.append((u, pt, bpt))
            if len(pend) > 2:
                pv(*pend.pop(0))
        while pend:
            pv(*pend.pop(0))
        for kvh in range(2):
            po, bpo = pO[kvh]
            P.act(rsr[64:65, :], po[64:65, :], AF.Ln, reads=[bpo], writes=[b_rsr])
            P.act(rsb[64:65, kvh, :], rsr[64:65, :], AF.Exp, scale=-1.0, reads=[b_rsr], writes=[b_rsb])
        for kvh in range(2):
            pb_, bb_ = psum[2 + kvh]
            P.mm(pb_[0:64, :], ones_b[64:65, 0:64], rsb[64:65, kvh, :], reads=[b_const, b_rsb], writes=[bb_])
        for kvh in range(2):
            pb_, bb_ = psum[2 + kvh]
            P.v("dve", "tensor_copy", bcs[kvh][:], pb_[0:64, :], reads=[bb_], writes=[b_bcs[kvh]])
        for kvh in range(2):
            po, bpo = pO[kvh]
            P.v("dve", "tensor_tensor", OT[:, kvh * 4:(kvh + 1) * 4, :].rearrange("p h t -> p (h t)"), po[0:64, :], bcs[kvh][:],
                ALU.mult, reads=[bpo, b_bcs[kvh]], writes=[b_OT])
        for hh in range(2):
            pz, bz = psum[hh]
            for c in range(4):
                P.mm(pz[:], mT_in[:, c, :], w_out[:, c, hh * 512:(hh + 1) * 512], c == 0, False,
                     reads=[b_mTin, b_wout], writes=[bz])
            for h in range(8):
                P.mm(pz[:], OT[:, h, :], w_outB[:, h, hh * 512:(hh + 1) * 512], False, h == 7,
                     reads=[b_OT, b_woutB], writes=[bz])
            P.v("dve", "tensor_tensor", x1_sb[:, hh * 512:(hh + 1) * 512], pz[:],
                gateB_all[:, k.gslot[(0, 0, w)], hh * 512:(hh + 1) * 512],
                ALU.mult, reads=[bz, b_gateB[0]], writes=[b_x1])
        P.v("pool", "tensor_tensor", x1_sb[:], x1_sb[:], X[:], ALU.add, reads=[b_x1, bX], writes=[b_x1])
        P.dma(X1_d[slot * 128:(slot + 1) * 128, :], x1_sb[:], reads=[b_x1], writes=[b_scr["X1"]])

    tiles_c = [("ctx", 0), ("ctx", 1)] + [("own", t) for t in range(NOWN)]
    if "fewC" in dbg:
        tiles_c = [("ctx", 0), ("own", 0), ("own", 16)]
    s1 = k.s0_steps(1, "L1", psum[0], psum[0], psum[1])
    preC = loadC(*tiles_c[0])
    for si, (kind, ti) in enumerate(tiles_c):
        nxtC = loadC(*tiles_c[si + 1]) if si + 1 < len(tiles_c) else None
        if s1:
            s1.pop(0)()
        tileC(kind, ti, preC)
        preC = nxtC
    while s1:
        s1.pop(0)()
    if "stopL0mix" in dbg:
        return
    X2_d, X3_d, out_d = g["X2_d"], g["X3_d"], g["out_d"]
    b_scr["X2"] = Buf("X2"); b_scr["X3"] = Buf("X3")
    tiles = []
    for slot in range(NTOK0):
        w = 1 if slot >= NOWN else 0
        tiles.append((X1_d[slot * 128:(slot + 1) * 128, :], b_scr["X1"], X2_d[slot * 128:(slot + 1) * 128, :], b_scr["X2"], w))
    if "fewM" in dbg:
        tiles = [tiles[0], tiles[16], tiles[17]]
    moe(k, 0, tiles, "a")
    if "stopL0" in dbg or "stopD1" in dbg or "stopD2" in dbg:
        return
    layer1(k, dbg)
    if "stopL1" in dbg:
        return
    tiles = [(X3_d[t * 128:(t + 1) * 128, :], b_scr["X3"], out_d[t * 128:(t + 1) * 128, :], None, 0) for t in range(16)]
    moe(k, 1, tiles, "b")


def moe(k, l, tiles, tag):
    g = k.env
    nc, P = k.nc, k.P
    ar, psum = k.ar, k.psum
    identF, b_const = g["identF"], g["b_const"]
    NT = len(tiles)
    NTOK = NT * 128
    k.areset(0)
    hT_all = ar("hT_all" + tag, [128, 8, NTOK], BF16); b_hTall = Buf()
    yacc = ar("yacc" + tag, [128, NT, D]); b_yacc = [Buf() for _ in range(NT)]
    comb_all = ar("comb" + tag, [128, NT, 16]); b_combt = [Buf() for _ in range(NT)]
    mark = k.aoff
    k.xnf = ar("xnf" + tag, [128, D]); k.hTf = ar("hTf" + tag, [128, 8, 128])
    rw = ar("rw" + tag, [128, 8, 20]); rb = ar("rb" + tag, [128, 20]); b_rw = Buf()
    P.dma(rw[:], g["rw_d"][l].rearrange("(c p) n -> p c n", p=128), writes=[b_rw], eng="pool")
    P.dma(rb[:], g["rb_d"][l].partition_broadcast(128).rearrange("p o n -> p (o n)"), writes=[b_rw], eng="pool")
    lg = ar("lg" + tag, [128, 20]); b_lg = Buf()
    rt = ar("rt" + tag, [128, 64]); b_rt = Buf()
    pR, bR = psum[2]
    b_hTt = [Buf() for _ in range(NT)]

    def d1(ti):
        (src, sbuf_, dst, dbuf_, w) = tiles[ti]
        H, bH, X, bX = k.modulate(src, l, 1, w, fp32=True, src_buf=sbuf_)
        P.v("pool", "tensor_copy", hT_all[:, :, ti * 128:(ti + 1) * 128], H[:], reads=[bH], writes=[b_hTt[ti]])
        for c_ in range(8):
            P.mm(pR[:, 0:20], H[:, c_, :], rw[:, c_, :], c_ == 0, c_ == 7, reads=[bH, b_rw], writes=[bR])
        P.v("dve", "tensor_tensor", lg[:], pR[:, 0:20], rb[:], ALU.add, reads=[bR, b_rw], writes=[b_lg])
        gl, el = lg[:, 0:4], lg[:, 4:20]
        R_ = lambda a, b: rt[:, a:b]
        gmax, ngmax, sume, gw = R_(0, 1), R_(1, 2), R_(2, 3), R_(3, 4)
        ohg, eg, esel, oh1, msk, oh2, ew, sg = R_(4, 8), R_(8, 12), R_(12, 16), R_(16, 20), R_(20, 24), R_(24, 28), R_(28, 32), R_(32, 36)
        m1, m2, dd, e2, w1, w2 = R_(36, 37), R_(37, 38), R_(38, 39), R_(39, 40), R_(40, 41), R_(41, 42)
        rd, wr = [b_lg, b_rt], [b_rt]
        V = lambda name, *a, **kw: P.v("dve", name, *a, reads=rd, writes=wr, **kw)
        V("tensor_reduce", gmax, gl, AX.X, ALU.max)
        V("tensor_scalar", ohg, gl, gmax, None, ALU.is_equal)
        V("tensor_scalar", ngmax, gmax, -1.0, None, ALU.mult)
        P.act(eg, gl, AF.Exp, bias=ngmax, accum_out=sume, reads=rd, writes=wr)
        V("reciprocal", gw, sume)
        V("tensor_scalar", esel, el[:, 0:4], ohg[:, 0:1], None, ALU.mult)
        for gi in range(1, 4):
            V("scalar_tensor_tensor", esel, el[:, gi * 4:(gi + 1) * 4], ohg[:, gi:gi + 1], esel, ALU.mult, ALU.add)
        V("tensor_reduce", m1, esel, AX.X, ALU.max)
        V("tensor_scalar", oh1, esel, m1, None, ALU.is_equal)
        V("scalar_tensor_tensor", msk, oh1, -NEG_BIG, esel, ALU.mult, ALU.add)
        V("tensor_reduce", m2, msk, AX.X, ALU.max)
        V("tensor_scalar", oh2, msk, m2, None, ALU.is_equal)
        V("tensor_tensor", dd, m2, m1, ALU.subtract)
        P.act(e2, dd, AF.Exp, reads=rd, writes=wr)
        V("tensor_scalar", w1, e2, 1.0, None, ALU.add)
        V("reciprocal", w1, w1)
        V("tensor_tensor", w2, e2, w1, ALU.mult)
        V("tensor_scalar", ew, oh1, w1, None, ALU.mult)
        V("scalar_tensor_tensor", ew, oh2, w2, ew, ALU.mult, ALU.add)
        V("tensor_scalar", sg, ohg, gw, None, ALU.mult)
        for gi in range(4):
            P.v("dve", "tensor_scalar", comb_all[:, ti, gi * 4:(gi + 1) * 4], ew, sg[:, gi:gi + 1], None, ALU.mult,
                reads=[b_rt], writes=[b_combt[ti]])
    wst = [ar("mwst%d" % i + tag, [128, 512]) for i in range(2)]; b_wst = [Buf(), Buf()]
    Wg = [ar("Wg%d" % i + tag, [128, 8, 256], BF16) for i in range(2)]
    Wu = [ar("Wu%d" % i + tag, [128, 8, 256], BF16) for i in range(2)]
    Wd = [ar("Wd%d" % i + tag, [128, 2, D], BF16) for i in range(2)]
    b_W = [[Buf(), Buf(), Buf()] for _ in range(2)]
    sa = [ar("sa%d" % i + tag, [128, 512]) for i in range(2)]; b_sa = [Buf(), Buf()]
    hid = [ar("hid%d" % i + tag, [128, 2, 512], BF16) for i in range(2)]; b_hid = [Buf(), Buf()]
    pCW, bCW = psum[0]
    pAs = [psum[1], psum[2]]
    pUs = [psum[3], psum[4]]
    pYs = [psum[5], psum[6], psum[0]]
    wcnt = 0
    blocks = [(s, min(512, NTOK - s)) for s in range(0, NTOK, 512)]
    it = 0
    yi = 0
    def load_w(e):
        nonlocal wcnt
        pe_ = e % 2
        srcs = (g["wg_d"][l, e].rearrange("(c p) f -> p c f", p=128), g["wu_d"][l, e].rearrange("(c p) f -> p c f", p=128),
                g["wd_d"][l, e].rearrange("(c p) n -> p c n", p=128))
        dsts = (Wg[pe_], Wu[pe_], Wd[pe_])
        for wi in range(3):
            for q4 in range(4):
                s_, bs_ = wst[wcnt % 2], b_wst[wcnt % 2]
                if wi < 2:
                    sv = s_[:].rearrange("p (c f) -> p c f", c=2)
                    sview = srcs[wi][:, 2 * q4:2 * q4 + 2, :]
                    dview = dsts[wi][:, 2 * q4:2 * q4 + 2, :]
                else:
                    sv = s_[:]
                    sview = srcs[wi][:, q4 // 2, (q4 % 2) * 512:(q4 % 2 + 1) * 512]
                    dview = dsts[wi][:, q4 // 2, (q4 % 2) * 512:(q4 % 2 + 1) * 512]
                P.dma(sv, sview, writes=[bs_], eng=("sp" if wcnt % 2 == 0 else "pool"))
                P.v("pool", "tensor_copy", dview, sv, reads=[bs_], writes=[b_W[pe_][wi]])
                wcnt += 1

    def gu(e, t0, n, i):
        pe_ = e % 2
        for fc in range(2):
            pa, ba = pAs[fc]
            pu, bu = pUs[fc]
            for c_ in range(8):
                P.mm(pa[:, 0:n], Wg[pe_][:, c_, fc * 128:(fc + 1) * 128], hT_all[:, c_, t0:t0 + n], c_ == 0, c_ == 7,
                     reads=[b_W[pe_][0]] + b_hTt[t0 // 128:(t0 + n) // 128], writes=[ba])
            for c_ in range(8):
                P.mm(pu[:, 0:n], Wu[pe_][:, c_, fc * 128:(fc + 1) * 128], hT_all[:, c_, t0:t0 + n], c_ == 0, c_ == 7,
                     reads=[b_W[pe_][1]] + b_hTt[t0 // 128:(t0 + n) // 128], writes=[bu])
            P.act(sa[fc][:, 0:n], pa[:, 0:n], AF.Silu, reads=[ba], writes=[b_sa[fc]])
            P.v("dve", "tensor_tensor", hid[i][:, fc, 0:n], sa[fc][:, 0:n], pu[:, 0:n], ALU.mult,
                reads=[b_sa[fc], bu], writes=[b_hid[i]])

    def dn(e, t0, n, i):
        nonlocal yi
        pe_ = e % 2
        ntile = n // 128
        for j in range(ntile):
            tile_i = t0 // 128 + j
            for dh in range(2):
                py, by = pYs[yi % 3]
                yi += 1
                for fc in range(2):
                    P.mm(py[:], hid[i][:, fc, j * 128:(j + 1) * 128], Wd[pe_][:, fc, dh * 512:(dh + 1) * 512],
                         fc == 0, fc == 1, reads=[b_hid[i], b_W[pe_][2]], writes=[by])
                ya = yacc[:, tile_i, dh * 512:(dh + 1) * 512]
                cs = comb_all[:, tile_i, e:e + 1]
                if e == 0:
                    P.v("dve", "tensor_scalar", ya, py[:], cs, None, ALU.mult, reads=[by, b_combt[tile_i]], writes=[b_yacc[tile_i]])
                else:
                    P.v("dve", "scalar_tensor_tensor", ya, py[:], cs, ya, ALU.mult, ALU.add,
                        reads=[by, b_combt[tile_i], b_yacc[tile_i]], writes=[b_yacc[tile_i]])

    items = [(e, t0, n) for e in range(16) for (t0, n) in blocks]
    pend = None
    last_e = -1
    for idx, (e, t0, n) in enumerate(items):
        if e != last_e:
            if e == 0:
                load_w(0)
            if e + 1 < 16:
                pass
            last_e = e
        if e == 0:
            for tq in range(t0 // 128, (t0 + n) // 128):
                d1(tq)
        gu(e, t0, n, idx % 2)
        if pend is not None:
            dn(*pend)
        pend = (e, t0, n, idx % 2)
        if t0 == blocks[0][0] and e + 1 < 16:
            load_w(e + 1)
    dn(*pend)
    xo = [k.xnf, k.hTf[:].rearrange("p c t -> p (c t)")]; b_xo = [k.env["b_xnf"], k.env["b_hTf"]]
    for ti, (src, sbuf_, dst, dbuf_, w) in enumerate(tiles):
        i = ti % 2
        X, bX = g["xt"][i], g["b_xt"][i]
        P.dma(X[:], src, reads=([sbuf_] if sbuf_ else []), writes=[bX], eng="pool")
        gs = k.gslot[(l, 1, w)]
        P.v("dve", "tensor_tensor", xo[i][:], yacc[:, ti, :], k.gateB_all[:, gs, :], ALU.mult,
            reads=[b_yacc[ti], k.b_gateB[l]], writes=[b_xo[i]])
        P.v("pool", "tensor_tensor", xo[i][:], xo[i][:], X[:], ALU.add, reads=[b_xo[i], bX], writes=[b_xo[i]])
        P.dma(dst, xo[i][:], reads=[b_xo[i]], writes=([dbuf_] if dbuf_ else []))


def layer1(k, dbg):
    g = k.env
    nc, P = k.nc, k.P
    ar, psum = k.ar, k.psum
    identF, identB, b_const = g["identF"], g["identB"], g["b_const"]
    X2_d, X3_d, QT1_d = g["X2_d"], g["X3_d"], g["QT1_d"]
    bnd, b_bnd = k.bnd, k.b_bnd
    sqn, skn, sinkb = g["sqn"], g["skn"], g["sinkb"]
    b_X2, b_X3 = k.b_scr["X2"], k.b_scr["X3"]
    b_QT1 = Buf()
    k.areset(0)
    NK = 19
    KT = ar("KT1", [128, 2, NK * 128], BF16); b_KT = Buf()
    P.v("pool", "memset", KT[64:128], 0.0, writes=[b_KT])
    Vt = ar("Vt1", [128, NK, 2, 66], BF16); b_Vt = Buf()
    P.v("pool", "memset", Vt[:], 1.0, writes=[b_Vt])
    wmask = ar("wmask", [128, 2, 128], BF16)
    P.dma(wmask[:], g["wmask_d"], writes=[b_const], eng="pool")
    k.wstage = [ar("wstage1%d" % i, [128, 1280]) for i in range(2)]
    k.swt = ar("swt1", [128, 1024])
    w_in = ar("w_in1", [128, 8, 1280], BF16); b_win = Buf()
    odin_d, odout_d = g["odin_d"], g["odout_d"]
    k.load_weight_bf16(w_in, b_win, lambda c: odin_d[c * 128:(c + 1) * 128, :], 8, 1280, k.wstage, k.b_wstage)
    w_outB = ar("w_out1B", [64, 16, D], BF16); b_woutB = Buf()
    for h in range(16):
        s_, bs_ = k.wstage[h % 2], k.b_wstage[h % 2]
        P.dma(s_[0:64, 0:D], odout_d[h * 64:(h + 1) * 64, :], writes=[bs_], eng=("sp" if h % 2 == 0 else "pool"))
        k.cast(("pool", "dve", "act")[h % 3], w_outB[:, h, :], s_[0:64, 0:D], [bs_], [b_woutB])
    ones_f = ar("ones_f1", [128, 64]); P.v("pool", "memset", ones_f[:], 1.0, writes=[b_const])
    z_sb = ar("z_sb1", [128, 1280]); b_z = Buf()
    ropeC = ar("ropeC1", [128, 64]); ropeS = ar("ropeS1", [128, 64]); b_rope = Buf()
    tmpA = ar("tmpA1", [128, 1024]); b_tmpA = Buf()
    tmpB = ar("tmpB1", [128, 1024]); b_tmpB = Buf()
    scr = ar("scr1", [128, 64]); b_scr_ = Buf()
    qb = ar("qb1", [128, 1024], BF16); b_qb = Buf()
    kb = ar("kb1", [128, 128], BF16); b_kb = Buf()
    qT_sb = ar("qT_sb1", [64, 16, 128], BF16); b_qTsb = Buf()
    pMisc, bMisc = psum[3]
    pMisc2, bMisc2 = psum[4]

    def modE(kind, ti):
        if kind == "ctx":
            return k.modulate(X2_d[(NOWN + ti) * 128:(NOWN + ti + 1) * 128, :], 1, 0, 1, src_buf=b_X2)
        return k.modulate(X2_d[ti * 128:(ti + 1) * 128, :], 1, 0, 0, src_buf=b_X2)

    def tileE(kind, ti, pre):
        if kind == "ctx":
            src, w, kidx = X2_d[(NOWN + ti) * 128:(NOWN + ti + 1) * 128, :], 1, ti
        else:
            src, w, kidx = X2_d[ti * 128:(ti + 1) * 128, :], 0, 2 + ti
        need_q = (kind != "ctx" and ti < 16)
        H, bH, X, bX = pre
        blocks = ((0, 512), (512, 1024), (1024, 1280)) if need_q else ((1024, 1280),)
        for bi, (lo, hi) in enumerate(blocks):
            pz, bz = psum[1 + bi % 2]
            for c in range(8):
                P.mm(pz[:, 0:hi - lo], H[:, c, :], w_in[:, c, lo:hi], c == 0, c == 7, reads=[bH, b_win], writes=[bz])
            P.act(z_sb[:, lo:hi], pz[:, 0:hi - lo], AF.Copy, reads=[bz], writes=[b_z])
        rope = None
        if kind != "ctx":
            P.dma(ropeC[:], g["ropeC_d"][ti * 128:(ti + 1) * 128, :], writes=[b_rope], eng="pool")
            P.dma(ropeS[:], g["ropeS_d"][ti * 128:(ti + 1) * 128, :], writes=[b_rope], eng="pool")
            rope = (ropeC, ropeS, b_rope, tmpB, b_tmpB)
        pM8 = pMisc[0:64, :].bitcast(BF16).rearrange("p (h t) -> p h t", h=8)
        pM8b = pMisc2[0:64, :].bitcast(BF16).rearrange("p (h t) -> p h t", h=8)
        if need_q:
            k.head_norm_rope(qb[:], b_qb, z_sb[:, 0:1024], b_z, 16, sqn, rope, tmpA, b_tmpA, scr, b_scr_)
            for h in range(16):
                pm, bm = (pM8, bMisc) if h < 8 else (pM8b, bMisc2)
                P.tr(pm[:, h % 8, :], qb[:, h * 64:(h + 1) * 64], identB[:], reads=[b_qb, b_const], writes=[bm])
            P.act(qT_sb[:, 0:8, :], pM8, AF.Copy, reads=[bMisc], writes=[b_qTsb])
            P.act(qT_sb[:, 8:16, :], pM8b, AF.Copy, reads=[bMisc2], writes=[b_qTsb])
            P.dma(QT1_d[ti], qT_sb[:], reads=[b_qTsb], writes=[b_QT1])
        k.head_norm_rope(kb[:], b_kb, z_sb[:, 1024:1152], b_z, 2, skn, rope, tmpA, b_tmpA, scr, b_scr_)
        for h in range(2):
            P.tr(pM8[:, h, :], kb[:, h * 64:(h + 1) * 64], identB[:], reads=[b_kb, b_const], writes=[bMisc])
        P.act(KT[0:64, :, kidx * 128:(kidx + 1) * 128], pM8[:, 0:2, :], AF.Copy, reads=[bMisc], writes=[b_KT])
        P.v("pool", "tensor_copy", Vt[:, kidx, :, 0:64], z_sb[:, 1152:1280].rearrange("p (h d) -> p h d", h=2),
            reads=[b_z], writes=[b_Vt])

    seqE = [("ctx", 0), ("ctx", 1)] + [("own", ti) for ti in range(17)]
    SETE = [(z_sb, b_z, qb, b_qb, kb, b_kb, qT_sb, b_qTsb),
            (ar("z_sb1b", [128, 1280]), Buf(), ar("qb1b", [128, 1024], BF16), Buf(), ar("kb1b", [128, 128], BF16), Buf(),
             ar("qT_sb1b", [64, 16, 128], BF16), Buf())]
    pre = modE(*seqE[0])
    for si, (kind, ti) in enumerate(seqE):
        nxt = modE(*seqE[si + 1]) if si + 1 < len(seqE) else None
        (z_sb, b_z, qb, b_qb, kb, b_kb, qT_sb, b_qTsb) = SETE[si % 2]
        tileE(kind, ti, pre)
        pre = nxt

    if "stopE1" in dbg:
        return
    qT_in = [ar("qT_in1%d" % i, [128, 16, 128], BF16) for i in range(2)]; b_qTin = [Buf(), Buf()]
    for i_ in range(2):
        P.v("pool", "memset", qT_in[i_][64:128], 0.0, writes=[b_qTin[i_]])
    NPT = 3
    PT = [ar("PT1%d" % i, [128, 1024], BF16) for i in range(NPT)]; b_PT = [Buf() for _ in range(NPT)]
    esink = ar("esink1", [128, 16]); b_esink = Buf()
    P.act(esink[:], sinkb[:], AF.Exp, bias=bnd[:, 6:7], reads=[b_const, b_bnd], writes=[b_esink])
    rsr = ar("rsr1", [128, 512]); b_rsr = Buf()
    bc_sb = ar("bc_sb1", [64, 512]); b_bc = Buf()
    OT = ar("OT1", [64, 16, 128], BF16); b_OT = Buf()
    x3_sb = ar("x3_sb", [128, D]); b_x3 = Buf()
    pO = [psum[4], psum[5], psum[6], psum[7]]
    SG = [(k.psall[:, 2 * g_ * 512:(2 * g_ + 2) * 512], [psum[2 * g_][1], psum[2 * g_ + 1][1]]) for g_ in range(2)]
    pBC, bBC = psum[0]
    ones_b = ar("ones_b1", [128, 64], BF16); P.v("pool", "memset", ones_b[:], 1.0, writes=[b_const])
    rsb = ar("rsb1", [128, 4, 512], BF16); b_rsb = Buf()
    bcs = [ar("bcs1%d" % i_, [64, 512]) for i_ in range(4)]; b_bcs = [Buf() for _ in range(4)]
    x_ins = [ar("xE%d" % i_, [128, D]) for i_ in range(2)]; b_xins = [Buf(), Buf()]
    vsink = ar("vsink", [128, 66], BF16); b_vs = Buf()
    P.v("pool", "memset", vsink[:], 0.0, writes=[b_vs])
    P.v("pool", "memset", vsink[:, 64:65], 1.0, writes=[b_vs])
    esrow = ar("esrow", [128, 16, 128], BF16); b_esrow = Buf()
    P.v("dve", "tensor_copy", esrow[64:65], esink[64:65, :].unsqueeze(2).to_broadcast([1, 16, 128]),
        reads=[b_esink], writes=[b_esrow])

    def loadQ(qi):
        i_ = qi % 2
        P.dma(qT_in[i_][0:64], QT1_d[qi], reads=[b_QT1], writes=[b_qTin[i_]])
        P.dma(x_ins[i_][:], X2_d[qi * 128:(qi + 1) * 128, :], reads=[b_X2], writes=[b_xins[i_]])

    it = 0
    loadQ(0)
    for qi in range(16):
        i = qi % 2
        Q, bQ = qT_in[i], b_qTin[i]
        if qi + 1 < 16:
            loadQ(qi + 1)
        keys = [(0, None), (1, None)]
        if qi > 0:
            keys.append((2 + qi - 1, 0))
        keys.append((2 + qi, None))
        keys.append((2 + qi + 1, 1))
        nk = len(keys)
        units = [(gq, list(range(a_, min(a_ + 2, nk)))) for gq in range(4) for a_ in range(0, nk, 2)]
        pend = None

        def pv(gq, kks, pt, bpt):
            po, bpo = pO[gq]
            for j, kk in enumerate(kks):
                kt, mk = keys[kk]
                P.mm(po[0:65, :], Vt[:, kt, gq // 2, 0:65], pt[:, j * 512:(j + 1) * 512], kk == 0, False,
                     reads=[bpt, b_Vt], writes=[bpo])
            if kks[-1] == nk - 1:
                P.mm(po[0:65, :], vsink[64:65, 0:65], esrow[64:65, gq * 4:(gq + 1) * 4, :].rearrange("p h t -> p (h t)"),
                     False, True, reads=[b_vs, b_esrow], writes=[bpo])

        for (gq, kks) in units:
            kvh = gq // 2
            psc, bscs = SG[it % 2]
            pt, bpt = PT[it % NPT], b_PT[it % NPT]
            it += 1
            n = len(kks)
            for j, kk in enumerate(kks):
                kt, mk = keys[kk]
                P.mm(psc[:, j * 512:(j + 1) * 512], KT[:, kvh, kt * 128:(kt + 1) * 128],
                     Q[:, gq * 4:(gq + 1) * 4, :].rearrange("p h t -> p (h t)"), reads=[b_KT, bQ], writes=[bscs[j]])
            P.act(pt[:, 0:n * 512], psc[:, 0:n * 512], AF.Exp, scale=0.125, bias=bnd[:, 6:7],
                  reads=bscs[0:n] + [b_bnd], writes=[bpt])
            for j, kk in enumerate(kks):
                kt, mk = keys[kk]
                if mk is not None:
                    p3 = pt[:, j * 512:(j + 1) * 512].rearrange("p (h t) -> p h t", h=4)
                    P.v("dve", "tensor_tensor", p3, p3, wmask[:, mk, :].unsqueeze(1).to_broadcast([128, 4, 128]), ALU.mult,
                        reads=[bpt, b_const], writes=[bpt])
            if pend is not None:
                pv(*pend)
            pend = (gq, kks, pt, bpt)
        pv(*pend)
        for gq in range(4):
            po, bpo = pO[gq]
            P.act(rsr[64:65, :], po[64:65, :], AF.Ln, reads=[bpo], writes=[b_rsr])
            P.act(rsb[64:65, gq, :], rsr[64:65, :], AF.Exp, scale=-1.0, reads=[b_rsr], writes=[b_rsb])
        for gq in range(4):
            pb_, bb_ = psum[gq]
            P.mm(pb_[0:64, :], ones_b[64:65, 0:64], rsb[64:65, gq, :], reads=[b_const, b_rsb], writes=[bb_])
        for gq in range(4):
            pb_, bb_ = psum[gq]
            P.v("dve", "tensor_copy", bcs[gq][:], pb_[0:64, :], reads=[bb_], writes=[b_bcs[gq]])
        for gq in range(4):
            po, bpo = pO[gq]
            P.v("dve", "tensor_tensor", OT[:, gq * 4:(gq + 1) * 4, :].rearrange("p h t -> p (h t)"), po[0:64, :], bcs[gq][:],
                ALU.mult, reads=[bpo, b_bcs[gq]], writes=[b_OT])
        X, bX = x_ins[i], b_xins[i]
        for hh in range(2):
            pz, bz = psum[1 + hh]
            for h in range(16):
                P.mm(pz[:], OT[:, h, :], w_outB[:, h, hh * 512:(hh + 1) * 512], h == 0, h == 15,
                     reads=[b_OT, b_woutB], writes=[bz])
            P.v("dve", "tensor_tensor", x3_sb[:, hh * 512:(hh + 1) * 512], pz[:],
                k.gateB_all[:, k.gslot[(1, 0, 0)], hh * 512:(hh + 1) * 512], ALU.mult,
                reads=[bz, k.b_gateB[1]], writes=[b_x3])
        P.v("pool", "tensor_tensor", x3_sb[:], x3_sb[:], X[:], ALU.add, reads=[b_x3, bX], writes=[b_x3])
        P.dma(X3_d[qi * 128:(qi + 1) * 128, :], x3_sb[:], reads=[b_x3], writes=[b_X3])


def rope_tables():
    t = np.arange(SEQ)
    row = (t // 64).astype(np.float32)
    col = (t % 64).astype(np.float32)
    inv = (10000.0 ** (-np.arange(0, 32, 2, dtype=np.float32) / 32)).astype(np.float32)
    ang = np.stack([row[:, None] * inv, col[:, None] * inv], axis=1)
    c = np.cos(ang).astype(np.float32)
    s = np.sin(ang).astype(np.float32)
    C = np.zeros((SEQ, 2, 2, 16), np.float32)
    S = np.zeros((SEQ, 2, 2, 16), np.float32)
    C[:, :, 0] = c
    C[:, :, 1] = c
    S[:, :, 0] = -s
    S[:, :, 1] = s
    return C.reshape(SEQ, 64), S.reshape(SEQ, 64)


def host_consts():
    p = np.arange(128)
    same = (p[:, None] // 64) == (p[None, :] // 64)
    s, t = p[:, None], p[None, :]
    tri = np.stack([same & (s <= t), same & (s >= t), same & (s > t), same & (s < t)], axis=1).astype(np.float32)
    sm = (p % 64)[:, None]
    tt = np.arange(64)[None, :]
    mA = np.tile((sm <= tt), (1, 4))
    mB = np.tile((sm >= tt), (1, 4))
    maskAB = np.stack([mA, mB], axis=1).astype(ml_dtypes.bfloat16)
    sel2 = np.zeros((2, 2, 128), np.float32)
    sel2[0, 0] = 1
    sel2[1, 1] = 1
    wmask = np.stack([(s >= t), (s <= t)], axis=1).astype(ml_dtypes.bfloat16)
    return dict(identF=np.eye(128, dtype=np.float32), identB=np.eye(128).astype(ml_dtypes.bfloat16),
                tri=tri, maskAB=maskAB, sel2=sel2, wmask=wmask)


def make_in_maps(inp):
    f = lambda a: np.ascontiguousarray(np.asarray(a, dtype=np.float32))
    C, S = rope_tables()
    consts = host_consts()
    maps = []
    rw = np.concatenate([f(inp["router_group_w"]), f(inp["router_expert_w"])], axis=-1)
    rb = np.concatenate([f(inp["router_group_b"]), f(inp["router_expert_b"])], axis=-1)[:, None, :]
    shared = dict(
        mod_w=f(inp["mod_w"]), mod_b=f(inp["mod_b"]), norm_mix=f(inp["norm_mix"]), norm_ffn=f(inp["norm_ffn"]),
        ev_w_in=f(inp["ev_w_in"])[0], ev_w_out=f(inp["ev_w_out"])[0],
        gla_out_norm=f(inp["gla_out_norm"]), att_q_norm=f(inp["att_q_norm"]), att_k_norm=f(inp["att_k_norm"]),
        od_w_in=f(inp["od_w_in"])[0], od_w_out=f(inp["od_w_out"])[0], swa_sink=f(inp["swa_sink"]),
        swa_q_norm=f(inp["swa_q_norm"]), swa_k_norm=f(inp["swa_k_norm"]),
        router_w=np.ascontiguousarray(rw), router_b=np.ascontiguousarray(rb),
        exp_w_gate=f(inp["exp_w_gate"]).reshape(2, 16, D, 256), exp_w_up=f(inp["exp_w_up"]).reshape(2, 16, D, 256),
        exp_w_down=f(inp["exp_w_down"]).reshape(2, 16, 256, D), **consts)
    gw = f(inp["gla_gate_w"])[0]
    gb = f(inp["gla_gate_b"])[0]
    for core in range(8):
        b, half = core // 2, core % 2
        x = f(inp["x"])[b]
        cx = f(inp["ctx"])[b]
        order = (0, 1)
        if half == 1:
            x = x[::-1]
            cx = cx[::-1]
            order = (1, 0)
        gwe = np.zeros((33, 512), np.float32)
        for i, dr in enumerate(order):
            gwe[16 * dr:16 * dr + 16, 256 * i:256 * i + 256] = gw[dr]
            gwe[32, 256 * i:256 * i + 256] = gb[dr]
        m = dict(shared)
        m.update(x=np.ascontiguousarray(x), ctx=np.ascontiguousarray(cx),
                 crow=np.ascontiguousarray(np.stack([f(inp["c"])[b], f(inp["c_ctx"])])),
                 gw_ext=gwe,
                 ropeC=np.ascontiguousarray(C[::-1] if half else C),
                 ropeS=np.ascontiguousarray(S[::-1] if half else S))
        maps.append(m)
    return maps


_CACHE = {}


def kernel(**inputs):
    if "nc" not in _CACHE:
        _CACHE["nc"] = build()[0]
    nc = _CACHE["nc"]
    maps = make_in_maps(inputs)
    res = run_bass_kernel_spmd(nc, maps, core_ids=list(range(8)))
    out = np.zeros((4, SEQ, D), np.float32)
    for core in range(8):
        b, half = core // 2, core % 2
        o = np.asarray(res.results[core]["out"], dtype=np.float32)
        if half == 0:
            out[b, 0:2048] = o
        else:
            out[b, 2048:] = o[::-1]
    return out
```

```python
import numpy as np
import ml_dtypes
from contextlib import ExitStack
import concourse.bass as bass
import concourse.mybir as mybir
from concourse.bass_utils import run_bass_kernel_spmd

F32 = mybir.dt.float32
BF16 = mybir.dt.bfloat16
ALU = mybir.AluOpType
AF = mybir.ActivationFunctionType
AX = mybir.AxisListType

import os
COMPUTE = ("pe", "act", "dve", "pool")
SCHED_W = int(os.environ.get("SCHED_W", "128"))
PE_A = float(os.environ.get("PE_A", "0.05"))
PE_B = float(os.environ.get("PE_B", "0.00035"))
PE_F = float(os.environ.get("PE_F", "2.5"))
DMA_L = float(os.environ.get("DMA_L", "2.2"))
ACT_S = float(os.environ.get("ACT_S", "0.8"))
DVE_S = float(os.environ.get("DVE_S", "1.0"))
POOL_S = float(os.environ.get("POOL_S", "1.0"))
SEM_L = float(os.environ.get("SEM_L", "0.1"))
NDMASEM = 12

D = 1024
SEQ = 4096
NOWN = 17
NTOK0 = 19
EVEN_IN = 2336
NEG_BIG = 1.0e30


class Buf:
    __slots__ = ("name", "w", "r")

    def __init__(self, name=""):
        self.name = name
        self.w = None
        self.r = {}


class Prog:
    def __init__(self, nc):
        self.nc = nc
        self.ops = []
        self.cost = []
        self.reorder = True
        self.base = set()
        self.eng = {"pe": nc.tensor, "act": nc.scalar, "dve": nc.vector,
                    "pool": nc.gpsimd, "sp": nc.sync}

    def op(self, eng, fn, reads=(), writes=(), dma=False, cost=0.4):
        idx = len(self.ops)
        deps = set(self.base)
        for b in reads:
            if b.w is not None:
                deps.add(b.w)
        for b in writes:
            if b.w is not None:
                deps.add(b.w)
            for v in b.r.values():
                if isinstance(v, list):
                    deps.update(v)
                else:
                    deps.add(v)
        key = (eng, dma)
        for b in reads:
            b.r.setdefault(key, []).append(idx)
        for b in writes:
            b.w = idx
            b.r = {}
        deps.discard(idx)
        self.ops.append((eng, dma, fn, deps))
        self.cost.append(cost)
        return idx

    def barrier(self):
        last = {}
        dm = {}
        for i, (eng, dma, fn, deps) in enumerate(self.ops):
            if dma:
                dm.setdefault(eng, []).append(i)
            else:
                last[eng] = i
        base = set(last.values())
        for q, lst in dm.items():
            base.update(lst[-NDMASEM:])
        self.base = base

    @staticmethod
    def _fs(ap):
        n = 1
        for s in ap.shape[1:]:
            n *= s
        return n

    def dma(self, out, in_, reads=(), writes=(), eng="sp"):
        e = self.eng[eng]
        nb = self._fs(out) * out.shape[0] * (2 if out.dtype == BF16 else 4)
        return self.op(eng, lambda: e.dma_start(out=out, in_=in_), reads, writes, dma=True,
                       cost=DMA_L + nb / 150e3)

    def mm(self, out, lhsT, rhs, start=True, stop=True, reads=(), writes=()):
        nc = self.nc
        c = PE_A + self._fs(rhs) * PE_B
        if rhs.dtype == F32:
            c *= PE_F
        return self.op("pe", lambda: nc.tensor.matmul(out, lhsT, rhs, start=start, stop=stop),
                       reads, writes, cost=c)

    def tr(self, out, in_, ident, reads=(), writes=()):
        nc = self.nc
        return self.op("pe", lambda: nc.tensor.transpose(out, in_, ident), reads, writes, cost=0.12)

    def act(self, out, in_, func, reads=(), writes=(), **kw):
        nc = self.nc
        return self.op("act", lambda: nc.scalar.activation(out=out, in_=in_, func=func, **kw),
                       reads, writes, cost=ACT_S * (0.25 + self._fs(in_) * 0.0009))

    def v(self, eng, name, *args, reads=(), writes=(), **kw):
        f = getattr(self.eng[eng], name)
        c = DVE_S * (0.15 + self._fs(args[0]) * 0.0011)
        if eng == "pool":
            c = POOL_S * (0.3 + self._fs(args[0]) * 0.0025)
        return self.op(eng, lambda: f(*args, **kw), reads, writes, cost=c)

    def schedule(self, W=SCHED_W):
        ops, cost = self.ops, self.cost
        n = len(ops)
        fin = [None] * n
        start = [0.0] * n
        rem = {}
        for i, (eng, dma, fn, deps) in enumerate(ops):
            rem.setdefault(eng, []).append(i)
        etime = {e: 0.0 for e in rem}
        nsched = 0
        while nsched < n:
            best = None
            for e, lst in rem.items():
                if not lst:
                    continue
                te = etime[e]
                for i in lst[:W]:
                    deps = ops[i][3]
                    r = te
                    ok = True
                    for d in deps:
                        f = fin[d]
                        if f is None:
                            ok = False
                            break
                        if f > r:
                            r = f
                    if not ok:
                        continue
                    key = (r, i)
                    if best is None or key < best[0]:
                        best = (key, e, i)
                    if r <= te:
                        break
            (r, i), e, _ = best
            eng, dma, fn, deps = ops[i]
            start[i] = r
            if dma:
                etime[e] = r + 0.5
                fin[i] = r + cost[i]
            else:
                etime[e] = r + cost[i]
                fin[i] = r + cost[i] + SEM_L
            rem[e].remove(i)
            nsched += 1
        order = sorted(range(n), key=lambda j: (start[j], j))
        self.sim_time = max(f for f in fin)
        self.sim_start, self.sim_fin = start, fin
        return order

    def emit(self, sems):
        ops = self.ops
        n = len(ops)
        need = [False] * n
        for (eng, dma, fn, deps) in ops:
            for d in deps:
                deng, ddma, _, _ = ops[d]
                if (not ddma) and deng == "pe" and eng == "pe" and not dma:
                    continue
                need[d] = True
        sig = [None] * n
        cnt = {e: 0 for e in COMPUTE}
        dcnt = {}
        waited = {}
        order = self.schedule() if self.reorder else range(n)
        for i in order:
            eng, dma, fn, deps = ops[i]
            e = self.eng[eng]
            w = {}
            for d in deps:
                deng, ddma, _, _ = ops[d]
                if (not ddma) and deng == "pe" and eng == "pe" and not dma:
                    continue
                s, val = sig[d]
                if w.get(id(s), (None, -1))[1] < val:
                    w[id(s)] = (s, val)
            if dma:
                j = dcnt.get(eng, 0)
                pool = sems[("dma", eng)]
                s = pool[j % len(pool)]
                val = 16 * (j // len(pool) + 1)
                if val > 16 and w.get(id(s), (None, -1))[1] < val - 16:
                    w[id(s)] = (s, val - 16)
                dcnt[eng] = j + 1
                sig[i] = (s, val)
            for sid, (s, val) in w.items():
                k = (eng, sid)
                if waited.get(k, -1) >= val:
                    continue
                waited[k] = val
                e.wait_ge(s, val)
            ins = fn()
            if dma:
                ins.then_inc(sig[i][0], 16)
            elif need[i]:
                cnt[eng] += 1
                sig[i] = (sems[eng], cnt[eng])
                ins.then_inc(sems[eng], 1)
        for (k, pool) in sems.items():
            if isinstance(k, tuple):
                eng = k[1]
                j = dcnt.get(eng, 0)
                e = self.eng[eng]
                for q, s in enumerate(pool):
                    c = (j - q + len(pool) - 1) // len(pool) if j > q else 0
                    if c > 0:
                        e.wait_ge(s, 16 * c)
        return dict(n_ops=n, sig=cnt, dmas=dcnt)


class K:
    pass


def build(stages=("all",), dbg=()):
    nc = bass.Bass("TRN2", target_bir_lowering=False)
    P = Prog(nc)
    k = K()
    k.nc, k.P = nc, P
    k.dbgset = set(dbg)
    es = ExitStack()
    k.es = es
    k.dbg = {}

    def din(name, shape, dt=F32):
        return nc.dram_tensor(name, list(shape), dt, kind="ExternalInput").ap()

    def dscr(name, shape, dt=F32):
        if name in dbg:
            return nc.dram_tensor(name, list(shape), dt, kind="ExternalOutput").ap()
        return nc.dram_tensor(name, list(shape), dt).ap()

    def dout(name, shape, dt=F32):
        return nc.dram_tensor(name, list(shape), dt, kind="ExternalOutput").ap()

    def sb(name, shape, dt=F32):
        return es.enter_context(nc.sbuf_tensor("sb_" + name, list(shape), dt))

    def ps(name, shape, dt=F32):
        return es.enter_context(nc.psum_tensor("ps_" + name, list(shape), dt))

    k.din, k.dscr, k.dout, k.sb, k.ps = din, dscr, dout, sb, ps
    ARW = 41700
    k.arena = None
    k.aoff = 0

    def ar(name, shape, dt=F32):
        if k.arena is None:
            k.arena = sb("arena", [128, ARW])
        n = 1
        for s in shape[1:]:
            n *= s
        w = n if dt == F32 else (n + 1) // 2
        off = k.aoff
        k.aoff += w + (w % 2)
        assert k.aoff <= ARW, (name, k.aoff)
        a = k.arena[0:shape[0], off:off + w]
        if dt != F32:
            a = a.bitcast(dt)
        if len(shape) > 2:
            names = " ".join("d%d" % i for i in range(1, len(shape)))
            kw = {"d%d" % i: shape[i] for i in range(1, len(shape))}
            a = a.rearrange("p (%s) -> p %s" % (names, names), **kw)
        return a

    def areset(mark=0):
        P.barrier()
        k.peak = getattr(k, "peak", [])
        k.peak.append(k.aoff * 4 // 1024)
        k.marks = getattr(k, "marks", [])
        k.marks.append(len(P.ops))
        k.aoff = mark

    k.ar, k.areset = ar, areset

    with es:
        sems = {e: es.enter_context(nc.semaphore("s_" + e)) for e in COMPUTE}
        for q in ("sp", "pool"):
            sems[("dma", q)] = [es.enter_context(nc.semaphore(f"d_{q}{i}")) for i in range(NDMASEM)]
        body(k, stages, dbg)
        st = P.emit(sems)
        k.stats = st
    return nc, k


def body(k, stages, dbg):
    nc, P = k.nc, k.P
    din, dscr, dout, sb, ps, ar = k.din, k.dscr, k.dout, k.sb, k.ps, k.ar

    x_d = din("x", [SEQ, D])
    ctx_d = din("ctx", [256, D])
    crow_d = din("crow", [2, D])
    modw_d = din("mod_w", [2, D, 6 * D])
    modb_d = din("mod_b", [2, 6 * D])
    nmix_d = din("norm_mix", [2, D])
    nffn_d = din("norm_ffn", [2, D])
    evin_d = din("ev_w_in", [D, EVEN_IN])
    evout_d = din("ev_w_out", [D, D])
    gwext_d = din("gw_ext", [33, 512])
    glan_d = din("gla_out_norm", [1, 128])
    aqn_d = din("att_q_norm", [1, 64])
    akn_d = din("att_k_norm", [1, 64])
    odin_d = din("od_w_in", [D, 1280])
    odout_d = din("od_w_out", [D, D])
    sink_d = din("swa_sink", [1, 16])
    sqn_d = din("swa_q_norm", [1, 64])
    skn_d = din("swa_k_norm", [1, 64])
    rw_d = din("router_w", [2, D, 20])
    rb_d = din("router_b", [2, 1, 20])
    wg_d = din("exp_w_gate", [2, 16, D, 256])
    wu_d = din("exp_w_up", [2, 16, D, 256])
    wd_d = din("exp_w_down", [2, 16, 256, D])
    ropeC_d = din("ropeC", [SEQ, 64])
    ropeS_d = din("ropeS", [SEQ, 64])
    identF_d = din("identF", [128, 128])
    identB_d = din("identB", [128, 128], BF16)
    tri_d = din("tri", [128, 4, 128])
    maskAB_d = din("maskAB", [128, 2, 256], BF16)
    sel2_d = din("sel2", [2, 2, 128])
    wmask_d = din("wmask", [128, 2, 128], BF16)
    out_d = dout("out", [2048, D])

    X1_d = dscr("X1", [NTOK0 * 128, D])
    X2_d = dscr("X2", [NTOK0 * 128, D])
    X3_d = dscr("X3", [2048, D])
    OA_d = dscr("OA", [NTOK0, 128, 512])
    QT_d = dscr("QT", [NTOK0, 64, 8, 128], BF16)
    MIXT_d = dscr("MIXT", [NTOK0, 128, 8, 128], BF16)
    PB_lt = dscr("PB_lt", [NTOK0, 64, 512], BF16)
    PB_ke = dscr("PB_ke", [NTOK0, 64, 512], BF16)
    PB_kd = dscr("PB_kd", [NTOK0, 128, 256], BF16)
    PB_v = dscr("PB_v", [NTOK0, 128, 512], BF16)
    PB_ee = dscr("PB_ee", [NTOK0, 64, 8])
    PB_rg = dscr("PB_rg", [NTOK0, 128, 512])
    QT1_d = dscr("QT1", [16, 64, 16, 128], BF16)

    identF = sb("identF", [128, 128]); identB = sb("identB", [128, 128], BF16)
    b_const = Buf("const")
    for t, d in ((identF, identF_d), (identB, identB_d)):
        P.dma(t[:], d, writes=[b_const], eng="pool")
    k.identF, k.identB, k.b_const = identF, identB, b_const

    psall = ps("psall", [128, 4096])
    k.psall = psall
    pA, pB, pC, pD, pE, pF, pG, pH = [psall[:, i * 512:(i + 1) * 512] for i in range(8)]
    bA, bB, bC, bD, bE, bF_, bG, bH = [Buf("ps%d" % i) for i in range(8)]
    k.psum = [(pA, bA), (pB, bB), (pC, bC), (pD, bD), (pE, bE), (pF, bF_), (pG, bG), (pH, bH)]

    crow = ar("crow", [2, D]); b_crow = Buf()
    scT = sb("scT", [128, 8, 2]); b_scT = Buf()
    P.dma(crow[:], crow_d, writes=[b_crow])
    P.act(crow[:], crow[:], AF.Silu, reads=[b_crow], writes=[b_crow])
    for c in range(8):
        P.tr(pA[:, 2 * c:2 * c + 2], crow[:, c * 128:(c + 1) * 128], identF[0:2, 0:2],
             reads=[b_crow, b_const], writes=[bA])
    P.v("dve", "tensor_copy", scT[:].rearrange("p c t -> p (c t)"), pA[:, 0:16], reads=[bA], writes=[b_scT])
    gcol = sb("gcol", [128, 2, 2, 2, 8, 2])
    b_gcol = [Buf(), Buf()]
    gateB_all = sb("gateB", [128, 6, D])
    b_gateB = [Buf(), Buf()]
    k.gslot = {(0, 0, 0): 0, (0, 0, 1): 1, (0, 1, 0): 2, (0, 1, 1): 3, (1, 0, 0): 4, (1, 1, 0): 5}
    k.gcol, k.b_gcol = gcol, b_gcol
    k.gateB_all, k.b_gateB = gateB_all, b_gateB

    def s0_steps(l, tg, bankT, bank1, bank2):
        st = {}

        def alloc():
            st["modv"] = ar("modv" + tg, [2, 6 * D]); st["b_modv"] = Buf()
            st["modb"] = [ar("modb%d" % i + tg, [2, 512]) for i in range(2)]; st["b_modb"] = [Buf(), Buf()]
            st["wst"] = [ar("modwst%d" % i + tg, [128, 8, 512]) for i in range(2)]; st["b_wst"] = [Buf(), Buf()]
            st["nrm"] = ar("nrm" + tg, [2, D]); st["b_nrm"] = Buf()
            st["grow"] = st["nrm"]; st["b_grow"] = st["b_nrm"]
            st["sel2"] = ar("sel2" + tg, [2, 2, 128]); st["b_sel2"] = Buf()
            P.dma(st["sel2"][:], sel2_d, writes=[st["b_sel2"]], eng="pool")

        def blk(j):
            if j == 0:
                alloc()
            modv, b_modv = st["modv"], st["b_modv"]
            w = st["wst"][j % 2]; bw = st["b_wst"][j % 2]
            mb = st["modb"][j % 2]; bmb = st["b_modb"][j % 2]
            pz, bz = (bank1, bank2)[j % 2]
            for r in range(2):
                P.dma(mb[r:r + 1, :], modb_d[l:l + 1, j * 512:(j + 1) * 512], writes=[bmb], eng="pool")
            P.dma(w[:], modw_d[l].rearrange("(c p) n -> p c n", p=128)[:, :, j * 512:(j + 1) * 512],
                  writes=[bw], eng=("sp" if j % 2 == 0 else "pool"))
            for c in range(8):
                P.mm(pz[0:2, :], scT[:, c, :], w[:, c, :], c == 0, c == 7, reads=[b_scT, bw], writes=[bz])
            P.v("dve", "tensor_tensor", modv[:, j * 512:(j + 1) * 512], pz[0:2, :], mb[:],
                ALU.add, reads=[bz, bmb], writes=[b_modv])

        def fin():
            modv, b_modv = st["modv"], st["b_modv"]
            nrm, b_nrm, grow, b_grow = st["nrm"], st["b_nrm"], st["grow"], st["b_grow"]
            pT_, bT_ = bankT
            for sub in range(2):
                nd = (nmix_d, nffn_d)[sub]
                for r in range(2):
                    P.dma(nrm[r:r + 1, :], nd[l:l + 1, :], writes=[b_nrm], eng="pool")
                base = sub * 3 * D
                P.v("dve", "scalar_tensor_tensor", grow[:], modv[:, base + D:base + 2 * D], 1.0, nrm[:],
                    ALU.add, ALU.mult, reads=[b_modv, b_nrm], writes=[b_grow])
                for gi, src_ in enumerate((grow[:], modv[:, base:base + D])):
                    for c in range(8):
                        P.tr(pT_[:, 2 * c:2 * c + 2], src_[:, c * 128:(c + 1) * 128], identF[0:2, 0:2],
                             reads=[b_grow, b_modv, b_const], writes=[bT_])
                    P.v("dve", "tensor_copy", gcol[:, l, sub, gi].rearrange("p c t -> p (c t)"), pT_[:, 0:16],
                        reads=[bT_], writes=[b_gcol[l]])
            for sub in range(2):
                for w in range(2):
                    if l == 1 and w == 1:
                        continue
                    for hh in range(2):
                        pz, bz = (bank1, bank2)[hh]
                        col = (sub * 3 + 2) * D + hh * 512
                        P.mm(pz[:], st["sel2"][:, w, :], modv[:, col:col + 512], reads=[st["b_sel2"], b_modv], writes=[bz])
                        P.v("dve", "tensor_copy", gateB_all[:, k.gslot[(l, sub, w)], hh * 512:(hh + 1) * 512], pz[:],
                            reads=[bz], writes=[b_gateB[l]])

        return [(lambda j=j: blk(j)) for j in range(12)] + [fin]

    k.s0_steps = s0_steps
    for stp in s0_steps(0, "L0", k.psum[0], k.psum[1], k.psum[2]):
        stp()

    xt = [sb("xt%d" % i, [128, D]) for i in range(2)]; b_xt = [Buf(), Buf()]
    stat = [sb("stat%d" % i, [128, 8]) for i in range(2)]; b_stat = [Buf(), Buf()]
    xn = [sb("xn%d" % i, [128, D], BF16) for i in range(2)]; b_xn = [Buf(), Buf()]
    b_xnf = Buf()
    hT = [sb("hT%d" % i, [128, 8, 128], BF16) for i in range(2)]; b_hT = [Buf(), Buf()]
    b_hTf = Buf()
    k.cnt = 0

    def modulate(src_ap, l, sub, w, fp32=False, src_buf=None, xt_out=None):
        i = k.cnt % 2
        k.cnt += 1
        X, bX = xt[i], b_xt[i]
        P.dma(X[:], src_ap, reads=([src_buf] if src_buf else []), writes=[bX])
        st, bst = stat[i], b_stat[i]
        P.v("dve", "scalar_tensor_tensor", xn[i][:], X[:], 1.0, X[:], ALU.mult, ALU.mult,
            accum_out=st[:, 0:1], reads=[bX], writes=[b_xn[i], bst])
        P.act(st[:, 1:2], st[:, 0:1], AF.Ln, scale=1.0 / D, bias=1e-6, reads=[bst], writes=[bst])
        P.act(st[:, 2:3], st[:, 1:2], AF.Exp, scale=-0.5, reads=[bst], writes=[bst])
        if not fp32:
            N_, bN = xn[i], b_xn[i]
            H, bH_ = hT[i], b_hT[i]
            pz, bz = k.psum[0]
            pzv = pz[:].bitcast(BF16).rearrange("p (c t) -> p c t", c=8)
            P.v("dve", "tensor_scalar", N_[:], X[:], st[:, 2:3], None, ALU.mult, reads=[bX, bst], writes=[bN])
            for c in range(8):
                P.tr(pzv[:, c, :], N_[:, c * 128:(c + 1) * 128], identB[:], reads=[bN, b_const], writes=[bz])
            for c in range(8):
                P.act(H[:, c, :], pzv[:, c, :], AF.Identity, scale=gcol[:, l, sub, 0, c, w:w + 1],
                      bias=gcol[:, l, sub, 1, c, w:w + 1], reads=[bz, b_gcol[l]], writes=[bH_])
            return H, bH_, X, bX
        else:
            xnf, hTf = k.xnf, k.hTf
            P.v("dve", "tensor_scalar", xnf[:], X[:], st[:, 2:3], None, ALU.mult, reads=[bX, bst], writes=[b_xnf])
            for hh in range(2):
                pz, bz = k.psum[hh]
                for c in range(4):
                    cc = hh * 4 + c
                    P.tr(pz[:, c * 128:(c + 1) * 128], xnf[:, cc * 128:(cc + 1) * 128], identF[:],
                         reads=[b_xnf, b_const], writes=[bz])
                for c in range(4):
                    cc = hh * 4 + c
                    P.act(hTf[:, cc, :], pz[:, c * 128:(c + 1) * 128], AF.Identity,
                          scale=gcol[:, l, sub, 0, cc, w:w + 1], bias=gcol[:, l, sub, 1, cc, w:w + 1],
                          reads=[bz, b_gcol[l]], writes=[b_hTf])
            return hTf, b_hTf, X, bX

    k.modulate = modulate

    def cast(eng, dst, src, reads, writes):
        if eng == "act":
            P.act(dst, src, AF.Copy, reads=reads, writes=writes)
        else:
            P.v(eng, "tensor_copy", dst, src, reads=reads, writes=writes)

    k.cast = cast

    def load_weight_bf16(dst, b_dst, src_ap_fn, nchunk, ncol, stage, b_stage, cast_engs=("pool", "dve", "act")):
        for c in range(nchunk):
            s, bs = stage[c % 2], b_stage[c % 2]
            P.dma(s[:, 0:ncol], src_ap_fn(c), writes=[bs], eng=("sp" if c % 2 == 0 else "pool"))
            cast(cast_engs[c % len(cast_engs)], dst[:, c, :], s[:, 0:ncol], [bs], [b_dst])

    k.b_wstage = [Buf(), Buf()]
    k.load_weight_bf16 = load_weight_bf16

    def bcast_row(name, d_ap, n):
        t = sb(name, [128, n])
        P.dma(t[:], d_ap.partition_broadcast(128).rearrange("p o n -> p (o n)"), writes=[b_const], eng="pool")
        return t

    aqn = bcast_row("aqn", aqn_d, 64); akn = bcast_row("akn", akn_d, 64)
    sqn = bcast_row("sqn", sqn_d, 64); skn = bcast_row("skn", skn_d, 64)
    glan = bcast_row("glan", glan_d, 128)
    sinkb = bcast_row("sinkb", sink_d, 16)

    bnd = sb("bnd", [128, 8]); b_bnd = Buf()

    def make_bound(col, gq, gk):
        P.v("dve", "tensor_reduce", bnd[:, col:col + 1], gq[:], AX.X, ALU.max, apply_absolute_value=True,
            reads=[b_const], writes=[b_bnd])
        P.v("dve", "tensor_reduce", bnd[:, col + 1:col + 2], gk[:], AX.X, ALU.max, apply_absolute_value=True,
            reads=[b_const], writes=[b_bnd])
        P.v("dve", "scalar_tensor_tensor", bnd[:, col + 2:col + 3], bnd[:, col:col + 1], -8.0, bnd[:, col + 1:col + 2],
            ALU.mult, ALU.mult, reads=[b_bnd], writes=[b_bnd])
    make_bound(0, aqn, akn)
    make_bound(4, sqn, skn)
    k.bnd, k.b_bnd = bnd, b_bnd

    def head_norm_rope(dst, b_dst, src, b_src, nh, gain, rope, tmp, b_tmp, scr, b_scr):
        s3 = src.rearrange("p (h d) -> p h d", h=nh)
        t3 = tmp[:, 0:nh * 64].rearrange("p (h d) -> p h d", h=nh)
        P.v("dve", "tensor_tensor", t3, s3, s3, ALU.mult, reads=[b_src], writes=[b_tmp])
        P.v("dve", "tensor_reduce", scr[:, 0:nh], t3, AX.X, ALU.add, reads=[b_tmp], writes=[b_scr])
        P.act(scr[:, 16:16 + nh], scr[:, 0:nh], AF.Ln, scale=1.0 / 64, bias=1e-6, reads=[b_scr], writes=[b_scr])
        P.act(scr[:, 32:32 + nh], scr[:, 16:16 + nh], AF.Exp, scale=-0.5, reads=[b_scr], writes=[b_scr])
        rs = scr[:, 32:32 + nh]
        P.v("dve", "tensor_tensor", t3, s3, rs.unsqueeze(2).to_broadcast([128, nh, 64]), ALU.mult,
            reads=[b_src, b_scr], writes=[b_tmp])
        g3 = gain[:, 0:64].unsqueeze(1).to_broadcast([128, nh, 64])
        if rope is None:
            d3 = dst.rearrange("p (h d) -> p h d", h=nh)
            P.v("dve", "tensor_tensor", d3, t3, g3, ALU.mult, reads=[b_tmp, b_const], writes=[b_dst])
            return
        C, S, b_rope, tmp2, b_tmp2 = rope
        P.v("dve", "tensor_tensor", t3, t3, g3, ALU.mult, reads=[b_tmp, b_const], writes=[b_tmp])
        u3 = tmp2[:, 0:nh * 64].rearrange("p (h d) -> p h d", h=nh)
        P.v("pool", "tensor_tensor", u3, t3, C[:, 0:64].unsqueeze(1).to_broadcast([128, nh, 64]), ALU.mult,
            reads=[b_tmp, b_rope], writes=[b_tmp2])
        t5 = tmp[:, 0:nh * 64].rearrange("p (h a f e) -> p h a f e", h=nh, a=2, f=2)
        S5 = S[:, 0:64].rearrange("p (a f e) -> p a f e", a=2, f=2)
        d5 = dst.rearrange("p (h a f e) -> p h a f e", h=nh, a=2, f=2)
        u5 = tmp2[:, 0:nh * 64].rearrange("p (h a f e) -> p h a f e", h=nh, a=2, f=2)
        for f in range(2):
            sw = t5[:, :, :, 1 - f, :]
            sv = S5[:, :, f, :].unsqueeze(1).to_broadcast([128, nh, 2, 16])
            w_ = scr
            P.v("dve", "tensor_tensor", sw_tmp(k, nh)[:, :, :, f, :], sw, sv, ALU.mult,
                reads=[b_tmp, b_rope], writes=[k.b_swt])
        st5 = sw_tmp(k, nh)
        P.v("dve", "tensor_tensor", d5, st5, u5, ALU.add, reads=[k.b_swt, b_tmp2], writes=[b_dst])

    k.b_swt = Buf()

    def sw_tmp(k_, nh):
        return k_.swt[:, 0:nh * 64].rearrange("p (h a f e) -> p h a f e", h=nh, a=2, f=2)

    k.head_norm_rope = head_norm_rope

    from_l0 = dict(locals())
    k.env = from_l0
    k.areset(0)
    if "stopS0" in dbg:
        return
    layer0(k, stages, dbg)


def layer0(k, stages, dbg):
    g = k.env
    nc, P = k.nc, k.P
    sb, ps, dout = k.ar, k.ps, k.dout
    identF, identB, b_const = g["identF"], g["identB"], g["b_const"]
    modulate = k.modulate
    psum = k.psum
    x_d, ctx_d = g["x_d"], g["ctx_d"]
    (OA_d, QT_d, MIXT_d, PB_lt, PB_ke, PB_kd, PB_v, PB_ee, PB_rg, X1_d) = (
        g["OA_d"], g["QT_d"], g["MIXT_d"], g["PB_lt"], g["PB_ke"], g["PB_kd"], g["PB_v"], g["PB_ee"], g["PB_rg"], g["X1_d"])
    aqn, akn, glan = g["aqn"], g["akn"], g["glan"]
    bnd, b_bnd = k.bnd, k.b_bnd
    gateB_all, b_gateB = k.gateB_all, k.b_gateB
    b_scr = {n: Buf(n) for n in ("OA", "QT", "MIXT", "PBlt", "PBke", "PBkd", "PBv", "PBee", "PBrg", "X1")}
    k.b_scr = b_scr

    KT = sb("KT", [128, 2, 34 * 128], BF16); b_KT = Buf()
    P.v("pool", "memset", KT[64:128], 0.0, writes=[b_KT])
    Vt = sb("Vt", [128, 34, 2, 66], BF16); b_Vt = Buf()
    P.v("pool", "memset", Vt[:], 1.0, writes=[b_Vt])
    mark = k.aoff
    k.swt = sb("swt", [128, 1024])
    w_in = sb("w_in", [128, 8, EVEN_IN], BF16); b_win = Buf()
    tri = sb("tri", [128, 4, 128]); maskAB = sb("maskAB", [128, 2, 256], BF16)
    P.dma(tri[:], g["tri_d"], writes=[b_const], eng="pool")
    P.dma(maskAB[:], g["maskAB_d"], writes=[b_const], eng="pool")
    gwext = sb("gwext", [33, 512]); P.dma(gwext[:], g["gwext_d"], writes=[b_const], eng="pool")
    mark_w = k.aoff
    k.wstage = [sb("wstage%d" % i, [128, 1168]) for i in range(2)]
    evin_d = g["evin_d"]
    for hf in range(2):
        lo_ = hf * 1168
        k.load_weight_bf16(w_in[:, :, lo_:lo_ + 1168], b_win,
                           lambda c, lo_=lo_: evin_d[c * 128:(c + 1) * 128, lo_:lo_ + 1168], 8, 1168, k.wstage, k.b_wstage)
    P.barrier()
    k.aoff = mark_w

    z_sb = sb("z_sb", [128, EVEN_IN]); b_z = Buf()
    ropeC = sb("ropeC", [128, 64]); ropeS = sb("ropeS", [128, 64]); b_rope = Buf()
    tmpA = sb("tmpA", [128, 1024]); b_tmpA = Buf()
    tmpB = sb("tmpB", [128, 1024]); b_tmpB = Buf()
    scr = sb("scr", [128, 64]); b_scr_ = Buf()
    qb = sb("qb", [128, 512], BF16); b_qb = Buf()
    kb = sb("kb", [128, 128], BF16); b_kb = Buf()
    qT_sb = sb("qT_sb", [64, 8, 128], BF16); b_qTsb = Buf()
    lrT = sb("lrT", [33, 128]); b_lrT = Buf()
    P.v("pool", "memset", lrT[:], 1.0, writes=[b_lrT])
    e_sb = sb("e_sb", [128, 512]); b_e = Buf()
    sp_ = sb("sp_", [128, 512]); b_sp = Buf()
    gqT = sb("gqT", [64, 4, 128]); gkT = sb("gkT", [64, 4, 128]); b_gq = Buf(); b_gk = Buf()
    EbT = [sb("EbT%d" % i, [64, 4, 128]) for i in range(2)]; b_EbT = [Buf(), Buf()]
    EnbT = [sb("EnbT%d" % i, [64, 4, 128]) for i in range(2)]; b_EnbT = [Buf(), Buf()]
    kdec = sb("kdec", [128, 256]); b_kdec = Buf()
    LT = [sb("LT%d" % i, [128, 2, 4, 64], BF16) for i in range(2)]
    b_LTq = [Buf(), Buf()]; b_LTa = [Buf(), Buf()]
    keT = [sb("keT%d" % i, [64, 4, 128], BF16) for i in range(2)]; b_keT = [Buf(), Buf()]
    kd = [sb("kd%d" % i, [128, 256], BF16) for i in range(2)]; b_kd = [Buf(), Buf()]
    v_bf = sb("v_bf", [128, 512], BF16); b_vbf = Buf()
    vhi = sb("vhi", [128, 2, 512], BF16); b_vhi = Buf()
    eend = [sb("eend%d" % i, [64, 4, 2]) for i in range(2)]; b_eend = [Buf(), Buf()]
    RH = [[sb("RH%d%d" % (i, j), [128, 4, 128], BF16) for j in range(2)] for i in range(2)]
    b_RHs = [[Buf(), Buf()], [Buf(), Buf()]]; b_RHv = [[Buf(), Buf()], [Buf(), Buf()]]
    S32 = [sb("S32_%d" % i, [64, 4, 128]) for i in range(2)]; b_S32 = [Buf(), Buf()]
    par = [0, 0]
    for d_ in range(2):
        P.v("pool", "memset", S32[d_][:], 0.0, writes=[b_S32[d_]])
        for j in range(2):
            P.v("pool", "memset", RH[d_][j][:], 0.0, writes=[b_RHs[d_][j], b_RHv[d_][j]])
    oa_sb = sb("oa_sb", [128, 512]); b_oa = Buf()
    rg_sb = sb("rg_sb", [128, 512]); b_rg = Buf()
    mixT_sb = sb("mixT_sb", [128, 8, 128], BF16); b_mixT = Buf()

    def mkset2():
        z2 = sb("z_sb2", [128, EVEN_IN])
        qb2 = sb("qb2", [128, 512], BF16); kb2 = sb("kb2", [128, 128], BF16); qT2 = sb("qT_sb2", [64, 8, 128], BF16)
        lrT2 = sb("lrT2", [33, 128]); bl2 = Buf()
        P.v("pool", "memset", lrT2[:], 1.0, writes=[bl2])
        gq2 = sb("gqT2", [64, 4, 128]); gk2 = sb("gkT2", [64, 4, 128])
        kdec2 = sb("kdec2", [128, 256])
        LT2 = [sb("LT2%d" % i, [128, 2, 4, 64], BF16) for i in range(2)]
        keT2 = [sb("keT2%d" % i, [64, 4, 128], BF16) for i in range(2)]
        kd2 = [sb("kd2%d" % i, [128, 256], BF16) for i in range(2)]
        v2 = sb("v_bf2", [128, 512], BF16); vh2 = sb("vhi2", [128, 2, 512], BF16)
        ee2 = [sb("eend2%d" % i, [64, 4, 2]) for i in range(2)]
        oa2 = sb("oa_sb2", [128, 512]); rg2 = sb("rg_sb2", [128, 512])
        return (z2, Buf(), qb2, Buf(), kb2, Buf(), qT2, Buf(), lrT2, bl2, gq2, gk2, Buf(), Buf(), kdec2, Buf(),
                LT2, [Buf(), Buf()], [Buf(), Buf()], keT2, [Buf(), Buf()], kd2, [Buf(), Buf()], v2, Buf(), vh2, Buf(),
                ee2, [Buf(), Buf()], oa2, Buf(), rg2, Buf())

    SETS = [(z_sb, b_z, qb, b_qb, kb, b_kb, qT_sb, b_qTsb, lrT, b_lrT, gqT, gkT, b_gq, b_gk, kdec, b_kdec,
             LT, b_LTq, b_LTa, keT, b_keT, kd, b_kd, v_bf, b_vbf, vhi, b_vhi, eend, b_eend, oa_sb, b_oa, rg_sb, b_rg),
            mkset2()]

    pMisc, bMisc = psum[3]
    pZG, bZG = psum[4]
    pBT, bBT = psum[5]
    pAT, bAT = psum[7][0], Buf("pAT")
    pS, bS = psum[7]
    pPO, bPO = psum[6]

    def scan_tile(d_, lt, b_ltq, b_lta, ke, b_ke, kdt, b_kdt, vb, b_vb, vh, b_vh, ee, b_ee, with_out):
        order = (0, 1) if d_ == 0 else (1, 0)
        pAT4 = pAT[:, 0:256].rearrange("p (h t) -> p h t", h=4)
        pS4 = pS[0:64, :].rearrange("p (h v) -> p h v", h=4)
        po, bpo = pPO, bPO
        for ch in order:
            p_ = par[d_]
            rh, brs, brv = RH[d_][p_], b_RHs[d_][p_], b_RHv[d_][p_]
            if with_out:
                for h in range(4):
                    P.mm(pAT4[64:128, h, :], ke[0:64, h, ch * 64:(ch + 1) * 64], lt[0:64, ch, h, :],
                         reads=[b_ke, b_ltq], writes=[bAT])
                P.v("dve", "tensor_tensor", lt[64:128, ch, :, :], pAT4[64:128, :, :],
                    maskAB[64:128, d_, :].rearrange("p (h t) -> p h t", h=4), ALU.mult,
                    reads=[bAT, b_const], writes=[b_lta])
                P.v("pool", "tensor_copy", rh[64:128, :, :], vh[64:128, ch, :].rearrange("p (h v) -> p h v", h=4),
                    reads=[b_vh], writes=[brv])
                for h in range(4):
                    P.mm(po[ch * 64:(ch + 1) * 64, h * 128:(h + 1) * 128], lt[:, ch, h, :], rh[:, h, :],
                         reads=[b_ltq, b_lta, brs, brv], writes=[bpo])
            for h in range(4):
                P.mm(pS4[:, h, :], kdt[ch * 64:(ch + 1) * 64, h * 64:(h + 1) * 64],
                     vb[ch * 64:(ch + 1) * 64, h * 128:(h + 1) * 128], reads=[b_kdt, b_vb], writes=[bS])
            for h in range(4):
                P.v("dve", "scalar_tensor_tensor", S32[d_][:, h, :], S32[d_][:, h, :], ee[:, h, ch:ch + 1],
                    pS4[:, h, :], ALU.mult, ALU.add, reads=[b_S32[d_], b_ee, bS], writes=[b_S32[d_]])
            nrh = RH[d_][1 - p_]
            P.act(nrh[0:64, :, :], S32[d_][:], AF.Copy, reads=[b_S32[d_]], writes=[b_RHs[d_][1 - p_]])
            par[d_] = 1 - p_

    def gla_prep(dirs, need_q):
        P.tr(pBT[0:32, 0:128], z_sb[:, 1536:1568], identF[:], reads=[b_z, b_const], writes=[bBT])
        P.act(lrT[0:32, :], pBT[0:32, 0:128], AF.Copy, reads=[bBT], writes=[b_lrT])
        P.mm(pZG[:], lrT[:], gwext[:], reads=[b_lrT, b_const], writes=[bZG])
        P.act(e_sb[:], pZG[:], AF.Exp, scale=-1.0, reads=[bZG], writes=[b_e])
        P.act(sp_[:], e_sb[:], AF.Ln, bias=1.0, reads=[b_e], writes=[b_sp])
        pBT4 = pBT[0:64, :].rearrange("p (h t) -> p h t", h=4)
        if need_q:
            for h in range(4):
                P.tr(pBT4[:, h, :], z_sb[:, h * 64:(h + 1) * 64], identF[:], reads=[b_z, b_const], writes=[bBT])
            P.act(gqT[:], pBT4, AF.Copy, reads=[bBT], writes=[b_gq])
        for h in range(4):
            P.tr(pBT4[:, h, :], z_sb[:, 256 + h * 64:256 + (h + 1) * 64], identF[:], reads=[b_z, b_const], writes=[bBT])
        P.act(gkT[:], pBT4, AF.Copy, reads=[bBT], writes=[b_gk])
        P.v("pool", "tensor_copy", v_bf[:], z_sb[:, 512:1024], reads=[b_z], writes=[b_vbf])
        P.dma(vhi[64:128, 0, :], v_bf[0:64, :], reads=[b_vbf], writes=[b_vhi], eng="pool")
        P.v("pool", "tensor_copy", vhi[64:128, 1, :], v_bf[64:128, :], reads=[b_vbf], writes=[b_vhi])
        for d_ in dirs:
            for h in range(4):
                P.mm(pBT4[:, h, :], sp_[:, d_ * 256 + h * 64:d_ * 256 + (h + 1) * 64], tri[:, d_, :],
                     reads=[b_sp, b_const], writes=[bBT])
            P.act(EbT[d_][:], pBT4, AF.Exp, scale=-1.0 / 16, reads=[bBT], writes=[b_EbT[d_]])
            P.act(EnbT[d_][:], pBT4, AF.Exp, scale=1.0 / 16, reads=[bBT], writes=[b_EnbT[d_]])
            P.mm(pZG[:, 0:256], tri[:, 2 + d_, :], sp_[:, d_ * 256:(d_ + 1) * 256], reads=[b_sp, b_const], writes=[bZG])
            P.act(kdec[:], pZG[:, 0:256], AF.Exp, scale=-1.0 / 16, reads=[bZG], writes=[b_kdec])
            if need_q:
                for ch in range(2):
                    P.v("dve", "scalar_tensor_tensor", LT[d_][0:64, ch, :, :],
                        gqT[:, :, ch * 64:(ch + 1) * 64], 0.125,
                        EbT[d_][:, :, ch * 64:(ch + 1) * 64], ALU.mult, ALU.mult,
                        reads=[b_gq, b_EbT[d_]], writes=[b_LTq[d_]])
            P.v("dve", "tensor_tensor", keT[d_][:], gkT[:], EnbT[d_][:], ALU.mult,
                reads=[b_gk, b_EnbT[d_]], writes=[b_keT[d_]])
            P.v("dve", "tensor_tensor", kd[d_][:], z_sb[:, 256:512], kdec[:], ALU.mult,
                reads=[b_z, b_kdec], writes=[b_kd[d_]])
            cols = (63, 127) if d_ == 0 else (0, 64)
            for ch in range(2):
                P.v("dve", "tensor_copy", eend[d_][:, :, ch:ch + 1], EbT[d_][:, :, cols[ch]:cols[ch] + 1],
                    reads=[b_EbT[d_]], writes=[b_eend[d_]])

    def inproj(H, bH, blocks):
        for bi, (lo, hi) in enumerate(blocks):
            pz, bz = psum[1 + bi % 2]
            for c in range(8):
                P.mm(pz[:, 0:hi - lo], H[:, c, :], w_in[:, c, lo:hi], c == 0, c == 7, reads=[bH, b_win], writes=[bz])
            if bi % 2 == 0:
                P.act(z_sb[:, lo:hi], pz[:, 0:hi - lo], AF.Copy, reads=[bz], writes=[b_z])
            else:
                P.v("dve", "tensor_copy", z_sb[:, lo:hi], pz[:, 0:hi - lo], reads=[bz], writes=[b_z])

    FULL = ((0, 512), (512, 1024), (1024, 1536), (1536, 2048), (2048, 2336))
    RBLK = ((256, 768), (768, 1024), (1536, 1568), (2080, 2336))

    def modA(kind, ti):
        if kind == "ctx":
            return modulate(ctx_d[ti * 128:(ti + 1) * 128, :], 0, 0, 1)
        return modulate(x_d[ti * 128:(ti + 1) * 128, :], 0, 0, 0)

    def tileA(kind, ti, pre):
        if kind == "ctx":
            src, w, slot, kidx = ctx_d[ti * 128:(ti + 1) * 128, :], 1, NOWN + ti, ti
        else:
            src, w, slot, kidx = x_d[ti * 128:(ti + 1) * 128, :], 0, ti, 2 + ti
        H, bH, X, bX = pre
        inproj(H, bH, RBLK if kind == "R" else FULL)
        rope = None
        if kind != "ctx":
            P.dma(ropeC[:], g["ropeC_d"][ti * 128:(ti + 1) * 128, :], writes=[b_rope], eng="pool")
            P.dma(ropeS[:], g["ropeS_d"][ti * 128:(ti + 1) * 128, :], writes=[b_rope], eng="pool")
            rope = (ropeC, ropeS, b_rope, tmpB, b_tmpB)
        pM8 = pMisc[0:64, :].bitcast(BF16).rearrange("p (h t) -> p h t", h=8)
        if kind != "R":
            k.head_norm_rope(qb[:], b_qb, z_sb[:, 1568:2080], b_z, 8, aqn, rope, tmpA, b_tmpA, scr, b_scr_)
            for h in range(8):
                P.tr(pM8[:, h, :], qb[:, h * 64:(h + 1) * 64], identB[:], reads=[b_qb, b_const], writes=[bMisc])
            P.act(qT_sb[:], pM8, AF.Copy, reads=[bMisc], writes=[b_qTsb])
            P.dma(QT_d[slot], qT_sb[:], reads=[b_qTsb], writes=[b_scr["QT"]])
        k.head_norm_rope(kb[:], b_kb, z_sb[:, 2080:2208], b_z, 2, akn, rope, tmpA, b_tmpA, scr, b_scr_)
        for h in range(2):
            P.tr(pM8[:, h, :], kb[:, h * 64:(h + 1) * 64], identB[:], reads=[b_kb, b_const], writes=[bMisc])
        P.act(KT[0:64, :, kidx * 128:(kidx + 1) * 128], pM8[:, 0:2, :], AF.Copy, reads=[bMisc], writes=[b_KT])
        P.v("pool", "tensor_copy", Vt[:, kidx, :, 0:64], z_sb[:, 2208:2336].rearrange("p (h d) -> p h d", h=2),
            reads=[b_z], writes=[b_Vt])
        if kind == "R":
            gla_prep((1,), False)
            scan_tile(1, LT[1], b_LTq[1], b_LTa[1], keT[1], b_keT[1], kd[1], b_kd[1], v_bf, b_vbf, vhi, b_vhi,
                      eend[1], b_eend[1], False)
            return
        gla_prep((0, 1), True)
        P.act(rg_sb[:], z_sb[:, 1024:1536], AF.Exp, scale=-1.0, reads=[b_z], writes=[b_rg])
        P.act(rg_sb[:], rg_sb[:], AF.Ln, bias=1.0, reads=[b_rg], writes=[b_rg])
        P.act(rg_sb[:], rg_sb[:], AF.Exp, scale=-1.0, reads=[b_rg], writes=[b_rg])
        P.v("pool", "tensor_tensor", rg_sb[:].rearrange("p (h v) -> p h v", h=4), rg_sb[:].rearrange("p (h v) -> p h v", h=4),
            glan[:, 0:128].unsqueeze(1).to_broadcast([128, 4, 128]), ALU.mult, reads=[b_rg, b_const], writes=[b_rg])
        P.v("pool", "tensor_tensor", rg_sb[:], rg_sb[:], z_sb[:, 1024:1536], ALU.mult, reads=[b_rg, b_z], writes=[b_rg])
        P.dma(PB_rg[slot], rg_sb[:], reads=[b_rg], writes=[b_scr["PBrg"]])
        P.dma(PB_lt[slot], LT[1][0:64].rearrange("p c h t -> p (c h t)"), reads=[b_LTq[1]], writes=[b_scr["PBlt"]])
        P.dma(PB_ke[slot], keT[1][:].rearrange("p h t -> p (h t)"), reads=[b_keT[1]], writes=[b_scr["PBke"]])
        P.dma(PB_kd[slot], kd[1][:], reads=[b_kd[1]], writes=[b_scr["PBkd"]])
        P.dma(PB_v[slot], v_bf[:], reads=[b_vbf], writes=[b_scr["PBv"]])
        P.dma(PB_ee[slot], eend[1][:].rearrange("p h c -> p (h c)"), reads=[b_eend[1]], writes=[b_scr["PBee"]])
        scan_tile(0, LT[0], b_LTq[0], b_LTa[0], keT[0], b_keT[0], kd[0], b_kd[0], v_bf, b_vbf, vhi, b_vhi,
                  eend[0], b_eend[0], True)
        P.act(oa_sb[:], pPO[:], AF.Copy, reads=[bPO], writes=[b_oa])
        P.dma(OA_d[slot], oa_sb[:], reads=[b_oa], writes=[b_scr["OA"]])

    ob_sb = tmpB[:, 0:512]; b_ob = b_tmpB
    ab_sb = tmpB[:, 512:768].bitcast(BF16); b_ab = Buf()

    def tileB(slot):
        P.dma(LT[1][0:64].rearrange("p c h t -> p (c h t)"), PB_lt[slot], reads=[b_scr["PBlt"]], writes=[b_LTq[1]])
        P.dma(keT[1][:].rearrange("p h t -> p (h t)"), PB_ke[slot], reads=[b_scr["PBke"]], writes=[b_keT[1]])
        P.dma(kd[1][:], PB_kd[slot], reads=[b_scr["PBkd"]], writes=[b_kd[1]])
        P.dma(v_bf[:], PB_v[slot], reads=[b_scr["PBv"]], writes=[b_vbf])
        P.dma(vhi[64:128, 0, :], PB_v[slot][0:64, :], reads=[b_scr["PBv"]], writes=[b_vhi], eng="pool")
        P.dma(vhi[64:128, 1, :], PB_v[slot][64:128, :], reads=[b_scr["PBv"]], writes=[b_vhi], eng="pool")
        P.dma(eend[1][:].rearrange("p h c -> p (h c)"), PB_ee[slot], reads=[b_scr["PBee"]], writes=[b_eend[1]])
        P.dma(oa_sb[:], OA_d[slot], reads=[b_scr["OA"]], writes=[b_oa], eng="pool")
        P.dma(rg_sb[:], PB_rg[slot], reads=[b_scr["PBrg"]], writes=[b_rg], eng="pool")
        scan_tile(1, LT[1], b_LTq[1], b_LTa[1], keT[1], b_keT[1], kd[1], b_kd[1], v_bf, b_vbf, vhi, b_vhi,
                  eend[1], b_eend[1], True)
        P.v("dve", "tensor_tensor", ob_sb[:], pPO[:], oa_sb[:], ALU.add, reads=[bPO, b_oa], writes=[b_ob])
        o3 = ob_sb[:].rearrange("p (h v) -> p h v", h=4)
        t3 = tmpA[:, 0:512].rearrange("p (h v) -> p h v", h=4)
        P.v("dve", "tensor_tensor", t3, o3, o3, ALU.mult, reads=[b_ob], writes=[b_tmpA])
        P.v("dve", "tensor_reduce", scr[:, 0:4], t3, AX.X, ALU.add, reads=[b_tmpA], writes=[b_scr_])
        P.act(scr[:, 16:20], scr[:, 0:4], AF.Ln, scale=1.0 / 128, bias=1e-6, reads=[b_scr_], writes=[b_scr_])
        P.act(scr[:, 32:36], scr[:, 16:20], AF.Exp, scale=-0.5, reads=[b_scr_], writes=[b_scr_])
        P.v("dve", "tensor_tensor", t3, o3, scr[:, 32:36].unsqueeze(2).to_broadcast([128, 4, 128]), ALU.mult,
            reads=[b_ob, b_scr_], writes=[b_tmpA])
        P.v("dve", "tensor_tensor", ab_sb[:], tmpA[:, 0:512], rg_sb[:], ALU.mult, reads=[b_tmpA, b_rg], writes=[b_ab])
        pM4 = pMisc[:].bitcast(BF16)[:, 0:512].rearrange("p (c t) -> p c t", c=4)
        for c in range(4):
            P.tr(pM4[:, c, :], ab_sb[:, c * 128:(c + 1) * 128], identB[:], reads=[b_ab, b_const], writes=[bMisc])
        P.act(mixT_sb[:, 0:4, :], pM4, AF.Copy, reads=[bMisc], writes=[b_mixT])
        P.dma(MIXT_d[slot][:, 0:4, :], mixT_sb[:, 0:4, :], reads=[b_mixT], writes=[b_scr["MIXT"]])

    seqA = [("ctx", 0), ("ctx", 1)] + [("R", ti) for ti in range(31, NOWN - 1, -1)] + [("own", ti) for ti in range(NOWN)]
    pre = modA(*seqA[0])
    nset = 0
    for si, (kind, ti) in enumerate(seqA):
        nxt = modA(*seqA[si + 1]) if si + 1 < len(seqA) else None
        (z_sb, b_z, qb, b_qb, kb, b_kb, qT_sb, b_qTsb, lrT, b_lrT, gqT, gkT, b_gq, b_gk, kdec, b_kdec,
         LT, b_LTq, b_LTa, keT, b_keT, kd, b_kd, v_bf, b_vbf, vhi, b_vhi, eend, b_eend, oa_sb, b_oa, rg_sb, b_rg) = SETS[nset % 2]
        nset += 1
        tileA(kind, ti, pre)
        pre = nxt
        if (kind, ti) == ("ctx", 1):
            for sl in (NOWN + 1, NOWN + 0):
                (z_sb, b_z, qb, b_qb, kb, b_kb, qT_sb, b_qTsb, lrT, b_lrT, gqT, gkT, b_gq, b_gk, kdec, b_kdec,
                 LT, b_LTq, b_LTa, keT, b_keT, kd, b_kd, v_bf, b_vbf, vhi, b_vhi, eend, b_eend, oa_sb, b_oa, rg_sb, b_rg) = SETS[nset % 2]
                nset += 1
                tileB(sl)
    for ti in range(NOWN - 1, -1, -1):
        (z_sb, b_z, qb, b_qb, kb, b_kb, qT_sb, b_qTsb, lrT, b_lrT, gqT, gkT, b_gq, b_gk, kdec, b_kdec,
         LT, b_LTq, b_LTa, keT, b_keT, kd, b_kd, v_bf, b_vbf, vhi, b_vhi, eend, b_eend, oa_sb, b_oa, rg_sb, b_rg) = SETS[nset % 2]
        nset += 1
        tileB(ti)

    if "stopAB" in dbg:
        return
    k.areset(mark)
    k.wstage = [sb("wstage%d" % i, [128, D]) for i in range(2)]
    w_out = sb("w_out", [128, 4, D], BF16); b_wout = Buf()
    w_outB = sb("w_outB", [64, 8, D], BF16); b_woutB = Buf()
    evout_d = g["evout_d"]
    k.load_weight_bf16(w_out, b_wout, lambda c: evout_d[c * 128:(c + 1) * 128, :], 4, D, k.wstage, k.b_wstage)
    for h in range(8):
        s_, bs_ = k.wstage[h % 2], k.b_wstage[h % 2]
        P.dma(s_[0:64, :], evout_d[512 + h * 64:512 + (h + 1) * 64, :], writes=[bs_], eng=("sp" if h % 2 == 0 else "pool"))
        k.cast(("pool", "dve", "act")[h % 3], w_outB[:, h, :], s_[0:64, :], [bs_], [b_woutB])
    ones_f = sb("ones_f", [128, 64]); P.v("pool", "memset", ones_f[:], 1.0, writes=[b_const])
    qT_in = [sb("qT_in%d" % i, [128, 8, 128], BF16) for i in range(2)]; b_qTin = [Buf(), Buf()]
    for i_ in range(2):
        P.v("pool", "memset", qT_in[i_][64:128], 0.0, writes=[b_qTin[i_]])
    rsr = sb("rsr", [128, 512]); b_rsr = Buf()
    bc_sb = sb("bc_sb", [64, 512]); b_bc = Buf()
    OT = sb("OT", [64, 8, 128], BF16); b_OT = Buf()
    mT_in = sb("mT_in", [128, 4, 128], BF16); b_mTin = Buf()
    x1_sb = sb("x1_sb", [128, D]); b_x1 = Buf()
    pO = [psum[6], psum[7]]
    SG = [(k.psall[:, 2 * g_ * 512:(2 * g_ + 2) * 512], [psum[2 * g_][1], psum[2 * g_ + 1][1]]) for g_ in range(3)]
    pBC, bBC = psum[0]
    NPT = 4
    PT = [sb("PTp%d" % i, [128, 1024], BF16) for i in range(NPT)]; b_PT = [Buf() for _ in range(NPT)]
    cstate = dict(it=0)

    mT_ins = [mT_in, sb("mT_in2", [128, 4, 128], BF16)]; b_mTins = [b_mTin, Buf()]
    x_ins = [sb("xC%d" % i, [128, D]) for i in range(2)]; b_xins = [Buf(), Buf()]
    ones_b = sb("ones_b", [128, 64], BF16); P.v("pool", "memset", ones_b[:], 1.0, writes=[b_const])
    rsb = sb("rsb", [128, 2, 512], BF16); b_rsb = Buf()
    bcs = [sb("bcs%d" % i, [64, 512]) for i in range(2)]; b_bcs = [Buf(), Buf()]
    cpre = dict(n=0)

    def loadC(kind, ti):
        if kind == "ctx":
            slot, src = NOWN + ti, ctx_d[ti * 128:(ti + 1) * 128, :]
        else:
            slot, src = ti, x_d[ti * 128:(ti + 1) * 128, :]
        i = cpre["n"] % 2
        cpre["n"] += 1
        P.dma(qT_in[i][0:64], QT_d[slot], reads=[b_scr["QT"]], writes=[b_qTin[i]])
        P.dma(mT_ins[i][:], MIXT_d[slot][:, 0:4, :], reads=[b_scr["MIXT"]], writes=[b_mTins[i]], eng="pool")
        P.dma(x_ins[i][:], src, writes=[b_xins[i]])
        return (qT_in[i], b_qTin[i], mT_ins[i], b_mTins[i], x_ins[i], b_xins[i])

    def tileC(kind, ti, pre):
        if kind == "ctx":
            slot, w, src, keys = NOWN + ti, 1, ctx_d[ti * 128:(ti + 1) * 128, :], [0, 1]
        else:
            slot, w, src, keys = ti, 0, x_d[ti * 128:(ti + 1) * 128, :], list(range(34))
        Q, bQ, mT_in, b_mTin, X, bX = pre
        units = [(kvh, keys[a_:a_ + 2]) for kvh in range(2) for a_ in range(0, len(keys), 2)]
        pend = []

        def pv(u, pt, bpt):
            kvh, kts = u
            po, bpo = pO[kvh]
            for j, kt in enumerate(kts):
                P.mm(po[0:65, :], Vt[:, kt, kvh, 0:65], pt[:, j * 512:(j + 1) * 512], kt == keys[0], kt == keys[-1],
                     reads=[bpt, b_Vt], writes=[bpo])

        for u in units:
            kvh, kts = u
            it = cstate["it"]
            cstate["it"] += 1
            psc, bscs = SG[it % 3]
            pt, bpt = PT[it % NPT], b_PT[it % NPT]
            n = len(kts)
            for j, kt in enumerate(kts):
                P.mm(psc[:, j * 512:(j + 1) * 512], KT[:, kvh, kt * 128:(kt + 1) * 128],
                     Q[:, kvh * 4:(kvh + 1) * 4, :].rearrange("p h t -> p (h t)"),
                     reads=[b_KT, bQ], writes=[bscs[j]])
            P.act(pt[:, 0:n * 512], psc[:, 0:n * 512], AF.Exp, scale=0.125, bias=bnd[:, 2:3],
                  reads=bscs[0:n] + [b_bnd], writes=[bpt])
            pend.append((u, pt, bpt))
            if len(pend) > 2:
                pv(*pend.pop(0))
        while pend:
            pv(*pend.pop(0))
        for kvh in range(2):
            po, bpo = pO[kvh]
            P.act(rsr[64:65, :], po[64:65, :], AF.Ln, reads=[bpo], writes=[b_rsr])
            P.act(rsb[64:65, kvh, :], rsr[64:65, :], AF.Exp, scale=-1.0, reads=[b_rsr], writes=[b_rsb])
        for kvh in range(2):
            pb_, bb_ = psum[2 + kvh]
            P.mm(pb_[0:64, :], ones_b[64:65, 0:64], rsb[64:65, kvh, :], reads=[b_const, b_rsb], writes=[bb_])
        for kvh in range(2):
            pb_, bb_ = psum[2 + kvh]
            P.v("dve", "tensor_copy", bcs[kvh][:], pb_[0:64, :], reads=[bb_], writes=[b_bcs[kvh]])
        for kvh in range(2):
            po, bpo = pO[kvh]
            P.v("dve", "tensor_tensor", OT[:, kvh * 4:(kvh + 1) * 4, :].rearrange("p h t -> p (h t)"), po[0:64, :], bcs[kvh][:],
                ALU.mult, reads=[bpo, b_bcs[kvh]], writes=[b_OT])
        for hh in range(2):
            pz, bz = psum[hh]
            for c in range(4):
                P.mm(pz[:], mT_in[:, c, :], w_out[:, c, hh * 512:(hh + 1) * 512], c == 0, False,
                     reads=[b_mTin, b_wout], writes=[bz])
            for h in range(8):
                P.mm(pz[:], OT[:, h, :], w_outB[:, h, hh * 512:(hh + 1) * 512], False, h == 7,
                     reads=[b_OT, b_woutB], writes=[bz])
            P.v("dve", "tensor_tensor", x1_sb[:, hh * 512:(hh + 1) * 512], pz[:],
                gateB_all[:, k.gslot[(0, 0, w)], hh * 512:(hh + 1) * 512],
                ALU.mult, reads=[bz, b_gateB[0]], writes=[b_x1])
        P.v("pool", "tensor_tensor", x1_sb[:], x1_sb[:], X[:], ALU.add, reads=[b_x1, bX], writes=[b_x1])
        P.dma(X1_d[slot * 128:(slot + 1) * 128, :], x1_sb[:], reads=[b_x1], writes=[b_scr["X1"]])

    tiles_c = [("ctx", 0), ("ctx", 1)] + [("own", t) for t in range(NOWN)]
    if "fewC" in dbg:
        tiles_c = [("ctx", 0), ("own", 0), ("own", 16)]
    s1 = k.s0_steps(1, "L1", psum[0], psum[0], psum[1])
    preC = loadC(*tiles_c[0])
    for si, (kind, ti) in enumerate(tiles_c):
        nxtC = loadC(*tiles_c[si + 1]) if si + 1 < len(tiles_c) else None
        if s1:
            s1.pop(0)()
        tileC(kind, ti, preC)
        preC = nxtC
    while s1:
        s1.pop(0)()
    if "stopL0mix" in dbg:
        return
    X2_d, X3_d, out_d = g["X2_d"], g["X3_d"], g["out_d"]
    b_scr["X2"] = Buf("X2"); b_scr["X3"] = Buf("X3")
    tiles = []
    for slot in range(NTOK0):
        w = 1 if slot >= NOWN else 0
        tiles.append((X1_d[slot * 128:(slot + 1) * 128, :], b_scr["X1"], X2_d[slot * 128:(slot + 1) * 128, :], b_scr["X2"], w))
    if "fewM" in dbg:
        tiles = [tiles[0], tiles[16], tiles[17]]
    moe(k, 0, tiles, "a")
    if "stopL0" in dbg or "stopD1" in dbg or "stopD2" in dbg:
        return
    layer1(k, dbg)
    if "stopL1" in dbg:
        return
    tiles = [(X3_d[t * 128:(t + 1) * 128, :], b_scr["X3"], out_d[t * 128:(t + 1) * 128, :], None, 0) for t in range(16)]
    moe(k, 1, tiles, "b")


def moe(k, l, tiles, tag):
    g = k.env
    nc, P = k.nc, k.P
    ar, psum = k.ar, k.psum
    identF, b_const = g["identF"], g["b_const"]
    NT = len(tiles)
    NTOK = NT * 128
    k.areset(0)
    hT_all = ar("hT_all" + tag, [128, 8, NTOK], BF16); b_hTall = Buf()
    yacc = ar("yacc" + tag, [128, NT, D]); b_yacc = [Buf() for _ in range(NT)]
    comb_all = ar("comb" + tag, [128, NT, 16]); b_combt = [Buf() for _ in range(NT)]
    mark = k.aoff
    k.xnf = ar("xnf" + tag, [128, D]); k.hTf = ar("hTf" + tag, [128, 8, 128])
    rw = ar("rw" + tag, [128, 8, 20]); rb = ar("rb" + tag, [128, 20]); b_rw = Buf()
    P.dma(rw[:], g["rw_d"][l].rearrange("(c p) n -> p c n", p=128), writes=[b_rw], eng="pool")
    P.dma(rb[:], g["rb_d"][l].partition_broadcast(128).rearrange("p o n -> p (o n)"), writes=[b_rw], eng="pool")
    lg = ar("lg" + tag, [128, 20]); b_lg = Buf()
    rt = ar("rt" + tag, [128, 64]); b_rt = Buf()
    pR, bR = psum[2]
    b_hTt = [Buf() for _ in range(NT)]

    def d1(ti):
        (src, sbuf_, dst, dbuf_, w) = tiles[ti]
        H, bH, X, bX = k.modulate(src, l, 1, w, fp32=True, src_buf=sbuf_)
        P.v("pool", "tensor_copy", hT_all[:, :, ti * 128:(ti + 1) * 128], H[:], reads=[bH], writes=[b_hTt[ti]])
        for c_ in range(8):
            P.mm(pR[:, 0:20], H[:, c_, :], rw[:, c_, :], c_ == 0, c_ == 7, reads=[bH, b_rw], writes=[bR])
        P.v("dve", "tensor_tensor", lg[:], pR[:, 0:20], rb[:], ALU.add, reads=[bR, b_rw], writes=[b_lg])
        gl, el = lg[:, 0:4], lg[:, 4:20]
        R_ = lambda a, b: rt[:, a:b]
        gmax, ngmax, sume, gw = R_(0, 1), R_(1, 2), R_(2, 3), R_(3, 4)
        ohg, eg, esel, oh1, msk, oh2, ew, sg = R_(4, 8), R_(8, 12), R_(12, 16), R_(16, 20), R_(20, 24), R_(24, 28), R_(28, 32), R_(32, 36)
        m1, m2, dd, e2, w1, w2 = R_(36, 37), R_(37, 38), R_(38, 39), R_(39, 40), R_(40, 41), R_(41, 42)
        rd, wr = [b_lg, b_rt], [b_rt]
        V = lambda name, *a, **kw: P.v("dve", name, *a, reads=rd, writes=wr, **kw)
        V("tensor_reduce", gmax, gl, AX.X, ALU.max)
        V("tensor_scalar", ohg, gl, gmax, None, ALU.is_equal)
        V("tensor_scalar", ngmax, gmax, -1.0, None, ALU.mult)
        P.act(eg, gl, AF.Exp, bias=ngmax, accum_out=sume, reads=rd, writes=wr)
        V("reciprocal", gw, sume)
        V("tensor_scalar", esel, el[:, 0:4], ohg[:, 0:1], None, ALU.mult)
        for gi in range(1, 4):
            V("scalar_tensor_tensor", esel, el[:, gi * 4:(gi + 1) * 4], ohg[:, gi:gi + 1], esel, ALU.mult, ALU.add)
        V("tensor_reduce", m1, esel, AX.X, ALU.max)
        V("tensor_scalar", oh1, esel, m1, None, ALU.is_equal)
        V("scalar_tensor_tensor", msk, oh1, -NEG_BIG, esel, ALU.mult, ALU.add)
        V("tensor_reduce", m2, msk, AX.X, ALU.max)
        V("tensor_scalar", oh2, msk, m2, None, ALU.is_equal)
        V("tensor_tensor", dd, m2, m1, ALU.subtract)
        P.act(e2, dd, AF.Exp, reads=rd, writes=wr)
        V("tensor_scalar", w1, e2, 1.0, None, ALU.add)
        V("reciprocal", w1, w1)
        V("tensor_tensor", w2, e2, w1, ALU.mult)
        V("tensor_scalar", ew, oh1, w1, None, ALU.mult)
        V("scalar_tensor_tensor", ew, oh2, w2, ew, ALU.mult, ALU.add)
        V("tensor_scalar", sg, ohg, gw, None, ALU.mult)
        for gi in range(4):
            P.v("dve", "tensor_scalar", comb_all[:, ti, gi * 4:(gi + 1) * 4], ew, sg[:, gi:gi + 1], None, ALU.mult,
                reads=[b_rt], writes=[b_combt[ti]])
    wst = [ar("mwst%d" % i + tag, [128, 512]) for i in range(2)]; b_wst = [Buf(), Buf()]
    Wg = [ar("Wg%d" % i + tag, [128, 8, 256], BF16) for i in range(2)]
    Wu = [ar("Wu%d" % i + tag, [128, 8, 256], BF16) for i in range(2)]
    Wd = [ar("Wd%d" % i + tag, [128, 2, D], BF16) for i in range(2)]
    b_W = [[Buf(), Buf(), Buf()] for _ in range(2)]
    sa = [ar("sa%d" % i + tag, [128, 512]) for i in range(2)]; b_sa = [Buf(), Buf()]
    hid = [ar("hid%d" % i + tag, [128, 2, 512], BF16) for i in range(2)]; b_hid = [Buf(), Buf()]
    pCW, bCW = psum[0]
    pAs = [psum[1], psum[2]]
    pUs = [psum[3], psum[4]]
    pYs = [psum[5], psum[6], psum[0]]
    wcnt = 0
    blocks = [(s, min(512, NTOK - s)) for s in range(0, NTOK, 512)]
    it = 0
    yi = 0
    def load_w(e):
        nonlocal wcnt
        pe_ = e % 2
        srcs = (g["wg_d"][l, e].rearrange("(c p) f -> p c f", p=128), g["wu_d"][l, e].rearrange("(c p) f -> p c f", p=128),
                g["wd_d"][l, e].rearrange("(c p) n -> p c n", p=128))
        dsts = (Wg[pe_], Wu[pe_], Wd[pe_])
        for wi in range(3):
            for q4 in range(4):
                s_, bs_ = wst[wcnt % 2], b_wst[wcnt % 2]
                if wi < 2:
                    sv = s_[:].rearrange("p (c f) -> p c f", c=2)
                    sview = srcs[wi][:, 2 * q4:2 * q4 + 2, :]
                    dview = dsts[wi][:, 2 * q4:2 * q4 + 2, :]
                else:
                    sv = s_[:]
                    sview = srcs[wi][:, q4 // 2, (q4 % 2) * 512:(q4 % 2 + 1) * 512]
                    dview = dsts[wi][:, q4 // 2, (q4 % 2) * 512:(q4 % 2 + 1) * 512]
                P.dma(sv, sview, writes=[bs_], eng=("sp" if wcnt % 2 == 0 else "pool"))
                P.v("pool", "tensor_copy", dview, sv, reads=[bs_], writes=[b_W[pe_][wi]])
                wcnt += 1

    def gu(e, t0, n, i):
        pe_ = e % 2
        for fc in range(2):
            pa, ba = pAs[fc]
            pu, bu = pUs[fc]
            for c_ in range(8):
                P.mm(pa[:, 0:n], Wg[pe_][:, c_, fc * 128:(fc + 1) * 128], hT_all[:, c_, t0:t0 + n], c_ == 0, c_ == 7,
                     reads=[b_W[pe_][0]] + b_hTt[t0 // 128:(t0 + n) // 128], writes=[ba])
            for c_ in range(8):
                P.mm(pu[:, 0:n], Wu[pe_][:, c_, fc * 128:(fc + 1) * 128], hT_all[:, c_, t0:t0 + n], c_ == 0, c_ == 7,
                     reads=[b_W[pe_][1]] + b_hTt[t0 // 128:(t0 + n) // 128], writes=[bu])
            P.act(sa[fc][:, 0:n], pa[:, 0:n], AF.Silu, reads=[ba], writes=[b_sa[fc]])
            P.v("dve", "tensor_tensor", hid[i][:, fc, 0:n], sa[fc][:, 0:n], pu[:, 0:n], ALU.mult,
                reads=[b_sa[fc], bu], writes=[b_hid[i]])

    def dn(e, t0, n, i):
        nonlocal yi
        pe_ = e % 2
        ntile = n // 128
        for j in range(ntile):
            tile_i = t0 // 128 + j
            for dh in range(2):
                py, by = pYs[yi % 3]
                yi += 1
                for fc in range(2):
                    P.mm(py[:], hid[i][:, fc, j * 128:(j + 1) * 128], Wd[pe_][:, fc, dh * 512:(dh + 1) * 512],
                         fc == 0, fc == 1, reads=[b_hid[i], b_W[pe_][2]], writes=[by])
                ya = yacc[:, tile_i, dh * 512:(dh + 1) * 512]
                cs = comb_all[:, tile_i, e:e + 1]
                if e == 0:
                    P.v("dve", "tensor_scalar", ya, py[:], cs, None, ALU.mult, reads=[by, b_combt[tile_i]], writes=[b_yacc[tile_i]])
                else:
                    P.v("dve", "scalar_tensor_tensor", ya, py[:], cs, ya, ALU.mult, ALU.add,
                        reads=[by, b_combt[tile_i], b_yacc[tile_i]], writes=[b_yacc[tile_i]])

    items = [(e, t0, n) for e in range(16) for (t0, n) in blocks]
    pend = None
    last_e = -1
    for idx, (e, t0, n) in enumerate(items):
        if e != last_e:
            if e == 0:
                load_w(0)
            if e + 1 < 16:
                pass
            last_e = e
        if e == 0:
            for tq in range(t0 // 128, (t0 + n) // 128):
                d1(tq)
        gu(e, t0, n, idx % 2)
        if pend is not None:
            dn(*pend)
        pend = (e, t0, n, idx % 2)
        if t0 == blocks[0][0] and e + 1 < 16:
            load_w(e + 1)
    dn(*pend)
    xo = [k.xnf, k.hTf[:].rearrange("p c t -> p (c t)")]; b_xo = [k.env["b_xnf"], k.env["b_hTf"]]
    for ti, (src, sbuf_, dst, dbuf_, w) in enumerate(tiles):
        i = ti % 2
        X, bX = g["xt"][i], g["b_xt"][i]
        P.dma(X[:], src, reads=([sbuf_] if sbuf_ else []), writes=[bX], eng="pool")
        gs = k.gslot[(l, 1, w)]
        P.v("dve", "tensor_tensor", xo[i][:], yacc[:, ti, :], k.gateB_all[:, gs, :], ALU.mult,
            reads=[b_yacc[ti], k.b_gateB[l]], writes=[b_xo[i]])
        P.v("pool", "tensor_tensor", xo[i][:], xo[i][:], X[:], ALU.add, reads=[b_xo[i], bX], writes=[b_xo[i]])
        P.dma(dst, xo[i][:], reads=[b_xo[i]], writes=([dbuf_] if dbuf_ else []))


def layer1(k, dbg):
    g = k.env
    nc, P = k.nc, k.P
    ar, psum = k.ar, k.psum
    identF, identB, b_const = g["identF"], g["identB"], g["b_const"]
    X2_d, X3_d, QT1_d = g["X2_d"], g["X3_d"], g["QT1_d"]
    bnd, b_bnd = k.bnd, k.b_bnd
    sqn, skn, sinkb = g["sqn"], g["skn"], g["sinkb"]
    b_X2, b_X3 = k.b_scr["X2"], k.b_scr["X3"]
    b_QT1 = Buf()
    k.areset(0)
    NK = 19
    KT = ar("KT1", [128, 2, NK * 128], BF16); b_KT = Buf()
    P.v("pool", "memset", KT[64:128], 0.0, writes=[b_KT])
    Vt = ar("Vt1", [128, NK, 2, 66], BF16); b_Vt = Buf()
    P.v("pool", "memset", Vt[:], 1.0, writes=[b_Vt])
    wmask = ar("wmask", [128, 2, 128], BF16)
    P.dma(wmask[:], g["wmask_d"], writes=[b_const], eng="pool")
    k.wstage = [ar("wstage1%d" % i, [128, 1280]) for i in range(2)]
    k.swt = ar("swt1", [128, 1024])
    w_in = ar("w_in1", [128, 8, 1280], BF16); b_win = Buf()
    odin_d, odout_d = g["odin_d"], g["odout_d"]
    k.load_weight_bf16(w_in, b_win, lambda c: odin_d[c * 128:(c + 1) * 128, :], 8, 1280, k.wstage, k.b_wstage)
    w_outB = ar("w_out1B", [64, 16, D], BF16); b_woutB = Buf()
    for h in range(16):
        s_, bs_ = k.wstage[h % 2], k.b_wstage[h % 2]
        P.dma(s_[0:64, 0:D], odout_d[h * 64:(h + 1) * 64, :], writes=[bs_], eng=("sp" if h % 2 == 0 else "pool"))
        k.cast(("pool", "dve", "act")[h % 3], w_outB[:, h, :], s_[0:64, 0:D], [bs_], [b_woutB])
    ones_f = ar("ones_f1", [128, 64]); P.v("pool", "memset", ones_f[:], 1.0, writes=[b_const])
    z_sb = ar("z_sb1", [128, 1280]); b_z = Buf()
    ropeC = ar("ropeC1", [128, 64]); ropeS = ar("ropeS1", [128, 64]); b_rope = Buf()
    tmpA = ar("tmpA1", [128, 1024]); b_tmpA = Buf()
    tmpB = ar("tmpB1", [128, 1024]); b_tmpB = Buf()
    scr = ar("scr1", [128, 64]); b_scr_ = Buf()
    qb = ar("qb1", [128, 1024], BF16); b_qb = Buf()
    kb = ar("kb1", [128, 128], BF16); b_kb = Buf()
    qT_sb = ar("qT_sb1", [64, 16, 128], BF16); b_qTsb = Buf()
    pMisc, bMisc = psum[3]
    pMisc2, bMisc2 = psum[4]

    def modE(kind, ti):
        if kind == "ctx":
            return k.modulate(X2_d[(NOWN + ti) * 128:(NOWN + ti + 1) * 128, :], 1, 0, 1, src_buf=b_X2)
        return k.modulate(X2_d[ti * 128:(ti + 1) * 128, :], 1, 0, 0, src_buf=b_X2)

    def tileE(kind, ti, pre):
        if kind == "ctx":
            src, w, kidx = X2_d[(NOWN + ti) * 128:(NOWN + ti + 1) * 128, :], 1, ti
        else:
            src, w, kidx = X2_d[ti * 128:(ti + 1) * 128, :], 0, 2 + ti
        need_q = (kind != "ctx" and ti < 16)
        H, bH, X, bX = pre
        blocks = ((0, 512), (512, 1024), (1024, 1280)) if need_q else ((1024, 1280),)
        for bi, (lo, hi) in enumerate(blocks):
            pz, bz = psum[1 + bi % 2]
            for c in range(8):
                P.mm(pz[:, 0:hi - lo], H[:, c, :], w_in[:, c, lo:hi], c == 0, c == 7, reads=[bH, b_win], writes=[bz])
            P.act(z_sb[:, lo:hi], pz[:, 0:hi - lo], AF.Copy, reads=[bz], writes=[b_z])
        rope = None
        if kind != "ctx":
            P.dma(ropeC[:], g["ropeC_d"][ti * 128:(ti + 1) * 128, :], writes=[b_rope], eng="pool")
            P.dma(ropeS[:], g["ropeS_d"][ti * 128:(ti + 1) * 128, :], writes=[b_rope], eng="pool")
            rope = (ropeC, ropeS, b_rope, tmpB, b_tmpB)
        pM8 = pMisc[0:64, :].bitcast(BF16).rearrange("p (h t) -> p h t", h=8)
        pM8b = pMisc2[0:64, :].bitcast(BF16).rearrange("p (h t) -> p h t", h=8)
        if need_q:
            k.head_norm_rope(qb[:], b_qb, z_sb[:, 0:1024], b_z, 16, sqn, rope, tmpA, b_tmpA, scr, b_scr_)
            for h in range(16):
                pm, bm = (pM8, bMisc) if h < 8 else (pM8b, bMisc2)
                P.tr(pm[:, h % 8, :], qb[:, h * 64:(h + 1) * 64], identB[:], reads=[b_qb, b_const], writes=[bm])
            P.act(qT_sb[:, 0:8, :], pM8, AF.Copy, reads=[bMisc], writes=[b_qTsb])
            P.act(qT_sb[:, 8:16, :], pM8b, AF.Copy, reads=[bMisc2], writes=[b_qTsb])
            P.dma(QT1_d[ti], qT_sb[:], reads=[b_qTsb], writes=[b_QT1])
        k.head_norm_rope(kb[:], b_kb, z_sb[:, 1024:1152], b_z, 2, skn, rope, tmpA, b_tmpA, scr, b_scr_)
        for h in range(2):
            P.tr(pM8[:, h, :], kb[:, h * 64:(h + 1) * 64], identB[:], reads=[b_kb, b_const], writes=[bMisc])
        P.act(KT[0:64, :, kidx * 128:(kidx + 1) * 128], pM8[:, 0:2, :], AF.Copy, reads=[bMisc], writes=[b_KT])
        P.v("pool", "tensor_copy", Vt[:, kidx, :, 0:64], z_sb[:, 1152:1280].rearrange("p (h d) -> p h d", h=2),
            reads=[b_z], writes=[b_Vt])

    seqE = [("ctx", 0), ("ctx", 1)] + [("own", ti) for ti in range(17)]
    SETE = [(z_sb, b_z, qb, b_qb, kb, b_kb, qT_sb, b_qTsb),
            (ar("z_sb1b", [128, 1280]), Buf(), ar("qb1b", [128, 1024], BF16), Buf(), ar("kb1b", [128, 128], BF16), Buf(),
             ar("qT_sb1b", [64, 16, 128], BF16), Buf())]
    pre = modE(*seqE[0])
    for si, (kind, ti) in enumerate(seqE):
        nxt = modE(*seqE[si + 1]) if si + 1 < len(seqE) else None
        (z_sb, b_z, qb, b_qb, kb, b_kb, qT_sb, b_qTsb) = SETE[si % 2]
        tileE(kind, ti, pre)
        pre = nxt

    if "stopE1" in dbg:
        return
    qT_in = [ar("qT_in1%d" % i, [128, 16, 128], BF16) for i in range(2)]; b_qTin = [Buf(), Buf()]
    for i_ in range(2):
        P.v("pool", "memset", qT_in[i_][64:128], 0.0, writes=[b_qTin[i_]])
    NPT = 3
    PT = [ar("PT1%d" % i, [128, 1024], BF16) for i in range(NPT)]; b_PT = [Buf() for _ in range(NPT)]
    esink = ar("esink1", [128, 16]); b_esink = Buf()
    P.act(esink[:], sinkb[:], AF.Exp, bias=bnd[:, 6:7], reads=[b_const, b_bnd], writes=[b_esink])
    rsr = ar("rsr1", [128, 512]); b_rsr = Buf()
    bc_sb = ar("bc_sb1", [64, 512]); b_bc = Buf()
    OT = ar("OT1", [64, 16, 128], BF16); b_OT = Buf()
    x3_sb = ar("x3_sb", [128, D]); b_x3 = Buf()
    pO = [psum[4], psum[5], psum[6], psum[7]]
    SG = [(k.psall[:, 2 * g_ * 512:(2 * g_ + 2) * 512], [psum[2 * g_][1], psum[2 * g_ + 1][1]]) for g_ in range(2)]
    pBC, bBC = psum[0]
    ones_b = ar("ones_b1", [128, 64], BF16); P.v("pool", "memset", ones_b[:], 1.0, writes=[b_const])
    rsb = ar("rsb1", [128, 4, 512], BF16); b_rsb = Buf()
    bcs = [ar("bcs1%d" % i_, [64, 512]) for i_ in range(4)]; b_bcs = [Buf() for _ in range(4)]
    x_ins = [ar("xE%d" % i_, [128, D]) for i_ in range(2)]; b_xins = [Buf(), Buf()]
    vsink = ar("vsink", [128, 66], BF16); b_vs = Buf()
    P.v("pool", "memset", vsink[:], 0.0, writes=[b_vs])
    P.v("pool", "memset", vsink[:, 64:65], 1.0, writes=[b_vs])
    esrow = ar("esrow", [128, 16, 128], BF16); b_esrow = Buf()
    P.v("dve", "tensor_copy", esrow[64:65], esink[64:65, :].unsqueeze(2).to_broadcast([1, 16, 128]),
        reads=[b_esink], writes=[b_esrow])

    def loadQ(qi):
        i_ = qi % 2
        P.dma(qT_in[i_][0:64], QT1_d[qi], reads=[b_QT1], writes=[b_qTin[i_]])
        P.dma(x_ins[i_][:], X2_d[qi * 128:(qi + 1) * 128, :], reads=[b_X2], writes=[b_xins[i_]])

    it = 0
    loadQ(0)
    for qi in range(16):
        i = qi % 2
        Q, bQ = qT_in[i], b_qTin[i]
        if qi + 1 < 16:
            loadQ(qi + 1)
        keys = [(0, None), (1, None)]
        if qi > 0:
            keys.append((2 + qi - 1, 0))
        keys.append((2 + qi, None))
        keys.append((2 + qi + 1, 1))
        nk = len(keys)
        units = [(gq, list(range(a_, min(a_ + 2, nk)))) for gq in range(4) for a_ in range(0, nk, 2)]
        pend = None

        def pv(gq, kks, pt, bpt):
            po, bpo = pO[gq]
            for j, kk in enumerate(kks):
                kt, mk = keys[kk]
                P.mm(po[0:65, :], Vt[:, kt, gq // 2, 0:65], pt[:, j * 512:(j + 1) * 512], kk == 0, False,
                     reads=[bpt, b_Vt], writes=[bpo])
            if kks[-1] == nk - 1:
                P.mm(po[0:65, :], vsink[64:65, 0:65], esrow[64:65, gq * 4:(gq + 1) * 4, :].rearrange("p h t -> p (h t)"),
                     False, True, reads=[b_vs, b_esrow], writes=[bpo])

        for (gq, kks) in units:
            kvh = gq // 2
            psc, bscs = SG[it % 2]
            pt, bpt = PT[it % NPT], b_PT[it % NPT]
            it += 1
            n = len(kks)
            for j, kk in enumerate(kks):
                kt, mk = keys[kk]
                P.mm(psc[:, j * 512:(j + 1) * 512], KT[:, kvh, kt * 128:(kt + 1) * 128],
                     Q[:, gq * 4:(gq + 1) * 4, :].rearrange("p h t -> p (h t)"), reads=[b_KT, bQ], writes=[bscs[j]])
            P.act(pt[:, 0:n * 512], psc[:, 0:n * 512], AF.Exp, scale=0.125, bias=bnd[:, 6:7],
                  reads=bscs[0:n] + [b_bnd], writes=[bpt])
            for j, kk in enumerate(kks):
                kt, mk = keys[kk]
                if mk is not None:
                    p3 = pt[:, j * 512:(j + 1) * 512].rearrange("p (h t) -> p h t", h=4)
                    P.v("dve", "tensor_tensor", p3, p3, wmask[:, mk, :].unsqueeze(1).to_broadcast([128, 4, 128]), ALU.mult,
                        reads=[bpt, b_const], writes=[bpt])
            if pend is not None:
                pv(*pend)
            pend = (gq, kks, pt, bpt)
        pv(*pend)
        for gq in range(4):
            po, bpo = pO[gq]
            P.act(rsr[64:65, :], po[64:65, :], AF.Ln, reads=[bpo], writes=[b_rsr])
            P.act(rsb[64:65, gq, :], rsr[64:65, :], AF.Exp, scale=-1.0, reads=[b_rsr], writes=[b_rsb])
        for gq in range(4):
            pb_, bb_ = psum[gq]
            P.mm(pb_[0:64, :], ones_b[64:65, 0:64], rsb[64:65, gq, :], reads=[b_const, b_rsb], writes=[bb_])
        for gq in range(4):
            pb_, bb_ = psum[gq]
            P.v("dve", "tensor_copy", bcs[gq][:], pb_[0:64, :], reads=[bb_], writes=[b_bcs[gq]])
        for gq in range(4):
            po, bpo = pO[gq]
            P.v("dve", "tensor_tensor", OT[:, gq * 4:(gq + 1) * 4, :].rearrange("p h t -> p (h t)"), po[0:64, :], bcs[gq][:],
                ALU.mult, reads=[bpo, b_bcs[gq]], writes=[b_OT])
        X, bX = x_ins[i], b_xins[i]
        for hh in range(2):
            pz, bz = psum[1 + hh]
            for h in range(16):
                P.mm(pz[:], OT[:, h, :], w_outB[:, h, hh * 512:(hh + 1) * 512], h == 0, h == 15,
                     reads=[b_OT, b_woutB], writes=[bz])
            P.v("dve", "tensor_tensor", x3_sb[:, hh * 512:(hh + 1) * 512], pz[:],
                k.gateB_all[:, k.gslot[(1, 0, 0)], hh * 512:(hh + 1) * 512], ALU.mult,
                reads=[bz, k.b_gateB[1]], writes=[b_x3])
        P.v("pool", "tensor_tensor", x3_sb[:], x3_sb[:], X[:], ALU.add, reads=[b_x3, bX], writes=[b_x3])
        P.dma(X3_d[qi * 128:(qi + 1) * 128, :], x3_sb[:], reads=[b_x3], writes=[b_X3])


def rope_tables():
    t = np.arange(SEQ)
    row = (t // 64).astype(np.float32)
    col = (t % 64).astype(np.float32)
    inv = (10000.0 ** (-np.arange(0, 32, 2, dtype=np.float32) / 32)).astype(np.float32)
    ang = np.stack([row[:, None] * inv, col[:, None] * inv], axis=1)
    c = np.cos(ang).astype(np.float32)
    s = np.sin(ang).astype(np.float32)
    C = np.zeros((SEQ, 2, 2, 16), np.float32)
    S = np.zeros((SEQ, 2, 2, 16), np.float32)
    C[:, :, 0] = c
    C[:, :, 1] = c
    S[:, :, 0] = -s
    S[:, :, 1] = s
    return C.reshape(SEQ, 64), S.reshape(SEQ, 64)


def host_consts():
    p = np.arange(128)
    same = (p[:, None] // 64) == (p[None, :] // 64)
    s, t = p[:, None], p[None, :]
    tri = np.stack([same & (s <= t), same & (s >= t), same & (s > t), same & (s < t)], axis=1).astype(np.float32)
    sm = (p % 64)[:, None]
    tt = np.arange(64)[None, :]
    mA = np.tile((sm <= tt), (1, 4))
    mB = np.tile((sm >= tt), (1, 4))
    maskAB = np.stack([mA, mB], axis=1).astype(ml_dtypes.bfloat16)
    sel2 = np.zeros((2, 2, 128), np.float32)
    sel2[0, 0] = 1
    sel2[1, 1] = 1
    wmask = np.stack([(s >= t), (s <= t)], axis=1).astype(ml_dtypes.bfloat16)
    return dict(identF=np.eye(128, dtype=np.float32), identB=np.eye(128).astype(ml_dtypes.bfloat16),
                tri=tri, maskAB=maskAB, sel2=sel2, wmask=wmask)


def make_in_maps(inp):
    f = lambda a: np.ascontiguousarray(np.asarray(a, dtype=np.float32))
    C, S = rope_tables()
    consts = host_consts()
    maps = []
    rw = np.concatenate([f(inp["router_group_w"]), f(inp["router_expert_w"])], axis=-1)
    rb = np.concatenate([f(inp["router_group_b"]), f(inp["router_expert_b"])], axis=-1)[:, None, :]
    shared = dict(
        mod_w=f(inp["mod_w"]), mod_b=f(inp["mod_b"]), norm_mix=f(inp["norm_mix"]), norm_ffn=f(inp["norm_ffn"]),
        ev_w_in=f(inp["ev_w_in"])[0], ev_w_out=f(inp["ev_w_out"])[0],
        gla_out_norm=f(inp["gla_out_norm"]), att_q_norm=f(inp["att_q_norm"]), att_k_norm=f(inp["att_k_norm"]),
        od_w_in=f(inp["od_w_in"])[0], od_w_out=f(inp["od_w_out"])[0], swa_sink=f(inp["swa_sink"]),
        swa_q_norm=f(inp["swa_q_norm"]), swa_k_norm=f(inp["swa_k_norm"]),
        router_w=np.ascontiguousarray(rw), router_b=np.ascontiguousarray(rb),
        exp_w_gate=f(inp["exp_w_gate"]).reshape(2, 16, D, 256), exp_w_up=f(inp["exp_w_up"]).reshape(2, 16, D, 256),
        exp_w_down=f(inp["exp_w_down"]).reshape(2, 16, 256, D), **consts)
    gw = f(inp["gla_gate_w"])[0]
    gb = f(inp["gla_gate_b"])[0]
    for core in range(8):
        b, half = core // 2, core % 2
        x = f(inp["x"])[b]
        cx = f(inp["ctx"])[b]
        order = (0, 1)
        if half == 1:
            x = x[::-1]
            cx = cx[::-1]
            order = (1, 0)
        gwe = np.zeros((33, 512), np.float32)
        for i, dr in enumerate(order):
            gwe[16 * dr:16 * dr + 16, 256 * i:256 * i + 256] = gw[dr]
            gwe[32, 256 * i:256 * i + 256] = gb[dr]
        m = dict(shared)
        m.update(x=np.ascontiguousarray(x), ctx=np.ascontiguousarray(cx),
                 crow=np.ascontiguousarray(np.stack([f(inp["c"])[b], f(inp["c_ctx"])])),
                 gw_ext=gwe,
                 ropeC=np.ascontiguousarray(C[::-1] if half else C),
                 ropeS=np.ascontiguousarray(S[::-1] if half else S))
        maps.append(m)
    return maps


_CACHE = {}


def kernel(**inputs):
    if "nc" not in _CACHE:
        _CACHE["nc"] = build()[0]
    nc = _CACHE["nc"]
    maps = make_in_maps(inputs)
    res = run_bass_kernel_spmd(nc, maps, core_ids=list(range(8)))
    out = np.zeros((4, SEQ, D), np.float32)
    for core in range(8):
        b, half = core // 2, core % 2
        o = np.asarray(res.results[core]["out"], dtype=np.float32)
        if half == 0:
            out[b, 0:2048] = o
        else:
            out[b, 2048:] = o[::-1]
    return out
```

```python
import numpy as np
import ml_dtypes
from contextlib import ExitStack
import concourse.bass as bass
import concourse.mybir as mybir
from concourse.bass_utils import run_bass_kernel_spmd

F32 = mybir.dt.float32
BF16 = mybir.dt.bfloat16
ALU = mybir.AluOpType
AF = mybir.ActivationFunctionType
AX = mybir.AxisListType

import os
COMPUTE = ("pe", "act", "dve", "pool")
SCHED_W = int(os.environ.get("SCHED_W", "128"))
PE_A = float(os.environ.get("PE_A", "0.05"))
PE_B = float(os.environ.get("PE_B", "0.00035"))
PE_F = float(os.environ.get("PE_F", "2.5"))
DMA_L = float(os.environ.get("DMA_L", "2.2"))
ACT_S = float(os.environ.get("ACT_S", "0.8"))
DVE_S = float(os.environ.get("DVE_S", "1.0"))
POOL_S = float(os.environ.get("POOL_S", "1.0"))
SEM_L = float(os.environ.get("SEM_L", "0.1"))
NDMASEM = 12

D = 1024
SEQ = 4096
NOWN = 17
NTOK0 = 19
EVEN_IN = 2336
NEG_BIG = 1.0e30


class Buf:
    __slots__ = ("name", "w", "r")

    def __init__(self, name=""):
        self.name = name
        self.w = None
        self.r = {}


class Prog:
    def __init__(self, nc):
        self.nc = nc
        self.ops = []
        self.cost = []
        self.reorder = True
        self.base = set()
        self.eng = {"pe": nc.tensor, "act": nc.scalar, "dve": nc.vector,
                    "pool": nc.gpsimd, "sp": nc.sync}

    def op(self, eng, fn, reads=(), writes=(), dma=False, cost=0.4):
        idx = len(self.ops)
        deps = set(self.base)
        for b in reads:
            if b.w is not None:
                deps.add(b.w)
        for b in writes:
            if b.w is not None:
                deps.add(b.w)
            for v in b.r.values():
                if isinstance(v, list):
                    deps.update(v)
                else:
                    deps.add(v)
        key = (eng, dma)
        for b in reads:
            b.r.setdefault(key, []).append(idx)
        for b in writes:
            b.w = idx
            b.r = {}
        deps.discard(idx)
        self.ops.append((eng, dma, fn, deps))
        self.cost.append(cost)
        return idx

    def barrier(self):
        last = {}
        dm = {}
        for i, (eng, dma, fn, deps) in enumerate(self.ops):
            if dma:
                dm.setdefault(eng, []).append(i)
            else:
                last[eng] = i
        base = set(last.values())
        for q, lst in dm.items():
            base.update(lst[-NDMASEM:])
        self.base = base

    @staticmethod
    def _fs(ap):
        n = 1
        for s in ap.shape[1:]:
            n *= s
        return n

    def dma(self, out, in_, reads=(), writes=(), eng="sp"):
        e = self.eng[eng]
        nb = self._fs(out) * out.shape[0] * (2 if out.dtype == BF16 else 4)
        return self.op(eng, lambda: e.dma_start(out=out, in_=in_), reads, writes, dma=True,
                       cost=DMA_L + nb / 150e3)

    def mm(self, out, lhsT, rhs, start=True, stop=True, reads=(), writes=()):
        nc = self.nc
        c = PE_A + self._fs(rhs) * PE_B
        if rhs.dtype == F32:
            c *= PE_F
        return self.op("pe", lambda: nc.tensor.matmul(out, lhsT, rhs, start=start, stop=stop),
                       reads, writes, cost=c)

    def tr(self, out, in_, ident, reads=(), writes=()):
        nc = self.nc
        return self.op("pe", lambda: nc.tensor.transpose(out, in_, ident), reads, writes, cost=0.12)

    def act(self, out, in_, func, reads=(), writes=(), **kw):
        nc = self.nc
        return self.op("act", lambda: nc.scalar.activation(out=out, in_=in_, func=func, **kw),
                       reads, writes, cost=ACT_S * (0.25 + self._fs(in_) * 0.0009))

    def v(self, eng, name, *args, reads=(), writes=(), **kw):
        f = getattr(self.eng[eng], name)
        c = DVE_S * (0.15 + self._fs(args[0]) * 0.0011)
        if eng == "pool":
            c = POOL_S * (0.3 + self._fs(args[0]) * 0.0025)
        return self.op(eng, lambda: f(*args, **kw), reads, writes, cost=c)

    def schedule(self, W=SCHED_W):
        ops, cost = self.ops, self.cost
        n = len(ops)
        fin = [None] * n
        start = [0.0] * n
        rem = {}
        for i, (eng, dma, fn, deps) in enumerate(ops):
            rem.setdefault(eng, []).append(i)
        etime = {e: 0.0 for e in rem}
        nsched = 0
        while nsched < n:
            best = None
            for e, lst in rem.items():
                if not lst:
                    continue
                te = etime[e]
                for i in lst[:W]:
                    deps = ops[i][3]
                    r = te
                    ok = True
                    for d in deps:
                        f = fin[d]
                        if f is None:
                            ok = False
                            break
                        if f > r:
                            r = f
                    if not ok:
                        continue
                    key = (r, i)
                    if best is None or key < best[0]:
                        best = (key, e, i)
                    if r <= te:
                        break
            (r, i), e, _ = best
            eng, dma, fn, deps = ops[i]
            start[i] = r
            if dma:
                etime[e] = r + 0.5
                fin[i] = r + cost[i]
            else:
                etime[e] = r + cost[i]
                fin[i] = r + cost[i] + SEM_L
            rem[e].remove(i)
            nsched += 1
        order = sorted(range(n), key=lambda j: (start[j], j))
        self.sim_time = max(f for f in fin)
        self.sim_start, self.sim_fin = start, fin
        return order

    def emit(self, sems):
        ops = self.ops
        n = len(ops)
        need = [False] * n
        for (eng, dma, fn, deps) in ops:
            for d in deps:
                deng, ddma, _, _ = ops[d]
                if (not ddma) and deng == "pe" and eng == "pe" and not dma:
                    continue
                need[d] = True
        sig = [None] * n
        cnt = {e: 0 for e in COMPUTE}
        dcnt = {}
        waited = {}
        order = self.schedule() if self.reorder else range(n)
        for i in order:
            eng, dma, fn, deps = ops[i]
            e = self.eng[eng]
            w = {}
            for d in deps:
                deng, ddma, _, _ = ops[d]
                if (not ddma) and deng == "pe" and eng == "pe" and not dma:
                    continue
                s, val = sig[d]
                if w.get(id(s), (None, -1))[1] < val:
                    w[id(s)] = (s, val)
            if dma:
                j = dcnt.get(eng, 0)
                pool = sems[("dma", eng)]
                s = pool[j % len(pool)]
                val = 16 * (j // len(pool) + 1)
                if val > 16 and w.get(id(s), (None, -1))[1] < val - 16:
                    w[id(s)] = (s, val - 16)
                dcnt[eng] = j + 1
                sig[i] = (s, val)
            for sid, (s, val) in w.items():
                k = (eng, sid)
                if waited.get(k, -1) >= val:
                    continue
                waited[k] = val
                e.wait_ge(s, val)
            ins = fn()
            if dma:
                ins.then_inc(sig[i][0], 16)
            elif need[i]:
                cnt[eng] += 1
                sig[i] = (sems[eng], cnt[eng])
                ins.then_inc(sems[eng], 1)
        for (k, pool) in sems.items():
            if isinstance(k, tuple):
                eng = k[1]
                j = dcnt.get(eng, 0)
                e = self.eng[eng]
                for q, s in enumerate(pool):
                    c = (j - q + len(pool) - 1) // len(pool) if j > q else 0
                    if c > 0:
                        e.wait_ge(s, 16 * c)
        return dict(n_ops=n, sig=cnt, dmas=dcnt)


class K:
    pass


def build(stages=("all",), dbg=()):
    nc = bass.Bass("TRN2", target_bir_lowering=False)
    P = Prog(nc)
    k = K()
    k.nc, k.P = nc, P
    k.dbgset = set(dbg)
    es = ExitStack()
    k.es = es
    k.dbg = {}

    def din(name, shape, dt=F32):
        return nc.dram_tensor(name, list(shape), dt, kind="ExternalInput").ap()

    def dscr(name, shape, dt=F32):
        if name in dbg:
            return nc.dram_tensor(name, list(shape), dt, kind="ExternalOutput").ap()
        return nc.dram_tensor(name, list(shape), dt).ap()

    def dout(name, shape, dt=F32):
        return nc.dram_tensor(name, list(shape), dt, kind="ExternalOutput").ap()

    def sb(name, shape, dt=F32):
        return es.enter_context(nc.sbuf_tensor("sb_" + name, list(shape), dt))

    def ps(name, shape, dt=F32):
        return es.enter_context(nc.psum_tensor("ps_" + name, list(shape), dt))

    k.din, k.dscr, k.dout, k.sb, k.ps = din, dscr, dout, sb, ps
    ARW = 41700
    k.arena = None
    k.aoff = 0

    def ar(name, shape, dt=F32):
        if k.arena is None:
            k.arena = sb("arena", [128, ARW])
        n = 1
        for s in shape[1:]:
            n *= s
        w = n if dt == F32 else (n + 1) // 2
        off = k.aoff
        k.aoff += w + (w % 2)
        assert k.aoff <= ARW, (name, k.aoff)
        a = k.arena[0:shape[0], off:off + w]
        if dt != F32:
            a = a.bitcast(dt)
        if len(shape) > 2:
            names = " ".join("d%d" % i for i in range(1, len(shape)))
            kw = {"d%d" % i: shape[i] for i in range(1, len(shape))}
            a = a.rearrange("p (%s) -> p %s" % (names, names), **kw)
        return a

    def areset(mark=0):
        P.barrier()
        k.peak = getattr(k, "peak", [])
        k.peak.append(k.aoff * 4 // 1024)
        k.marks = getattr(k, "marks", [])
        k.marks.append(len(P.ops))
        k.aoff = mark

    k.ar, k.areset = ar, areset

    with es:
        sems = {e: es.enter_context(nc.semaphore("s_" + e)) for e in COMPUTE}
        for q in ("sp", "pool"):
            sems[("dma", q)] = [es.enter_context(nc.semaphore(f"d_{q}{i}")) for i in range(NDMASEM)]
        body(k, stages, dbg)
        st = P.emit(sems)
        k.stats = st
    return nc, k


def body(k, stages, dbg):
    nc, P = k.nc, k.P
    din, dscr, dout, sb, ps, ar = k.din, k.dscr, k.dout, k.sb, k.ps, k.ar

    x_d = din("x", [SEQ, D])
    ctx_d = din("ctx", [256, D])
    crow_d = din("crow", [2, D])
    modw_d = din("mod_w", [2, D, 6 * D])
    modb_d = din("mod_b", [2, 6 * D])
    nmix_d = din("norm_mix", [2, D])
    nffn_d = din("norm_ffn", [2, D])
    evin_d = din("ev_w_in", [D, EVEN_IN])
    evout_d = din("ev_w_out", [D, D])
    gwext_d = din("gw_ext", [33, 512])
    glan_d = din("gla_out_norm", [1, 128])
    aqn_d = din("att_q_norm", [1, 64])
    akn_d = din("att_k_norm", [1, 64])
    odin_d = din("od_w_in", [D, 1280])
    odout_d = din("od_w_out", [D, D])
    sink_d = din("swa_sink", [1, 16])
    sqn_d = din("swa_q_norm", [1, 64])
    skn_d = din("swa_k_norm", [1, 64])
    rw_d = din("router_w", [2, D, 20])
    rb_d = din("router_b", [2, 1, 20])
    wg_d = din("exp_w_gate", [2, 16, D, 256])
    wu_d = din("exp_w_up", [2, 16, D, 256])
    wd_d = din("exp_w_down", [2, 16, 256, D])
    ropeC_d = din("ropeC", [SEQ, 64])
    ropeS_d = din("ropeS", [SEQ, 64])
    identF_d = din("identF", [128, 128])
    identB_d = din("identB", [128, 128], BF16)
    tri_d = din("tri", [128, 4, 128])
    maskAB_d = din("maskAB", [128, 2, 256], BF16)
    sel2_d = din("sel2", [2, 2, 128])
    wmask_d = din("wmask", [128, 2, 128], BF16)
    out_d = dout("out", [2048, D])

    X1_d = dscr("X1", [NTOK0 * 128, D])
    X2_d = dscr("X2", [NTOK0 * 128, D])
    X3_d = dscr("X3", [2048, D])
    OA_d = dscr("OA", [NTOK0, 128, 512])
    QT_d = dscr("QT", [NTOK0, 64, 8, 128], BF16)
    MIXT_d = dscr("MIXT", [NTOK0, 128, 8, 128], BF16)
    PB_lt = dscr("PB_lt", [NTOK0, 64, 512], BF16)
    PB_ke = dscr("PB_ke", [NTOK0, 64, 512], BF16)
    PB_kd = dscr("PB_kd", [NTOK0, 128, 256], BF16)
    PB_v = dscr("PB_v", [NTOK0, 128, 512], BF16)
    PB_ee = dscr("PB_ee", [NTOK0, 64, 8])
    PB_rg = dscr("PB_rg", [NTOK0, 128, 512])
    QT1_d = dscr("QT1", [16, 64, 16, 128], BF16)

    identF = sb("identF", [128, 128]); identB = sb("identB", [128, 128], BF16)
    b_const = Buf("const")
    for t, d in ((identF, identF_d), (identB, identB_d)):
        P.dma(t[:], d, writes=[b_const], eng="pool")
    k.identF, k.identB, k.b_const = identF, identB, b_const

    psall = ps("psall", [128, 4096])
    k.psall = psall
    pA, pB, pC, pD, pE, pF, pG, pH = [psall[:, i * 512:(i + 1) * 512] for i in range(8)]
    bA, bB, bC, bD, bE, bF_, bG, bH = [Buf("ps%d" % i) for i in range(8)]
    k.psum = [(pA, bA), (pB, bB), (pC, bC), (pD, bD), (pE, bE), (pF, bF_), (pG, bG), (pH, bH)]

    crow = ar("crow", [2, D]); b_crow = Buf()
    scT = sb("scT", [128, 8, 2]); b_scT = Buf()
    P.dma(crow[:], crow_d, writes=[b_crow])
    P.act(crow[:], crow[:], AF.Silu, reads=[b_crow], writes=[b_crow])
    for c in range(8):
        P.tr(pA[:, 2 * c:2 * c + 2], crow[:, c * 128:(c + 1) * 128], identF[0:2, 0:2],
             reads=[b_crow, b_const], writes=[bA])
    P.v("dve", "tensor_copy", scT[:].rearrange("p c t -> p (c t)"), pA[:, 0:16], reads=[bA], writes=[b_scT])
    gcol = sb("gcol", [128, 2, 2, 2, 8, 2])
    b_gcol = [Buf(), Buf()]
    gateB_all = sb("gateB", [128, 6, D])
    b_gateB = [Buf(), Buf()]
    k.gslot = {(0, 0, 0): 0, (0, 0, 1): 1, (0, 1, 0): 2, (0, 1, 1): 3, (1, 0, 0): 4, (1, 1, 0): 5}
    k.gcol, k.b_gcol = gcol, b_gcol
    k.gateB_all, k.b_gateB = gateB_all, b_gateB

    def s0_steps(l, tg, bankT, bank1, bank2):
        st = {}

        def alloc():
            st["modv"] = ar("modv" + tg, [2, 6 * D]); st["b_modv"] = Buf()
            st["modb"] = [ar("modb%d" % i + tg, [2, 512]) for i in range(2)]; st["b_modb"] = [Buf(), Buf()]
            st["wst"] = [ar("modwst%d" % i + tg, [128, 8, 512]) for i in range(2)]; st["b_wst"] = [Buf(), Buf()]
            st["nrm"] = ar("nrm" + tg, [2, D]); st["b_nrm"] = Buf()
            st["grow"] = st["nrm"]; st["b_grow"] = st["b_nrm"]
            st["sel2"] = ar("sel2" + tg, [2, 2, 128]); st["b_sel2"] = Buf()
            P.dma(st["sel2"][:], sel2_d, writes=[st["b_sel2"]], eng="pool")

        def blk(j):
            if j == 0:
                alloc()
            modv, b_modv = st["modv"], st["b_modv"]
            w = st["wst"][j % 2]; bw = st["b_wst"][j % 2]
            mb = st["modb"][j % 2]; bmb = st["b_modb"][j % 2]
            pz, bz = (bank1, bank2)[j % 2]
            for r in range(2):
                P.dma(mb[r:r + 1, :], modb_d[l:l + 1, j * 512:(j + 1) * 512], writes=[bmb], eng="pool")
            P.dma(w[:], modw_d[l].rearrange("(c p) n -> p c n", p=128)[:, :, j * 512:(j + 1) * 512],
                  writes=[bw], eng=("sp" if j % 2 == 0 else "pool"))
            for c in range(8):
                P.mm(pz[0:2, :], scT[:, c, :], w[:, c, :], c == 0, c == 7, reads=[b_scT, bw], writes=[bz])
            P.v("dve", "tensor_tensor", modv[:, j * 512:(j + 1) * 512], pz[0:2, :], mb[:],
                ALU.add, reads=[bz, bmb], writes=[b_modv])

        def fin():
            modv, b_modv = st["modv"], st["b_modv"]
            nrm, b_nrm, grow, b_grow = st["nrm"], st["b_nrm"], st["grow"], st["b_grow"]
            pT_, bT_ = bankT
            for sub in range(2):
                nd = (nmix_d, nffn_d)[sub]
                for r in range(2):
                    P.dma(nrm[r:r + 1, :], nd[l:l + 1, :], writes=[b_nrm], eng="pool")
                base = sub * 3 * D
                P.v("dve", "scalar_tensor_tensor", grow[:], modv[:, base + D:base + 2 * D], 1.0, nrm[:],
                    ALU.add, ALU.mult, reads=[b_modv, b_nrm], writes=[b_grow])
                for gi, src_ in enumerate((grow[:], modv[:, base:base + D])):
                    for c in range(8):
                        P.tr(pT_[:, 2 * c:2 * c + 2], src_[:, c * 128:(c + 1) * 128], identF[0:2, 0:2],
                             reads=[b_grow, b_modv, b_const], writes=[bT_])
                    P.v("dve", "tensor_copy", gcol[:, l, sub, gi].rearrange("p c t -> p (c t)"), pT_[:, 0:16],
                        reads=[bT_], writes=[b_gcol[l]])
            for sub in range(2):
                for w in range(2):
                    if l == 1 and w == 1:
                        continue
                    for hh in range(2):
                        pz, bz = (bank1, bank2)[hh]
                        col = (sub * 3 + 2) * D + hh * 512
                        P.mm(pz[:], st["sel2"][:, w, :], modv[:, col:col + 512], reads=[st["b_sel2"], b_modv], writes=[bz])
                        P.v("dve", "tensor_copy", gateB_all[:, k.gslot[(l, sub, w)], hh * 512:(hh + 1) * 512], pz[:],
                            reads=[bz], writes=[b_gateB[l]])

        return [(lambda j=j: blk(j)) for j in range(12)] + [fin]

    k.s0_steps = s0_steps
    for stp in s0_steps(0, "L0", k.psum[0], k.psum[1], k.psum[2]):
        stp()

    xt = [sb("xt%d" % i, [128, D]) for i in range(2)]; b_xt = [Buf(), Buf()]
    stat = [sb("stat%d" % i, [128, 8]) for i in range(2)]; b_stat = [Buf(), Buf()]
    xn = [sb("xn%d" % i, [128, D], BF16) for i in range(2)]; b_xn = [Buf(), Buf()]
    b_xnf = Buf()
    hT = [sb("hT%d" % i, [128, 8, 128], BF16) for i in range(2)]; b_hT = [Buf(), Buf()]
    b_hTf = Buf()
    k.cnt = 0

    def modulate(src_ap, l, sub, w, fp32=False, src_buf=None, xt_out=None):
        i = k.cnt % 2
        k.cnt += 1
        X, bX = xt[i], b_xt[i]
        P.dma(X[:], src_ap, reads=([src_buf] if src_buf else []), writes=[bX])
        st, bst = stat[i], b_stat[i]
        P.v("dve", "scalar_tensor_tensor", xn[i][:], X[:], 1.0, X[:], ALU.mult, ALU.mult,
            accum_out=st[:, 0:1], reads=[bX], writes=[b_xn[i], bst])
        P.act(st[:, 1:2], st[:, 0:1], AF.Ln, scale=1.0 / D, bias=1e-6, reads=[bst], writes=[bst])
        P.act(st[:, 2:3], st[:, 1:2], AF.Exp, scale=-0.5, reads=[bst], writes=[bst])
        if not fp32:
            N_, bN = xn[i], b_xn[i]
            H, bH_ = hT[i], b_hT[i]
            pz, bz = k.psum[0]
            pzv = pz[:].bitcast(BF16).rearrange("p (c t) -> p c t", c=8)
            P.v("dve", "tensor_scalar", N_[:], X[:], st[:, 2:3], None, ALU.mult, reads=[bX, bst], writes=[bN])
            for c in range(8):
                P.tr(pzv[:, c, :], N_[:, c * 128:(c + 1) * 128], identB[:], reads=[bN, b_const], writes=[bz])
            for c in range(8):
                P.act(H[:, c, :], pzv[:, c, :], AF.Identity, scale=gcol[:, l, sub, 0, c, w:w + 1],
                      bias=gcol[:, l, sub, 1, c, w:w + 1], reads=[bz, b_gcol[l]], writes=[bH_])
            return H, bH_, X, bX
        else:
            xnf, hTf = k.xnf, k.hTf
            P.v("dve", "tensor_scalar", xnf[:], X[:], st[:, 2:3], None, ALU.mult, reads=[bX, bst], writes=[b_xnf])
            for hh in range(2):
                pz, bz = k.psum[hh]
                for c in range(4):
                    cc = hh * 4 + c
                    P.tr(pz[:, c * 128:(c + 1) * 128], xnf[:, cc * 128:(cc + 1) * 128], identF[:],
                         reads=[b_xnf, b_const], writes=[bz])
                for c in range(4):
                    cc = hh * 4 + c
                    P.act(hTf[:, cc, :], pz[:, c * 128:(c + 1) * 128], AF.Identity,
                          scale=gcol[:, l, sub, 0, cc, w:w + 1], bias=gcol[:, l, sub, 1, cc, w:w + 1],
                          reads=[bz, b_gcol[l]], writes=[b_hTf])
            return hTf, b_hTf, X, bX

    k.modulate = modulate

    def cast(eng, dst, src, reads, writes):
        if eng == "act":
            P.act(dst, src, AF.Copy, reads=reads, writes=writes)
        else:
            P.v(eng, "tensor_copy", dst, src, reads=reads, writes=writes)

    k.cast = cast

    def load_weight_bf16(dst, b_dst, src_ap_fn, nchunk, ncol, stage, b_stage, cast_engs=("pool", "dve", "act")):
        for c in range(nchunk):
            s, bs = stage[c % 2], b_stage[c % 2]
            P.dma(s[:, 0:ncol], src_ap_fn(c), writes=[bs], eng=("sp" if c % 2 == 0 else "pool"))
            cast(cast_engs[c % len(cast_engs)], dst[:, c, :], s[:, 0:ncol], [bs], [b_dst])

    k.b_wstage = [Buf(), Buf()]
    k.load_weight_bf16 = load_weight_bf16

    def bcast_row(name, d_ap, n):
        t = sb(name, [128, n])
        P.dma(t[:], d_ap.partition_broadcast(128).rearrange("p o n -> p (o n)"), writes=[b_const], eng="pool")
        return t

    aqn = bcast_row("aqn", aqn_d, 64); akn = bcast_row("akn", akn_d, 64)
    sqn = bcast_row("sqn", sqn_d, 64); skn = bcast_row("skn", skn_d, 64)
    glan = bcast_row("glan", glan_d, 128)
    sinkb = bcast_row("sinkb", sink_d, 16)

    bnd = sb("bnd", [128, 8]); b_bnd = Buf()

    def make_bound(col, gq, gk):
        P.v("dve", "tensor_reduce", bnd[:, col:col + 1], gq[:], AX.X, ALU.max, apply_absolute_value=True,
            reads=[b_const], writes=[b_bnd])
        P.v("dve", "tensor_reduce", bnd[:, col + 1:col + 2], gk[:], AX.X, ALU.max, apply_absolute_value=True,
            reads=[b_const], writes=[b_bnd])
        P.v("dve", "scalar_tensor_tensor", bnd[:, col + 2:col + 3], bnd[:, col:col + 1], -8.0, bnd[:, col + 1:col + 2],
            ALU.mult, ALU.mult, reads=[b_bnd], writes=[b_bnd])
    make_bound(0, aqn, akn)
    make_bound(4, sqn, skn)
    k.bnd, k.b_bnd = bnd, b_bnd

    def head_norm_rope(dst, b_dst, src, b_src, nh, gain, rope, tmp, b_tmp, scr, b_scr):
        s3 = src.rearrange("p (h d) -> p h d", h=nh)
        t3 = tmp[:, 0:nh * 64].rearrange("p (h d) -> p h d", h=nh)
        P.v("dve", "tensor_tensor", t3, s3, s3, ALU.mult, reads=[b_src], writes=[b_tmp])
        P.v("dve", "tensor_reduce", scr[:, 0:nh], t3, AX.X, ALU.add, reads=[b_tmp], writes=[b_scr])
        P.act(scr[:, 16:16 + nh], scr[:, 0:nh], AF.Ln, scale=1.0 / 64, bias=1e-6, reads=[b_scr], writes=[b_scr])
        P.act(scr[:, 32:32 + nh], scr[:, 16:16 + nh], AF.Exp, scale=-0.5, reads=[b_scr], writes=[b_scr])
        rs = scr[:, 32:32 + nh]
        P.v("dve", "tensor_tensor", t3, s3, rs.unsqueeze(2).to_broadcast([128, nh, 64]), ALU.mult,
            reads=[b_src, b_scr], writes=[b_tmp])
        g3 = gain[:, 0:64].unsqueeze(1).to_broadcast([128, nh, 64])
        if rope is None:
            d3 = dst.rearrange("p (h d) -> p h d", h=nh)
            P.v("dve", "tensor_tensor", d3, t3, g3, ALU.mult, reads=[b_tmp, b_const], writes=[b_dst])
            return
        C, S, b_rope, tmp2, b_tmp2 = rope
        P.v("dve", "tensor_tensor", t3, t3, g3, ALU.mult, reads=[b_tmp, b_const], writes=[b_tmp])
        u3 = tmp2[:, 0:nh * 64].rearrange("p (h d) -> p h d", h=nh)
        P.v("pool", "tensor_tensor", u3, t3, C[:, 0:64].unsqueeze(1).to_broadcast([128, nh, 64]), ALU.mult,
            reads=[b_tmp, b_rope], writes=[b_tmp2])
        t5 = tmp[:, 0:nh * 64].rearrange("p (h a f e) -> p h a f e", h=nh, a=2, f=2)
        S5 = S[:, 0:64].rearrange("p (a f e) -> p a f e", a=2, f=2)
        d5 = dst.rearrange("p (h a f e) -> p h a f e", h=nh, a=2, f=2)
        u5 = tmp2[:, 0:nh * 64].rearrange("p (h a f e) -> p h a f e", h=nh, a=2, f=2)
        for f in range(2):
            sw = t5[:, :, :, 1 - f, :]
            sv = S5[:, :, f, :].unsqueeze(1).to_broadcast([128, nh, 2, 16])
            w_ = scr
            P.v("dve", "tensor_tensor", sw_tmp(k, nh)[:, :, :, f, :], sw, sv, ALU.mult,
                reads=[b_tmp, b_rope], writes=[k.b_swt])
        st5 = sw_tmp(k, nh)
        P.v("dve", "tensor_tensor", d5, st5, u5, ALU.add, reads=[k.b_swt, b_tmp2], writes=[b_dst])

    k.b_swt = Buf()

    def sw_tmp(k_, nh):
        return k_.swt[:, 0:nh * 64].rearrange("p (h a f e) -> p h a f e", h=nh, a=2, f=2)

    k.head_norm_rope = head_norm_rope

    from_l0 = dict(locals())
    k.env = from_l0
    k.areset(0)
    if "stopS0" in dbg:
        return
    layer0(k, stages, dbg)


def layer0(k, stages, dbg):
    g = k.env
    nc, P = k.nc, k.P
    sb, ps, dout = k.ar, k.ps, k.dout
    identF, identB, b_const = g["identF"], g["identB"], g["b_const"]
    modulate = k.modulate
    psum = k.psum
    x_d, ctx_d = g["x_d"], g["ctx_d"]
    (OA_d, QT_d, MIXT_d, PB_lt, PB_ke, PB_kd, PB_v, PB_ee, PB_rg, X1_d) = (
        g["OA_d"], g["QT_d"], g["MIXT_d"], g["PB_lt"], g["PB_ke"], g["PB_kd"], g["PB_v"], g["PB_ee"], g["PB_rg"], g["X1_d"])
    aqn, akn, glan = g["aqn"], g["akn"], g["glan"]
    bnd, b_bnd = k.bnd, k.b_bnd
    gateB_all, b_gateB = k.gateB_all, k.b_gateB
    b_scr = {n: Buf(n) for n in ("OA", "QT", "MIXT", "PBlt", "PBke", "PBkd", "PBv", "PBee", "PBrg", "X1")}
    k.b_scr = b_scr

    KT = sb("KT", [128, 2, 34 * 128], BF16); b_KT = Buf()
    P.v("pool", "memset", KT[64:128], 0.0, writes=[b_KT])
    P.v("pool", "memset", KT[64:65], 1.0, writes=[b_KT])
    P.v("dve", "tensor_scalar", KT[64:65].rearrange("p a b -> p (a b)"), KT[64:65].rearrange("p a b -> p (a b)"),
        bnd[64:65, 2:3], 8.0, ALU.mult, ALU.mult, reads=[b_KT, b_bnd], writes=[b_KT])
    Vt = sb("Vt", [128, 34, 2, 66], BF16); b_Vt = Buf()
    P.v("pool", "memset", Vt[:], 1.0, writes=[b_Vt])
    mark = k.aoff
    k.swt = sb("swt", [128, 1024])
    w_in = sb("w_in", [128, 8, EVEN_IN], BF16); b_win = Buf()
    tri = sb("tri", [128, 4, 128]); maskAB = sb("maskAB", [128, 2, 256], BF16)
    P.dma(tri[:], g["tri_d"], writes=[b_const], eng="pool")
    P.dma(maskAB[:], g["maskAB_d"], writes=[b_const], eng="pool")
    gwext = sb("gwext", [33, 512]); P.dma(gwext[:], g["gwext_d"], writes=[b_const], eng="pool")
    mark_w = k.aoff
    k.wstage = [sb("wstage%d" % i, [128, 1168]) for i in range(2)]
    evin_d = g["evin_d"]
    for hf in range(2):
        lo_ = hf * 1168
        k.load_weight_bf16(w_in[:, :, lo_:lo_ + 1168], b_win,
                           lambda c, lo_=lo_: evin_d[c * 128:(c + 1) * 128, lo_:lo_ + 1168], 8, 1168, k.wstage, k.b_wstage)
    P.barrier()
    k.aoff = mark_w

    z_sb = sb("z_sb", [128, EVEN_IN]); b_z = Buf()
    ropeC = sb("ropeC", [128, 64]); ropeS = sb("ropeS", [128, 64]); b_rope = Buf()
    tmpA = sb("tmpA", [128, 1024]); b_tmpA = Buf()
    tmpB = sb("tmpB", [128, 1024]); b_tmpB = Buf()
    scr = sb("scr", [128, 64]); b_scr_ = Buf()
    qb = sb("qb", [128, 512], BF16); b_qb = Buf()
    kb = sb("kb", [128, 128], BF16); b_kb = Buf()
    qT_sb = sb("qT_sb", [64, 8, 128], BF16); b_qTsb = Buf()
    lrT = sb("lrT", [33, 128]); b_lrT = Buf()
    P.v("pool", "memset", lrT[:], 1.0, writes=[b_lrT])
    e_sb = sb("e_sb", [128, 512]); b_e = Buf()
    sp_ = sb("sp_", [128, 512]); b_sp = Buf()
    gqT = sb("gqT", [64, 4, 128]); gkT = sb("gkT", [64, 4, 128]); b_gq = Buf(); b_gk = Buf()
    EbT = [sb("EbT%d" % i, [64, 4, 128]) for i in range(2)]; b_EbT = [Buf(), Buf()]
    EnbT = [sb("EnbT%d" % i, [64, 4, 128]) for i in range(2)]; b_EnbT = [Buf(), Buf()]
    kdec = sb("kdec", [128, 256]); b_kdec = Buf()
    LT = [sb("LT%d" % i, [128, 2, 4, 64], BF16) for i in range(2)]
    b_LTq = [Buf(), Buf()]; b_LTa = [Buf(), Buf()]
    keT = [sb("keT%d" % i, [64, 4, 128], BF16) for i in range(2)]; b_keT = [Buf(), Buf()]
    kd = [sb("kd%d" % i, [128, 256], BF16) for i in range(2)]; b_kd = [Buf(), Buf()]
    v_bf = sb("v_bf", [128, 512], BF16); b_vbf = Buf()
    vhi = sb("vhi", [128, 2, 512], BF16); b_vhi = Buf()
    eend = [sb("eend%d" % i, [64, 4, 2]) for i in range(2)]; b_eend = [Buf(), Buf()]
    RH = [[sb("RH%d%d" % (i, j), [128, 4, 128], BF16) for j in range(2)] for i in range(2)]
    b_RHs = [[Buf(), Buf()], [Buf(), Buf()]]; b_RHv = [[Buf(), Buf()], [Buf(), Buf()]]
    S32 = [sb("S32_%d" % i, [64, 4, 128]) for i in range(2)]; b_S32 = [Buf(), Buf()]
    par = [0, 0]
    for d_ in range(2):
        P.v("pool", "memset", S32[d_][:], 0.0, writes=[b_S32[d_]])
        for j in range(2):
            P.v("pool", "memset", RH[d_][j][:], 0.0, writes=[b_RHs[d_][j], b_RHv[d_][j]])
    oa_sb = sb("oa_sb", [128, 512]); b_oa = Buf()
    rg_sb = sb("rg_sb", [128, 512]); b_rg = Buf()
    mixT_sb = sb("mixT_sb", [128, 8, 128], BF16); b_mixT = Buf()

    def mkset2():
        z2 = sb("z_sb2", [128, EVEN_IN])
        qb2 = sb("qb2", [128, 512], BF16); kb2 = sb("kb2", [128, 128], BF16); qT2 = sb("qT_sb2", [64, 8, 128], BF16)
        lrT2 = sb("lrT2", [33, 128]); bl2 = Buf()
        P.v("pool", "memset", lrT2[:], 1.0, writes=[bl2])
        gq2 = sb("gqT2", [64, 4, 128]); gk2 = sb("gkT2", [64, 4, 128])
        kdec2 = sb("kdec2", [128, 256])
        LT2 = [sb("LT2%d" % i, [128, 2, 4, 64], BF16) for i in range(2)]
        keT2 = [sb("keT2%d" % i, [64, 4, 128], BF16) for i in range(2)]
        kd2 = [sb("kd2%d" % i, [128, 256], BF16) for i in range(2)]
        v2 = sb("v_bf2", [128, 512], BF16); vh2 = sb("vhi2", [128, 2, 512], BF16)
        ee2 = [sb("eend2%d" % i, [64, 4, 2]) for i in range(2)]
        oa2 = sb("oa_sb2", [128, 512]); rg2 = sb("rg_sb2", [128, 512])
        return (z2, Buf(), qb2, Buf(), kb2, Buf(), qT2, Buf(), lrT2, bl2, gq2, gk2, Buf(), Buf(), kdec2, Buf(),
                LT2, [Buf(), Buf()], [Buf(), Buf()], keT2, [Buf(), Buf()], kd2, [Buf(), Buf()], v2, Buf(), vh2, Buf(),
                ee2, [Buf(), Buf()], oa2, Buf(), rg2, Buf())

    SETS = [(z_sb, b_z, qb, b_qb, kb, b_kb, qT_sb, b_qTsb, lrT, b_lrT, gqT, gkT, b_gq, b_gk, kdec, b_kdec,
             LT, b_LTq, b_LTa, keT, b_keT, kd, b_kd, v_bf, b_vbf, vhi, b_vhi, eend, b_eend, oa_sb, b_oa, rg_sb, b_rg),
            mkset2()]

    pMisc, bMisc = psum[3]
    pZG, bZG = psum[4]
    pBT, bBT = psum[5]
    pAT, bAT = psum[7][0], Buf("pAT")
    pS, bS = psum[7]
    pPO, bPO = psum[6]

    def scan_tile(d_, lt, b_ltq, b_lta, ke, b_ke, kdt, b_kdt, vb, b_vb, vh, b_vh, ee, b_ee, with_out):
        order = (0, 1) if d_ == 0 else (1, 0)
        pAT4 = pAT[:, 0:256].rearrange("p (h t) -> p h t", h=4)
        pS4 = pS[0:64, :].rearrange("p (h v) -> p h v", h=4)
        po, bpo = pPO, bPO
        for ch in order:
            p_ = par[d_]
            rh, brs, brv = RH[d_][p_], b_RHs[d_][p_], b_RHv[d_][p_]
            if with_out:
                for h in range(4):
                    P.mm(pAT4[64:128, h, :], ke[0:64, h, ch * 64:(ch + 1) * 64], lt[0:64, ch, h, :],
                         reads=[b_ke, b_ltq], writes=[bAT])
                P.v("dve", "tensor_tensor", lt[64:128, ch, :, :], pAT4[64:128, :, :],
                    maskAB[64:128, d_, :].rearrange("p (h t) -> p h t", h=4), ALU.mult,
                    reads=[bAT, b_const], writes=[b_lta])
                P.v("pool", "tensor_copy", rh[64:128, :, :], vh[64:128, ch, :].rearrange("p (h v) -> p h v", h=4),
                    reads=[b_vh], writes=[brv])
                for h in range(4):
                    P.mm(po[ch * 64:(ch + 1) * 64, h * 128:(h + 1) * 128], lt[:, ch, h, :], rh[:, h, :],
                         reads=[b_ltq, b_lta, brs, brv], writes=[bpo])
            for h in range(4):
                P.mm(pS4[:, h, :], kdt[ch * 64:(ch + 1) * 64, h * 64:(h + 1) * 64],
                     vb[ch * 64:(ch + 1) * 64, h * 128:(h + 1) * 128], reads=[b_kdt, b_vb], writes=[bS])
            for h in range(4):
                P.v("dve", "scalar_tensor_tensor", S32[d_][:, h, :], S32[d_][:, h, :], ee[:, h, ch:ch + 1],
                    pS4[:, h, :], ALU.mult, ALU.add, reads=[b_S32[d_], b_ee, bS], writes=[b_S32[d_]])
            nrh = RH[d_][1 - p_]
            P.act(nrh[0:64, :, :], S32[d_][:], AF.Copy, reads=[b_S32[d_]], writes=[b_RHs[d_][1 - p_]])
            par[d_] = 1 - p_

    def gla_prep(dirs, need_q):
        P.tr(pBT[0:32, 0:128], z_sb[:, 1536:1568], identF[:], reads=[b_z, b_const], writes=[bBT])
        P.act(lrT[0:32, :], pBT[0:32, 0:128], AF.Copy, reads=[bBT], writes=[b_lrT])
        P.mm(pZG[:], lrT[:], gwext[:], reads=[b_lrT, b_const], writes=[bZG])
        P.act(e_sb[:], pZG[:], AF.Exp, scale=-1.0, reads=[bZG], writes=[b_e])
        P.act(sp_[:], e_sb[:], AF.Ln, bias=1.0, reads=[b_e], writes=[b_sp])
        pBT4 = pBT[0:64, :].rearrange("p (h t) -> p h t", h=4)
        if need_q:
            for h in range(4):
                P.tr(pBT4[:, h, :], z_sb[:, h * 64:(h + 1) * 64], identF[:], reads=[b_z, b_const], writes=[bBT])
            P.act(gqT[:], pBT4, AF.Copy, reads=[bBT], writes=[b_gq])
        for h in range(4):
            P.tr(pBT4[:, h, :], z_sb[:, 256 + h * 64:256 + (h + 1) * 64], identF[:], reads=[b_z, b_const], writes=[bBT])
        P.act(gkT[:], pBT4, AF.Copy, reads=[bBT], writes=[b_gk])
        P.v("pool", "tensor_copy", v_bf[:], z_sb[:, 512:1024], reads=[b_z], writes=[b_vbf])
        P.dma(vhi[64:128, 0, :], v_bf[0:64, :], reads=[b_vbf], writes=[b_vhi], eng="pool")
        P.v("pool", "tensor_copy", vhi[64:128, 1, :], v_bf[64:128, :], reads=[b_vbf], writes=[b_vhi])
        for d_ in dirs:
            for h in range(4):
                P.mm(pBT4[:, h, :], sp_[:, d_ * 256 + h * 64:d_ * 256 + (h + 1) * 64], tri[:, d_, :],
                     reads=[b_sp, b_const], writes=[bBT])
            P.act(EbT[d_][:], pBT4, AF.Exp, scale=-1.0 / 16, reads=[bBT], writes=[b_EbT[d_]])
            P.act(EnbT[d_][:], pBT4, AF.Exp, scale=1.0 / 16, reads=[bBT], writes=[b_EnbT[d_]])
            P.mm(pZG[:, 0:256], tri[:, 2 + d_, :], sp_[:, d_ * 256:(d_ + 1) * 256], reads=[b_sp, b_const], writes=[bZG])
            P.act(kdec[:], pZG[:, 0:256], AF.Exp, scale=-1.0 / 16, reads=[bZG], writes=[b_kdec])
            if need_q:
                for ch in range(2):
                    P.v("dve", "scalar_tensor_tensor", LT[d_][0:64, ch, :, :],
                        gqT[:, :, ch * 64:(ch + 1) * 64], 0.125,
                        EbT[d_][:, :, ch * 64:(ch + 1) * 64], ALU.mult, ALU.mult,
                        reads=[b_gq, b_EbT[d_]], writes=[b_LTq[d_]])
            P.v("dve", "tensor_tensor", keT[d_][:], gkT[:], EnbT[d_][:], ALU.mult,
                reads=[b_gk, b_EnbT[d_]], writes=[b_keT[d_]])
            P.v("dve", "tensor_tensor", kd[d_][:], z_sb[:, 256:512], kdec[:], ALU.mult,
                reads=[b_z, b_kdec], writes=[b_kd[d_]])
            cols = (63, 127) if d_ == 0 else (0, 64)
            for ch in range(2):
                P.v("dve", "tensor_copy", eend[d_][:, :, ch:ch + 1], EbT[d_][:, :, cols[ch]:cols[ch] + 1],
                    reads=[b_EbT[d_]], writes=[b_eend[d_]])

    def inproj(H, bH, blocks):
        for bi, (lo, hi) in enumerate(blocks):
            pz, bz = psum[1 + bi % 2]
            for c in range(8):
                P.mm(pz[:, 0:hi - lo], H[:, c, :], w_in[:, c, lo:hi], c == 0, c == 7, reads=[bH, b_win], writes=[bz])
            if bi % 2 == 0:
                P.act(z_sb[:, lo:hi], pz[:, 0:hi - lo], AF.Copy, reads=[bz], writes=[b_z])
            else:
                P.v("dve", "tensor_copy", z_sb[:, lo:hi], pz[:, 0:hi - lo], reads=[bz], writes=[b_z])

    FULL = ((0, 512), (512, 1024), (1024, 1536), (1536, 2048), (2048, 2336))
    RBLK = ((256, 768), (768, 1024), (1536, 1568), (2080, 2336))

    def modA(kind, ti):
        if kind == "ctx":
            return modulate(ctx_d[ti * 128:(ti + 1) * 128, :], 0, 0, 1)
        return modulate(x_d[ti * 128:(ti + 1) * 128, :], 0, 0, 0)

    def tileA(kind, ti, pre):
        if kind == "ctx":
            src, w, slot, kidx = ctx_d[ti * 128:(ti + 1) * 128, :], 1, NOWN + ti, ti
        else:
            src, w, slot, kidx = x_d[ti * 128:(ti + 1) * 128, :], 0, ti, 2 + ti
        H, bH, X, bX = pre
        inproj(H, bH, RBLK if kind == "R" else FULL)
        rope = None
        if kind != "ctx":
            P.dma(ropeC[:], g["ropeC_d"][ti * 128:(ti + 1) * 128, :], writes=[b_rope], eng="pool")
            P.dma(ropeS[:], g["ropeS_d"][ti * 128:(ti + 1) * 128, :], writes=[b_rope], eng="pool")
            rope = (ropeC, ropeS, b_rope, tmpB, b_tmpB)
        pM8 = pMisc[0:64, :].bitcast(BF16).rearrange("p (h t) -> p h t", h=8)
        if kind != "R":
            k.head_norm_rope(qb[:], b_qb, z_sb[:, 1568:2080], b_z, 8, aqn, rope, tmpA, b_tmpA, scr, b_scr_)
            for h in range(8):
                P.tr(pM8[:, h, :], qb[:, h * 64:(h + 1) * 64], identB[:], reads=[b_qb, b_const], writes=[bMisc])
            P.act(qT_sb[:], pM8, AF.Copy, reads=[bMisc], writes=[b_qTsb])
            P.dma(QT_d[slot], qT_sb[:], reads=[b_qTsb], writes=[b_scr["QT"]])
        k.head_norm_rope(kb[:], b_kb, z_sb[:, 2080:2208], b_z, 2, akn, rope, tmpA, b_tmpA, scr, b_scr_)
        for h in range(2):
            P.tr(pM8[:, h, :], kb[:, h * 64:(h + 1) * 64], identB[:], reads=[b_kb, b_const], writes=[bMisc])
        P.act(KT[0:64, :, kidx * 128:(kidx + 1) * 128], pM8[:, 0:2, :], AF.Copy, reads=[bMisc], writes=[b_KT])
        P.v("pool", "tensor_copy", Vt[:, kidx, :, 0:64], z_sb[:, 2208:2336].rearrange("p (h d) -> p h d", h=2),
            reads=[b_z], writes=[b_Vt])
        if kind == "R":
            gla_prep((1,), False)
            scan_tile(1, LT[1], b_LTq[1], b_LTa[1], keT[1], b_keT[1], kd[1], b_kd[1], v_bf, b_vbf, vhi, b_vhi,
                      eend[1], b_eend[1], False)
            return
        gla_prep((0, 1), True)
        P.act(rg_sb[:], z_sb[:, 1024:1536], AF.Exp, scale=-1.0, reads=[b_z], writes=[b_rg])
        P.act(rg_sb[:], rg_sb[:], AF.Ln, bias=1.0, reads=[b_rg], writes=[b_rg])
        P.act(rg_sb[:], rg_sb[:], AF.Exp, scale=-1.0, reads=[b_rg], writes=[b_rg])
        P.v("pool", "tensor_tensor", rg_sb[:].rearrange("p (h v) -> p h v", h=4), rg_sb[:].rearrange("p (h v) -> p h v", h=4),
            glan[:, 0:128].unsqueeze(1).to_broadcast([128, 4, 128]), ALU.mult, reads=[b_rg, b_const], writes=[b_rg])
        P.v("pool", "tensor_tensor", rg_sb[:], rg_sb[:], z_sb[:, 1024:1536], ALU.mult, reads=[b_rg, b_z], writes=[b_rg])
        P.dma(PB_rg[slot], rg_sb[:], reads=[b_rg], writes=[b_scr["PBrg"]])
        P.dma(PB_lt[slot], LT[1][0:64].rearrange("p c h t -> p (c h t)"), reads=[b_LTq[1]], writes=[b_scr["PBlt"]])
        P.dma(PB_ke[slot], keT[1][:].rearrange("p h t -> p (h t)"), reads=[b_keT[1]], writes=[b_scr["PBke"]])
        P.dma(PB_kd[slot], kd[1][:], reads=[b_kd[1]], writes=[b_scr["PBkd"]])
        P.dma(PB_v[slot], v_bf[:], reads=[b_vbf], writes=[b_scr["PBv"]])
        P.dma(PB_ee[slot], eend[1][:].rearrange("p h c -> p (h c)"), reads=[b_eend[1]], writes=[b_scr["PBee"]])
        scan_tile(0, LT[0], b_LTq[0], b_LTa[0], keT[0], b_keT[0], kd[0], b_kd[0], v_bf, b_vbf, vhi, b_vhi,
                  eend[0], b_eend[0], True)
        P.act(oa_sb[:], pPO[:], AF.Copy, reads=[bPO], writes=[b_oa])
        P.dma(OA_d[slot], oa_sb[:], reads=[b_oa], writes=[b_scr["OA"]])

    ob_sb = tmpB[:, 0:512]; b_ob = b_tmpB
    ab_sb = tmpB[:, 512:768].bitcast(BF16); b_ab = Buf()

    def tileB(slot):
        P.dma(LT[1][0:64].rearrange("p c h t -> p (c h t)"), PB_lt[slot], reads=[b_scr["PBlt"]], writes=[b_LTq[1]])
        P.dma(keT[1][:].rearrange("p h t -> p (h t)"), PB_ke[slot], reads=[b_scr["PBke"]], writes=[b_keT[1]])
        P.dma(kd[1][:], PB_kd[slot], reads=[b_scr["PBkd"]], writes=[b_kd[1]])
        P.dma(v_bf[:], PB_v[slot], reads=[b_scr["PBv"]], writes=[b_vbf])
        P.dma(vhi[64:128, 0, :], PB_v[slot][0:64, :], reads=[b_scr["PBv"]], writes=[b_vhi], eng="pool")
        P.dma(vhi[64:128, 1, :], PB_v[slot][64:128, :], reads=[b_scr["PBv"]], writes=[b_vhi], eng="pool")
        P.dma(eend[1][:].rearrange("p h c -> p (h c)"), PB_ee[slot], reads=[b_scr["PBee"]], writes=[b_eend[1]])
        P.dma(oa_sb[:], OA_d[slot], reads=[b_scr["OA"]], writes=[b_oa], eng="pool")
        P.dma(rg_sb[:], PB_rg[slot], reads=[b_scr["PBrg"]], writes=[b_rg], eng="pool")
        scan_tile(1, LT[1], b_LTq[1], b_LTa[1], keT[1], b_keT[1], kd[1], b_kd[1], v_bf, b_vbf, vhi, b_vhi,
                  eend[1], b_eend[1], True)
        P.v("dve", "tensor_tensor", ob_sb[:], pPO[:], oa_sb[:], ALU.add, reads=[bPO, b_oa], writes=[b_ob])
        o3 = ob_sb[:].rearrange("p (h v) -> p h v", h=4)
        t3 = tmpA[:, 0:512].rearrange("p (h v) -> p h v", h=4)
        P.v("dve", "tensor_tensor", t3, o3, o3, ALU.mult, reads=[b_ob], writes=[b_tmpA])
        P.v("dve", "tensor_reduce", scr[:, 0:4], t3, AX.X, ALU.add, reads=[b_tmpA], writes=[b_scr_])
        P.act(scr[:, 16:20], scr[:, 0:4], AF.Ln, scale=1.0 / 128, bias=1e-6, reads=[b_scr_], writes=[b_scr_])
        P.act(scr[:, 32:36], scr[:, 16:20], AF.Exp, scale=-0.5, reads=[b_scr_], writes=[b_scr_])
        P.v("dve", "tensor_tensor", t3, o3, scr[:, 32:36].unsqueeze(2).to_broadcast([128, 4, 128]), ALU.mult,
            reads=[b_ob, b_scr_], writes=[b_tmpA])
        P.v("dve", "tensor_tensor", ab_sb[:], tmpA[:, 0:512], rg_sb[:], ALU.mult, reads=[b_tmpA, b_rg], writes=[b_ab])
        pM4 = pMisc[:].bitcast(BF16)[:, 0:512].rearrange("p (c t) -> p c t", c=4)
        for c in range(4):
            P.tr(pM4[:, c, :], ab_sb[:, c * 128:(c + 1) * 128], identB[:], reads=[b_ab, b_const], writes=[bMisc])
        P.act(mixT_sb[:, 0:4, :], pM4, AF.Copy, reads=[bMisc], writes=[b_mixT])
        P.dma(MIXT_d[slot][:, 0:4, :], mixT_sb[:, 0:4, :], reads=[b_mixT], writes=[b_scr["MIXT"]])

    seqA = [("ctx", 0), ("ctx", 1)] + [("R", ti) for ti in range(31, NOWN - 1, -1)] + [("own", ti) for ti in range(NOWN)]
    pre = modA(*seqA[0])
    nset = 0
    for si, (kind, ti) in enumerate(seqA):
        nxt = modA(*seqA[si + 1]) if si + 1 < len(seqA) else None
        (z_sb, b_z, qb, b_qb, kb, b_kb, qT_sb, b_qTsb, lrT, b_lrT, gqT, gkT, b_gq, b_gk, kdec, b_kdec,
         LT, b_LTq, b_LTa, keT, b_keT, kd, b_kd, v_bf, b_vbf, vhi, b_vhi, eend, b_eend, oa_sb, b_oa, rg_sb, b_rg) = SETS[nset % 2]
        nset += 1
        tileA(kind, ti, pre)
        pre = nxt
        if (kind, ti) == ("ctx", 1):
            for sl in (NOWN + 1, NOWN + 0):
                (z_sb, b_z, qb, b_qb, kb, b_kb, qT_sb, b_qTsb, lrT, b_lrT, gqT, gkT, b_gq, b_gk, kdec, b_kdec,
                 LT, b_LTq, b_LTa, keT, b_keT, kd, b_kd, v_bf, b_vbf, vhi, b_vhi, eend, b_eend, oa_sb, b_oa, rg_sb, b_rg) = SETS[nset % 2]
                nset += 1
                tileB(sl)
    for ti in range(NOWN - 1, -1, -1):
        (z_sb, b_z, qb, b_qb, kb, b_kb, qT_sb, b_qTsb, lrT, b_lrT, gqT, gkT, b_gq, b_gk, kdec, b_kdec,
         LT, b_LTq, b_LTa, keT, b_keT, kd, b_kd, v_bf, b_vbf, vhi, b_vhi, eend, b_eend, oa_sb, b_oa, rg_sb, b_rg) = SETS[nset % 2]
        nset += 1
        tileB(ti)

    if "stopAB" in dbg:
        return
    k.areset(mark)
    k.wstage = [sb("wstage%d" % i, [128, D]) for i in range(2)]
    w_out = sb("w_out", [128, 4, D], BF16); b_wout = Buf()
    w_outB = sb("w_outB", [64, 8, D], BF16); b_woutB = Buf()
    evout_d = g["evout_d"]
    k.load_weight_bf16(w_out, b_wout, lambda c: evout_d[c * 128:(c + 1) * 128, :], 4, D, k.wstage, k.b_wstage)
    for h in range(8):
        s_, bs_ = k.wstage[h % 2], k.b_wstage[h % 2]
        P.dma(s_[0:64, :], evout_d[512 + h * 64:512 + (h + 1) * 64, :], writes=[bs_], eng=("sp" if h % 2 == 0 else "pool"))
        k.cast(("pool", "dve", "act")[h % 3], w_outB[:, h, :], s_[0:64, :], [bs_], [b_woutB])
    ones_f = sb("ones_f", [128, 64]); P.v("pool", "memset", ones_f[:], 1.0, writes=[b_const])
    qT_in = [sb("qT_in%d" % i, [128, 8, 128], BF16) for i in range(2)]; b_qTin = [Buf(), Buf()]
    for i_ in range(2):
        P.v("pool", "memset", qT_in[i_][64:128], 0.0, writes=[b_qTin[i_]])
        P.v("pool", "memset", qT_in[i_][64:65], 1.0, writes=[b_qTin[i_]])
    rsr = sb("rsr", [128, 512]); b_rsr = Buf()
    bc_sb = sb("bc_sb", [64, 512]); b_bc = Buf()
    OT = sb("OT", [64, 8, 128], BF16); b_OT = Buf()
    mT_in = sb("mT_in", [128, 4, 128], BF16); b_mTin = Buf()
    x1_sb = sb("x1_sb", [128, D]); b_x1 = Buf()
    pO = [psum[6], psum[7]]
    SG = [(k.psall[:, 2 * g_ * 512:(2 * g_ + 2) * 512], [psum[2 * g_][1], psum[2 * g_ + 1][1]]) for g_ in range(3)]
    pBC, bBC = psum[0]
    NPT = 4
    PT = [sb("PTp%d" % i, [128, 1024], BF16) for i in range(NPT)]; b_PT = [Buf() for _ in range(NPT)]
    cstate = dict(it=0)

    mT_ins = [mT_in, sb("mT_in2", [128, 4, 128], BF16)]; b_mTins = [b_mTin, Buf()]
    x_ins = [sb("xC%d" % i, [128, D]) for i in range(2)]; b_xins = [Buf(), Buf()]
    ones_b = sb("ones_b", [128, 64], BF16); P.v("pool", "memset", ones_b[:], 1.0, writes=[b_const])
    rsb = sb("rsb", [128, 2, 512], BF16); b_rsb = Buf()
    bcs = [sb("bcs%d" % i, [64, 512]) for i in range(2)]; b_bcs = [Buf(), Buf()]
    cpre = dict(n=0)

    def loadC(kind, ti):
        if kind == "ctx":
            slot, src = NOWN + ti, ctx_d[ti * 128:(ti + 1) * 128, :]
        else:
            slot, src = ti, x_d[ti * 128:(ti + 1) * 128, :]
        i = cpre["n"] % 2
        cpre["n"] += 1
        P.dma(qT_in[i][0:64], QT_d[slot], reads=[b_scr["QT"]], writes=[b_qTin[i]])
        P.dma(mT_ins[i][:], MIXT_d[slot][:, 0:4, :], reads=[b_scr["MIXT"]], writes=[b_mTins[i]], eng="pool")
        P.dma(x_ins[i][:], src, writes=[b_xins[i]])
        return (qT_in[i], b_qTin[i], mT_ins[i], b_mTins[i], x_ins[i], b_xins[i])

    def tileC(kind, ti, pre):
        if kind == "ctx":
            slot, w, src, keys = NOWN + ti, 1, ctx_d[ti * 128:(ti + 1) * 128, :], [0, 1]
        else:
            slot, w, src, keys = ti, 0, x_d[ti * 128:(ti + 1) * 128, :], list(range(34))
        Q, bQ, mT_in, b_mTin, X, bX = pre
        units = [(kvh, keys[a_:a_ + 2]) for kvh in range(2) for a_ in range(0, len(keys), 2)]
        pend = []

        def pv(u, pt, bpt):
            kvh, kts = u
            po, bpo = pO[kvh]
            for j, kt in enumerate(kts):
                P.mm(po[0:65, :], Vt[:, kt, kvh, 0:65], pt[:, j * 512:(j + 1) * 512], kt == keys[0], kt == keys[-1],
                     reads=[bpt, b_Vt], writes=[bpo])

        for u in units:
            kvh, kts = u
            it = cstate["it"]
            cstate["it"] += 1
            psc, bscs = SG[it % 3]
            pt, bpt = PT[it % NPT], b_PT[it % NPT]
            n = len(kts)
            for j, kt in enumerate(kts):
                P.mm(psc[:, j * 512:(j + 1) * 512], KT[:, kvh, kt * 128:(kt + 1) * 128],
                     Q[:, kvh * 4:(kvh + 1) * 4, :].rearrange("p h t -> p (h t)"),
                     reads=[b_KT, bQ], writes=[bscs[j]])
            P.act(pt[:, 0:n * 512], psc[:, 0:n * 512], AF.Exp, scale=0.125,
                  reads=bscs[0:n], writes=[bpt])
            pend.append((u, pt, bpt))
            if len(pend) > 2:
                pv(*pend.pop(0))
        while pend:
            pv(*pend.pop(0))
        for kvh in range(2):
            po, bpo = pO[kvh]
            P.act(rsr[64:65, :], po[64:65, :], AF.Ln, reads=[bpo], writes=[b_rsr])
            P.act(rsb[64:65, kvh, :], rsr[64:65, :], AF.Exp, scale=-1.0, reads=[b_rsr], writes=[b_rsb])
        for kvh in range(2):
            pb_, bb_ = psum[2 + kvh]
            P.mm(pb_[0:64, :], ones_b[64:65, 0:64], rsb[64:65, kvh, :], reads=[b_const, b_rsb], writes=[bb_])
        for kvh in range(2):
            pb_, bb_ = psum[2 + kvh]
            P.v("dve", "tensor_copy", bcs[kvh][:], pb_[0:64, :], reads=[bb_], writes=[b_bcs[kvh]])
        for kvh in range(2):
            po, bpo = pO[kvh]
            P.v("dve", "tensor_tensor", OT[:, kvh * 4:(kvh + 1) * 4, :].rearrange("p h t -> p (h t)"), po[0:64, :], bcs[kvh][:],
                ALU.mult, reads=[bpo, b_bcs[kvh]], writes=[b_OT])
        for hh in range(2):
            pz, bz = psum[hh]
            for c in range(4):
                P.mm(pz[:], mT_in[:, c, :], w_out[:, c, hh * 512:(hh + 1) * 512], c == 0, False,
                     reads=[b_mTin, b_wout], writes=[bz])
            for h in range(8):
                P.mm(pz[:], OT[:, h, :], w_outB[:, h, hh * 512:(hh + 1) * 512], False, h == 7,
                     reads=[b_OT, b_woutB], writes=[bz])
            P.v("dve", "tensor_tensor", x1_sb[:, hh * 512:(hh + 1) * 512], pz[:],
                gateB_all[:, k.gslot[(0, 0, w)], hh * 512:(hh + 1) * 512],
                ALU.mult, reads=[bz, b_gateB[0]], writes=[b_x1])
        P.v("pool", "tensor_tensor", x1_sb[:], x1_sb[:], X[:], ALU.add, reads=[b_x1, bX], writes=[b_x1])
        P.dma(X1_d[slot * 128:(slot + 1) * 128, :], x1_sb[:], reads=[b_x1], writes=[b_scr["X1"]])

    tiles_c = [("ctx", 0), ("ctx", 1)] + [("own", t) for t in range(NOWN)]
    if "fewC" in dbg:
        tiles_c = [("ctx", 0), ("own", 0), ("own", 16)]
    s1 = k.s0_steps(1, "L1", psum[0], psum[0], psum[1])
    preC = loadC(*tiles_c[0])
    for si, (kind, ti) in enumerate(tiles_c):
        nxtC = loadC(*tiles_c[si + 1]) if si + 1 < len(tiles_c) else None
        if s1:
            s1.pop(0)()
        tileC(kind, ti, preC)
        preC = nxtC
    while s1:
        s1.pop(0)()
    if "stopL0mix" in dbg:
        return
    X2_d, X3_d, out_d = g["X2_d"], g["X3_d"], g["out_d"]
    b_scr["X2"] = Buf("X2"); b_scr["X3"] = Buf("X3")
    tiles = []
    for slot in range(NTOK0):
        w = 1 if slot >= NOWN else 0
        tiles.append((X1_d[slot * 128:(slot + 1) * 128, :], b_scr["X1"], X2_d[slot * 128:(slot + 1) * 128, :], b_scr["X2"], w))
    if "fewM" in dbg:
        tiles = [tiles[0], tiles[16], tiles[17]]
    moe(k, 0, tiles, "a")
    if "stopL0" in dbg or "stopD1" in dbg or "stopD2" in dbg:
        return
    layer1(k, dbg)
    if "stopL1" in dbg:
        return
    tiles = [(X3_d[t * 128:(t + 1) * 128, :], b_scr["X3"], out_d[t * 128:(t + 1) * 128, :], None, 0) for t in range(16)]
    moe(k, 1, tiles, "b")


def moe(k, l, tiles, tag):
    g = k.env
    nc, P = k.nc, k.P
    ar, psum = k.ar, k.psum
    identF, b_const = g["identF"], g["b_const"]
    NT = len(tiles)
    NTOK = NT * 128
    k.areset(0)
    hT_all = ar("hT_all" + tag, [128, 8, NTOK], BF16); b_hTall = Buf()
    yacc = ar("yacc" + tag, [128, NT, D]); b_yacc = [Buf() for _ in range(NT)]
    comb_all = ar("comb" + tag, [128, NT, 16]); b_combt = [Buf() for _ in range(NT)]
    mark = k.aoff
    k.xnf = ar("xnf" + tag, [128, D]); k.hTf = ar("hTf" + tag, [128, 8, 128])
    rw = ar("rw" + tag, [128, 8, 20]); rb = ar("rb" + tag, [128, 20]); b_rw = Buf()
    P.dma(rw[:], g["rw_d"][l].rearrange("(c p) n -> p c n", p=128), writes=[b_rw], eng="pool")
    P.dma(rb[:], g["rb_d"][l].partition_broadcast(128).rearrange("p o n -> p (o n)"), writes=[b_rw], eng="pool")
    lg = ar("lg" + tag, [128, 20]); b_lg = Buf()
    rt = ar("rt" + tag, [128, 64]); b_rt = Buf()
    pR, bR = psum[2]
    b_hTt = [Buf() for _ in range(NT)]

    def d1(ti):
        (src, sbuf_, dst, dbuf_, w) = tiles[ti]
        H, bH, X, bX = k.modulate(src, l, 1, w, fp32=True, src_buf=sbuf_)
        P.v("pool", "tensor_copy", hT_all[:, :, ti * 128:(ti + 1) * 128], H[:], reads=[bH], writes=[b_hTt[ti]])
        for c_ in range(8):
            P.mm(pR[:, 0:20], H[:, c_, :], rw[:, c_, :], c_ == 0, c_ == 7, reads=[bH, b_rw], writes=[bR])
        P.v("dve", "tensor_tensor", lg[:], pR[:, 0:20], rb[:], ALU.add, reads=[bR, b_rw], writes=[b_lg])
        gl, el = lg[:, 0:4], lg[:, 4:20]
        R_ = lambda a, b: rt[:, a:b]
        gmax, ngmax, sume, gw = R_(0, 1), R_(1, 2), R_(2, 3), R_(3, 4)
        ohg, eg, esel, oh1, msk, oh2, ew, sg = R_(4, 8), R_(8, 12), R_(12, 16), R_(16, 20), R_(20, 24), R_(24, 28), R_(28, 32), R_(32, 36)
        m1, m2, dd, e2, w1, w2 = R_(36, 37), R_(37, 38), R_(38, 39), R_(39, 40), R_(40, 41), R_(41, 42)
        rd, wr = [b_lg, b_rt], [b_rt]
        V = lambda name, *a, **kw: P.v("dve", name, *a, reads=rd, writes=wr, **kw)
        V("tensor_reduce", gmax, gl, AX.X, ALU.max)
        V("tensor_scalar", ohg, gl, gmax, None, ALU.is_equal)
        V("tensor_scalar", ngmax, gmax, -1.0, None, ALU.mult)
        P.act(eg, gl, AF.Exp, bias=ngmax, accum_out=sume, reads=rd, writes=wr)
        V("reciprocal", gw, sume)
        V("tensor_scalar", esel, el[:, 0:4], ohg[:, 0:1], None, ALU.mult)
        for gi in range(1, 4):
            V("scalar_tensor_tensor", esel, el[:, gi * 4:(gi + 1) * 4], ohg[:, gi:gi + 1], esel, ALU.mult, ALU.add)
        V("tensor_reduce", m1, esel, AX.X, ALU.max)
        V("tensor_scalar", oh1, esel, m1, None, ALU.is_equal)
        V("scalar_tensor_tensor", msk, oh1, -NEG_BIG, esel, ALU.mult, ALU.add)
        V("tensor_reduce", m2, msk, AX.X, ALU.max)
        V("tensor_scalar", oh2, msk, m2, None, ALU.is_equal)
        V("tensor_tensor", dd, m2, m1, ALU.subtract)
        P.act(e2, dd, AF.Exp, reads=rd, writes=wr)
        V("tensor_scalar", w1, e2, 1.0, None, ALU.add)
        V("reciprocal", w1, w1)
        V("tensor_tensor", w2, e2, w1, ALU.mult)
        V("tensor_scalar", ew, oh1, w1, None, ALU.mult)
        V("scalar_tensor_tensor", ew, oh2, w2, ew, ALU.mult, ALU.add)
        V("tensor_scalar", sg, ohg, gw, None, ALU.mult)
        for gi in range(4):
            P.v("dve", "tensor_scalar", comb_all[:, ti, gi * 4:(gi + 1) * 4], ew, sg[:, gi:gi + 1], None, ALU.mult,
                reads=[b_rt], writes=[b_combt[ti]])
    wst = [ar("mwst%d" % i + tag, [128, 512]) for i in range(2)]; b_wst = [Buf(), Buf()]
    Wg = [ar("Wg%d" % i + tag, [128, 8, 256], BF16) for i in range(2)]
    Wu = [ar("Wu%d" % i + tag, [128, 8, 256], BF16) for i in range(2)]
    Wd = [ar("Wd%d" % i + tag, [128, 2, D], BF16) for i in range(2)]
    b_W = [[Buf(), Buf(), Buf()] for _ in range(2)]
    sa = [ar("sa%d" % i + tag, [128, 512]) for i in range(2)]; b_sa = [Buf(), Buf()]
    hid = [ar("hid%d" % i + tag, [128, 2, 512], BF16) for i in range(2)]; b_hid = [Buf(), Buf()]
    pCW, bCW = psum[0]
    pAs = [psum[1], psum[2]]
    pUs = [psum[3], psum[4]]
    pYs = [psum[5], psum[6], psum[0]]
    wcnt = 0
    blocks = [(s, min(512, NTOK - s)) for s in range(0, NTOK, 512)]
    it = 0
    yi = 0
    def load_w(e):
        nonlocal wcnt
        pe_ = e % 2
        srcs = (g["wg_d"][l, e].rearrange("(c p) f -> p c f", p=128), g["wu_d"][l, e].rearrange("(c p) f -> p c f", p=128),
                g["wd_d"][l, e].rearrange("(c p) n -> p c n", p=128))
        dsts = (Wg[pe_], Wu[pe_], Wd[pe_])
        for wi in range(3):
            for q4 in range(4):
                s_, bs_ = wst[wcnt % 2], b_wst[wcnt % 2]
                if wi < 2:
                    sv = s_[:].rearrange("p (c f) -> p c f", c=2)
                    sview = srcs[wi][:, 2 * q4:2 * q4 + 2, :]
                    dview = dsts[wi][:, 2 * q4:2 * q4 + 2, :]
                else:
                    sv = s_[:]
                    sview = srcs[wi][:, q4 // 2, (q4 % 2) * 512:(q4 % 2 + 1) * 512]
                    dview = dsts[wi][:, q4 // 2, (q4 % 2) * 512:(q4 % 2 + 1) * 512]
                P.dma(sv, sview, writes=[bs_], eng=("sp" if wcnt % 2 == 0 else "pool"))
                P.v("pool", "tensor_copy", dview, sv, reads=[bs_], writes=[b_W[pe_][wi]])
                wcnt += 1

    def gu(e, t0, n, i):
        pe_ = e % 2
        for fc in range(2):
            pa, ba = pAs[fc]
            pu, bu = pUs[fc]
            for c_ in range(8):
                P.mm(pa[:, 0:n], Wg[pe_][:, c_, fc * 128:(fc + 1) * 128], hT_all[:, c_, t0:t0 + n], c_ == 0, c_ == 7,
                     reads=[b_W[pe_][0]] + b_hTt[t0 // 128:(t0 + n) // 128], writes=[ba])
            for c_ in range(8):
                P.mm(pu[:, 0:n], Wu[pe_][:, c_, fc * 128:(fc + 1) * 128], hT_all[:, c_, t0:t0 + n], c_ == 0, c_ == 7,
                     reads=[b_W[pe_][1]] + b_hTt[t0 // 128:(t0 + n) // 128], writes=[bu])
            P.act(sa[fc][:, 0:n], pa[:, 0:n], AF.Silu, reads=[ba], writes=[b_sa[fc]])
            P.v("dve", "tensor_tensor", hid[i][:, fc, 0:n], sa[fc][:, 0:n], pu[:, 0:n], ALU.mult,
                reads=[b_sa[fc], bu], writes=[b_hid[i]])

    def dn(e, t0, n, i):
        nonlocal yi
        pe_ = e % 2
        ntile = n // 128
        for j in range(ntile):
            tile_i = t0 // 128 + j
            for dh in range(2):
                py, by = pYs[yi % 3]
                yi += 1
                for fc in range(2):
                    P.mm(py[:], hid[i][:, fc, j * 128:(j + 1) * 128], Wd[pe_][:, fc, dh * 512:(dh + 1) * 512],
                         fc == 0, fc == 1, reads=[b_hid[i], b_W[pe_][2]], writes=[by])
                ya = yacc[:, tile_i, dh * 512:(dh + 1) * 512]
                cs = comb_all[:, tile_i, e:e + 1]
                if e == 0:
                    P.v("dve", "tensor_scalar", ya, py[:], cs, None, ALU.mult, reads=[by, b_combt[tile_i]], writes=[b_yacc[tile_i]])
                else:
                    P.v("dve", "scalar_tensor_tensor", ya, py[:], cs, ya, ALU.mult, ALU.add,
                        reads=[by, b_combt[tile_i], b_yacc[tile_i]], writes=[b_yacc[tile_i]])

    items = [(e, t0, n) for e in range(16) for (t0, n) in blocks]
    pend = None
    last_e = -1
    for idx, (e, t0, n) in enumerate(items):
        if e != last_e:
            if e == 0:
                load_w(0)
            if e + 1 < 16:
                pass
            last_e = e
        if e == 0:
            for tq in range(t0 // 128, (t0 + n) // 128):
                d1(tq)
        gu(e, t0, n, idx % 2)
        if pend is not None:
            dn(*pend)
        pend = (e, t0, n, idx % 2)
        if t0 == blocks[0][0] and e + 1 < 16:
            load_w(e + 1)
    dn(*pend)
    xo = [k.xnf, k.hTf[:].rearrange("p c t -> p (c t)")]; b_xo = [k.env["b_xnf"], k.env["b_hTf"]]
    for ti, (src, sbuf_, dst, dbuf_, w) in enumerate(tiles):
        i = ti % 2
        X, bX = g["xt"][i], g["b_xt"][i]
        P.dma(X[:], src, reads=([sbuf_] if sbuf_ else []), writes=[bX], eng="pool")
        gs = k.gslot[(l, 1, w)]
        P.v("dve", "tensor_tensor", xo[i][:], yacc[:, ti, :], k.gateB_all[:, gs, :], ALU.mult,
            reads=[b_yacc[ti], k.b_gateB[l]], writes=[b_xo[i]])
        P.v("pool", "tensor_tensor", xo[i][:], xo[i][:], X[:], ALU.add, reads=[b_xo[i], bX], writes=[b_xo[i]])
        P.dma(dst, xo[i][:], reads=[b_xo[i]], writes=([dbuf_] if dbuf_ else []))


def layer1(k, dbg):
    g = k.env
    nc, P = k.nc, k.P
    ar, psum = k.ar, k.psum
    identF, identB, b_const = g["identF"], g["identB"], g["b_const"]
    X2_d, X3_d, QT1_d = g["X2_d"], g["X3_d"], g["QT1_d"]
    bnd, b_bnd = k.bnd, k.b_bnd
    sqn, skn, sinkb = g["sqn"], g["skn"], g["sinkb"]
    b_X2, b_X3 = k.b_scr["X2"], k.b_scr["X3"]
    b_QT1 = Buf()
    k.areset(0)
    NK = 19
    KT = ar("KT1", [128, 2, NK * 128], BF16); b_KT = Buf()
    P.v("pool", "memset", KT[64:128], 0.0, writes=[b_KT])
    Vt = ar("Vt1", [128, NK, 2, 66], BF16); b_Vt = Buf()
    P.v("pool", "memset", Vt[:], 1.0, writes=[b_Vt])
    wmask = ar("wmask", [128, 2, 128], BF16)
    P.dma(wmask[:], g["wmask_d"], writes=[b_const], eng="pool")
    k.wstage = [ar("wstage1%d" % i, [128, 1280]) for i in range(2)]
    k.swt = ar("swt1", [128, 1024])
    w_in = ar("w_in1", [128, 8, 1280], BF16); b_win = Buf()
    odin_d, odout_d = g["odin_d"], g["odout_d"]
    k.load_weight_bf16(w_in, b_win, lambda c: odin_d[c * 128:(c + 1) * 128, :], 8, 1280, k.wstage, k.b_wstage)
    w_outB = ar("w_out1B", [64, 16, D], BF16); b_woutB = Buf()
    for h in range(16):
        s_, bs_ = k.wstage[h % 2], k.b_wstage[h % 2]
        P.dma(s_[0:64, 0:D], odout_d[h * 64:(h + 1) * 64, :], writes=[bs_], eng=("sp" if h % 2 == 0 else "pool"))
        k.cast(("pool", "dve", "act")[h % 3], w_outB[:, h, :], s_[0:64, 0:D], [bs_], [b_woutB])
    ones_f = ar("ones_f1", [128, 64]); P.v("pool", "memset", ones_f[:], 1.0, writes=[b_const])
    z_sb = ar("z_sb1", [128, 1280]); b_z = Buf()
    ropeC = ar("ropeC1", [128, 64]); ropeS = ar("ropeS1", [128, 64]); b_rope = Buf()
    tmpA = ar("tmpA1", [128, 1024]); b_tmpA = Buf()
    tmpB = ar("tmpB1", [128, 1024]); b_tmpB = Buf()
    scr = ar("scr1", [128, 64]); b_scr_ = Buf()
    qb = ar("qb1", [128, 1024], BF16); b_qb = Buf()
    kb = ar("kb1", [128, 128], BF16); b_kb = Buf()
    qT_sb = ar("qT_sb1", [64, 16, 128], BF16); b_qTsb = Buf()
    pMisc, bMisc = psum[3]
    pMisc2, bMisc2 = psum[4]

    def modE(kind, ti):
        if kind == "ctx":
            return k.modulate(X2_d[(NOWN + ti) * 128:(NOWN + ti + 1) * 128, :], 1, 0, 1, src_buf=b_X2)
        return k.modulate(X2_d[ti * 128:(ti + 1) * 128, :], 1, 0, 0, src_buf=b_X2)

    def tileE(kind, ti, pre):
        if kind == "ctx":
            src, w, kidx = X2_d[(NOWN + ti) * 128:(NOWN + ti + 1) * 128, :], 1, ti
        else:
            src, w, kidx = X2_d[ti * 128:(ti + 1) * 128, :], 0, 2 + ti
        need_q = (kind != "ctx" and ti < 16)
        H, bH, X, bX = pre
        blocks = ((0, 512), (512, 1024), (1024, 1280)) if need_q else ((1024, 1280),)
        for bi, (lo, hi) in enumerate(blocks):
            pz, bz = psum[1 + bi % 2]
            for c in range(8):
                P.mm(pz[:, 0:hi - lo], H[:, c, :], w_in[:, c, lo:hi], c == 0, c == 7, reads=[bH, b_win], writes=[bz])
            P.act(z_sb[:, lo:hi], pz[:, 0:hi - lo], AF.Copy, reads=[bz], writes=[b_z])
        rope = None
        if kind != "ctx":
            P.dma(ropeC[:], g["ropeC_d"][ti * 128:(ti + 1) * 128, :], writes=[b_rope], eng="pool")
            P.dma(ropeS[:], g["ropeS_d"][ti * 128:(ti + 1) * 128, :], writes=[b_rope], eng="pool")
            rope = (ropeC, ropeS, b_rope, tmpB, b_tmpB)
        pM8 = pMisc[0:64, :].bitcast(BF16).rearrange("p (h t) -> p h t", h=8)
        pM8b = pMisc2[0:64, :].bitcast(BF16).rearrange("p (h t) -> p h t", h=8)
        if need_q:
            k.head_norm_rope(qb[:], b_qb, z_sb[:, 0:1024], b_z, 16, sqn, rope, tmpA, b_tmpA, scr, b_scr_)
            for h in range(16):
                pm, bm = (pM8, bMisc) if h < 8 else (pM8b, bMisc2)
                P.tr(pm[:, h % 8, :], qb[:, h * 64:(h + 1) * 64], identB[:], reads=[b_qb, b_const], writes=[bm])
            P.act(qT_sb[:, 0:8, :], pM8, AF.Copy, reads=[bMisc], writes=[b_qTsb])
            P.act(qT_sb[:, 8:16, :], pM8b, AF.Copy, reads=[bMisc2], writes=[b_qTsb])
            P.dma(QT1_d[ti], qT_sb[:], reads=[b_qTsb], writes=[b_QT1])
        k.head_norm_rope(kb[:], b_kb, z_sb[:, 1024:1152], b_z, 2, skn, rope, tmpA, b_tmpA, scr, b_scr_)
        for h in range(2):
            P.tr(pM8[:, h, :], kb[:, h * 64:(h + 1) * 64], identB[:], reads=[b_kb, b_const], writes=[bMisc])
        P.act(KT[0:64, :, kidx * 128:(kidx + 1) * 128], pM8[:, 0:2, :], AF.Copy, reads=[bMisc], writes=[b_KT])
        P.v("pool", "tensor_copy", Vt[:, kidx, :, 0:64], z_sb[:, 1152:1280].rearrange("p (h d) -> p h d", h=2),
            reads=[b_z], writes=[b_Vt])

    seqE = [("ctx", 0), ("ctx", 1)] + [("own", ti) for ti in range(17)]
    SETE = [(z_sb, b_z, qb, b_qb, kb, b_kb, qT_sb, b_qTsb),
            (ar("z_sb1b", [128, 1280]), Buf(), ar("qb1b", [128, 1024], BF16), Buf(), ar("kb1b", [128, 128], BF16), Buf(),
             ar("qT_sb1b", [64, 16, 128], BF16), Buf())]
    pre = modE(*seqE[0])
    for si, (kind, ti) in enumerate(seqE):
        nxt = modE(*seqE[si + 1]) if si + 1 < len(seqE) else None
        (z_sb, b_z, qb, b_qb, kb, b_kb, qT_sb, b_qTsb) = SETE[si % 2]
        tileE(kind, ti, pre)
        pre = nxt

    if "stopE1" in dbg:
        return
    qT_in = [ar("qT_in1%d" % i, [128, 16, 128], BF16) for i in range(2)]; b_qTin = [Buf(), Buf()]
    for i_ in range(2):
        P.v("pool", "memset", qT_in[i_][64:128], 0.0, writes=[b_qTin[i_]])
    NPT = 3
    PT = [ar("PT1%d" % i, [128, 1024], BF16) for i in range(NPT)]; b_PT = [Buf() for _ in range(NPT)]
    esink = ar("esink1", [128, 16]); b_esink = Buf()
    P.act(esink[:], sinkb[:], AF.Exp, bias=bnd[:, 6:7], reads=[b_const, b_bnd], writes=[b_esink])
    rsr = ar("rsr1", [128, 512]); b_rsr = Buf()
    bc_sb = ar("bc_sb1", [64, 512]); b_bc = Buf()
    OT = ar("OT1", [64, 16, 128], BF16); b_OT = Buf()
    x3_sb = ar("x3_sb", [128, D]); b_x3 = Buf()
    pO = [psum[4], psum[5], psum[6], psum[7]]
    SG = [(k.psall[:, 2 * g_ * 512:(2 * g_ + 2) * 512], [psum[2 * g_][1], psum[2 * g_ + 1][1]]) for g_ in range(2)]
    pBC, bBC = psum[0]
    ones_b = ar("ones_b1", [128, 64], BF16); P.v("pool", "memset", ones_b[:], 1.0, writes=[b_const])
    rsb = ar("rsb1", [128, 4, 512], BF16); b_rsb = Buf()
    bcs = [ar("bcs1%d" % i_, [64, 512]) for i_ in range(4)]; b_bcs = [Buf() for _ in range(4)]
    x_ins = [ar("xE%d" % i_, [128, D]) for i_ in range(2)]; b_xins = [Buf(), Buf()]
    vsink = ar("vsink", [128, 66], BF16); b_vs = Buf()
    P.v("pool", "memset", vsink[:], 0.0, writes=[b_vs])
    P.v("pool", "memset", vsink[:, 64:65], 1.0, writes=[b_vs])
    esrow = ar("esrow", [128, 16, 128], BF16); b_esrow = Buf()
    P.v("dve", "tensor_copy", esrow[64:65], esink[64:65, :].unsqueeze(2).to_broadcast([1, 16, 128]),
        reads=[b_esink], writes=[b_esrow])

    def loadQ(qi):
        i_ = qi % 2
        P.dma(qT_in[i_][0:64], QT1_d[qi], reads=[b_QT1], writes=[b_qTin[i_]])
        P.dma(x_ins[i_][:], X2_d[qi * 128:(qi + 1) * 128, :], reads=[b_X2], writes=[b_xins[i_]])

    it = 0
    loadQ(0)
    for qi in range(16):
        i = qi % 2
        Q, bQ = qT_in[i], b_qTin[i]
        if qi + 1 < 16:
            loadQ(qi + 1)
        keys = [(0, None), (1, None)]
        if qi > 0:
            keys.append((2 + qi - 1, 0))
        keys.append((2 + qi, None))
        keys.append((2 + qi + 1, 1))
        nk = len(keys)
        units = [(gq, list(range(a_, min(a_ + 2, nk)))) for gq in range(4) for a_ in range(0, nk, 2)]
        pend = None

        def pv(gq, kks, pt, bpt):
            po, bpo = pO[gq]
            for j, kk in enumerate(kks):
                kt, mk = keys[kk]
                P.mm(po[0:65, :], Vt[:, kt, gq // 2, 0:65], pt[:, j * 512:(j + 1) * 512], kk == 0, False,
                     reads=[bpt, b_Vt], writes=[bpo])
            if kks[-1] == nk - 1:
                P.mm(po[0:65, :], vsink[64:65, 0:65], esrow[64:65, gq * 4:(gq + 1) * 4, :].rearrange("p h t -> p (h t)"),
                     False, True, reads=[b_vs, b_esrow], writes=[bpo])

        for (gq, kks) in units:
            kvh = gq // 2
            psc, bscs = SG[it % 2]
            pt, bpt = PT[it % NPT], b_PT[it % NPT]
            it += 1
            n = len(kks)
            for j, kk in enumerate(kks):
                kt, mk = keys[kk]
                P.mm(psc[:, j * 512:(j + 1) * 512], KT[:, kvh, kt * 128:(kt + 1) * 128],
                     Q[:, gq * 4:(gq + 1) * 4, :].rearrange("p h t -> p (h t)"), reads=[b_KT, bQ], writes=[bscs[j]])
            P.act(pt[:, 0:n * 512], psc[:, 0:n * 512], AF.Exp, scale=0.125, bias=bnd[:, 6:7],
                  reads=bscs[0:n] + [b_bnd], writes=[bpt])
            for j, kk in enumerate(kks):
                kt, mk = keys[kk]
                if mk is not None:
                    p3 = pt[:, j * 512:(j + 1) * 512].rearrange("p (h t) -> p h t", h=4)
                    P.v("dve", "tensor_tensor", p3, p3, wmask[:, mk, :].unsqueeze(1).to_broadcast([128, 4, 128]), ALU.mult,
                        reads=[bpt, b_const], writes=[bpt])
            if pend is not None:
                pv(*pend)
            pend = (gq, kks, pt, bpt)
        pv(*pend)
        for gq in range(4):
            po, bpo = pO[gq]
            P.act(rsr[64:65, :], po[64:65, :], AF.Ln, reads=[bpo], writes=[b_rsr])
            P.act(rsb[64:65, gq, :], rsr[64:65, :], AF.Exp, scale=-1.0, reads=[b_rsr], writes=[b_rsb])
        for gq in range(4):
            pb_, bb_ = psum[gq]
            P.mm(pb_[0:64, :], ones_b[64:65, 0:64], rsb[64:65, gq, :], reads=[b_const, b_rsb], writes=[bb_])
        for gq in range(4):
            pb_, bb_ = psum[gq]
            P.v("dve", "tensor_copy", bcs[gq][:], pb_[0:64, :], reads=[bb_], writes=[b_bcs[gq]])
        for gq in range(4):
            po, bpo = pO[gq]
            P.v("dve", "tensor_tensor", OT[:, gq * 4:(gq + 1) * 4, :].rearrange("p h t -> p (h t)"), po[0:64, :], bcs[gq][:],
                ALU.mult, reads=[bpo, b_bcs[gq]], writes=[b_OT])
        X, bX = x_ins[i], b_xins[i]
        for hh in range(2):
            pz, bz = psum[1 + hh]
            for h in range(16):
                P.mm(pz[:], OT[:, h, :], w_outB[:, h, hh * 512:(hh + 1) * 512], h == 0, h == 15,
                     reads=[b_OT, b_woutB], writes=[bz])
            P.v("dve", "tensor_tensor", x3_sb[:, hh * 512:(hh + 1) * 512], pz[:],
                k.gateB_all[:, k.gslot[(1, 0, 0)], hh * 512:(hh + 1) * 512], ALU.mult,
                reads=[bz, k.b_gateB[1]], writes=[b_x3])
        P.v("pool", "tensor_tensor", x3_sb[:], x3_sb[:], X[:], ALU.add, reads=[b_x3, bX], writes=[b_x3])
        P.dma(X3_d[qi * 128:(qi + 1) * 128, :], x3_sb[:], reads=[b_x3], writes=[b_X3])


def rope_tables():
    t = np.arange(SEQ)
    row = (t // 64).astype(np.float32)
    col = (t % 64).astype(np.float32)
    inv = (10000.0 ** (-np.arange(0, 32, 2, dtype=np.float32) / 32)).astype(np.float32)
    ang = np.stack([row[:, None] * inv, col[:, None] * inv], axis=1)
    c = np.cos(ang).astype(np.float32)
    s = np.sin(ang).astype(np.float32)
    C = np.zeros((SEQ, 2, 2, 16), np.float32)
    S = np.zeros((SEQ, 2, 2, 16), np.float32)
    C[:, :, 0] = c
    C[:, :, 1] = c
    S[:, :, 0] = -s
    S[:, :, 1] = s
    return C.reshape(SEQ, 64), S.reshape(SEQ, 64)


def host_consts():
    p = np.arange(128)
    same = (p[:, None] // 64) == (p[None, :] // 64)
    s, t = p[:, None], p[None, :]
    tri = np.stack([same & (s <= t), same & (s >= t), same & (s > t), same & (s < t)], axis=1).astype(np.float32)
    sm = (p % 64)[:, None]
    tt = np.arange(64)[None, :]
    mA = np.tile((sm <= tt), (1, 4))
    mB = np.tile((sm >= tt), (1, 4))
    maskAB = np.stack([mA, mB], axis=1).astype(ml_dtypes.bfloat16)
    sel2 = np.zeros((2, 2, 128), np.float32)
    sel2[0, 0] = 1
    sel2[1, 1] = 1
    wmask = np.stack([(s >= t), (s <= t)], axis=1).astype(ml_dtypes.bfloat16)
    return dict(identF=np.eye(128, dtype=np.float32), identB=np.eye(128).astype(ml_dtypes.bfloat16),
                tri=tri, maskAB=maskAB, sel2=sel2, wmask=wmask)


def make_in_maps(inp):
    f = lambda a: np.ascontiguousarray(np.asarray(a, dtype=np.float32))
    C, S = rope_tables()
    consts = host_consts()
    maps = []
    rw = np.concatenate([f(inp["router_group_w"]), f(inp["router_expert_w"])], axis=-1)
    rb = np.concatenate([f(inp["router_group_b"]), f(inp["router_expert_b"])], axis=-1)[:, None, :]
    shared = dict(
        mod_w=f(inp["mod_w"]), mod_b=f(inp["mod_b"]), norm_mix=f(inp["norm_mix"]), norm_ffn=f(inp["norm_ffn"]),
        ev_w_in=f(inp["ev_w_in"])[0], ev_w_out=f(inp["ev_w_out"])[0],
        gla_out_norm=f(inp["gla_out_norm"]), att_q_norm=f(inp["att_q_norm"]), att_k_norm=f(inp["att_k_norm"]),
        od_w_in=f(inp["od_w_in"])[0], od_w_out=f(inp["od_w_out"])[0], swa_sink=f(inp["swa_sink"]),
        swa_q_norm=f(inp["swa_q_norm"]), swa_k_norm=f(inp["swa_k_norm"]),
        router_w=np.ascontiguousarray(rw), router_b=np.ascontiguousarray(rb),
        exp_w_gate=f(inp["exp_w_gate"]).reshape(2, 16, D, 256), exp_w_up=f(inp["exp_w_up"]).reshape(2, 16, D, 256),
        exp_w_down=f(inp["exp_w_down"]).reshape(2, 16, 256, D), **consts)
    gw = f(inp["gla_gate_w"])[0]
    gb = f(inp["gla_gate_b"])[0]
    for core in range(8):
        b, half = core // 2, core % 2
        x = f(inp["x"])[b]
        cx = f(inp["ctx"])[b]
        order = (0, 1)
        if half == 1:
            x = x[::-1]
            cx = cx[::-1]
            order = (1, 0)
        gwe = np.zeros((33, 512), np.float32)
        for i, dr in enumerate(order):
            gwe[16 * dr:16 * dr + 16, 256 * i:256 * i + 256] = gw[dr]
            gwe[32, 256 * i:256 * i + 256] = gb[dr]
        m = dict(shared)
        m.update(x=np.ascontiguousarray(x), ctx=np.ascontiguousarray(cx),
                 crow=np.ascontiguousarray(np.stack([f(inp["c"])[b], f(inp["c_ctx"])])),
                 gw_ext=gwe,
                 ropeC=np.ascontiguousarray(C[::-1] if half else C),
                 ropeS=np.ascontiguousarray(S[::-1] if half else S))
        maps.append(m)
    return maps


_CACHE = {}


def kernel(**inputs):
    if "nc" not in _CACHE:
        _CACHE["nc"] = build()[0]
    nc = _CACHE["nc"]
    maps = make_in_maps(inputs)
    res = run_bass_kernel_spmd(nc, maps, core_ids=list(range(8)))
    out = np.zeros((4, SEQ, D), np.float32)
    for core in range(8):
        b, half = core // 2, core % 2
        o = np.asarray(res.results[core]["out"], dtype=np.float32)
        if half == 0:
            out[b, 0:2048] = o
        else:
            out[b, 2048:] = o[::-1]
    return out
```
